# Optimizing a Trainium2 kernel written in Bass

```python
import math
import jax
import jax.numpy as jnp
from jax import lax
import numpy as np

D_MODEL = 2048
BATCH = 2
SEQ = 8192
DEPTH = 2

F32 = jnp.float32
GRID_W = 64
CTX_LEN = 256
EPS = 1e-6

SSD_HEADS = 32
SSD_HEAD_DIM = 64
SSD_WIDTH = SSD_HEADS * SSD_HEAD_DIM
SSD_GROUPS = 4
SSD_HEADS_PER_GROUP = SSD_HEADS // SSD_GROUPS
SSD_STATE = 128
SSD_CHUNK = 128
D_CONV = 3
XBC_WIDTH = SSD_WIDTH + 2 * SSD_GROUPS * SSD_STATE

GM_GROUPS = 16
GM_GROUP_DIM = 128
GM_WIDTH = GM_GROUPS * GM_GROUP_DIM
GM_CHUNK = 128

HYB_CUTS = (SSD_WIDTH, SSD_WIDTH + XBC_WIDTH, SSD_WIDTH + XBC_WIDTH + 2 * SSD_HEADS,
            SSD_WIDTH + XBC_WIDTH + 2 * SSD_HEADS + GM_WIDTH)
HYB_IN = SSD_WIDTH + XBC_WIDTH + 2 * SSD_HEADS + 2 * GM_WIDTH
HYB_MIX = SSD_WIDTH + GM_WIDTH

MLA_HEADS = 16
Q_LORA = 768
KV_LORA = 512
QK_NOPE = 128
QK_ROPE = 64
V_DIM = 128
ROPE_PAIRS = QK_ROPE // 4
ROPE_THETA = 10000.0
MLA_IN = Q_LORA + KV_LORA + QK_ROPE
MLA_SCALE = (QK_NOPE + QK_ROPE) ** -0.5
Q_BLOCK = 128

N_EXPERTS = 32
TOP_K = 4
EXPERT_FF = D_MODEL
SWIGLU_LIMIT = 7.0
SWIGLU_ALPHA = 1.702
MOE_BLOCK = 128

N_EVEN = (DEPTH + 1) // 2
N_ODD = DEPTH // 2

kernel_name = 'hybrid_ssd_gmlp_mla_moe_prefix_dit'


def rms_norm(x, g):
    xf = x.astype(F32)
    y = xf * lax.rsqrt(jnp.mean(xf * xf, axis=-1, keepdims=True) + EPS)
    return y.astype(x.dtype) * g


def centred_dwconv(x, w, b):
    k = w.shape[0]
    pad = (k - 1) // 2
    n = x.shape[1]
    xp = jnp.pad(x, ((0, 0), (pad, k - 1 - pad), (0, 0)))
    out = b
    for i in range(k):
        out = out + xp[:, i:i + n] * w[i]
    return out


def axial_rope(rows, dtype):
    row = jnp.broadcast_to(jnp.arange(rows)[:, None], (rows, GRID_W)).reshape(-1)
    col = jnp.broadcast_to(jnp.arange(GRID_W)[None, :], (rows, GRID_W)).reshape(-1)
    freqs = ROPE_THETA ** (-jnp.arange(ROPE_PAIRS, dtype=F32) / ROPE_PAIRS)
    ang = jnp.stack([row[:, None] * freqs, col[:, None] * freqs], axis=1)
    return jnp.cos(ang).astype(dtype), jnp.sin(ang).astype(dtype)


def apply_rope(x, cos, sin):
    xr = x.reshape(*x.shape[:-1], 2, 2, ROPE_PAIRS)
    x1, x2 = xr[..., 0, :], xr[..., 1, :]
    c, s = cos[:, None], sin[:, None]
    out = jnp.stack([x1 * c - x2 * s, x1 * s + x2 * c], axis=-2)
    return out.reshape(x.shape)


def ssd_chunked(xh, dt, a_neg, bm, cm, h0):
    bsz, n = xh.shape[:2]
    nc = n // SSD_CHUNK
    G, J, P, N = SSD_GROUPS, SSD_HEADS_PER_GROUP, SSD_HEAD_DIM, SSD_STATE
    xc = xh.reshape(bsz, nc, SSD_CHUNK, G, J, P)
    dtc = dt.reshape(bsz, nc, SSD_CHUNK, G, J)
    bc = bm.reshape(bsz, nc, SSD_CHUNK, G, N)
    cc = cm.reshape(bsz, nc, SSD_CHUNK, G, N)
    a_cs = jnp.cumsum(dtc * a_neg, axis=2)
    xdt = xc * dtc[..., None]
    tril = jnp.tril(jnp.ones((SSD_CHUNK, SSD_CHUNK), dtype=bool))[:, :, None, None]
    seg = a_cs[:, :, :, None] - a_cs[:, :, None, :]
    lmat = jnp.exp(jnp.where(tril, seg, -jnp.inf))
    cb = jnp.einsum('bcqgn,bckgn->bcqkg', cc, bc)
    y_diag = jnp.einsum('bcqkgj,bckgjp->bcqgjp', cb[..., None] * lmat, xdt)
    decay_end = jnp.exp(a_cs[:, :, -1:] - a_cs)
    states = jnp.einsum('bckgn,bckgj,bckgjp->bcgjpn', bc, decay_end, xdt)
    chunk_decay = jnp.exp(a_cs[:, :, -1])

    def step(h, inp):
        s, dec = inp
        return h * dec[..., None, None] + s, h

    h_final, h_start = lax.scan(step, h0, (jnp.moveaxis(states, 1, 0), jnp.moveaxis(chunk_decay, 1, 0)))
    h_start = jnp.moveaxis(h_start, 0, 1)
    y_off = jnp.einsum('bcqgn,bcgjpn,bcqgj->bcqgjp', cc, h_start, jnp.exp(a_cs))
    return (y_diag + y_off).reshape(bsz, n, G, J, P), h_final


def chunk_gmlp(u, v, v_norm_g, w_s, b_s):
    bsz, n, _ = u.shape
    u = jax.nn.gelu(u)
    v = rms_norm(jax.nn.gelu(v), v_norm_g)
    vc = v.reshape(bsz, n // GM_CHUNK, GM_CHUNK, GM_GROUPS, GM_GROUP_DIM)
    s = jnp.einsum('gqk,bckgd->bcqgd', w_s, vc) + b_s.T[:, :, None]
    return u * s.reshape(bsz, n, GM_WIDTH)


def hybrid_mixer(h_ctx, h_lat, w_in, conv_w, conv_b, dt_bias, a_log, d_skip, ssd_norm_g,
                 v_norm_g, w_s, b_s, w_out, need_ctx):
    G, J = SSD_GROUPS, SSD_HEADS_PER_GROUP
    dt_b = dt_bias.astype(F32).reshape(2, G, J)
    a_neg = -jnp.exp(a_log.astype(F32)).reshape(2, G, J)

    def project(h):
        bsz, n, _ = h.shape
        z, xbc, dt_raw, u, v = jnp.split(h @ w_in, HYB_CUTS, axis=-1)
        xbc = jax.nn.silu(centred_dwconv(xbc, conv_w, conv_b))
        xs, bm, cm = jnp.split(xbc, [SSD_WIDTH, SSD_WIDTH + G * SSD_STATE], axis=-1)
        xs = xs.reshape(bsz, n, G, J, SSD_HEAD_DIM)
        bm = bm.reshape(bsz, n, G, SSD_STATE)
        cm = cm.reshape(bsz, n, G, SSD_STATE)
        dt = jax.nn.softplus(dt_raw.astype(F32).reshape(bsz, n, 2, G, J) + dt_b)
        return z, xs, bm, cm, dt, u, v

    zc, xc, bc, cc, dtc, uc, vc = project(h_ctx)
    zl, xl, bl, cl, dtl, ul, vl = project(h_lat)
    flip = lambda t: jnp.flip(t, axis=1)
    h0 = jnp.zeros((h_lat.shape[0], G, J, SSD_HEAD_DIM, SSD_STATE), F32)
    yc_f, hc_f = ssd_chunked(xc, dtc[:, :, 0], a_neg[0], bc, cc, h0)
    yl_f, _ = ssd_chunked(xl, dtl[:, :, 0], a_neg[0], bl, cl, hc_f)
    yc_b, hc_b = ssd_chunked(flip(xc), flip(dtc[:, :, 1]), a_neg[1], flip(bc), flip(cc), h0)
    yl_b, _ = ssd_chunked(flip(xl), flip(dtl[:, :, 1]), a_neg[1], flip(bl), flip(cl), hc_b)
    d = d_skip.reshape(G, J)[:, :, None]

    def merge(xs, yf, yb, z, u, v):
        y = (yf + flip(yb) + xs * d).reshape(z.shape).astype(z.dtype)
        y_ssd = rms_norm(y * jax.nn.silu(z), ssd_norm_g)
        y_gm = chunk_gmlp(u, v, v_norm_g, w_s, b_s)
        return jnp.concatenate([y_ssd, y_gm], axis=-1) @ w_out

    y_lat = merge(xl, yl_f, yl_b, zl, ul, vl)
    y_ctx = merge(xc, yc_f, yc_b, zc, uc, vc) if need_ctx else None
    return y_ctx, y_lat


def attend(q_nope, q_pe, k_nope, k_pe, v):
    s = jnp.einsum('bqhd,bkhd->bhqk', q_nope, k_nope) + jnp.einsum('bqhr,bkr->bhqk', q_pe, k_pe)
    p = jax.nn.softmax(s.astype(F32) * MLA_SCALE, axis=-1).astype(v.dtype)
    return jnp.einsum('bhqk,bkhd->bqhd', p, v)


def mla_mixer(h_ctx, h_lat, w_in, q_norm_g, kv_norm_g, w_uq, w_ukv, w_o, cos, sin, need_ctx):
    bsz, n, _ = h_lat.shape

    def queries(qp):
        q = (rms_norm(qp, q_norm_g) @ w_uq).reshape(*qp.shape[:2], MLA_HEADS, QK_NOPE + QK_ROPE)
        return q[..., :QK_NOPE], q[..., QK_NOPE:]

    def keys_values(kvp):
        c_kv, k_pe = jnp.split(kvp, [KV_LORA], axis=-1)
        kv = (rms_norm(c_kv, kv_norm_g) @ w_ukv).reshape(*kvp.shape[:2], MLA_HEADS, QK_NOPE + V_DIM)
        return kv[..., :QK_NOPE], k_pe, kv[..., QK_NOPE:]

    p_lat = h_lat @ w_in
    qn_l, qp_l = queries(p_lat[..., :Q_LORA])
    kn_l, kp_l, v_l = keys_values(p_lat[..., Q_LORA:])
    qp_l = apply_rope(qp_l, cos, sin)
    kp_l = apply_rope(kp_l[:, :, None], cos, sin)[:, :, 0]
    if need_ctx:
        p_ctx = h_ctx @ w_in
        kvp_ctx = p_ctx[..., Q_LORA:]
    else:
        kvp_ctx = h_ctx @ w_in[:, Q_LORA:]
    kn_c, kp_c, v_c = keys_values(kvp_ctx)
    kn_all = jnp.concatenate([kn_c, kn_l], axis=1)
    kp_all = jnp.concatenate([kp_c, kp_l], axis=1)
    v_all = jnp.concatenate([v_c, v_l], axis=1)
    nblk = n // Q_BLOCK
    qn_b = jnp.moveaxis(qn_l.reshape(bsz, nblk, Q_BLOCK, MLA_HEADS, QK_NOPE), 1, 0)
    qp_b = jnp.moveaxis(qp_l.reshape(bsz, nblk, Q_BLOCK, MLA_HEADS, QK_ROPE), 1, 0)
    o = lax.map(lambda qb: attend(qb[0], qb[1], kn_all, kp_all, v_all), (qn_b, qp_b))
    y_lat = jnp.moveaxis(o, 0, 1).reshape(bsz, n, MLA_HEADS * V_DIM) @ w_o
    y_ctx = None
    if need_ctx:
        qn_c, qp_c = queries(p_ctx[..., :Q_LORA])
        y_ctx = attend(qn_c, qp_c, kn_c, kp_c, v_c).reshape(bsz, h_ctx.shape[1], MLA_HEADS * V_DIM) @ w_o
    return y_ctx, y_lat


def moe(h, router_w, router_b, w1, b1, w2, b2):
    t_tok, d = h.shape
    logits = (h @ router_w + router_b).astype(F32)
    top_val, top_idx = lax.top_k(logits, TOP_K)
    gates = jax.nn.softmax(top_val, axis=-1).astype(h.dtype)
    flat_e = top_idx.reshape(-1)
    flat_tok = jnp.repeat(jnp.arange(t_tok, dtype=jnp.int32), TOP_K)
    flat_gate = gates.reshape(-1)
    order = jnp.argsort(flat_e)
    e_sorted, tok_sorted, gate_sorted = flat_e[order], flat_tok[order], flat_gate[order]
    counts = jnp.bincount(flat_e, length=N_EXPERTS)
    padded = (counts + MOE_BLOCK - 1) // MOE_BLOCK * MOE_BLOCK
    cum_pad = jnp.cumsum(padded)
    pad_start = cum_pad - padded
    start = jnp.cumsum(counts) - counts
    dest = pad_start[e_sorted] + jnp.arange(t_tok * TOP_K) - start[e_sorted]
    n_blocks = -(-(t_tok * TOP_K) // MOE_BLOCK) + N_EXPERTS
    n_slots = n_blocks * MOE_BLOCK
    slot_tok = jnp.full((n_slots,), t_tok, jnp.int32).at[dest].set(tok_sorted)
    slot_gate = jnp.zeros((n_slots,), h.dtype).at[dest].set(gate_sorted)
    block_expert = jnp.clip(jnp.searchsorted(cum_pad, jnp.arange(n_blocks) * MOE_BLOCK, side='right'),
                            0, N_EXPERTS - 1)
    h_pad = jnp.concatenate([h, jnp.zeros((1, d), h.dtype)], axis=0)
    xb = h_pad[slot_tok].reshape(n_blocks, MOE_BLOCK, d)

    def expert_block(args):
        xblk, e = args
        a = xblk @ w1[e] + b1[e]
        glu = jnp.minimum(a[:, 0::2], SWIGLU_LIMIT)
        lin = jnp.clip(a[:, 1::2], -SWIGLU_LIMIT, SWIGLU_LIMIT)
        y = glu * jax.nn.sigmoid(SWIGLU_ALPHA * glu) * (lin + 1.0)
        return y @ w2[e] + b2[e]

    yb = lax.map(expert_block, (xb, block_expert)).reshape(n_slots, d) * slot_gate[:, None]
    return jnp.zeros((t_tok + 1, d), h.dtype).at[slot_tok].add(yb)[:t_tok]


def setup_inputs(seed: int = 0) -> dict:
    key = jax.random.key(seed)
    ks = iter(jax.random.split(key, 48))
    D = D_MODEL

    def nrm(shape, scale):
        return jax.random.normal(next(ks), shape, F32) * scale

    def gain(shape):
        return 1.0 + nrm(shape, 0.02)

    x = nrm((BATCH, SEQ, D), 1.0)
    c = nrm((BATCH, D), 1.0)
    ctx = nrm((BATCH, CTX_LEN, D), 1.0)
    c_ctx = nrm((D,), 1.0)
    mix_norm_g = gain((DEPTH, D))
    ffn_norm_g = gain((DEPTH, D))
    ada_w = nrm((DEPTH, D, 6 * D), 0.5 * D ** -0.5)
    ada_b = nrm((DEPTH, 6 * D), 0.02)
    hyb_w_in = nrm((N_EVEN, D, HYB_IN), D ** -0.5)
    hyb_conv_w = nrm((N_EVEN, D_CONV, XBC_WIDTH), D_CONV ** -0.5)
    hyb_conv_b = nrm((N_EVEN, XBC_WIDTH), 0.02)
    dt0 = jnp.exp(jax.random.uniform(next(ks), (N_EVEN, 2, SSD_HEADS), F32, math.log(1e-3), math.log(1e-1)))
    hyb_dt_bias = dt0 + jnp.log(-jnp.expm1(-dt0))
    hyb_a_log = jnp.log(jax.random.uniform(next(ks), (N_EVEN, 2, SSD_HEADS), F32, 1.0, 16.0))
    hyb_d_skip = gain((N_EVEN, SSD_HEADS))
    hyb_ssd_norm_g = gain((N_EVEN, SSD_WIDTH))
    hyb_v_norm_g = gain((N_EVEN, GM_WIDTH))
    hyb_w_s = nrm((N_EVEN, GM_GROUPS, GM_CHUNK, GM_CHUNK), GM_CHUNK ** -0.5)
    hyb_b_s = gain((N_EVEN, GM_GROUPS, GM_CHUNK))
    hyb_w_out = nrm((N_EVEN, HYB_MIX, D), HYB_MIX ** -0.5)
    mla_w_in = nrm((N_ODD, D, MLA_IN), D ** -0.5)
    mla_q_norm_g = gain((N_ODD, Q_LORA))
    mla_kv_norm_g = gain((N_ODD, KV_LORA))
    mla_w_uq = nrm((N_ODD, Q_LORA, MLA_HEADS * (QK_NOPE + QK_ROPE)), Q_LORA ** -0.5)
    mla_w_ukv = nrm((N_ODD, KV_LORA, MLA_HEADS * (QK_NOPE + V_DIM)), KV_LORA ** -0.5)
    mla_w_o = nrm((N_ODD, MLA_HEADS * V_DIM, D), (MLA_HEADS * V_DIM) ** -0.5)
    router_w = nrm((DEPTH, D, N_EXPERTS), D ** -0.5)
    router_b = nrm((DEPTH, N_EXPERTS), 0.01)
    exp_w1 = nrm((DEPTH, N_EXPERTS, D, 2 * EXPERT_FF), D ** -0.5)
    exp_b1 = nrm((DEPTH, N_EXPERTS, 2 * EXPERT_FF), 0.02)
    exp_w2 = nrm((DEPTH, N_EXPERTS, EXPERT_FF, D), EXPERT_FF ** -0.5)
    exp_b2 = nrm((DEPTH, N_EXPERTS, D), 0.02)
    final_norm_g = gain((D,))
    return {'x': x, 'c': c, 'ctx': ctx, 'c_ctx': c_ctx,
            'mix_norm_g': mix_norm_g, 'ffn_norm_g': ffn_norm_g, 'ada_w': ada_w, 'ada_b': ada_b,
            'hyb_w_in': hyb_w_in, 'hyb_conv_w': hyb_conv_w, 'hyb_conv_b': hyb_conv_b,
            'hyb_dt_bias': hyb_dt_bias, 'hyb_a_log': hyb_a_log, 'hyb_d_skip': hyb_d_skip,
            'hyb_ssd_norm_g': hyb_ssd_norm_g, 'hyb_v_norm_g': hyb_v_norm_g,
            'hyb_w_s': hyb_w_s, 'hyb_b_s': hyb_b_s, 'hyb_w_out': hyb_w_out,
            'mla_w_in': mla_w_in, 'mla_q_norm_g': mla_q_norm_g, 'mla_kv_norm_g': mla_kv_norm_g,
            'mla_w_uq': mla_w_uq, 'mla_w_ukv': mla_w_ukv, 'mla_w_o': mla_w_o,
            'router_w': router_w, 'router_b': router_b,
            'exp_w1': exp_w1, 'exp_b1': exp_b1, 'exp_w2': exp_w2, 'exp_b2': exp_b2,
            'final_norm_g': final_norm_g}


def reference(x, c, ctx, c_ctx, mix_norm_g, ffn_norm_g, ada_w, ada_b,
              hyb_w_in, hyb_conv_w, hyb_conv_b, hyb_dt_bias, hyb_a_log, hyb_d_skip,
              hyb_ssd_norm_g, hyb_v_norm_g, hyb_w_s, hyb_b_s, hyb_w_out,
              mla_w_in, mla_q_norm_g, mla_kv_norm_g, mla_w_uq, mla_w_ukv, mla_w_o,
              router_w, router_b, exp_w1, exp_b1, exp_w2, exp_b2, final_norm_g):
    bsz, n, d = x.shape
    rows = n // GRID_W
    cos, sin = axial_rope(rows, x.dtype)
    silu_c = jax.nn.silu(c)
    silu_cc = jax.nn.silu(c_ctx)
    lat, cx = x, ctx
    n_ctx_tok = bsz * ctx.shape[1]
    for i in range(DEPTH):
        last = i == DEPTH - 1
        j = i // 2
        mod_l = (silu_c @ ada_w[i] + ada_b[i])[:, None, :]
        sh1, sc1, g1, sh2, sc2, g2 = jnp.split(mod_l, 6, axis=-1)
        n_mod_c = 2 if last else 6
        mods_c = jnp.split(silu_cc @ ada_w[i][:, :n_mod_c * d] + ada_b[i][:n_mod_c * d], n_mod_c)
        h_lat = rms_norm(lat, mix_norm_g[i]) * (1.0 + sc1) + sh1
        h_ctx = rms_norm(cx, mix_norm_g[i]) * (1.0 + mods_c[1]) + mods_c[0]
        if i % 2 == 0:
            y_ctx, y_lat = hybrid_mixer(h_ctx, h_lat, hyb_w_in[j], hyb_conv_w[j], hyb_conv_b[j],
                                        hyb_dt_bias[j], hyb_a_log[j], hyb_d_skip[j], hyb_ssd_norm_g[j],
                                        hyb_v_norm_g[j], hyb_w_s[j], hyb_b_s[j], hyb_w_out[j],
                                        need_ctx=not last)
        else:
            y_ctx, y_lat = mla_mixer(h_ctx, h_lat, mla_w_in[j], mla_q_norm_g[j], mla_kv_norm_g[j],
                                     mla_w_uq[j], mla_w_ukv[j], mla_w_o[j], cos, sin,
                                     need_ctx=not last)
        lat = lat + g1 * y_lat
        hf_lat = rms_norm(lat, ffn_norm_g[i]) * (1.0 + sc2) + sh2
        if last:
            f = moe(hf_lat.reshape(-1, d), router_w[i], router_b[i], exp_w1[i], exp_b1[i], exp_w2[i], exp_b2[i])
            lat = lat + g2 * f.reshape(bsz, n, d)
        else:
            cx = cx + mods_c[2] * y_ctx
            hf_ctx = rms_norm(cx, ffn_norm_g[i]) * (1.0 + mods_c[4]) + mods_c[3]
            tokens = jnp.concatenate([hf_ctx.reshape(-1, d), hf_lat.reshape(-1, d)], axis=0)
            f = moe(tokens, router_w[i], router_b[i], exp_w1[i], exp_b1[i], exp_w2[i], exp_b2[i])
            cx = cx + mods_c[5] * f[:n_ctx_tok].reshape(cx.shape)
            lat = lat + g2 * f[n_ctx_tok:].reshape(bsz, n, d)
    return rms_norm(lat, final_norm_g)
```

```python
import contextlib
import numpy as np
import concourse.bass as bass
import concourse.mybir as mybir
from concourse.bass_utils import run_bass_kernel_spmd

F32 = mybir.dt.float32
BF16 = mybir.dt.bfloat16
I32 = mybir.dt.int32
AF = mybir.ActivationFunctionType
ALU = mybir.AluOpType
AX = mybir.AxisListType

NDSEM = 8


class Ctx:
    def __init__(self):
        nc = bass.Bass("TRN2", target_bir_lowering=False)
        self.nc = nc
        self.E = {"pe": nc.tensor, "act": nc.scalar, "dve": nc.vector, "pool": nc.gpsimd, "sp": nc.sync}
        self.csem = {e: nc.alloc_semaphore(name=f"c_{e}") for e in ("pe", "act", "dve", "pool")}
        self.ccount = {e: 0 for e in self.csem}
        self.dsem = {q: [nc.alloc_semaphore(name=f"d_{q}{i}") for i in range(NDSEM)] for q in ("sp", "pool")}
        self.dval = {q: [0] * NDSEM for q in self.dsem}
        self.dnext = {q: 0 for q in self.dsem}
        self.ccsem = nc.alloc_semaphore(name="cc")
        self.ccval = 0
        self.known = {e: {} for e in self.E}
        self.lastw = {}
        self.readers = {}
        self.uid = 0
        self.n_instr = 0

    def name(self, base):
        self.uid += 1
        return f"{base}_{self.uid}"

    def dram(self, name, shape, dtype, kind="Internal"):
        key = _re.sub(r"^b\d", "", name) if kind == "Internal" and not name.endswith("RES") else name
        if not hasattr(self, "_dcache"):
            self._dcache = {}
        if key in self._dcache:
            return self._dcache[key]
        ap = self.nc.dram_tensor(key, list(shape), dtype, kind=kind).ap()
        self._dcache[key] = ap
        return ap

    def _deps(self, eng, reads, writes):
        need = []
        for r in reads:
            w = self.lastw.get(r)
            if w is not None:
                need.append(w)
        for r in writes:
            w = self.lastw.get(r)
            if w is not None:
                need.append(w)
            rd = self.readers.get(r)
            if rd:
                need.extend(rd.values())
        out = {}
        kn = self.known[eng]
        for (sem, val, src) in need:
            if src == "pe" and eng == "pe":
                continue
            key = id(sem)
            if kn.get(key, 0) >= val:
                continue
            if key not in out or out[key][1] < val:
                out[key] = (sem, val)
        for key, (sem, val) in out.items():
            kn[key] = val
            self.E[eng].wait_ge(sem, val)
            self.n_instr += 1

    def _record(self, rec, reads, writes):
        for r in reads:
            d = self.readers.setdefault(r, {})
            k = id(rec[0])
            if k not in d or d[k][1] < rec[1]:
                d[k] = rec
        for r in writes:
            self.lastw[r] = rec
            self.readers[r] = {}

    def op(self, eng, fn, reads=(), writes=(), inc=True):
        self._deps(eng, reads, writes)
        ins = fn(self.E[eng])
        self.n_instr += 1
        sem = self.csem[eng]
        if inc:
            self.ccount[eng] += 1
            ins.then_inc(sem, 1)
            rec = (sem, self.ccount[eng], eng)
        else:
            rec = (sem, self.ccount[eng] + 1, eng)
        self._record(rec, reads, writes)
        return ins

    def dma(self, q, out, in_, reads=(), writes=(), **kw):
        i = self.dnext[q]
        self.dnext[q] = (i + 1) % NDSEM
        sem = self.dsem[q][i]
        kn = self.known[q]
        if kn.get(id(sem), 0) < self.dval[q][i]:
            self.E[q].wait_ge(sem, self.dval[q][i])
            kn[id(sem)] = self.dval[q][i]
            self.n_instr += 1
        self._deps(q, reads, writes)
        ins = self.E[q].dma_start(out=out, in_=in_, **kw)
        self.n_instr += 1
        self.dval[q][i] += 16
        ins.then_inc(sem, 16)
        rec = (sem, self.dval[q][i], "dma_" + q)
        self._record(rec, reads, writes)
        return ins

    def idma(self, out, out_off, in_, in_off, reads=(), writes=(), **kw):
        q = "pool"
        i = self.dnext[q]
        self.dnext[q] = (i + 1) % NDSEM
        sem = self.dsem[q][i]
        kn = self.known[q]
        if kn.get(id(sem), 0) < self.dval[q][i]:
            self.E[q].wait_ge(sem, self.dval[q][i])
            kn[id(sem)] = self.dval[q][i]
        self._deps(q, reads, writes)
        ins = self.nc.gpsimd.indirect_dma_start(out=out, out_offset=out_off, in_=in_, in_offset=in_off, **kw)
        self.n_instr += 1
        self.dval[q][i] += 16
        ins.then_inc(sem, 16)
        rec = (sem, self.dval[q][i], "dma_" + q)
        self._record(rec, reads, writes)
        return ins

    def collective(self, kind, op, groups, in_ap, out_ap, reads=(), writes=()):
        q = "pool"
        self._deps(q, reads, writes)
        ins = self.nc.gpsimd.collective_compute(kind, op, replica_groups=groups, ins=[in_ap], outs=[out_ap])
        self.ccval += 1
        ins.then_inc(self.ccsem)
        self.nc.gpsimd.wait_ge(self.ccsem, self.ccval)
        self.known[q][id(self.ccsem)] = self.ccval
        rec = (self.ccsem, self.ccval, "cc")
        self._record(rec, reads, writes)
        return ins

    def barrier(self):
        for e in self.E:
            kn = self.known[e]
            eng = self.E[e]
            for f, sem in self.csem.items():
                if self.ccount[f] > kn.get(id(sem), 0):
                    eng.wait_ge(sem, self.ccount[f])
                    kn[id(sem)] = self.ccount[f]
            for q in self.dsem:
                for i, sem in enumerate(self.dsem[q]):
                    if self.dval[q][i] > kn.get(id(sem), 0):
                        eng.wait_ge(sem, self.dval[q][i])
                        kn[id(sem)] = self.dval[q][i]
            if self.ccval > kn.get(id(self.ccsem), 0):
                eng.wait_ge(self.ccsem, self.ccval)
                kn[id(self.ccsem)] = self.ccval

    def finish(self):
        sp = self.E["sp"]
        kn = self.known["sp"]
        for q in self.dsem:
            for i, sem in enumerate(self.dsem[q]):
                if self.dval[q][i] > kn.get(id(sem), 0):
                    sp.wait_ge(sem, self.dval[q][i])
        for e, sem in self.csem.items():
            if self.ccount[e] > kn.get(id(sem), 0):
                sp.wait_ge(sem, self.ccount[e])
        if self.ccval:
            sp.wait_ge(self.ccsem, self.ccval)


def bcast_rows(ap_row, nparts=128):
    return ap_row.partition_broadcast(nparts)


def _mk(c):
    return c


def mm(c, out, lhsT, rhs, start, stop, reads, writes, inc=None):
    inc = stop if inc is None else inc
    return c.op("pe", lambda e: e.matmul(out=out, lhsT=lhsT, rhs=rhs, start=start, stop=stop), reads, writes, inc=inc)


def tr(c, out, in_, ident, reads, writes, inc=True):
    return c.op("pe", lambda e: e.transpose(out=out, in_=in_, identity=ident), reads, writes, inc=inc)


def act(c, out, in_, func, reads, writes, **kw):
    return c.op("act", lambda e: e.activation(out=out, in_=in_, func=func, **kw), reads, writes)


def cp(c, eng, out, in_, reads, writes):
    if eng == "act":
        return c.op("act", lambda e: e.copy(out=out, in_=in_), reads, writes)
    return c.op(eng, lambda e: e.tensor_copy(out=out, in_=in_), reads, writes)


def tt(c, eng, out, in0, in1, op, reads, writes):
    return c.op(eng, lambda e: e.tensor_tensor(out=out, in0=in0, in1=in1, op=op), reads, writes)


def ts(c, eng, out, in0, s1, s2, op0, op1, reads, writes, accum_out=None):
    if op1 is None:
        return c.op(eng, lambda e: e.tensor_scalar(out=out, in0=in0, scalar1=s1, scalar2=None, op0=op0), reads, writes)
    if accum_out is not None:
        return c.op(eng, lambda e: e.tensor_scalar(out=out, in0=in0, scalar1=s1, scalar2=s2, op0=op0, op1=op1, accum_out=accum_out), reads, writes)
    return c.op(eng, lambda e: e.tensor_scalar(out=out, in0=in0, scalar1=s1, scalar2=s2, op0=op0, op1=op1), reads, writes)


def stt(c, eng, out, in0, scalar, in1, op0, op1, reads, writes):
    return c.op(eng, lambda e: e.scalar_tensor_tensor(out=out, in0=in0, scalar=scalar, in1=in1, op0=op0, op1=op1), reads, writes)


def red(c, eng, out, in_, op, reads, writes, axis=None):
    axis = AX.X if axis is None else axis
    return c.op(eng, lambda e: e.tensor_reduce(out=out, in_=in_, axis=axis, op=op), reads, writes)


def mset(c, eng, ap, val, writes):
    return c.op(eng, lambda e: e.memset(ap, val), (), writes)


def rmsnorm_rstd(c, x_ap, junk_ap, ss_ap, D, reads, tag, eps=1e-6):
    act(c, junk_ap, x_ap, AF.Square, reads, [tag + "_junk", tag + "_ss0"], scale=float(D) ** -0.5, accum_out=ss_ap[:, 0:1])
    act(c, ss_ap[:, 1:2], ss_ap[:, 0:1], AF.Sqrt, [tag + "_ss0"], [tag + "_ss1"], bias=eps)
    c.op("dve", lambda e: e.reciprocal(out=ss_ap[:, 1:2], in_=ss_ap[:, 1:2]), [tag + "_ss1"], [tag + "_ss1"])
    return ss_ap[:, 1:2]


G4, J8, HP, NS = 4, 8, 64, 128
SW = 2048
XW = 3072
GW = 2048
HIN = 9280


def proj_rows(c, T, tag, D, src, tiles, mods, Wd, ncols, outs, K, ST=1024, src_res="RES"):
    nc = c.nc
    KC = D // 128
    with contextlib.ExitStack() as es:
        sb = lambda n, s, d: es.enter_context(nc.sbuf_tensor(T(tag + n), s, d))
        ps = lambda n, s, d: es.enter_context(nc.psum_tensor(T(tag + n), s, d))
        idt = sb("idt", [128, 128], F32)
        At = sb("At", [128, D], F32)
        Bt = sb("Bt", [128, D], F32)
        xt = [sb(f"xt{i}", [128, D], F32) for i in range(2)]
        junk = sb("junk", [128, D], F32)
        ss = sb("ss", [128, 2], F32)
        h = sb("h", [128, D], F32)
        hT = sb("hT", [128, KC, ST], BF16)
        wt = [sb(f"wt{i}", [128, KC, 512], BF16) for i in range(2)]
        ot = [sb(f"ot{i}", [128, 512], F32) for i in range(3)]
        pT = [ps(f"pT{i}", [128, 512], F32) for i in range(2)]
        pm = [ps(f"pm{i}", [128, 512], F32) for i in range(3)]
        R = lambda s: T(tag + s)
        c.dma("sp", idt[:], K["ident"][:, :], writes=[R("idt")])
        sts = []
        cur = []
        curn = 0
        for tl in tiles:
            if curn + tl[1] > ST:
                sts.append(cur)
                cur, curn = [], 0
            cur.append(tl)
            curn += tl[1]
        if cur:
            sts.append(cur)
        cur_ms = None
        ti = 0
        oi = 0
        for st_tiles in sts:
            off = 0
            offs = []
            for (r0, n, ms) in st_tiles:
                b = ti % 2
                ti += 1
                if ms != cur_ms:
                    c.dma("sp", At[:], mods[ms]["A"].partition_broadcast(128), reads=["MODS"], writes=[R("At")])
                    c.dma("sp", Bt[:], mods[ms]["B"].partition_broadcast(128), reads=["MODS"], writes=[R("Bt")])
                    cur_ms = ms
                c.dma("sp", xt[b][:n], src[r0:r0 + n, :], reads=[src_res], writes=[R(f"xt{b}")])
                rstd = rmsnorm_rstd(c, xt[b][:n], junk[:n], ss[:n], D, [R(f"xt{b}")], R("n"))
                stt(c, "dve", h[:n], xt[b][:n], rstd[:n], At[:n], ALU.mult, ALU.mult, [R(f"xt{b}"), R("n_ss1"), R("At")], [R("h")])
                tt(c, "pool", h[:n], h[:n], Bt[:n], ALU.add, [R("h"), R("Bt")], [R("h")])
                gsz = 4 if KC % 4 == 0 else 2
                for kg in range(KC // gsz):
                    pb = kg % 2
                    for jj in range(gsz):
                        k = kg * gsz + jj
                        tr(c, pT[pb][:, jj * 128:jj * 128 + n], h[:n, k * 128:(k + 1) * 128], idt[:n, :n],
                           [R("h"), R("idt")], [R(f"pT{pb}")], inc=(jj == gsz - 1))
                    cp(c, "act" if kg % 2 == 0 else "dve", hT[:, kg * gsz:(kg + 1) * gsz, off:off + n],
                       pT[pb][:, 0:gsz * 128].rearrange("p (a b) -> p a b", a=gsz)[:, :, :n], [R(f"pT{pb}")], [R("hT")])
                offs.append(off)
                off += n
            nblk = (ncols + 511) // 512
            for nb in range(nblk):
                c0 = nb * 512
                w = min(512, ncols - c0)
                wb = nb % 2
                c.dma("pool", wt[wb][:, :, :w], Wd[:, c0:c0 + w].rearrange("(k p) n -> p k n", p=128), writes=[R(f"wt{wb}")])
                for (r0, n, ms), o in zip(st_tiles, offs):
                    pb = oi % 3
                    oi += 1
                    for k in range(KC):
                        mm(c, pm[pb][:n, :w], hT[:, k, o:o + n], wt[wb][:, k, :w], k == 0, k == KC - 1, [R("hT"), R(f"wt{wb}")], [R(f"pm{pb}")])
                    cp(c, "act" if pb != 1 else "dve", ot[pb][:n, :w], pm[pb][:n, :w], [R(f"pm{pb}")], [R(f"ot{pb}")])
                    for (d0, d1, dst, rofs, res) in outs:
                        lo = max(c0, d0)
                        hi = min(c0 + w, d1)
                        if lo < hi:
                            rr = rofs(r0)
                            c.dma("sp", dst[rr:rr + n, lo - d0:hi - d0], ot[pb][:n, lo - c0:hi - c0], reads=[R(f"ot{pb}")], writes=[res])
    c.barrier()


def hybrid_phase(c, tag, D, NCTX, NLAT, RES, mods, W, K, dbg=None):
    nc = c.nc
    T = lambda s: f"{tag}_{s}"
    NTOK = NCTX + NLAT
    NCH = NTOK // 128
    PZ = c.dram(T("PZ"), [NTOK, SW], F32)
    PX = c.dram(T("PX"), [NTOK + 4, XW], F32)
    PD = c.dram(T("PD"), [NTOK, 64], F32)
    PU = c.dram(T("PU"), [NTOK, GW], F32)
    PV = c.dram(T("PV"), [NTOK, GW], F32)
    XS = c.dram(T("XS"), [NTOK, SW], F32)
    BM = c.dram(T("BM"), [NTOK, 512], BF16)
    CTd = c.dram(T("CTd"), [NCH * 128, 512], BF16)
    CBF = c.dram(T("CBF"), [NCH * 128, 512], F32)
    CBB = c.dram(T("CBB"), [NCH * 128, 512], F32)
    DT = c.dram(T("DT"), [NTOK, 64], F32)
    Y = c.dram(T("Y"), [NTOK, SW], F32)
    YG = c.dram(T("YG"), [NTOK, GW], F32)
    MIX = c.dram(T("MIX"), [NTOK, 2 * SW], BF16)
    YM = c.dram(T("YM"), [NTOK, D], F32)

    def pxrow(r):
        return r + 1 if r < NCTX else r + 3

    tiles = [(r0, 128, "c" if r0 < NCTX else "l") for r0 in range(0, NTOK, 128)]
    ident_rows = lambda r: r
    outs = [(0, SW, PZ, ident_rows, T("PZ")), (SW, SW + XW, PX, pxrow, T("PX")), (SW + XW, SW + XW + 64, PD, ident_rows, T("PD")),
            (SW + XW + 64, SW + XW + 64 + GW, PU, ident_rows, T("PU")), (SW + XW + 64 + GW, HIN, PV, ident_rows, T("PV"))]
    m1 = {ms: {"A": mods[ms]["A1"], "B": mods[ms]["B1"]} for ms in mods}
    proj_rows(c, T, "pj_", D, RES, tiles, m1, W["w_in"], HIN, outs, K)

    with contextlib.ExitStack() as es:
        sb = lambda n, s, d: es.enter_context(nc.sbuf_tensor(T(n), s, d))
        ps = lambda n, s, d: es.enter_context(nc.psum_tensor(T(n), s, d))
        idt = sb("c_idt", [128, 128], F32)
        trif = sb("c_trif", [128, 128], F32)
        trib = sb("c_trib", [128, 128], F32)
        cw = sb("c_cw", [128, 3, XW], F32)
        cb = sb("c_cb", [128, XW], F32)
        dtb = sb("c_dtb", [128, 64], F32)
        zr = sb("c_zr", [4, XW], F32)
        xp = [sb(f"c_xp{i}", [128, XW], F32) for i in range(2)]
        xc = [sb(f"c_xc{i}", [128, XW], F32) for i in range(2)]
        xn = [sb(f"c_xn{i}", [128, XW], F32) for i in range(2)]
        acc = sb("c_acc", [128, XW], F32)
        sg = sb("c_sg", [128, XW], F32)
        bmb = sb("c_bmb", [128, 512], BF16)
        btb = sb("c_btb", [128, 512], BF16)
        ctb = sb("c_ctb", [128, 512], BF16)
        cbf = sb("c_cbf", [128, 512], F32)
        cbb = sb("c_cbb", [128, 512], F32)
        dtt = sb("c_dtt", [128, 64], F32)
        pB = ps("c_pB", [128, 512], F32)
        pC = ps("c_pC", [128, 512], F32)
        pCB = ps("c_pCB", [128, 512], F32)
        c.dma("sp", idt[:], K["ident"][:, :], writes=[T("c_idt")])
        c.dma("sp", trif[:], K["trif"][:, :], writes=[T("c_trif")])
        c.dma("sp", trib[:], K["trib"][:, :], writes=[T("c_trib")])
        for i in range(3):
            c.dma("sp", cw[:, i, :], W["conv_w"][i:i + 1, :].partition_broadcast(128), writes=[T("c_cw")])
        c.dma("sp", cb[:], W["conv_b"].partition_broadcast(128), writes=[T("c_cb")])
        c.dma("sp", dtb[:], W["dtb"].partition_broadcast(128), writes=[T("c_dtb")])
        mset(c, "pool", zr[:], 0.0, [T("c_zr")])
        for zrow in (0, NCTX + 1, NCTX + 2, NTOK + 3):
            c.dma("sp", PX[zrow:zrow + 1, :], zr[0:1, :], reads=[T("c_zr")], writes=[T("PX")])
        for ch in range(NCH):
            b = ch % 2
            r0 = ch * 128
            p0 = pxrow(r0)
            c.dma("sp", xp[b][:], PX[p0 - 1:p0 + 127, :], reads=[T("PX")], writes=[T(f"c_xp{b}")])
            c.dma("sp", xc[b][:], PX[p0:p0 + 128, :], reads=[T("PX")], writes=[T(f"c_xc{b}")])
            c.dma("sp", xn[b][:], PX[p0 + 1:p0 + 129, :], reads=[T("PX")], writes=[T(f"c_xn{b}")])
            tt(c, "dve", acc[:], xc[b][:], cw[:, 1, :], ALU.mult, [T(f"c_xc{b}"), T("c_cw")], [T("c_acc")])
            tt(c, "pool", xp[b][:], xp[b][:], cw[:, 0, :], ALU.mult, [T(f"c_xp{b}"), T("c_cw")], [T(f"c_xp{b}")])
            tt(c, "pool", xn[b][:], xn[b][:], cw[:, 2, :], ALU.mult, [T(f"c_xn{b}"), T("c_cw")], [T(f"c_xn{b}")])
            tt(c, "dve", acc[:], acc[:], cb[:], ALU.add, [T("c_acc"), T("c_cb")], [T("c_acc")])
            tt(c, "dve", acc[:], acc[:], xp[b][:], ALU.add, [T("c_acc"), T(f"c_xp{b}")], [T("c_acc")])
            tt(c, "dve", acc[:], acc[:], xn[b][:], ALU.add, [T("c_acc"), T(f"c_xn{b}")], [T("c_acc")])
            act(c, sg[:], acc[:], AF.Silu, [T("c_acc")], [T("c_sg")])
            c.dma("sp", XS[r0:r0 + 128, :], sg[:, 0:SW], reads=[T("c_sg")], writes=[T("XS")])
            cp(c, "pool", bmb[:], sg[:, SW:SW + 512], [T("c_sg")], [T("c_bmb")])
            c.dma("sp", BM[r0:r0 + 128, :], bmb[:], reads=[T("c_bmb")], writes=[T("BM")])
            for g in range(G4):
                tr(c, pB[:, g * 128:(g + 1) * 128], sg[:, SW + g * 128:SW + (g + 1) * 128], idt[:], [T("c_sg"), T("c_idt")], [T("c_pB")], inc=(g == 3))
            for g in range(G4):
                tr(c, pC[:, g * 128:(g + 1) * 128], sg[:, SW + 512 + g * 128:SW + 512 + (g + 1) * 128], idt[:], [T("c_sg"), T("c_idt")], [T("c_pC")], inc=(g == 3))
            cp(c, "act", btb[:], pB[:], [T("c_pB")], [T("c_btb")])
            cp(c, "act", ctb[:], pC[:], [T("c_pC")], [T("c_ctb")])
            c.dma("sp", CTd[r0:r0 + 128, :], ctb[:], reads=[T("c_ctb")], writes=[T("CTd")])
            for g in range(G4):
                mm(c, pCB[:, g * 128:(g + 1) * 128], btb[:, g * 128:(g + 1) * 128], ctb[:, g * 128:(g + 1) * 128], True, True,
                   [T("c_btb"), T("c_ctb")], [T("c_pCB")], inc=(g == 3))
            tt(c, "dve", cbf[:].rearrange("p (g q) -> p g q", g=4), pCB[:].rearrange("p (g q) -> p g q", g=4),
               trif[:].unsqueeze(1).to_broadcast([128, 4, 128]), ALU.mult, [T("c_pCB"), T("c_trif")], [T("c_cbf")])
            tt(c, "dve", cbb[:].rearrange("p (g q) -> p g q", g=4), pCB[:].rearrange("p (g q) -> p g q", g=4),
               trib[:].unsqueeze(1).to_broadcast([128, 4, 128]), ALU.mult, [T("c_pCB"), T("c_trib")], [T("c_cbb")])
            c.dma("sp", CBF[r0:r0 + 128, :], cbf[:], reads=[T("c_cbf")], writes=[T("CBF")])
            c.dma("sp", CBB[r0:r0 + 128, :], cbb[:], reads=[T("c_cbb")], writes=[T("CBB")])
            c.dma("sp", dtt[:], PD[r0:r0 + 128, :], reads=[T("PD")], writes=[T("c_dtt")])
            tt(c, "dve", dtt[:], dtt[:], dtb[:], ALU.add, [T("c_dtt"), T("c_dtb")], [T("c_dtt")])
            act(c, dtt[:], dtt[:], AF.Exp, [T("c_dtt")], [T("c_dtt")])
            act(c, dtt[:], dtt[:], AF.Ln, [T("c_dtt")], [T("c_dtt")], bias=1.0)
            c.dma("sp", DT[r0:r0 + 128, :], dtt[:], reads=[T("c_dtt")], writes=[T("DT")])
    c.barrier()

    ctx_ch = list(range(NCTX // 128))
    lat_ch = list(range(NCTX // 128, NCH))
    with contextlib.ExitStack() as es:
        sb = lambda n, s, d: es.enter_context(nc.sbuf_tensor(T(n), s, d))
        ps = lambda n, s, d: es.enter_context(nc.psum_tensor(T(n), s, d))
        ones = sb("s_ones", [128, 128], F32)
        tri = [sb("s_trif", [128, 128], F32), sb("s_trib", [128, 128], F32)]
        aneg = sb("s_aneg", [128, 64], F32)
        dsk = sb("s_dsk", [128, 32], F32)
        xs = [sb(f"s_xs{i}", [128, SW], F32) for i in range(2)]
        bm = [sb(f"s_bm{i}", [128, 512], BF16) for i in range(2)]
        ct = [sb(f"s_ct{i}", [128, 512], BF16) for i in range(2)]
        cbm = [sb(f"s_cbm{i}", [128, 512], F32) for i in range(2)]
        dtt = [sb(f"s_dt{i}", [128, 64], F32) for i in range(2)]
        a = sb("s_a", [128, 32], F32)
        acs = sb("s_acs", [128, 32], F32)
        tot = sb("s_tot", [128, 32], F32)
        dend = sb("s_dend", [128, 32], F32)
        eacs = sb("s_eacs", [128, 32], F32)
        cdec = sb("s_cdec", [128, 32], F32)
        xdt = sb("s_xdt", [128, SW], BF16)
        xdtd = sb("s_xdtd", [128, SW], BF16)
        X4 = [sb(f"s_X4{i}", [128, 512], F32) for i in range(2)]
        seg = [sb(f"s_seg{i}", [128, 512], F32) for i in range(2)]
        Lx = [sb(f"s_Lx{i}", [128, 512], F32) for i in range(2)]
        MT = [sb(f"s_MT{i}", [128, 512], BF16) for i in range(2)]
        Hs = sb("s_H", [128, G4 * 512], F32)
        Hb = sb("s_Hb", [128, G4 * 512], BF16)
        yo = sb("s_yo", [128, SW], F32)
        yacc = [sb(f"s_yacc{i}", [128, SW], F32) for i in range(2)]
        pa = ps("s_pa", [128, 64], F32)
        pR = [ps(f"s_pR{i}", [128, 512], F32) for i in range(2)]
        pY = ps("s_pY", [128, SW], F32)
        pO = ps("s_pO", [128, 512], F32)
        c.dma("sp", ones[:], K["ones"][:, :], writes=[T("s_ones")])
        c.dma("sp", tri[0][:], K["trif"][:, :], writes=[T("s_trif")])
        c.dma("sp", tri[1][:], K["trib"][:, :], writes=[T("s_trib")])
        c.dma("sp", aneg[:], W["alog"].partition_broadcast(128), writes=[T("s_aneg")])
        act(c, aneg[:], aneg[:], AF.Exp, [T("s_aneg")], [T("s_aneg")])
        ts(c, "dve", aneg[:], aneg[:], -1.0, None, ALU.mult, None, [T("s_aneg")], [T("s_aneg")])
        c.dma("sp", dsk[:], W["dsk"].partition_broadcast(128), writes=[T("s_dsk")])
        it = 0
        for d in range(2):
            CBd = CBF if d == 0 else CBB
            order = (ctx_ch + lat_ch) if d == 0 else (ctx_ch[::-1] + lat_ch[::-1])
            mset(c, "pool", Hs[:], 0.0, [T("s_H")])
            mset(c, "pool", Hb[:], 0.0, [T("s_Hb")])
            for ch in order:
                b = it % 2
                it += 1
                r0 = ch * 128
                c.dma("sp", xs[b][:], XS[r0:r0 + 128, :], reads=[T("XS")], writes=[T(f"s_xs{b}")])
                c.dma("sp", bm[b][:], BM[r0:r0 + 128, :], reads=[T("BM")], writes=[T(f"s_bm{b}")])
                c.dma("sp", ct[b][:], CTd[r0:r0 + 128, :], reads=[T("CTd")], writes=[T(f"s_ct{b}")])
                c.dma("sp", cbm[b][:], CBd[r0:r0 + 128, :], reads=[T("CBF"), T("CBB")], writes=[T(f"s_cbm{b}")])
                c.dma("sp", dtt[b][:], DT[r0:r0 + 128, :], reads=[T("DT")], writes=[T(f"s_dt{b}")])
                dtd = dtt[b][:, d * 32:(d + 1) * 32]
                tt(c, "dve", a[:], dtd, aneg[:, d * 32:(d + 1) * 32], ALU.mult, [T(f"s_dt{b}"), T("s_aneg")], [T("s_a")])
                mm(c, pa[:, 0:32], tri[d][:], a[:], True, True, [T(f"s_tri{'fb'[d]}"), T("s_a")], [T("s_pa")])
                mm(c, pa[:, 32:64], ones[:], a[:], True, True, [T("s_ones"), T("s_a")], [T("s_pa")])
                cp(c, "dve", acs[:], pa[:, 0:32], [T("s_pa")], [T("s_acs")])
                cp(c, "dve", tot[:], pa[:, 32:64], [T("s_pa")], [T("s_tot")])
                tt(c, "dve", dend[:], tot[:], acs[:], ALU.subtract, [T("s_tot"), T("s_acs")], [T("s_dend")])
                act(c, dend[:], dend[:], AF.Exp, [T("s_dend")], [T("s_dend")])
                act(c, eacs[:], acs[:], AF.Exp, [T("s_acs")], [T("s_eacs")])
                act(c, cdec[:], tot[:], AF.Exp, [T("s_tot")], [T("s_cdec")])
                tt(c, "pool", xdt[:].rearrange("p (h e) -> p h e", e=HP), xs[b][:].rearrange("p (h e) -> p h e", e=HP),
                   dtd.unsqueeze(2).to_broadcast([128, 32, HP]), ALU.mult, [T(f"s_xs{b}"), T(f"s_dt{b}")], [T("s_xdt")])
                tt(c, "pool", xdtd[:].rearrange("p (h e) -> p h e", e=HP), xdt[:].rearrange("p (h e) -> p h e", e=HP),
                   dend[:].unsqueeze(2).to_broadcast([128, 32, HP]), ALU.mult, [T("s_xdt"), T("s_dend")], [T("s_xdtd")])
                for hq in range(8):
                    g = hq // 2
                    q2 = hq % 2
                    tt(c, "dve", X4[q2][:].rearrange("p (h q) -> p h q", h=4), tri[d][:].unsqueeze(1).to_broadcast([128, 4, 128]),
                       a[:, hq * 4:(hq + 1) * 4].unsqueeze(2).to_broadcast([128, 4, 128]), ALU.mult,
                       [T(f"s_tri{'fb'[d]}"), T("s_a")], [T(f"s_X4{q2}")])
                    mm(c, pR[q2][:], ones[:], X4[q2][:], True, True, [T("s_ones"), T(f"s_X4{q2}")], [T(f"s_pR{q2}")])
                    for j4 in range(4):
                        hh = hq * 4 + j4
                        ts(c, "dve", seg[q2][:, j4 * 128:(j4 + 1) * 128], pR[q2][:, j4 * 128:(j4 + 1) * 128], acs[:, hh:hh + 1], 0.0,
                           ALU.subtract, ALU.min, [T(f"s_pR{q2}"), T("s_acs")], [T(f"s_seg{q2}")])
                    act(c, Lx[q2][:], seg[q2][:], AF.Exp, [T(f"s_seg{q2}")], [T(f"s_Lx{q2}")])
                    tt(c, "pool", MT[q2][:].rearrange("p (h q) -> p h q", h=4), Lx[q2][:].rearrange("p (h q) -> p h q", h=4),
                       cbm[b][:, g * 128:(g + 1) * 128].unsqueeze(1).to_broadcast([128, 4, 128]), ALU.mult,
                       [T(f"s_Lx{q2}"), T(f"s_cbm{b}")], [T(f"s_MT{q2}")])
                    for j4 in range(4):
                        hh = hq * 4 + j4
                        mm(c, pY[:, hh * HP:(hh + 1) * HP], MT[q2][:, j4 * 128:(j4 + 1) * 128], xdt[:, hh * HP:(hh + 1) * HP], True, True,
                           [T(f"s_MT{q2}"), T("s_xdt")], [T("s_pY")], inc=(j4 == 3))
                if d == 0:
                    tt(c, "pool", yacc[b][:].rearrange("p (h e) -> p h e", e=HP), xs[b][:].rearrange("p (h e) -> p h e", e=HP),
                       dsk[:].unsqueeze(2).to_broadcast([128, 32, HP]), ALU.mult, [T(f"s_xs{b}"), T("s_dsk")], [T(f"s_yacc{b}")])
                else:
                    c.dma("sp", yacc[b][:], Y[r0:r0 + 128, :], reads=[T("Y")], writes=[T(f"s_yacc{b}")])
                for g in range(G4):
                    mm(c, pO[:], ct[b][:, g * 128:(g + 1) * 128], Hb[:, g * 512:(g + 1) * 512], True, True, [T(f"s_ct{b}"), T("s_Hb")], [T("s_pO")])
                    tt(c, "dve", yo[:, g * 512:(g + 1) * 512].rearrange("p (h e) -> p h e", e=HP), pO[:].rearrange("p (h e) -> p h e", e=HP),
                       eacs[:, g * 8:(g + 1) * 8].unsqueeze(2).to_broadcast([128, 8, HP]), ALU.mult, [T("s_pO"), T("s_eacs")], [T("s_yo")])
                tt(c, "dve", yo[:], yo[:], pY[:], ALU.add, [T("s_yo"), T("s_pY")], [T("s_yo")])
                tt(c, "pool", yacc[b][:], yacc[b][:], yo[:], ALU.add, [T(f"s_yacc{b}"), T("s_yo")], [T(f"s_yacc{b}")])
                c.dma("sp", Y[r0:r0 + 128, :], yacc[b][:], reads=[T(f"s_yacc{b}")], writes=[T("Y")])
                for g in range(G4):
                    mm(c, pO[:], bm[b][:, g * 128:(g + 1) * 128], xdtd[:, g * 512:(g + 1) * 512], True, True, [T(f"s_bm{b}"), T("s_xdtd")], [T("s_pO")])
                    tt(c, "dve", Hs[:, g * 512:(g + 1) * 512].rearrange("p (h e) -> p h e", e=HP),
                       Hs[:, g * 512:(g + 1) * 512].rearrange("p (h e) -> p h e", e=HP),
                       cdec[:, g * 8:(g + 1) * 8].unsqueeze(2).to_broadcast([128, 8, HP]), ALU.mult, [T("s_H"), T("s_cdec")], [T("s_H")])
                    tt(c, "dve", Hs[:, g * 512:(g + 1) * 512], Hs[:, g * 512:(g + 1) * 512], pO[:], ALU.add, [T("s_H"), T("s_pO")], [T("s_H")])
                cp(c, "act", Hb[:], Hs[:], [T("s_H")], [T("s_Hb")])
    c.barrier()

    with contextlib.ExitStack() as es:
        sb = lambda n, s, d: es.enter_context(nc.sbuf_tensor(T(n), s, d))
        ps = lambda n, s, d: es.enter_context(nc.psum_tensor(T(n), s, d))
        wst = sb("g_wst", [128, 16, 128], BF16)
        bst = sb("g_bst", [128, 16], F32)
        vng = sb("g_vng", [128, GW], F32)
        sng = sb("g_sng", [128, SW], F32)
        u = [sb(f"g_u{i}", [128, GW], F32) for i in range(2)]
        v = [sb(f"g_v{i}", [128, GW], F32) for i in range(2)]
        z = [sb(f"g_z{i}", [128, SW], F32) for i in range(2)]
        y = [sb(f"g_y{i}", [128, SW], F32) for i in range(2)]
        junk = sb("g_junk", [128, GW], F32)
        ss = sb("g_ss", [128, 2], F32)
        ss2 = sb("g_ss2", [128, 2], F32)
        vn = sb("g_vn", [128, GW], BF16)
        mix = [sb(f"g_mix{i}", [128, 2 * SW], BF16) for i in range(2)]
        sgm = sb("g_sgm", [128, GW], F32)
        pS = ps("g_pS", [128, GW], F32)
        c.dma("pool", wst[:], W["wsT"].rearrange("g k q -> k g q"), writes=[T("g_wst")])
        c.dma("sp", bst[:], W["bsT"][:, :], writes=[T("g_bst")])
        c.dma("sp", vng[:], W["vng"].partition_broadcast(128), writes=[T("g_vng")])
        c.dma("sp", sng[:], W["ssdg"].partition_broadcast(128), writes=[T("g_sng")])
        for ch in range(NCH):
            b = ch % 2
            r0 = ch * 128
            c.dma("sp", u[b][:], PU[r0:r0 + 128, :], reads=[T("PU")], writes=[T(f"g_u{b}")])
            c.dma("sp", v[b][:], PV[r0:r0 + 128, :], reads=[T("PV")], writes=[T(f"g_v{b}")])
            c.dma("sp", z[b][:], PZ[r0:r0 + 128, :], reads=[T("PZ")], writes=[T(f"g_z{b}")])
            c.dma("sp", y[b][:], Y[r0:r0 + 128, :], reads=[T("Y")], writes=[T(f"g_y{b}")])
            act(c, u[b][:], u[b][:], AF.Gelu_apprx_tanh, [T(f"g_u{b}")], [T(f"g_u{b}")])
            act(c, v[b][:], v[b][:], AF.Gelu_apprx_tanh, [T(f"g_v{b}")], [T(f"g_v{b}")])
            rstd = rmsnorm_rstd(c, v[b][:], junk[:], ss[:], GW, [T(f"g_v{b}")], T("g_n1"))
            stt(c, "dve", vn[:], v[b][:], rstd, vng[:], ALU.mult, ALU.mult, [T(f"g_v{b}"), T("g_n1_ss1"), T("g_vng")], [T("g_vn")])
            for gg in range(16):
                mm(c, pS[:, gg * 128:(gg + 1) * 128], wst[:, gg, :], vn[:, gg * 128:(gg + 1) * 128], True, True, [T("g_wst"), T("g_vn")], [T("g_pS")], inc=(gg == 15))
            tt(c, "dve", sgm[:].rearrange("p (g e) -> p g e", g=16), pS[:].rearrange("p (g e) -> p g e", g=16),
               bst[:].unsqueeze(2).to_broadcast([128, 16, 128]), ALU.add, [T("g_pS"), T("g_bst")], [T("g_sgm")])
            tt(c, "pool", mix[b][:, SW:2 * SW], sgm[:], u[b][:], ALU.mult, [T("g_sgm"), T(f"g_u{b}")], [T(f"g_mix{b}")])
            act(c, z[b][:], z[b][:], AF.Silu, [T(f"g_z{b}")], [T(f"g_z{b}")])
            tt(c, "dve", y[b][:], y[b][:], z[b][:], ALU.mult, [T(f"g_y{b}"), T(f"g_z{b}")], [T(f"g_y{b}")])
            rstd2 = rmsnorm_rstd(c, y[b][:], junk[:], ss2[:], SW, [T(f"g_y{b}")], T("g_n2"))
            stt(c, "dve", mix[b][:, 0:SW], y[b][:], rstd2, sng[:], ALU.mult, ALU.mult, [T(f"g_y{b}"), T("g_n2_ss1"), T("g_sng")], [T(f"g_mix{b}")])
            c.dma("sp", MIX[r0:r0 + 128, :], mix[b][:], reads=[T(f"g_mix{b}")], writes=[T("MIX")])
    c.barrier()
    if dbg is not None:
        dbg(dict(Y=Y, MIX=MIX, PZ=PZ, XS=XS, DT=DT))
    out_proj_res(c, T, "op_", D, 2 * SW, MIX, W["w_out"], RES, tiles, {ms: mods[ms]["G1"] for ms in mods}, K)


def out_proj_res(c, T, tag, D, KIN, SRC, Wd, RES, tiles, gates, K, ST=1024):
    nc = c.nc
    KC = KIN // 128
    R = lambda s: T(tag + s)
    with contextlib.ExitStack() as es:
        sb = lambda n, s, d: es.enter_context(nc.sbuf_tensor(R(n), s, d))
        ps = lambda n, s, d: es.enter_context(nc.psum_tensor(R(n), s, d))
        idb = sb("idb", [128, 128], BF16)
        idf = sb("idf", [128, 128], F32)
        Gm = sb("Gm", [128, D], F32)
        xin = [sb(f"xin{i}", [128, KIN], BF16) for i in range(2)]
        xT = sb("xT", [128, KC, ST], BF16)
        wt = [sb(f"wt{i}", [128, KC, 512], BF16) for i in range(2)]
        rr = [sb(f"rr{i}", [128, 512], F32) for i in range(2)]
        ot = [sb(f"ot{i}", [128, 512], F32) for i in range(2)]
        ptr = [ps(f"ptr{i}", [128, 1024], BF16) for i in range(2)]
        pm = [ps(f"pm{i}", [128, 512], F32) for i in range(2)]
        c.dma("sp", idf[:], K["ident"][:, :], writes=[R("idf")])
        cp(c, "dve", idb[:], idf[:], [R("idf")], [R("idb")])
        sts = []
        cur, curn = [], 0
        for tl in tiles:
            if curn + tl[1] > ST:
                sts.append(cur)
                cur, curn = [], 0
            cur.append(tl)
            curn += tl[1]
        if cur:
            sts.append(cur)
        ti = 0
        oi = 0
        for st_tiles in sts:
            off = 0
            offs = []
            for (r0, n, ms) in st_tiles:
                b = ti % 2
                ti += 1
                c.dma("sp", xin[b][:n], SRC[r0:r0 + n, :], reads=[R("SRC")], writes=[R(f"xin{b}")])
                for kg in range(KC // 8):
                    pb = kg % 2
                    for jj in range(8):
                        k = kg * 8 + jj
                        tr(c, ptr[pb][:, jj * 128:jj * 128 + n], xin[b][:n, k * 128:(k + 1) * 128], idb[:n, :n], [R(f"xin{b}"), R("idb")], [R(f"ptr{pb}")], inc=(jj == 7))
                    cp(c, "act" if pb == 0 else "dve", xT[:, kg * 8:(kg + 1) * 8, off:off + n],
                       ptr[pb][:].rearrange("p (a b) -> p a b", a=8)[:, :, :n], [R(f"ptr{pb}")], [R("xT")])
                offs.append(off)
                off += n
            for nb in range(D // 512):
                wb = nb % 2
                c.dma("pool", wt[wb][:], Wd[:, nb * 512:(nb + 1) * 512].rearrange("(k p) n -> p k n", p=128), writes=[R(f"wt{wb}")])
                cur_ms = None
                for (r0, n, ms), o in zip(st_tiles, offs):
                    pb = oi % 2
                    oi += 1
                    if ms != cur_ms:
                        c.dma("sp", Gm[:], gates[ms].partition_broadcast(128), reads=["MODS"], writes=[R("Gm")])
                        cur_ms = ms
                    for k in range(KC):
                        mm(c, pm[pb][:n, :], xT[:, k, o:o + n], wt[wb][:, k, :], k == 0, k == KC - 1, [R("xT"), R(f"wt{wb}")], [R(f"pm{pb}")])
                    c.dma("sp", rr[pb][:n], RES[r0:r0 + n, nb * 512:(nb + 1) * 512], reads=["RES"], writes=[R(f"rr{pb}")])
                    tt(c, "dve", ot[pb][:n], pm[pb][:n, :], Gm[:n, nb * 512:(nb + 1) * 512], ALU.mult, [R(f"pm{pb}"), R("Gm")], [R(f"ot{pb}")])
                    tt(c, "pool", ot[pb][:n], ot[pb][:n], rr[pb][:n], ALU.add, [R(f"ot{pb}"), R(f"rr{pb}")], [R(f"ot{pb}")])
                    c.dma("sp", RES[r0:r0 + n, nb * 512:(nb + 1) * 512], ot[pb][:n], reads=[R(f"ot{pb}")], writes=["RES"])
    c.barrier()


NH = 16
QL, KVL, DN, DR, DV = 768, 512, 128, 64, 128
SCALE = (DN + DR) ** -0.5


def mla_phase(c, tag, D, NCTX, NLAT, RES, mods, W, K, dbg=None, upto=None):
    nc = c.nc
    T = lambda s: f"{tag}_{s}"
    NTOK = NCTX + NLAT
    NKT = NTOK // 128
    NQT = NLAT // 128
    PQ = c.dram(T("PQ"), [NTOK, QL], F32)
    PKV = c.dram(T("PKV"), [NTOK, KVL + DR], F32)
    QD = c.dram(T("QD"), [NTOK, NH * 192], F32)
    QH = c.dram(T("QH"), [NH, NLAT, 192], F32)
    QSQ = c.dram(T("QSQ"), [NLAT, NH], F32)
    OD = c.dram(T("OD"), [NTOK, NH * DV], BF16)
    tiles = [(r0, 128, "c" if r0 < NCTX else "l") for r0 in range(0, NTOK, 128)]
    lat_tiles = [t for t in tiles if t[2] == "l"]
    m1 = {ms: {"A": mods[ms]["A1"], "B": mods[ms]["B1"]} for ms in mods}
    proj_rows(c, T, "pj_", D, RES, tiles, m1, W["w_in"], QL + KVL + DR, [(0, QL, PQ, lambda r: r, T("PQ")), (QL, QL + KVL + DR, PKV, lambda r: r, T("PKV"))], K)
    mq = {"l": {"A": W["qng"], "B": W["zero768"]}}
    proj_rows(c, T, "pq_", QL, PQ, lat_tiles, mq, W["w_uq"], NH * 192, [(0, NH * 192, QD, lambda r: r, T("QD"))], K, src_res=T("PQ"))

    if upto == "proj":
        return
    with contextlib.ExitStack() as es0:
        sb0 = lambda n, s, d: es0.enter_context(nc.sbuf_tensor(T(n), s, d))
        ckvT = sb0("ckvT", [128, 4, NTOK], BF16)
        kpT = sb0("kpT", [65, NTOK], BF16)
        kpsq = sb0("kpsq", [128, NTOK], F32)
        idt = sb0("idt", [128, 128], F32)
        ones = sb0("ones", [128, 128], F32)
        c.dma("sp", idt[:], K["ident"][:, :], writes=[T("idt")])
        c.dma("sp", ones[:], K["ones"][:, :], writes=[T("ones")])
        with contextlib.ExitStack() as es:
            sb = lambda n, s, d: es.enter_context(nc.sbuf_tensor(T(n), s, d))
            ps = lambda n, s, d: es.enter_context(nc.psum_tensor(T(n), s, d))
            kvg = sb("a_kvg", [128, KVL], F32)
            kv = [sb(f"a_kv{i}", [128, KVL + DR], F32) for i in range(2)]
            junk = sb("a_junk", [128, KVL], F32)
            ss = sb("a_ss", [128, 2], F32)
            kn = sb("a_kn", [128, KVL], F32)
            cs = [sb(f"a_cs{i}", [128, 64], F32) for i in range(2)]
            kr = sb("a_kr", [128, DR], F32)
            t1 = sb("a_t1", [128, 32], F32)
            t2 = sb("a_t2", [128, 32], F32)
            sq2 = sb("a_sq2", [64, 128], F32)
            qt = [sb(f"a_qt{i}", [128, NH * 192], F32) for i in range(2)]
            qr = sb("a_qr", [128, NH * 192], F32)
            q1 = sb("a_q1", [128, NH * 32], F32)
            q2 = sb("a_q2", [128, NH * 32], F32)
            qq = sb("a_qq", [128, NH * 192], F32)
            qs = sb("a_qs", [128, NH], F32)
            pT = [ps(f"a_pT{i}", [128, 512], F32) for i in range(2)]
            pk = ps("a_pk", [64, 128], F32)
            pn = ps("a_pn", [128, 128], F32)
            c.dma("sp", kvg[:], W["kvng"].partition_broadcast(128), writes=[T("a_kvg")])
            mset(c, "pool", kpT[64:65, :], 1.0, [T("kpT")])
            for ti, (r0, n, ms) in enumerate(tiles):
                b = ti % 2
                c.dma("sp", kv[b][:], PKV[r0:r0 + 128, :], reads=[T("PKV")], writes=[T(f"a_kv{b}")])
                rstd = rmsnorm_rstd(c, kv[b][:, 0:KVL], junk[:], ss[:], KVL, [T(f"a_kv{b}")], T("a_n"))
                stt(c, "dve", kn[:], kv[b][:, 0:KVL], rstd, kvg[:], ALU.mult, ALU.mult, [T(f"a_kv{b}"), T("a_n_ss1"), T("a_kvg")], [T("a_kn")])
                for k in range(4):
                    tr(c, pT[b][:, k * 128:(k + 1) * 128], kn[:, k * 128:(k + 1) * 128], idt[:], [T("a_kn"), T("idt")], [T(f"a_pT{b}")], inc=(k == 3))
                cp(c, "act", ckvT[:, :, r0:r0 + 128], pT[b][:].rearrange("p (a b) -> p a b", a=4), [T(f"a_pT{b}")], [T("ckvT")])
                kp = kv[b][:, KVL:KVL + DR]
                if ms == "l":
                    t0 = r0 - NCTX
                    c.dma("sp", cs[b][:, 0:32], W["cos"][t0:t0 + 128, :], writes=[T(f"a_cs{b}")])
                    c.dma("sp", cs[b][:, 32:64], W["sin"][t0:t0 + 128, :], writes=[T(f"a_cs{b}")])
                    kp4 = kp.rearrange("p (a h e) -> p a h e", a=2, h=2)
                    kr4 = kr[:].rearrange("p (a h e) -> p a h e", a=2, h=2)
                    co = cs[b][:, 0:32].rearrange("p (a e) -> p a e", a=2)
                    si = cs[b][:, 32:64].rearrange("p (a e) -> p a e", a=2)
                    t1v = t1[:].rearrange("p (a e) -> p a e", a=2)
                    t2v = t2[:].rearrange("p (a e) -> p a e", a=2)
                    rd = [T(f"a_kv{b}"), T(f"a_cs{b}")]
                    tt(c, "dve", t1v, kp4[:, :, 0, :], co, ALU.mult, rd, [T("a_t1")])
                    tt(c, "dve", t2v, kp4[:, :, 1, :], si, ALU.mult, rd, [T("a_t2")])
                    tt(c, "dve", kr4[:, :, 0, :], t1v, t2v, ALU.subtract, [T("a_t1"), T("a_t2")], [T("a_kr")])
                    tt(c, "dve", t1v, kp4[:, :, 0, :], si, ALU.mult, rd, [T("a_t1")])
                    tt(c, "dve", t2v, kp4[:, :, 1, :], co, ALU.mult, rd, [T("a_t2")])
                    tt(c, "dve", kr4[:, :, 1, :], t1v, t2v, ALU.add, [T("a_t1"), T("a_t2")], [T("a_kr")])
                else:
                    cp(c, "dve", kr[:], kp, [T(f"a_kv{b}")], [T("a_kr")])
                tr(c, pk[:, :], kr[:], idt[:], [T("a_kr"), T("idt")], [T("a_pk")])
                cp(c, "act", kpT[0:64, r0:r0 + 128], pk[:, :], [T("a_pk")], [T("kpT")])
                act(c, sq2[:, :], pk[:, :], AF.Square, [T("a_pk")], [T("a_sq2")])
                mm(c, pn[:, :], ones[0:64, :], sq2[:, :], True, True, [T("ones"), T("a_sq2")], [T("a_pn")])
                cp(c, "dve", kpsq[:, r0:r0 + 128], pn[:, :], [T("a_pn")], [T("kpsq")])
                if ms == "l":
                    t0 = r0 - NCTX
                    c.dma("sp", qt[b][:], QD[r0:r0 + 128, :], reads=[T("QD")], writes=[T(f"a_qt{b}")])
                    q3 = qt[b][:].rearrange("p (h e) -> p h e", h=NH)
                    qr3 = qr[:].rearrange("p (h e) -> p h e", h=NH)
                    cp(c, "pool", qr3[:, :, 0:DN], q3[:, :, 0:DN], [T(f"a_qt{b}")], [T("a_qr")])
                    rd = [T(f"a_qt{b}"), T(f"a_cs{b}")]
                    for ax in range(2):
                        x1 = q3[:, :, DN + ax * 32:DN + ax * 32 + 16]
                        x2 = q3[:, :, DN + ax * 32 + 16:DN + ax * 32 + 32]
                        o1 = qr3[:, :, DN + ax * 32:DN + ax * 32 + 16]
                        o2 = qr3[:, :, DN + ax * 32 + 16:DN + ax * 32 + 32]
                        co = cs[b][:, ax * 16:(ax + 1) * 16].unsqueeze(1).to_broadcast([128, NH, 16])
                        si = cs[b][:, 32 + ax * 16:32 + (ax + 1) * 16].unsqueeze(1).to_broadcast([128, NH, 16])
                        a1 = q1[:, 0:NH * 16].rearrange("p (h e) -> p h e", h=NH)
                        a2 = q2[:, 0:NH * 16].rearrange("p (h e) -> p h e", h=NH)
                        tt(c, "dve", a1, x1, co, ALU.mult, rd, [T("a_q1")])
                        tt(c, "dve", a2, x2, si, ALU.mult, rd, [T("a_q2")])
                        tt(c, "dve", o1, a1, a2, ALU.subtract, [T("a_q1"), T("a_q2")], [T("a_qr")])
                        tt(c, "dve", a1, x1, si, ALU.mult, rd, [T("a_q1")])
                        tt(c, "dve", a2, x2, co, ALU.mult, rd, [T("a_q2")])
                        tt(c, "dve", o2, a1, a2, ALU.add, [T("a_q1"), T("a_q2")], [T("a_qr")])
                    tt(c, "pool", qq[:], qr[:], qr[:], ALU.mult, [T("a_qr")], [T("a_qq")])
                    red(c, "dve", qs[:], qq[:].rearrange("p (h e) -> p h e", h=NH), ALU.add, [T("a_qq")], [T("a_qs")])
                    c.dma("sp", QSQ[t0:t0 + 128, :], qs[:], reads=[T("a_qs")], writes=[T("QSQ")])
                    c.dma("sp", QH[:, t0:t0 + 128, :].rearrange("h t e -> t h e"), qr3, reads=[T("a_qr")], writes=[T("QH")])
        c.barrier()
        if upto == "A2":
            return

        with contextlib.ExitStack() as es:
            sb = lambda n, s, d: es.enter_context(nc.sbuf_tensor(T(n), s, d))
            ps = lambda n, s, d: es.enter_context(nc.psum_tensor(T(n), s, d))
            wuk = sb("h_wuk", [128, 4, DN], BF16)
            wuv = sb("h_wuv", [128, 4, DV], BF16)
            KnT = sb("h_KnT", [128, NTOK], BF16)
            Vt = sb("h_Vt", [128, NKT, 132], BF16)
            sqk = sb("h_sqk", [128, 512], F32)
            ksq = sb("h_ksq", [128, NTOK], F32)
            km = sb("h_km", [128, 2], F32)
            kmb = sb("h_kmb", [128, 1], F32)
            qh = [sb(f"h_qh{i}", [128, 193], F32) for i in range(2)]
            qsq = [sb(f"h_qsq{i}", [128, NH], F32) for i in range(2)]
            nq = sb("h_nq", [128, 2], F32)
            QnT = [sb(f"h_QnT{i}", [128, 512], BF16) for i in range(2)]
            QpT = [sb(f"h_QpT{i}", [65, 512], BF16) for i in range(2)]
            PT = [sb(f"h_PT{i}", [128, 512], BF16) for i in range(3)]
            rc = sb("h_rc", [128, 4], F32)
            ob = [sb(f"h_ob{i}", [128, DV], BF16) for i in range(2)]
            pS = [ps(f"h_pS{i}", [128, 512], F32) for i in range(2)]
            pO = [ps(f"h_pO{i}", [128, 512], F32) for i in range(4)]
            pX = [ps(f"h_pX{i}", [128, 512], F32) for i in range(2)]
            mset(c, "dve", Vt[:], 1.0, [T("h_Vt")])
            pti = 0
            if upto == "K0":
                c.barrier()
                return
            for h in range(NH):
                c.dma("pool", wuk[:], W["w_uk"][:, h * DN:(h + 1) * DN].rearrange("(k p) n -> p k n", p=128), writes=[T("h_wuk")])
                c.dma("pool", wuv[:], W["w_uv"][:, h * DV:(h + 1) * DV].rearrange("(k p) n -> p k n", p=128), writes=[T("h_wuv")])
                if upto == "K0b":
                    c.barrier()
                    return
                nkb = (NTOK + 511) // 512
                for kb in range(nkb):
                    w = min(512, NTOK - kb * 512)
                    pb = kb % 2
                    for k in range(4):
                        mm(c, pX[pb][:, :w], wuk[:, k, :], ckvT[:, k, kb * 512:kb * 512 + w], k == 0, k == 3, [T("h_wuk"), T("ckvT")], [T(f"h_pX{pb}")])
                    cp(c, "dve", KnT[:, kb * 512:kb * 512 + w], pX[pb][:, :w], [T(f"h_pX{pb}")], [T("h_KnT")])
                    if upto == "K1a":
                        continue
                    act(c, sqk[:, :w], KnT[:, kb * 512:kb * 512 + w], AF.Square, [T("h_KnT")], [T("h_sqk")])
                    if upto == "K1b":
                        continue
                    mm(c, pS[pb][:, :w], ones[:, :], sqk[:, :w], True, True, [T("ones"), T("h_sqk")], [T(f"h_pS{pb}")])
                    tt(c, "dve", ksq[:, kb * 512:kb * 512 + w], pS[pb][:, :w], kpsq[:, kb * 512:kb * 512 + w], ALU.add, [T(f"h_pS{pb}"), T("kpsq")], [T("h_ksq")])
                if upto in ("K1", "K1a", "K1b"):
                    c.barrier()
                    return
                red(c, "dve", km[:, 0:1], ksq[:, :], ALU.max, [T("h_ksq")], [T("h_km")])
                act(c, km[:, 1:2], km[:, 0:1], AF.Sqrt, [T("h_km")], [T("h_km1")])
                ts(c, "dve", kmb[:], km[:, 1:2], -1.0, None, ALU.mult, None, [T("h_km1")], [T("h_kmb")])
                if upto == "K2":
                    c.barrier()
                    return
                for kg in range((NKT + 3) // 4):
                    pb = kg % 2
                    nk = min(4, NKT - kg * 4)
                    for j in range(nk):
                        kt = kg * 4 + j
                        for k in range(4):
                            mm(c, pX[pb][:, j * 128:(j + 1) * 128], ckvT[:, k, kt * 128:(kt + 1) * 128], wuv[:, k, :], k == 0, k == 3,
                               [T("ckvT"), T("h_wuv")], [T(f"h_pX{pb}")], inc=(k == 3 and j == nk - 1))
                    cp(c, "act" if pb == 0 else "dve", Vt[:, kg * 4:kg * 4 + nk, 0:DV], pX[pb][:, 0:nk * 128].rearrange("p (a b) -> p a b", a=nk),
                       [T(f"h_pX{pb}")], [T("h_Vt")])
                if upto == "KV":
                    c.barrier()
                    return
                for qb in range(NLAT // 512):
                    qi = qb % 2
                    for s4 in range(4):
                        t0 = qb * 512 + s4 * 128
                        b = s4 % 2
                        c.dma("sp", qh[b][:, 0:192], QH[h, t0:t0 + 128, :], reads=[T("QH")], writes=[T(f"h_qh{b}")])
                        c.dma("sp", qsq[b][:], QSQ[t0:t0 + 128, :], reads=[T("QSQ")], writes=[T(f"h_qsq{b}")])
                        act(c, nq[:, 0:1], qsq[b][:, h:h + 1], AF.Sqrt, [T(f"h_qsq{b}")], [T("h_nq")])
                        ts(c, "dve", qh[b][:, 192:193], nq[:, 0:1], kmb[:, 0:1], None, ALU.mult, None, [T("h_nq"), T("h_kmb")], [T(f"h_qh{b}")])
                        tr(c, pX[0][:, s4 * 128:(s4 + 1) * 128], qh[b][:, 0:DN], idt[:], [T(f"h_qh{b}"), T("idt")], [T("h_pX0")], inc=False)
                        tr(c, pX[1][0:65, s4 * 128:(s4 + 1) * 128], qh[b][:, DN:193], idt[:], [T(f"h_qh{b}"), T("idt")], [T("h_pX1")])
                    cp(c, "act", QnT[qi][:], pX[0][:], [T("h_pX0")], [T(f"h_QnT{qi}")])
                    cp(c, "dve", QpT[qi][:], pX[1][0:65, :], [T("h_pX1")], [T(f"h_QpT{qi}")])
                    for kt in range(NKT):
                        sbi = kt % 2
                        p3 = pti % 3
                        pti += 1
                        mm(c, pS[sbi][:], KnT[:, kt * 128:(kt + 1) * 128], QnT[qi][:], True, False, [T("h_KnT"), T(f"h_QnT{qi}")], [T(f"h_pS{sbi}")], inc=False)
                        mm(c, pS[sbi][:], kpT[:, kt * 128:(kt + 1) * 128], QpT[qi][:], False, True, [T("kpT"), T(f"h_QpT{qi}")], [T(f"h_pS{sbi}")])
                        act(c, PT[p3][:], pS[sbi][:], AF.Exp, [T(f"h_pS{sbi}")], [T(f"h_PT{p3}")], scale=SCALE)
                        for s4 in range(4):
                            mm(c, pO[s4][:, 0:129], PT[p3][:, s4 * 128:(s4 + 1) * 128], Vt[:, kt, 0:129], kt == 0, kt == NKT - 1,
                               [T(f"h_PT{p3}"), T("h_Vt")], [T(f"h_pO{s4}")], inc=(kt == NKT - 1 or s4 == 3))
                    for s4 in range(4):
                        t0 = qb * 512 + s4 * 128
                        b = s4 % 2
                        c.op("dve", lambda e: e.reciprocal(out=rc[:, s4:s4 + 1], in_=pO[s4][:, 128:129]), [T(f"h_pO{s4}")], [T("h_rc")])
                        ts(c, "dve", ob[b][:], pO[s4][:, 0:DV], rc[:, s4:s4 + 1], None, ALU.mult, None, [T(f"h_pO{s4}"), T("h_rc")], [T(f"h_ob{b}")])
                        c.dma("sp", OD[NCTX + t0:NCTX + t0 + 128, h * DV:(h + 1) * DV], ob[b][:], reads=[T(f"h_ob{b}")], writes=[T("OD")])
        c.barrier()
    c.barrier()
    if dbg is not None:
        dbg(dict(OD=OD, QH=QH, PKV=PKV))
    out_proj_res(c, T, "op_", D, NH * DV, OD, W["w_o"], RES, lat_tiles, {"l": mods["l"]["G1"]}, K)


NE = 32


def moe_local(c, tag, D, FF, NT, RES, tiles, mods, W, K, dbg=None):
    nc = c.nc
    KC = D // 128
    FC = FF // 128
    NBLK = (4 * NT + 511) // 512 + NE
    NSLOT = NBLK * 512
    assert NSLOT % 128 == 0
    T = lambda s: f"{tag}_{s}"
    HF = c.dram(T("HF"), [NT + 128, D], BF16)
    GD = c.dram(T("GD"), [NT, NE], F32)
    LISTF = c.dram(T("LISTF"), [NSLOT, 2], F32)
    ACC = c.dram(T("ACC"), [NT + 128, D], F32)
    W1G2 = W["w1gT"]
    W1L2 = W["w1lT"]
    W22 = W["w2T"]
    c.barrier()
    with contextlib.ExitStack() as es0:
        sb0 = lambda n, s, d: es0.enter_context(nc.sbuf_tensor(T(n), s, d))
        idt = sb0("idt", [128, 128], F32)
        ones = sb0("ones", [128, 128], F32)
        trif = sb0("trif", [128, 128], F32)
        iop = sb0("iop", [128, 1], F32)
        iob = sb0("iob", [128, NBLK], F32)
        cnt = sb0("cnt", [128, NE], F32)
        base = sb0("base", [128, NE], F32)
        widx = sb0("widx", [128, NBLK], I32)
        eidx = sb0("eidx", [128, NBLK], I32)
        widx1 = sb0("widx1", [128, FC, NBLK], I32)
        c.dma("sp", idt[:], K["ident"][:, :], writes=[T("idt")])
        c.dma("sp", ones[:], K["ones"][:, :], writes=[T("ones")])
        c.dma("sp", trif[:], K["trif"][:, :], writes=[T("trif")])
        c.dma("sp", iop[:], K["iota_p"][:, :], writes=[T("iop")])
        c.dma("sp", iob[:], K["iota_b"].partition_broadcast(128), writes=[T("iob")])
        with contextlib.ExitStack() as es:
            sb = lambda n, s, d: es.enter_context(nc.sbuf_tensor(T(n), s, d))
            ps = lambda n, s, d: es.enter_context(nc.psum_tensor(T(n), s, d))
            At = sb("At", [128, D], F32)
            Bt = sb("Bt", [128, D], F32)
            rwt = sb("rwt", [128, KC, NE], F32)
            rbt = sb("rbt", [128, NE], F32)
            xt = [sb(f"xt{i}", [128, D], F32) for i in range(2)]
            junk = sb("junk", [128, D], F32)
            ss = sb("ss", [128, 2], F32)
            h = sb("h", [128, D], F32)
            hb = [sb(f"hb{i}", [128, D], BF16) for i in range(2)]
            hT = sb("hT", [128, KC, 128], F32)
            lg = sb("lg", [128, NE], F32)
            m8 = sb("m8", [128, 8], F32)
            sm = sb("sm", [128, 4], F32)
            ex = sb("ex", [128, NE], F32)
            mk = [sb(f"mk{i}", [128, NE], F32) for i in range(2)]
            Gt = [sb(f"Gt{i}", [128, NE], F32) for i in range(2)]
            zt = sb("zt", [128, D], F32)
            zb = sb("zb", [128, D], BF16)
            lf = sb("lf", [128, NSLOT // 128, 2], F32)
            pT = [ps(f"pT{i}", [128, 512], F32) for i in range(2)]
            pl = ps("pl", [128, NE], F32)
            pc = ps("pc", [128, NE], F32)
            c.dma("sp", rwt[:], W["rw"].rearrange("(k p) e -> p k e", p=128), writes=[T("rwt")])
            c.dma("sp", rbt[:], W["rb"].partition_broadcast(128), writes=[T("rbt")])
            mset(c, "pool", zt[:], 0.0, [T("zt")])
            mset(c, "pool", zb[:], 0.0, [T("zb")])
            mset(c, "pool", lf[:, :, 0:1], float(NT), [T("lf")])
            mset(c, "pool", lf[:, :, 1:2], 0.0, [T("lf")])
            c.dma("sp", LISTF.rearrange("(p a) c -> p a c", p=128), lf[:], reads=[T("lf")], writes=[T("LISTF")])
            c.dma("sp", HF[NT:NT + 128, :], zb[:], reads=[T("zb")], writes=[T("HF")])
            for i in range((NT + 128) // 128):
                c.dma("sp", ACC[i * 128:(i + 1) * 128, :], zt[:], reads=[T("zt")], writes=[T("ACC")])
            cur_ms = None
            ntl = len(tiles)
            for ti, (r0, n, ms, tok0) in enumerate(tiles):
                assert n == 128
                b = ti % 2
                if ms != cur_ms:
                    c.dma("sp", At[:], mods[ms]["A2"].partition_broadcast(128), reads=["MODS"], writes=[T("At")])
                    c.dma("sp", Bt[:], mods[ms]["B2"].partition_broadcast(128), reads=["MODS"], writes=[T("Bt")])
                    cur_ms = ms
                c.dma("sp", xt[b][:], RES[r0:r0 + 128, :], reads=["RES"], writes=[T(f"xt{b}")])
                rstd = rmsnorm_rstd(c, xt[b][:], junk[:], ss[:], D, [T(f"xt{b}")], T("n1"))
                stt(c, "dve", h[:], xt[b][:], rstd, At[:], ALU.mult, ALU.mult, [T(f"xt{b}"), T("n1_ss1"), T("At")], [T("h")])
                tt(c, "pool", h[:], h[:], Bt[:], ALU.add, [T("h"), T("Bt")], [T("h")])
                cp(c, "act", hb[b][:], h[:], [T("h")], [T(f"hb{b}")])
                c.dma("sp", HF[tok0:tok0 + 128, :], hb[b][:], reads=[T(f"hb{b}")], writes=[T("HF")])
                for kg in range(KC // 4):
                    pb = kg % 2
                    for jj in range(4):
                        k = kg * 4 + jj
                        tr(c, pT[pb][:, jj * 128:(jj + 1) * 128], h[:, k * 128:(k + 1) * 128], idt[:], [T("h"), T("idt")], [T(f"pT{pb}")], inc=(jj == 3))
                    cp(c, "act" if kg % 2 == 0 else "dve", hT[:, kg * 4:(kg + 1) * 4, :], pT[pb][:].rearrange("p (a b) -> p a b", a=4), [T(f"pT{pb}")], [T("hT")])
                for k in range(KC):
                    mm(c, pl[:, :], hT[:, k, :], rwt[:, k, :], k == 0, k == KC - 1, [T("hT"), T("rwt")], [T("pl")])
                tt(c, "dve", lg[:], pl[:, :], rbt[:], ALU.add, [T("pl"), T("rbt")], [T("lg")])
                c.op("dve", lambda e: e.max(out=m8[:], in_=lg[:]), [T("lg")], [T("m8")])
                ts(c, "dve", mk[b][:], lg[:], m8[:, 3:4], None, ALU.is_ge, None, [T("lg"), T("m8")], [T(f"mk{b}")])
                ts(c, "dve", sm[:, 0:1], m8[:, 0:1], -1.0, None, ALU.mult, None, [T("m8")], [T("sm0")])
                act(c, ex[:], lg[:], AF.Exp, [T("lg"), T("sm0")], [T("ex")], bias=sm[:, 0:1])
                tt(c, "dve", ex[:], ex[:], mk[b][:], ALU.mult, [T("ex"), T(f"mk{b}")], [T("ex")])
                red(c, "dve", sm[:, 1:2], ex[:], ALU.add, [T("ex")], [T("sm1")])
                c.op("dve", lambda e: e.reciprocal(out=sm[:, 2:3], in_=sm[:, 1:2]), [T("sm1")], [T("sm2")])
                ts(c, "dve", Gt[b][:], ex[:], sm[:, 2:3], None, ALU.mult, None, [T("ex"), T("sm2")], [T(f"Gt{b}")])
                c.dma("sp", GD[tok0:tok0 + 128, :], Gt[b][:], reads=[T(f"Gt{b}")], writes=[T("GD")])
                mm(c, pc[:, :], ones[:], mk[b][:], ti == 0, ti == ntl - 1, [T("ones"), T(f"mk{b}")], [T("pc")], inc=True)
            cp(c, "dve", cnt[:], pc[:, :], [T("pc")], [T("cnt")])
        c.barrier()
        with contextlib.ExitStack() as es:
            sb = lambda n, s, d: es.enter_context(nc.sbuf_tensor(T(n), s, d))
            r = sb("r", [128, NE], F32)
            nb = sb("nb", [128, NE], F32)
            inc_ = [sb(f"inc{i}", [128, NE], F32) for i in range(2)]
            eid = sb("eid", [128, NBLK], F32)
            tmpb = sb("tmpb", [128, NBLK], F32)
            mset(c, "dve", nb[:], 0.0, [T("nb")])
            for j in range((4 * NT + 511) // 512 + 1):
                ts(c, "dve", r[:], cnt[:], 512.0 * j, None, ALU.is_gt, None, [T("cnt")], [T("r")])
                tt(c, "dve", nb[:], nb[:], r[:], ALU.add, [T("nb"), T("r")], [T("nb")])
            cp(c, "dve", inc_[0][:], nb[:], [T("nb")], [T("inc0")])
            src = 0
            sh = 1
            while sh < NE:
                dst = 1 - src
                tt(c, "dve", inc_[dst][:, sh:NE], inc_[src][:, sh:NE], inc_[src][:, 0:NE - sh], ALU.add, [T(f"inc{src}")], [T(f"inc{dst}")])
                cp(c, "dve", inc_[dst][:, 0:sh], inc_[src][:, 0:sh], [T(f"inc{src}")], [T(f"inc{dst}")])
                src = dst
                sh *= 2
            incl = inc_[src]
            tt(c, "dve", base[:], incl[:], nb[:], ALU.subtract, [T(f"inc{src}"), T("nb")], [T("base")])
            ts(c, "dve", base[:], base[:], 512.0, None, ALU.mult, None, [T("base")], [T("base")])
            mset(c, "dve", eid[:], 0.0, [T("eid")])
            for e in range(NE):
                ts(c, "dve", tmpb[:], iob[:], incl[:, e:e + 1], None, ALU.is_ge, None, [T("iob"), T(f"inc{src}")], [T("tmpb")])
                tt(c, "dve", eid[:], eid[:], tmpb[:], ALU.add, [T("eid"), T("tmpb")], [T("eid")])
            ts(c, "dve", eid[:], eid[:], float(NE - 1), None, ALU.min, None, [T("eid")], [T("eid")])
            cp(c, "dve", eidx[:], eid[:], [T("eid")], [T("eidx")])
            ts(c, "dve", tmpb[:], eid[:], 128.0, iop[:, 0:1], ALU.mult, ALU.add, [T("eid"), T("iop")], [T("tmpb")])
            cp(c, "dve", widx[:], tmpb[:], [T("tmpb")], [T("widx")])
            ts(c, "dve", eid[:], eid[:], 128.0 * FC, iop[:, 0:1], ALU.mult, ALU.add, [T("eid"), T("iop")], [T("eid")])
            for fc in range(FC):
                ts(c, "dve", tmpb[:], eid[:], 128.0 * fc, None, ALU.add, None, [T("eid")], [T("tmpb")])
                cp(c, "dve", widx1[:, fc, :], tmpb[:], [T("tmpb")], [T("widx")])
        c.barrier()
        with contextlib.ExitStack() as es:
            sb = lambda n, s, d: es.enter_context(nc.sbuf_tensor(T(n), s, d))
            ps = lambda n, s, d: es.enter_context(nc.psum_tensor(T(n), s, d))
            Gl = [sb(f"Gl{i}", [128, NE], F32) for i in range(2)]
            Mk = sb("Mk", [128, NE], F32)
            car = sb("car", [128, NE], F32)
            key = sb("key", [128, NE], F32)
            k8 = sb("k8", [128, 8], F32)
            eq = sb("eq", [128, NE], F32)
            dsti = [sb(f"dsti{i}", [128, 4], I32) for i in range(2)]
            dstf = sb("dstf", [128, 4], F32)
            pay = [sb(f"pay{i}", [128, 4, 2], F32) for i in range(2)]
            pcs = ps("pcs", [128, NE], F32)
            pcr = ps("pcr", [128, NE], F32)
            mset(c, "dve", car[:], 0.0, [T("car")])
            for ti, (r0, n, ms, tok0) in enumerate(tiles):
                b = ti % 2
                c.dma("sp", Gl[b][:], GD[tok0:tok0 + 128, :], reads=[T("GD")], writes=[T(f"Gl{b}")])
                ts(c, "dve", Mk[:], Gl[b][:], 0.0, None, ALU.is_gt, None, [T(f"Gl{b}")], [T("Mk")])
                mm(c, pcs[:, :], trif[:], Mk[:], True, True, [T("trif"), T("Mk")], [T("pcs")])
                mm(c, pcr[:, :], ones[:], Mk[:], True, True, [T("ones"), T("Mk")], [T("pcr")])
                tt(c, "dve", key[:], pcs[:, :], car[:], ALU.add, [T("pcs"), T("car")], [T("key")])
                tt(c, "dve", key[:], key[:], base[:], ALU.add, [T("key"), T("base")], [T("key")])
                tt(c, "dve", key[:], key[:], Mk[:], ALU.mult, [T("key"), T("Mk")], [T("key")])
                tt(c, "dve", car[:], car[:], pcr[:, :], ALU.add, [T("car"), T("pcr")], [T("car")])
                c.op("dve", lambda e: e.max(out=k8[:], in_=key[:]), [T("key")], [T("k8")])
                ts(c, "dve", dstf[:], k8[:, 0:4], -1.0, None, ALU.add, None, [T("k8")], [T("dstf")])
                cp(c, "dve", dsti[b][:], dstf[:], [T("dstf")], [T(f"dsti{b}")])
                for k in range(4):
                    ts(c, "dve", eq[:], key[:], k8[:, k:k + 1], None, ALU.is_equal, None, [T("key"), T("k8")], [T("eq")])
                    tt(c, "dve", eq[:], eq[:], Gl[b][:], ALU.mult, [T("eq"), T(f"Gl{b}")], [T("eq")])
                    red(c, "dve", pay[b][:, k, 1:2], eq[:], ALU.add, [T("eq")], [T(f"pay{b}")])
                    ts(c, "dve", pay[b][:, k, 0:1], iop[:, 0:1], float(tok0), None, ALU.add, None, [T("iop")], [T(f"pay{b}")])
                for k in range(4):
                    c.idma(LISTF[:, :], bass.IndirectOffsetOnAxis(ap=dsti[b][:, k:k + 1], axis=0), pay[b][:, k, :], None,
                           reads=[T(f"dsti{b}"), T(f"pay{b}")], writes=[T("LISTF")])
        c.barrier()
        with contextlib.ExitStack() as es:
            sb = lambda n, s, d: es.enter_context(nc.sbuf_tensor(T(n), s, d))
            ps = lambda n, s, d: es.enter_context(nc.psum_tensor(T(n), s, d))
            idb = sb("idb", [128, 128], BF16)
            lst = [sb(f"lst{i}", [128, 4, 2], F32) for i in range(2)]
            tkf = sb("tkf", [128, 4], F32)
            pad1 = sb("pad1", [128, 4], F32)
            tki = [sb(f"tki{i}", [128, 4], I32) for i in range(2)]
            xg = [sb(f"xg{i}", [128, D], BF16) for i in range(2)]
            xT = sb("xT", [128, KC, 512], BF16)
            yT = sb("yT", [128, FC, 512], BF16)
            w2t = sb("w2t", [128, FC, D], BF16)
            b2t = sb("b2t", [128, D], F32)
            b1gt = sb("b1gt", [128, FC], F32)
            b1lt = sb("b1lt", [128, FC], F32)
            w1gt = [sb(f"w1g{i}", [128, KC, 128], BF16) for i in range(2)]
            w1lt = [sb(f"w1l{i}", [128, KC, 128], BF16) for i in range(2)]
            stg = [sb(f"stg{i}", [128, max(D, KC * 128)], F32) for i in range(2)]
            nstg = 0
            gs = [sb(f"gs{i}", [128, 512], F32) for i in range(2)]
            sg = [sb(f"sg{i}", [128, 512], F32) for i in range(2)]
            ls = [sb(f"ls{i}", [128, 512], F32) for i in range(2)]
            yb = [sb(f"yb{i}", [128, D], F32) for i in range(2)]
            TW = min(8, KC)
            ptr = [ps(f"ptr{i}", [128, TW * 128], BF16) for i in range(2)]
            pgl = [ps(f"pgl{i}", [128, 512], F32) for i in range(4)]
            po = [ps(f"po{i}", [128, 512], F32) for i in range(2)]
            cp(c, "dve", idb[:], idt[:], [T("idt")], [T("idb")])
            gcount = 0
            for blk in range(NBLK):
                lb = blk % 2
                c.dma("sp", lst[lb][:], LISTF[blk * 512:(blk + 1) * 512, :].rearrange("(s p) c -> p s c", p=128), reads=[T("LISTF")], writes=[T(f"lst{lb}")])
                cp(c, "dve", tkf[:], lst[lb][:, :, 0], [T(f"lst{lb}")], [T("tkf")])
                ts(c, "dve", pad1[:], tkf[:], float(NT), None, ALU.is_ge, None, [T("tkf")], [T("pad1")])
                ts(c, "dve", pad1[:], pad1[:], iop[:, 0:1], None, ALU.mult, None, [T("pad1"), T("iop")], [T("pad1")])
                tt(c, "dve", tkf[:], tkf[:], pad1[:], ALU.add, [T("tkf"), T("pad1")], [T("tkf")])
                cp(c, "dve", tki[lb][:], tkf[:], [T("tkf")], [T(f"tki{lb}")])
                wofs = bass.IndirectOffsetOnAxis(ap=widx[:, blk:blk + 1], axis=0)
                for fc in range(FC):
                    sgi = nstg % 2
                    nstg += 1
                    c.idma(stg[sgi][:, 0:D], None, W22[:, :], bass.IndirectOffsetOnAxis(ap=widx1[:, fc, blk:blk + 1], axis=0),
                           reads=[T("widx")], writes=[T(f"stg{sgi}")])
                    cp(c, "act" if fc % 2 == 0 else "pool", w2t[:, fc, :], stg[sgi][:, 0:D], [T(f"stg{sgi}")], [T("w2t")])
                c.idma(b1gt[:], None, W["b1gT"][:, :], wofs, reads=[T("widx")], writes=[T("b1gt")])
                c.idma(b1lt[:], None, W["b1lT"][:, :], wofs, reads=[T("widx")], writes=[T("b1lt")])
                c.idma(b2t[:], None, W["b2"][:, :], bass.IndirectOffsetOnAxis(ap=eidx[:, blk:blk + 1], axis=0), reads=[T("eidx")], writes=[T("b2t")])
                for st in range(4):
                    b = gcount % 2
                    gcount += 1
                    c.idma(xg[b][:], None, HF[:, :], bass.IndirectOffsetOnAxis(ap=tki[lb][:, st:st + 1], axis=0),
                           reads=[T(f"tki{lb}"), T("HF")], writes=[T(f"xg{b}")])
                    for kg in range(KC // TW):
                        pb = (kg + st) % 2
                        for jj in range(TW):
                            k = kg * TW + jj
                            tr(c, ptr[pb][:, jj * 128:(jj + 1) * 128], xg[b][:, k * 128:(k + 1) * 128], idb[:], [T(f"xg{b}"), T("idb")], [T(f"ptr{pb}")], inc=(jj == TW - 1))
                        cp(c, "act" if pb == 0 else "dve", xT[:, kg * TW:(kg + 1) * TW, st * 128:(st + 1) * 128],
                           ptr[pb][:].rearrange("p (a b) -> p a b", a=TW), [T(f"ptr{pb}")], [T("xT")])
                for fc in range(FC):
                    wb = fc % 2
                    wofs1 = bass.IndirectOffsetOnAxis(ap=widx1[:, fc, blk:blk + 1], axis=0)
                    sgi = nstg % 2
                    nstg += 1
                    c.idma(stg[sgi][:, 0:KC * 128], None, W1G2[:, :], wofs1, reads=[T("widx")], writes=[T(f"stg{sgi}")])
                    cp(c, "act", w1gt[wb][:], stg[sgi][:, 0:KC * 128].rearrange("p (k f) -> p k f", k=KC), [T(f"stg{sgi}")], [T(f"w1g{wb}")])
                    sgi = nstg % 2
                    nstg += 1
                    c.idma(stg[sgi][:, 0:KC * 128], None, W1L2[:, :], wofs1, reads=[T("widx")], writes=[T(f"stg{sgi}")])
                    cp(c, "pool", w1lt[wb][:], stg[sgi][:, 0:KC * 128].rearrange("p (k f) -> p k f", k=KC), [T(f"stg{sgi}")], [T(f"w1l{wb}")])
                    pgi = pgl[2 * wb]
                    pli = pgl[2 * wb + 1]
                    for k in range(KC):
                        mm(c, pgi[:], w1gt[wb][:, k, :], xT[:, k, :], k == 0, k == KC - 1, [T(f"w1g{wb}"), T("xT")], [T(f"pgl{2 * wb}")])
                    for k in range(KC):
                        mm(c, pli[:], w1lt[wb][:, k, :], xT[:, k, :], k == 0, k == KC - 1, [T(f"w1l{wb}"), T("xT")], [T(f"pgl{2 * wb + 1}")])
                    ts(c, "dve", gs[wb][:], pgi[:], b1gt[:, fc:fc + 1], 7.0, ALU.add, ALU.min, [T(f"pgl{2 * wb}"), T("b1gt")], [T(f"gs{wb}")])
                    act(c, sg[wb][:], gs[wb][:], AF.Sigmoid, [T(f"gs{wb}")], [T(f"sg{wb}")], scale=1.702)
                    ts(c, "dve", ls[wb][:], pli[:], b1lt[:, fc:fc + 1], 7.0, ALU.add, ALU.min, [T(f"pgl{2 * wb + 1}"), T("b1lt")], [T(f"ls{wb}")])
                    ts(c, "pool", ls[wb][:], ls[wb][:], -7.0, 1.0, ALU.max, ALU.add, [T(f"ls{wb}")], [T(f"ls{wb}")])
                    tt(c, "pool", gs[wb][:], gs[wb][:], sg[wb][:], ALU.mult, [T(f"gs{wb}"), T(f"sg{wb}")], [T(f"gs{wb}")])
                    tt(c, "dve", yT[:, fc, :], gs[wb][:], ls[wb][:], ALU.mult, [T(f"gs{wb}"), T(f"ls{wb}")], [T("yT")])
                for st in range(4):
                    ob = st % 2
                    for nbk in range(D // 512):
                        pb = nbk % 2
                        for fc in range(FC):
                            mm(c, po[pb][:], yT[:, fc, st * 128:(st + 1) * 128], w2t[:, fc, nbk * 512:(nbk + 1) * 512], fc == 0, fc == FC - 1,
                               [T("yT"), T("w2t")], [T(f"po{pb}")])
                        tt(c, "dve", yb[ob][:, nbk * 512:(nbk + 1) * 512], po[pb][:], b2t[:, nbk * 512:(nbk + 1) * 512], ALU.add,
                           [T(f"po{pb}"), T("b2t")], [T(f"yb{ob}")])
                    ts(c, "pool", yb[ob][:], yb[ob][:], lst[lb][:, st, 1:2], None, ALU.mult, None, [T(f"yb{ob}"), T(f"lst{lb}")], [T(f"yb{ob}")])
                    c.idma(ACC[:, :], bass.IndirectOffsetOnAxis(ap=tki[lb][:, st:st + 1], axis=0), yb[ob][:], None,
                           reads=[T(f"tki{lb}"), T(f"yb{ob}")], writes=[T("ACC")], compute_op=ALU.add)
        c.barrier()
    c.barrier()
    if dbg is not None:
        dbg(dict(ACC=ACC, GD=GD, LISTF=LISTF))
    with contextlib.ExitStack() as es:
        sb = lambda n, s, d: es.enter_context(nc.sbuf_tensor(T(n), s, d))
        Gm = sb("Gm", [128, D], F32)
        xr = [sb(f"xr{i}", [128, D], F32) for i in range(2)]
        fr = [sb(f"fr{i}", [128, D], F32) for i in range(2)]
        cur_ms = None
        for ti, (r0, n, ms, tok0) in enumerate(tiles):
            b = ti % 2
            if ms != cur_ms:
                c.dma("sp", Gm[:], mods[ms]["G2"].partition_broadcast(128), reads=["MODS"], writes=[T("Gm")])
                cur_ms = ms
            c.dma("sp", xr[b][:], RES[r0:r0 + 128, :], reads=["RES"], writes=[T(f"xr{b}")])
            c.dma("sp", fr[b][:], ACC[tok0:tok0 + 128, :], reads=[T("ACC")], writes=[T(f"fr{b}")])
            tt(c, "dve", fr[b][:], fr[b][:], Gm[:], ALU.mult, [T(f"fr{b}"), T("Gm")], [T(f"fr{b}")])
            tt(c, "pool", xr[b][:], xr[b][:], fr[b][:], ALU.add, [T(f"xr{b}"), T(f"fr{b}")], [T(f"xr{b}")])
            c.dma("sp", RES[r0:r0 + 128, :], xr[b][:], reads=[T(f"xr{b}")], writes=["RES"])
    c.barrier()

import re as _re

D_MODEL = 2048
NBATCH = 2
SEQ = 8192
NCTX = 256
FFE = 2048


def mods_phase(c, CROW, ADAW, ADAB, MIXG, FFNG, MODV, K):
    nc = c.nc
    D = D_MODEL
    KC = D // 128
    T = lambda s: "md_" + s
    with contextlib.ExitStack() as es:
        sb = lambda n, s, d: es.enter_context(nc.sbuf_tensor(T(n), s, d))
        ps = lambda n, s, d: es.enter_context(nc.psum_tensor(T(n), s, d))
        idt = sb("idt", [128, 128], F32)
        cr = sb("cr", [3, D], F32)
        ST = sb("ST", [128, KC, 3], F32)
        wt = [sb(f"wt{i}", [128, KC, 512], F32) for i in range(2)]
        bt = [sb(f"bt{i}", [3, 512], F32) for i in range(2)]
        M = sb("M", [3, 6 * D], F32)
        g1 = sb("g1", [3, D], F32)
        g2 = sb("g2", [3, D], F32)
        MV = [sb(f"MV{i}", [3, D], F32) for i in range(2)]
        pT = ps("pT", [128, KC * 3], F32)
        pm = [ps(f"pm{i}", [3, 512], F32) for i in range(2)]
        c.dma("sp", idt[:], K["ident"][:, :], writes=[T("idt")])
        c.dma("sp", cr[:], CROW[:, :], writes=[T("cr")])
        act(c, cr[:], cr[:], AF.Silu, [T("cr")], [T("cr")])
        for k in range(KC):
            tr(c, pT[:, k * 3:(k + 1) * 3], cr[:, k * 128:(k + 1) * 128], idt[0:3, 0:3], [T("cr"), T("idt")], [T("pT")], inc=(k == KC - 1))
        cp(c, "dve", ST[:], pT[:].rearrange("p (k r) -> p k r", r=3), [T("pT")], [T("ST")])
        for i in range(2):
            c.dma("sp", g1[:], MIXG[i:i + 1, :].partition_broadcast(3), writes=[T("g1")])
            c.dma("sp", g2[:], FFNG[i:i + 1, :].partition_broadcast(3), writes=[T("g2")])
            for nb in range(6 * D // 512):
                wb = nb % 2
                c.dma("sp", wt[wb][:], ADAW[i * D:(i + 1) * D, nb * 512:(nb + 1) * 512].rearrange("(k p) n -> p k n", p=128), writes=[T(f"wt{wb}")])
                c.dma("sp", bt[wb][:], ADAB[i:i + 1, nb * 512:(nb + 1) * 512].partition_broadcast(3), writes=[T(f"bt{wb}")])
                for k in range(KC):
                    mm(c, pm[wb][:, :], ST[:, k, :], wt[wb][:, k, :], k == 0, k == KC - 1, [T("ST"), T(f"wt{wb}")], [T(f"pm{wb}")])
                tt(c, "dve", M[:, nb * 512:(nb + 1) * 512], pm[wb][:, :], bt[wb][:, :], ALU.add, [T(f"pm{wb}"), T(f"bt{wb}")], [T("M")])
            MO = MODV[i * 18:(i + 1) * 18, :].rearrange("(r k) d -> r k d", k=6)
            for k, (src0, gg) in enumerate(((D, g1), (0, None), (2 * D, None), (4 * D, g2), (3 * D, None), (5 * D, None))):
                mv = MV[k % 2]
                if gg is not None:
                    stt(c, "dve", mv[:], M[:, src0:src0 + D], 1.0, gg[:], ALU.add, ALU.mult, [T("M"), T("g1"), T("g2")], [T(f"MV{k % 2}")])
                else:
                    cp(c, "dve", mv[:], M[:, src0:src0 + D], [T("M")], [T(f"MV{k % 2}")])
                c.dma("sp", MO[:, k, :], mv[:], reads=[T(f"MV{k % 2}")], writes=["MODS"])
    c.barrier()


def modrow(MODV, i, r, k):
    j = (i * 3 + r) * 6 + k
    return MODV[j:j + 1, :]


def final_norm(c, tag, RES, r0, nrows, G, OUT, o0):
    nc = c.nc
    D = D_MODEL
    T = lambda s: f"{tag}_{s}"
    with contextlib.ExitStack() as es:
        sb = lambda n, s, d: es.enter_context(nc.sbuf_tensor(T(n), s, d))
        gt = sb("gt", [128, D], F32)
        xt = [sb(f"xt{i}", [128, D], F32) for i in range(2)]
        ot = [sb(f"ot{i}", [128, D], F32) for i in range(2)]
        junk = sb("junk", [128, D], F32)
        ss = sb("ss", [128, 2], F32)
        c.dma("sp", gt[:], G.partition_broadcast(128), writes=[T("gt")])
        for i in range(nrows // 128):
            b = i % 2
            c.dma("sp", xt[b][:], RES[r0 + i * 128:r0 + (i + 1) * 128, :], reads=["RES"], writes=[T(f"xt{b}")])
            rstd = rmsnorm_rstd(c, xt[b][:], junk[:], ss[:], D, [T(f"xt{b}")], T("n"))
            stt(c, "dve", ot[b][:], xt[b][:], rstd, gt[:], ALU.mult, ALU.mult, [T(f"xt{b}"), T("n_ss1"), T("gt")], [T(f"ot{b}")])
            c.dma("sp", OUT[o0 + i * 128:o0 + (i + 1) * 128, :], ot[b][:], reads=[T(f"ot{b}")], writes=["OUT"])
    c.barrier()


def build_program(nbatch=NBATCH, seq=SEQ, nctx=NCTX):
    c = Ctx()
    D = D_MODEL
    NTOK = nctx + seq
    ext = lambda n, s, dt=F32: c.dram(n, s, dt, "ExternalInput")
    X = ext("x", [nbatch * seq, D])
    CTX = ext("ctx", [nbatch * nctx, D])
    CROW = ext("crow", [3, D])
    ADAW = ext("ada_w", [2 * D, 6 * D])
    ADAB = ext("ada_b", [2, 6 * D])
    MIXG = ext("mix_g", [2, D])
    FFNG = ext("ffn_g", [2, D])
    FING = ext("fin_g", [1, D])
    WH = {"w_in": ext("h_w_in", [D, HIN]), "conv_w": ext("h_conv_w", [3, XW]), "conv_b": ext("h_conv_b", [1, XW]), "dtb": ext("h_dtb", [1, 64]),
          "alog": ext("h_alog", [1, 64]), "dsk": ext("h_dsk", [1, 32]), "ssdg": ext("h_ssdg", [1, SW]), "vng": ext("h_vng", [1, GW]),
          "wsT": ext("h_wsT", [16, 128, 128]), "bsT": ext("h_bsT", [128, 16]), "w_out": ext("h_w_out", [2 * SW, D])}
    WA = {"w_in": ext("a_w_in", [D, 1344]), "qng": ext("a_qng", [1, 768]), "kvng": ext("a_kvng", [1, 512]), "zero768": ext("a_zero768", [1, 768]),
          "w_uq": ext("a_w_uq", [768, 3072]), "w_uk": ext("a_w_uk", [512, 2048]), "w_uv": ext("a_w_uv", [512, 2048]), "w_o": ext("a_w_o", [2048, D]),
          "cos": ext("a_cos", [seq, 32]), "sin": ext("a_sin", [seq, 32])}
    FC = FFE // 128
    KC = D // 128
    WM = []
    for i in range(2):
        WM.append({"rw": ext(f"m{i}_rw", [D, 32]), "rb": ext(f"m{i}_rb", [1, 32]),
                   "w1gT": ext(f"m{i}_w1gT", [32 * FC * 128, KC * 128]), "w1lT": ext(f"m{i}_w1lT", [32 * FC * 128, KC * 128]),
                   "w2T": ext(f"m{i}_w2T", [32 * FFE, D]), "b1gT": ext(f"m{i}_b1gT", [32 * 128, FC]), "b1lT": ext(f"m{i}_b1lT", [32 * 128, FC]),
                   "b2": ext(f"m{i}_b2", [32, D])})
    NB0 = (4 * NTOK + 511) // 512 + 32
    NB1 = (4 * seq + 511) // 512 + 32
    K = {k: ext("k_" + k, [128, 128]) for k in ("ident", "ones", "trif", "trib")}
    K["iota_p"] = ext("k_iota_p", [128, 1])
    K0 = dict(K)
    K0["iota_b"] = ext("k_iota_b0", [1, NB0])
    K1 = dict(K)
    K1["iota_b"] = ext("k_iota_b1", [1, NB1])
    OUT = c.dram("out", [nbatch * seq, D], F32, "ExternalOutput")
    MODV = c.dram("MODV", [36, D], F32)
    mods_phase(c, CROW, ADAW, ADAB, MIXG, FFNG, MODV, K)
    for b in range(nbatch):
        RES = c.dram(f"b{b}RES", [NTOK, D], F32)
        for r0 in range(0, nctx, 128):
            c.dma("sp", RES[r0:r0 + 128, :], CTX[b * nctx + r0:b * nctx + r0 + 128, :], writes=["RES"])
        for r0 in range(0, seq, 128):
            c.dma("sp", RES[nctx + r0:nctx + r0 + 128, :], X[b * seq + r0:b * seq + r0 + 128, :], writes=["RES"])
        c.barrier()
        m = []
        for i in range(2):
            m.append({"l": {nm: modrow(MODV, i, b, k) for k, nm in enumerate(("A1", "B1", "G1", "A2", "B2", "G2"))},
                      "c": {nm: modrow(MODV, i, 2, k) for k, nm in enumerate(("A1", "B1", "G1", "A2", "B2", "G2"))}})
        hybrid_phase(c, f"b{b}H", D, nctx, seq, RES, m[0], WH, K)
        tiles0 = [(r0, 128, "c" if r0 < nctx else "l", r0) for r0 in range(0, NTOK, 128)]
        moe_local(c, f"b{b}M0", D, FFE, NTOK, RES, tiles0, m[0], WM[0], K0)
        mla_phase(c, f"b{b}A", D, nctx, seq, RES, m[1], WA, K)
        tiles1 = [(nctx + r0, 128, "l", r0) for r0 in range(0, seq, 128)]
        moe_local(c, f"b{b}M1", D, FFE, seq, RES, tiles1, m[1], WM[1], K1)
        final_norm(c, f"b{b}F", RES, nctx, seq, FING, OUT, b * seq)
    c.finish()
    return c


def host_inputs(inputs, nbatch=NBATCH, seq=SEQ, nctx=NCTX):
    f32 = np.float32
    A = lambda a: np.ascontiguousarray(np.asarray(a, dtype=f32))
    D = D_MODEL
    g = lambda k: np.asarray(inputs[k])
    m = {}
    m["x"] = A(g("x").reshape(nbatch * seq, D))
    m["ctx"] = A(g("ctx").reshape(nbatch * nctx, D))
    m["crow"] = A(np.concatenate([g("c").reshape(nbatch, D)[:2], g("c_ctx").reshape(1, D)], 0)) if nbatch == 2 else None
    m["ada_w"] = A(g("ada_w").reshape(2 * D, 6 * D))
    m["ada_b"] = A(g("ada_b").reshape(2, 6 * D))
    m["mix_g"] = A(g("mix_norm_g"))
    m["ffn_g"] = A(g("ffn_norm_g"))
    m["fin_g"] = A(g("final_norm_g").reshape(1, D))
    m["h_w_in"] = A(g("hyb_w_in")[0])
    m["h_conv_w"] = A(g("hyb_conv_w")[0])
    m["h_conv_b"] = A(g("hyb_conv_b")[0].reshape(1, -1))
    m["h_dtb"] = A(g("hyb_dt_bias")[0].reshape(1, 64))
    m["h_alog"] = A(g("hyb_a_log")[0].reshape(1, 64))
    m["h_dsk"] = A(g("hyb_d_skip")[0].reshape(1, 32))
    m["h_ssdg"] = A(g("hyb_ssd_norm_g")[0].reshape(1, -1))
    m["h_vng"] = A(g("hyb_v_norm_g")[0].reshape(1, -1))
    m["h_wsT"] = A(g("hyb_w_s")[0].transpose(0, 2, 1))
    m["h_bsT"] = A(g("hyb_b_s")[0].T)
    m["h_w_out"] = A(g("hyb_w_out")[0])
    m["a_w_in"] = A(g("mla_w_in")[0])
    m["a_qng"] = A(g("mla_q_norm_g")[0].reshape(1, -1))
    m["a_kvng"] = A(g("mla_kv_norm_g")[0].reshape(1, -1))
    m["a_zero768"] = np.zeros((1, 768), f32)
    m["a_w_uq"] = A(g("mla_w_uq")[0])
    wk = g("mla_w_ukv")[0].reshape(512, 16, 256)
    m["a_w_uk"] = A(wk[:, :, :128].reshape(512, 2048))
    m["a_w_uv"] = A(wk[:, :, 128:].reshape(512, 2048))
    m["a_w_o"] = A(g("mla_w_o")[0])
    rows = seq // 64
    row = np.repeat(np.arange(rows), 64).astype(np.float64)
    col = np.tile(np.arange(64), rows).astype(np.float64)
    freqs = (10000.0 ** (-np.arange(16, dtype=np.float32) / 16)).astype(np.float32)
    ang = np.stack([row[:, None].astype(f32) * freqs, col[:, None].astype(f32) * freqs], 1).astype(f32)
    m["a_cos"] = A(np.cos(ang).reshape(seq, 32))
    m["a_sin"] = A(np.sin(ang).reshape(seq, 32))
    FC = FFE // 128
    for i in range(2):
        m[f"m{i}_rw"] = A(g("router_w")[i])
        m[f"m{i}_rb"] = A(g("router_b")[i].reshape(1, 32))
        w1 = g("exp_w1")[i]
        w5 = w1.reshape(32, D // 128, 128, FC, 128, 2)
        m[f"m{i}_w1gT"] = A(w5[..., 0].transpose(0, 3, 2, 1, 4).reshape(32 * FC * 128, (D // 128) * 128))
        m[f"m{i}_w1lT"] = A(w5[..., 1].transpose(0, 3, 2, 1, 4).reshape(32 * FC * 128, (D // 128) * 128))
        m[f"m{i}_w2T"] = A(g("exp_w2")[i].reshape(32 * FFE, D))
        b1 = g("exp_b1")[i].reshape(32, FC, 128, 2)
        m[f"m{i}_b1gT"] = A(b1[..., 0].transpose(0, 2, 1).reshape(32 * 128, FC))
        m[f"m{i}_b1lT"] = A(b1[..., 1].transpose(0, 2, 1).reshape(32 * 128, FC))
        m[f"m{i}_b2"] = A(g("exp_b2")[i])
    tri = np.tril(np.ones((128, 128), f32))
    m["k_ident"] = np.eye(128, dtype=f32)
    m["k_ones"] = np.ones((128, 128), f32)
    m["k_trif"] = A(tri.T)
    m["k_trib"] = A(tri)
    m["k_iota_p"] = np.arange(128, dtype=f32).reshape(128, 1)
    NTOK = nctx + seq
    NB0 = (4 * NTOK + 511) // 512 + 32
    NB1 = (4 * seq + 511) // 512 + 32
    m["k_iota_b0"] = np.arange(NB0, dtype=f32).reshape(1, NB0)
    m["k_iota_b1"] = np.arange(NB1, dtype=f32).reshape(1, NB1)
    return m


def kernel(**inputs):
    c = build_program()
    m = host_inputs(inputs)
    res = run_bass_kernel_spmd(c.nc, [m], core_ids=[0])
    out = np.asarray(res.results[0]["out"], dtype=np.float32)
    return out.reshape(NBATCH, SEQ, D_MODEL)
```

```python
import contextlib
import numpy as np
import concourse.bass as bass
import concourse.mybir as mybir
from concourse.bass_utils import run_bass_kernel_spmd

F32 = mybir.dt.float32
BF16 = mybir.dt.bfloat16
I32 = mybir.dt.int32
AF = mybir.ActivationFunctionType
ALU = mybir.AluOpType
AX = mybir.AxisListType

NDSEM = 8


class Ctx:
    def __init__(self):
        nc = bass.Bass("TRN2", target_bir_lowering=False)
        self.nc = nc
        self.E = {"pe": nc.tensor, "act": nc.scalar, "dve": nc.vector, "pool": nc.gpsimd, "sp": nc.sync}
        self.csem = {e: nc.alloc_semaphore(name=f"c_{e}") for e in ("pe", "act", "dve", "pool")}
        self.ccount = {e: 0 for e in self.csem}
        self.dsem = {q: [nc.alloc_semaphore(name=f"d_{q}{i}") for i in range(NDSEM)] for q in ("sp", "pool")}
        self.dval = {q: [0] * NDSEM for q in self.dsem}
        self.dnext = {q: 0 for q in self.dsem}
        self.ccsem = nc.alloc_semaphore(name="cc")
        self.ccval = 0
        self.known = {e: {} for e in self.E}
        self.lastw = {}
        self.readers = {}
        self.uid = 0
        self.n_instr = 0

    def name(self, base):
        self.uid += 1
        return f"{base}_{self.uid}"

    def dram(self, name, shape, dtype, kind="Internal"):
        key = _re.sub(r"^b\d", "", name) if kind == "Internal" and not name.endswith("RES") else name
        if not hasattr(self, "_dcache"):
            self._dcache = {}
        if key in self._dcache:
            return self._dcache[key]
        ap = self.nc.dram_tensor(key, list(shape), dtype, kind=kind).ap()
        self._dcache[key] = ap
        return ap

    def _deps(self, eng, reads, writes):
        need = []
        for r in reads:
            w = self.lastw.get(r)
            if w is not None:
                need.append(w)
        for r in writes:
            w = self.lastw.get(r)
            if w is not None:
                need.append(w)
            rd = self.readers.get(r)
            if rd:
                need.extend(rd.values())
        out = {}
        kn = self.known[eng]
        for (sem, val, src) in need:
            if src == "pe" and eng == "pe":
                continue
            key = id(sem)
            if kn.get(key, 0) >= val:
                continue
            if key not in out or out[key][1] < val:
                out[key] = (sem, val)
        for key, (sem, val) in out.items():
            kn[key] = val
            self.E[eng].wait_ge(sem, val)
            self.n_instr += 1

    def _record(self, rec, reads, writes):
        for r in reads:
            d = self.readers.setdefault(r, {})
            k = id(rec[0])
            if k not in d or d[k][1] < rec[1]:
                d[k] = rec
        for r in writes:
            self.lastw[r] = rec
            self.readers[r] = {}

    def op(self, eng, fn, reads=(), writes=(), inc=True):
        self._deps(eng, reads, writes)
        ins = fn(self.E[eng])
        self.n_instr += 1
        sem = self.csem[eng]
        if inc:
            self.ccount[eng] += 1
            ins.then_inc(sem, 1)
            rec = (sem, self.ccount[eng], eng)
        else:
            rec = (sem, self.ccount[eng] + 1, eng)
        self._record(rec, reads, writes)
        return ins

    def dma(self, q, out, in_, reads=(), writes=(), **kw):
        i = self.dnext[q]
        self.dnext[q] = (i + 1) % NDSEM
        sem = self.dsem[q][i]
        kn = self.known[q]
        if kn.get(id(sem), 0) < self.dval[q][i]:
            self.E[q].wait_ge(sem, self.dval[q][i])
            kn[id(sem)] = self.dval[q][i]
            self.n_instr += 1
        self._deps(q, reads, writes)
        ins = self.E[q].dma_start(out=out, in_=in_, **kw)
        self.n_instr += 1
        self.dval[q][i] += 16
        ins.then_inc(sem, 16)
        rec = (sem, self.dval[q][i], "dma_" + q)
        self._record(rec, reads, writes)
        return ins

    def idma(self, out, out_off, in_, in_off, reads=(), writes=(), **kw):
        q = "pool"
        i = self.dnext[q]
        self.dnext[q] = (i + 1) % NDSEM
        sem = self.dsem[q][i]
        kn = self.known[q]
        if kn.get(id(sem), 0) < self.dval[q][i]:
            self.E[q].wait_ge(sem, self.dval[q][i])
            kn[id(sem)] = self.dval[q][i]
        self._deps(q, reads, writes)
        ins = self.nc.gpsimd.indirect_dma_start(out=out, out_offset=out_off, in_=in_, in_offset=in_off, **kw)
        self.n_instr += 1
        self.dval[q][i] += 16
        ins.then_inc(sem, 16)
        rec = (sem, self.dval[q][i], "dma_" + q)
        self._record(rec, reads, writes)
        return ins

    def collective(self, kind, op, groups, in_ap, out_ap, reads=(), writes=()):
        q = "pool"
        self._deps(q, reads, writes)
        ins = self.nc.gpsimd.collective_compute(kind, op, replica_groups=groups, ins=[in_ap], outs=[out_ap])
        self.ccval += 1
        ins.then_inc(self.ccsem)
        self.nc.gpsimd.wait_ge(self.ccsem, self.ccval)
        self.known[q][id(self.ccsem)] = self.ccval
        rec = (self.ccsem, self.ccval, "cc")
        self._record(rec, reads, writes)
        return ins

    def barrier(self):
        for e in self.E:
            kn = self.known[e]
            eng = self.E[e]
            for f, sem in self.csem.items():
                if self.ccount[f] > kn.get(id(sem), 0):
                    eng.wait_ge(sem, self.ccount[f])
                    kn[id(sem)] = self.ccount[f]
            for q in self.dsem:
                for i, sem in enumerate(self.dsem[q]):
                    if self.dval[q][i] > kn.get(id(sem), 0):
                        eng.wait_ge(sem, self.dval[q][i])
                        kn[id(sem)] = self.dval[q][i]
            if self.ccval > kn.get(id(self.ccsem), 0):
                eng.wait_ge(self.ccsem, self.ccval)
                kn[id(self.ccsem)] = self.ccval

    def finish(self):
        sp = self.E["sp"]
        kn = self.known["sp"]
        for q in self.dsem:
            for i, sem in enumerate(self.dsem[q]):
                if self.dval[q][i] > kn.get(id(sem), 0):
                    sp.wait_ge(sem, self.dval[q][i])
        for e, sem in self.csem.items():
            if self.ccount[e] > kn.get(id(sem), 0):
                sp.wait_ge(sem, self.ccount[e])
        if self.ccval:
            sp.wait_ge(self.ccsem, self.ccval)


def bcast_rows(ap_row, nparts=128):
    return ap_row.partition_broadcast(nparts)


def _mk(c):
    return c


def mm(c, out, lhsT, rhs, start, stop, reads, writes, inc=None):
    inc = stop if inc is None else inc
    return c.op("pe", lambda e: e.matmul(out=out, lhsT=lhsT, rhs=rhs, start=start, stop=stop), reads, writes, inc=inc)


def tr(c, out, in_, ident, reads, writes, inc=True):
    return c.op("pe", lambda e: e.transpose(out=out, in_=in_, identity=ident), reads, writes, inc=inc)


def act(c, out, in_, func, reads, writes, **kw):
    return c.op("act", lambda e: e.activation(out=out, in_=in_, func=func, **kw), reads, writes)


def cp(c, eng, out, in_, reads, writes):
    if eng == "act":
        return c.op("act", lambda e: e.copy(out=out, in_=in_), reads, writes)
    return c.op(eng, lambda e: e.tensor_copy(out=out, in_=in_), reads, writes)


def tt(c, eng, out, in0, in1, op, reads, writes):
    return c.op(eng, lambda e: e.tensor_tensor(out=out, in0=in0, in1=in1, op=op), reads, writes)


def ts(c, eng, out, in0, s1, s2, op0, op1, reads, writes, accum_out=None):
    if op1 is None:
        return c.op(eng, lambda e: e.tensor_scalar(out=out, in0=in0, scalar1=s1, scalar2=None, op0=op0), reads, writes)
    if accum_out is not None:
        return c.op(eng, lambda e: e.tensor_scalar(out=out, in0=in0, scalar1=s1, scalar2=s2, op0=op0, op1=op1, accum_out=accum_out), reads, writes)
    return c.op(eng, lambda e: e.tensor_scalar(out=out, in0=in0, scalar1=s1, scalar2=s2, op0=op0, op1=op1), reads, writes)


def stt(c, eng, out, in0, scalar, in1, op0, op1, reads, writes):
    return c.op(eng, lambda e: e.scalar_tensor_tensor(out=out, in0=in0, scalar=scalar, in1=in1, op0=op0, op1=op1), reads, writes)


def red(c, eng, out, in_, op, reads, writes, axis=None):
    axis = AX.X if axis is None else axis
    return c.op(eng, lambda e: e.tensor_reduce(out=out, in_=in_, axis=axis, op=op), reads, writes)


def mset(c, eng, ap, val, writes):
    return c.op(eng, lambda e: e.memset(ap, val), (), writes)


def rmsnorm_rstd(c, x_ap, junk_ap, ss_ap, D, reads, tag, eps=1e-6):
    act(c, junk_ap, x_ap, AF.Square, reads, [tag + "_junk", tag + "_ss0"], scale=float(D) ** -0.5, accum_out=ss_ap[:, 0:1])
    act(c, ss_ap[:, 1:2], ss_ap[:, 0:1], AF.Sqrt, [tag + "_ss0"], [tag + "_ss1"], bias=eps)
    c.op("dve", lambda e: e.reciprocal(out=ss_ap[:, 1:2], in_=ss_ap[:, 1:2]), [tag + "_ss1"], [tag + "_ss1"])
    return ss_ap[:, 1:2]


G4, J8, HP, NS = 4, 8, 64, 128
SW = 2048
XW = 3072
GW = 2048
HIN = 9280


def proj_rows(c, T, tag, D, src, tiles, mods, Wd, ncols, outs, K, ST=1024, src_res="RES"):
    nc = c.nc
    KC = D // 128
    with contextlib.ExitStack() as es:
        sb = lambda n, s, d: es.enter_context(nc.sbuf_tensor(T(tag + n), s, d))
        ps = lambda n, s, d: es.enter_context(nc.psum_tensor(T(tag + n), s, d))
        idt = sb("idt", [128, 128], F32)
        At = sb("At", [128, D], F32)
        Bt = sb("Bt", [128, D], F32)
        xt = [sb(f"xt{i}", [128, D], F32) for i in range(2)]
        junk = sb("junk", [128, D], F32)
        ss = sb("ss", [128, 2], F32)
        h = sb("h", [128, D], F32)
        hT = sb("hT", [128, KC, ST], BF16)
        wt = [sb(f"wt{i}", [128, KC, 512], BF16) for i in range(2)]
        ot = [sb(f"ot{i}", [128, 512], F32) for i in range(3)]
        pT = [ps(f"pT{i}", [128, 512], F32) for i in range(2)]
        pm = [ps(f"pm{i}", [128, 512], F32) for i in range(3)]
        R = lambda s: T(tag + s)
        c.dma("sp", idt[:], K["ident"][:, :], writes=[R("idt")])
        sts = []
        cur = []
        curn = 0
        for tl in tiles:
            if curn + tl[1] > ST:
                sts.append(cur)
                cur, curn = [], 0
            cur.append(tl)
            curn += tl[1]
        if cur:
            sts.append(cur)
        cur_ms = None
        ti = 0
        oi = 0
        for st_tiles in sts:
            off = 0
            offs = []
            for (r0, n, ms) in st_tiles:
                b = ti % 2
                ti += 1
                if ms != cur_ms:
                    c.dma("sp", At[:], mods[ms]["A"].partition_broadcast(128), reads=["MODS"], writes=[R("At")])
                    c.dma("sp", Bt[:], mods[ms]["B"].partition_broadcast(128), reads=["MODS"], writes=[R("Bt")])
                    cur_ms = ms
                c.dma("sp", xt[b][:n], src[r0:r0 + n, :], reads=[src_res], writes=[R(f"xt{b}")])
                rstd = rmsnorm_rstd(c, xt[b][:n], junk[:n], ss[:n], D, [R(f"xt{b}")], R("n"))
                stt(c, "dve", h[:n], xt[b][:n], rstd[:n], At[:n], ALU.mult, ALU.mult, [R(f"xt{b}"), R("n_ss1"), R("At")], [R("h")])
                tt(c, "pool", h[:n], h[:n], Bt[:n], ALU.add, [R("h"), R("Bt")], [R("h")])
                gsz = 4 if KC % 4 == 0 else 2
                for kg in range(KC // gsz):
                    pb = kg % 2
                    for jj in range(gsz):
                        k = kg * gsz + jj
                        tr(c, pT[pb][:, jj * 128:jj * 128 + n], h[:n, k * 128:(k + 1) * 128], idt[:n, :n],
                           [R("h"), R("idt")], [R(f"pT{pb}")], inc=(jj == gsz - 1))
                    cp(c, "act" if kg % 2 == 0 else "dve", hT[:, kg * gsz:(kg + 1) * gsz, off:off + n],
                       pT[pb][:, 0:gsz * 128].rearrange("p (a b) -> p a b", a=gsz)[:, :, :n], [R(f"pT{pb}")], [R("hT")])
                offs.append(off)
                off += n
            nblk = (ncols + 511) // 512
            for nb in range(nblk):
                c0 = nb * 512
                w = min(512, ncols - c0)
                wb = nb % 2
                c.dma("pool", wt[wb][:, :, :w], Wd[:, c0:c0 + w].rearrange("(k p) n -> p k n", p=128), writes=[R(f"wt{wb}")])
                for (r0, n, ms), o in zip(st_tiles, offs):
                    pb = oi % 3
                    oi += 1
                    for k in range(KC):
                        mm(c, pm[pb][:n, :w], hT[:, k, o:o + n], wt[wb][:, k, :w], k == 0, k == KC - 1, [R("hT"), R(f"wt{wb}")], [R(f"pm{pb}")])
                    cp(c, "act" if pb != 1 else "dve", ot[pb][:n, :w], pm[pb][:n, :w], [R(f"pm{pb}")], [R(f"ot{pb}")])
                    for (d0, d1, dst, rofs, res) in outs:
                        lo = max(c0, d0)
                        hi = min(c0 + w, d1)
                        if lo < hi:
                            rr = rofs(r0)
                            c.dma("sp", dst[rr:rr + n, lo - d0:hi - d0], ot[pb][:n, lo - c0:hi - c0], reads=[R(f"ot{pb}")], writes=[res])
    c.barrier()


def hybrid_phase(c, tag, D, NCTX, NLAT, RES, mods, W, K, dbg=None):
    nc = c.nc
    T = lambda s: f"{tag}_{s}"
    NTOK = NCTX + NLAT
    NCH = NTOK // 128
    PZ = c.dram(T("PZ"), [NTOK, SW], F32)
    PX = c.dram(T("PX"), [NTOK + 4, XW], F32)
    PD = c.dram(T("PD"), [NTOK, 64], F32)
    PU = c.dram(T("PU"), [NTOK, GW], F32)
    PV = c.dram(T("PV"), [NTOK, GW], F32)
    XS = c.dram(T("XS"), [NTOK, SW], F32)
    BM = c.dram(T("BM"), [NTOK, 512], BF16)
    CTd = c.dram(T("CTd"), [NCH * 128, 512], BF16)
    CBF = c.dram(T("CBF"), [NCH * 128, 512], F32)
    CBB = c.dram(T("CBB"), [NCH * 128, 512], F32)
    DT = c.dram(T("DT"), [NTOK, 64], F32)
    Y = c.dram(T("Y"), [NTOK, SW], F32)
    YG = c.dram(T("YG"), [NTOK, GW], F32)
    MIX = c.dram(T("MIX"), [NTOK, 2 * SW], BF16)
    YM = c.dram(T("YM"), [NTOK, D], F32)

    def pxrow(r):
        return r + 1 if r < NCTX else r + 3

    tiles = [(r0, 128, "c" if r0 < NCTX else "l") for r0 in range(0, NTOK, 128)]
    ident_rows = lambda r: r
    outs = [(0, SW, PZ, ident_rows, T("PZ")), (SW, SW + XW, PX, pxrow, T("PX")), (SW + XW, SW + XW + 64, PD, ident_rows, T("PD")),
            (SW + XW + 64, SW + XW + 64 + GW, PU, ident_rows, T("PU")), (SW + XW + 64 + GW, HIN, PV, ident_rows, T("PV"))]
    m1 = {ms: {"A": mods[ms]["A1"], "B": mods[ms]["B1"]} for ms in mods}
    proj_rows(c, T, "pj_", D, RES, tiles, m1, W["w_in"], HIN, outs, K)

    with contextlib.ExitStack() as es:
        sb = lambda n, s, d: es.enter_context(nc.sbuf_tensor(T(n), s, d))
        ps = lambda n, s, d: es.enter_context(nc.psum_tensor(T(n), s, d))
        idt = sb("c_idt", [128, 128], F32)
        trif = sb("c_trif", [128, 128], F32)
        trib = sb("c_trib", [128, 128], F32)
        cw = sb("c_cw", [128, 3, XW], F32)
        cb = sb("c_cb", [128, XW], F32)
        dtb = sb("c_dtb", [128, 64], F32)
        zr = sb("c_zr", [4, XW], F32)
        xp = [sb(f"c_xp{i}", [128, XW], F32) for i in range(2)]
        xc = [sb(f"c_xc{i}", [128, XW], F32) for i in range(2)]
        xn = [sb(f"c_xn{i}", [128, XW], F32) for i in range(2)]
        acc = sb("c_acc", [128, XW], F32)
        sg = sb("c_sg", [128, XW], F32)
        bmb = sb("c_bmb", [128, 512], BF16)
        btb = sb("c_btb", [128, 512], BF16)
        ctb = sb("c_ctb", [128, 512], BF16)
        cbf = sb("c_cbf", [128, 512], F32)
        cbb = sb("c_cbb", [128, 512], F32)
        dtt = sb("c_dtt", [128, 64], F32)
        pB = ps("c_pB", [128, 512], F32)
        pC = ps("c_pC", [128, 512], F32)
        pCB = ps("c_pCB", [128, 512], F32)
        c.dma("sp", idt[:], K["ident"][:, :], writes=[T("c_idt")])
        c.dma("sp", trif[:], K["trif"][:, :], writes=[T("c_trif")])
        c.dma("sp", trib[:], K["trib"][:, :], writes=[T("c_trib")])
        for i in range(3):
            c.dma("sp", cw[:, i, :], W["conv_w"][i:i + 1, :].partition_broadcast(128), writes=[T("c_cw")])
        c.dma("sp", cb[:], W["conv_b"].partition_broadcast(128), writes=[T("c_cb")])
        c.dma("sp", dtb[:], W["dtb"].partition_broadcast(128), writes=[T("c_dtb")])
        mset(c, "pool", zr[:], 0.0, [T("c_zr")])
        for zrow in (0, NCTX + 1, NCTX + 2, NTOK + 3):
            c.dma("sp", PX[zrow:zrow + 1, :], zr[0:1, :], reads=[T("c_zr")], writes=[T("PX")])
        for ch in range(NCH):
            b = ch % 2
            r0 = ch * 128
            p0 = pxrow(r0)
            c.dma("sp", xp[b][:], PX[p0 - 1:p0 + 127, :], reads=[T("PX")], writes=[T(f"c_xp{b}")])
            c.dma("sp", xc[b][:], PX[p0:p0 + 128, :], reads=[T("PX")], writes=[T(f"c_xc{b}")])
            c.dma("sp", xn[b][:], PX[p0 + 1:p0 + 129, :], reads=[T("PX")], writes=[T(f"c_xn{b}")])
            tt(c, "dve", acc[:], xc[b][:], cw[:, 1, :], ALU.mult, [T(f"c_xc{b}"), T("c_cw")], [T("c_acc")])
            tt(c, "pool", xp[b][:], xp[b][:], cw[:, 0, :], ALU.mult, [T(f"c_xp{b}"), T("c_cw")], [T(f"c_xp{b}")])
            tt(c, "pool", xn[b][:], xn[b][:], cw[:, 2, :], ALU.mult, [T(f"c_xn{b}"), T("c_cw")], [T(f"c_xn{b}")])
            tt(c, "dve", acc[:], acc[:], cb[:], ALU.add, [T("c_acc"), T("c_cb")], [T("c_acc")])
            tt(c, "dve", acc[:], acc[:], xp[b][:], ALU.add, [T("c_acc"), T(f"c_xp{b}")], [T("c_acc")])
            tt(c, "dve", acc[:], acc[:], xn[b][:], ALU.add, [T("c_acc"), T(f"c_xn{b}")], [T("c_acc")])
            act(c, sg[:], acc[:], AF.Silu, [T("c_acc")], [T("c_sg")])
            c.dma("sp", XS[r0:r0 + 128, :], sg[:, 0:SW], reads=[T("c_sg")], writes=[T("XS")])
            cp(c, "pool", bmb[:], sg[:, SW:SW + 512], [T("c_sg")], [T("c_bmb")])
            c.dma("sp", BM[r0:r0 + 128, :], bmb[:], reads=[T("c_bmb")], writes=[T("BM")])
            for g in range(G4):
                tr(c, pB[:, g * 128:(g + 1) * 128], sg[:, SW + g * 128:SW + (g + 1) * 128], idt[:], [T("c_sg"), T("c_idt")], [T("c_pB")], inc=(g == 3))
            for g in range(G4):
                tr(c, pC[:, g * 128:(g + 1) * 128], sg[:, SW + 512 + g * 128:SW + 512 + (g + 1) * 128], idt[:], [T("c_sg"), T("c_idt")], [T("c_pC")], inc=(g == 3))
            cp(c, "act", btb[:], pB[:], [T("c_pB")], [T("c_btb")])
            cp(c, "act", ctb[:], pC[:], [T("c_pC")], [T("c_ctb")])
            c.dma("sp", CTd[r0:r0 + 128, :], ctb[:], reads=[T("c_ctb")], writes=[T("CTd")])
            for g in range(G4):
                mm(c, pCB[:, g * 128:(g + 1) * 128], btb[:, g * 128:(g + 1) * 128], ctb[:, g * 128:(g + 1) * 128], True, True,
                   [T("c_btb"), T("c_ctb")], [T("c_pCB")], inc=(g == 3))
            tt(c, "dve", cbf[:].rearrange("p (g q) -> p g q", g=4), pCB[:].rearrange("p (g q) -> p g q", g=4),
               trif[:].unsqueeze(1).to_broadcast([128, 4, 128]), ALU.mult, [T("c_pCB"), T("c_trif")], [T("c_cbf")])
            tt(c, "dve", cbb[:].rearrange("p (g q) -> p g q", g=4), pCB[:].rearrange("p (g q) -> p g q", g=4),
               trib[:].unsqueeze(1).to_broadcast([128, 4, 128]), ALU.mult, [T("c_pCB"), T("c_trib")], [T("c_cbb")])
            c.dma("sp", CBF[r0:r0 + 128, :], cbf[:], reads=[T("c_cbf")], writes=[T("CBF")])
            c.dma("sp", CBB[r0:r0 + 128, :], cbb[:], reads=[T("c_cbb")], writes=[T("CBB")])
            c.dma("sp", dtt[:], PD[r0:r0 + 128, :], reads=[T("PD")], writes=[T("c_dtt")])
            tt(c, "dve", dtt[:], dtt[:], dtb[:], ALU.add, [T("c_dtt"), T("c_dtb")], [T("c_dtt")])
            act(c, dtt[:], dtt[:], AF.Exp, [T("c_dtt")], [T("c_dtt")])
            act(c, dtt[:], dtt[:], AF.Ln, [T("c_dtt")], [T("c_dtt")], bias=1.0)
            c.dma("sp", DT[r0:r0 + 128, :], dtt[:], reads=[T("c_dtt")], writes=[T("DT")])
    c.barrier()

    ctx_ch = list(range(NCTX // 128))
    lat_ch = list(range(NCTX // 128, NCH))
    with contextlib.ExitStack() as es:
        sb = lambda n, s, d: es.enter_context(nc.sbuf_tensor(T(n), s, d))
        ps = lambda n, s, d: es.enter_context(nc.psum_tensor(T(n), s, d))
        ones = sb("s_ones", [128, 128], F32)
        tri = [sb("s_trif", [128, 128], F32), sb("s_trib", [128, 128], F32)]
        aneg = sb("s_aneg", [128, 64], F32)
        dsk = sb("s_dsk", [128, 32], F32)
        xs = [sb(f"s_xs{i}", [128, SW], F32) for i in range(2)]
        bm = [sb(f"s_bm{i}", [128, 512], BF16) for i in range(2)]
        ct = [sb(f"s_ct{i}", [128, 512], BF16) for i in range(2)]
        cbm = [sb(f"s_cbm{i}", [128, 512], F32) for i in range(2)]
        dtt = [sb(f"s_dt{i}", [128, 64], F32) for i in range(2)]
        a = sb("s_a", [128, 32], F32)
        acs = sb("s_acs", [128, 32], F32)
        tot = sb("s_tot", [128, 32], F32)
        dend = sb("s_dend", [128, 32], F32)
        eacs = sb("s_eacs", [128, 32], F32)
        cdec = sb("s_cdec", [128, 32], F32)
        xdt = sb("s_xdt", [128, SW], BF16)
        xdtd = sb("s_xdtd", [128, SW], BF16)
        X4 = [sb(f"s_X4{i}", [128, 512], F32) for i in range(2)]
        seg = [sb(f"s_seg{i}", [128, 512], F32) for i in range(2)]
        Lx = [sb(f"s_Lx{i}", [128, 512], F32) for i in range(2)]
        MT = [sb(f"s_MT{i}", [128, 512], BF16) for i in range(2)]
        Hs = sb("s_H", [128, G4 * 512], F32)
        Hb = sb("s_Hb", [128, G4 * 512], BF16)
        yo = sb("s_yo", [128, SW], F32)
        yacc = [sb(f"s_yacc{i}", [128, SW], F32) for i in range(2)]
        pa = ps("s_pa", [128, 64], F32)
        pR = [ps(f"s_pR{i}", [128, 512], F32) for i in range(2)]
        pY = ps("s_pY", [128, SW], F32)
        pO = ps("s_pO", [128, 512], F32)
        c.dma("sp", ones[:], K["ones"][:, :], writes=[T("s_ones")])
        c.dma("sp", tri[0][:], K["trif"][:, :], writes=[T("s_trif")])
        c.dma("sp", tri[1][:], K["trib"][:, :], writes=[T("s_trib")])
        c.dma("sp", aneg[:], W["alog"].partition_broadcast(128), writes=[T("s_aneg")])
        act(c, aneg[:], aneg[:], AF.Exp, [T("s_aneg")], [T("s_aneg")])
        ts(c, "dve", aneg[:], aneg[:], -1.0, None, ALU.mult, None, [T("s_aneg")], [T("s_aneg")])
        c.dma("sp", dsk[:], W["dsk"].partition_broadcast(128), writes=[T("s_dsk")])
        it = 0
        for d in range(2):
            CBd = CBF if d == 0 else CBB
            order = (ctx_ch + lat_ch) if d == 0 else (ctx_ch[::-1] + lat_ch[::-1])
            mset(c, "pool", Hs[:], 0.0, [T("s_H")])
            mset(c, "pool", Hb[:], 0.0, [T("s_Hb")])
            for ch in order:
                b = it % 2
                it += 1
                r0 = ch * 128
                c.dma("sp", xs[b][:], XS[r0:r0 + 128, :], reads=[T("XS")], writes=[T(f"s_xs{b}")])
                c.dma("sp", bm[b][:], BM[r0:r0 + 128, :], reads=[T("BM")], writes=[T(f"s_bm{b}")])
                c.dma("sp", ct[b][:], CTd[r0:r0 + 128, :], reads=[T("CTd")], writes=[T(f"s_ct{b}")])
                c.dma("sp", cbm[b][:], CBd[r0:r0 + 128, :], reads=[T("CBF"), T("CBB")], writes=[T(f"s_cbm{b}")])
                c.dma("sp", dtt[b][:], DT[r0:r0 + 128, :], reads=[T("DT")], writes=[T(f"s_dt{b}")])
                dtd = dtt[b][:, d * 32:(d + 1) * 32]
                tt(c, "dve", a[:], dtd, aneg[:, d * 32:(d + 1) * 32], ALU.mult, [T(f"s_dt{b}"), T("s_aneg")], [T("s_a")])
                mm(c, pa[:, 0:32], tri[d][:], a[:], True, True, [T(f"s_tri{'fb'[d]}"), T("s_a")], [T("s_pa")])
                mm(c, pa[:, 32:64], ones[:], a[:], True, True, [T("s_ones"), T("s_a")], [T("s_pa")])
                cp(c, "dve", acs[:], pa[:, 0:32], [T("s_pa")], [T("s_acs")])
                cp(c, "dve", tot[:], pa[:, 32:64], [T("s_pa")], [T("s_tot")])
                tt(c, "dve", dend[:], tot[:], acs[:], ALU.subtract, [T("s_tot"), T("s_acs")], [T("s_dend")])
                act(c, dend[:], dend[:], AF.Exp, [T("s_dend")], [T("s_dend")])
                act(c, eacs[:], acs[:], AF.Exp, [T("s_acs")], [T("s_eacs")])
                act(c, cdec[:], tot[:], AF.Exp, [T("s_tot")], [T("s_cdec")])
                tt(c, "pool", xdt[:].rearrange("p (h e) -> p h e", e=HP), xs[b][:].rearrange("p (h e) -> p h e", e=HP),
                   dtd.unsqueeze(2).to_broadcast([128, 32, HP]), ALU.mult, [T(f"s_xs{b}"), T(f"s_dt{b}")], [T("s_xdt")])
                tt(c, "pool", xdtd[:].rearrange("p (h e) -> p h e", e=HP), xdt[:].rearrange("p (h e) -> p h e", e=HP),
                   dend[:].unsqueeze(2).to_broadcast([128, 32, HP]), ALU.mult, [T("s_xdt"), T("s_dend")], [T("s_xdtd")])
                for hq in range(8):
                    g = hq // 2
                    q2 = hq % 2
                    tt(c, "dve", X4[q2][:].rearrange("p (h q) -> p h q", h=4), tri[d][:].unsqueeze(1).to_broadcast([128, 4, 128]),
                       a[:, hq * 4:(hq + 1) * 4].unsqueeze(2).to_broadcast([128, 4, 128]), ALU.mult,
                       [T(f"s_tri{'fb'[d]}"), T("s_a")], [T(f"s_X4{q2}")])
                    mm(c, pR[q2][:], ones[:], X4[q2][:], True, True, [T("s_ones"), T(f"s_X4{q2}")], [T(f"s_pR{q2}")])
                    for j4 in range(4):
                        hh = hq * 4 + j4
                        ts(c, "dve", seg[q2][:, j4 * 128:(j4 + 1) * 128], pR[q2][:, j4 * 128:(j4 + 1) * 128], acs[:, hh:hh + 1], 0.0,
                           ALU.subtract, ALU.min, [T(f"s_pR{q2}"), T("s_acs")], [T(f"s_seg{q2}")])
                    act(c, Lx[q2][:], seg[q2][:], AF.Exp, [T(f"s_seg{q2}")], [T(f"s_Lx{q2}")])
                    tt(c, "pool", MT[q2][:].rearrange("p (h q) -> p h q", h=4), Lx[q2][:].rearrange("p (h q) -> p h q", h=4),
                       cbm[b][:, g * 128:(g + 1) * 128].unsqueeze(1).to_broadcast([128, 4, 128]), ALU.mult,
                       [T(f"s_Lx{q2}"), T(f"s_cbm{b}")], [T(f"s_MT{q2}")])
                    for j4 in range(4):
                        hh = hq * 4 + j4
                        mm(c, pY[:, hh * HP:(hh + 1) * HP], MT[q2][:, j4 * 128:(j4 + 1) * 128], xdt[:, hh * HP:(hh + 1) * HP], True, True,
                           [T(f"s_MT{q2}"), T("s_xdt")], [T("s_pY")], inc=(j4 == 3))
                if d == 0:
                    tt(c, "pool", yacc[b][:].rearrange("p (h e) -> p h e", e=HP), xs[b][:].rearrange("p (h e) -> p h e", e=HP),
                       dsk[:].unsqueeze(2).to_broadcast([128, 32, HP]), ALU.mult, [T(f"s_xs{b}"), T("s_dsk")], [T(f"s_yacc{b}")])
                else:
                    c.dma("sp", yacc[b][:], Y[r0:r0 + 128, :], reads=[T("Y")], writes=[T(f"s_yacc{b}")])
                for g in range(G4):
                    mm(c, pO[:], ct[b][:, g * 128:(g + 1) * 128], Hb[:, g * 512:(g + 1) * 512], True, True, [T(f"s_ct{b}"), T("s_Hb")], [T("s_pO")])
                    tt(c, "dve", yo[:, g * 512:(g + 1) * 512].rearrange("p (h e) -> p h e", e=HP), pO[:].rearrange("p (h e) -> p h e", e=HP),
                       eacs[:, g * 8:(g + 1) * 8].unsqueeze(2).to_broadcast([128, 8, HP]), ALU.mult, [T("s_pO"), T("s_eacs")], [T("s_yo")])
                tt(c, "dve", yo[:], yo[:], pY[:], ALU.add, [T("s_yo"), T("s_pY")], [T("s_yo")])
                tt(c, "pool", yacc[b][:], yacc[b][:], yo[:], ALU.add, [T(f"s_yacc{b}"), T("s_yo")], [T(f"s_yacc{b}")])
                c.dma("sp", Y[r0:r0 + 128, :], yacc[b][:], reads=[T(f"s_yacc{b}")], writes=[T("Y")])
                for g in range(G4):
                    mm(c, pO[:], bm[b][:, g * 128:(g + 1) * 128], xdtd[:, g * 512:(g + 1) * 512], True, True, [T(f"s_bm{b}"), T("s_xdtd")], [T("s_pO")])
                    tt(c, "dve", Hs[:, g * 512:(g + 1) * 512].rearrange("p (h e) -> p h e", e=HP),
                       Hs[:, g * 512:(g + 1) * 512].rearrange("p (h e) -> p h e", e=HP),
                       cdec[:, g * 8:(g + 1) * 8].unsqueeze(2).to_broadcast([128, 8, HP]), ALU.mult, [T("s_H"), T("s_cdec")], [T("s_H")])
                    tt(c, "dve", Hs[:, g * 512:(g + 1) * 512], Hs[:, g * 512:(g + 1) * 512], pO[:], ALU.add, [T("s_H"), T("s_pO")], [T("s_H")])
                cp(c, "act", Hb[:], Hs[:], [T("s_H")], [T("s_Hb")])
    c.barrier()

    with contextlib.ExitStack() as es:
        sb = lambda n, s, d: es.enter_context(nc.sbuf_tensor(T(n), s, d))
        ps = lambda n, s, d: es.enter_context(nc.psum_tensor(T(n), s, d))
        wst = sb("g_wst", [128, 16, 128], BF16)
        bst = sb("g_bst", [128, 16], F32)
        vng = sb("g_vng", [128, GW], F32)
        sng = sb("g_sng", [128, SW], F32)
        u = [sb(f"g_u{i}", [128, GW], F32) for i in range(2)]
        v = [sb(f"g_v{i}", [128, GW], F32) for i in range(2)]
        z = [sb(f"g_z{i}", [128, SW], F32) for i in range(2)]
        y = [sb(f"g_y{i}", [128, SW], F32) for i in range(2)]
        junk = sb("g_junk", [128, GW], F32)
        ss = sb("g_ss", [128, 2], F32)
        ss2 = sb("g_ss2", [128, 2], F32)
        vn = sb("g_vn", [128, GW], BF16)
        mix = [sb(f"g_mix{i}", [128, 2 * SW], BF16) for i in range(2)]
        sgm = sb("g_sgm", [128, GW], F32)
        pS = ps("g_pS", [128, GW], F32)
        c.dma("pool", wst[:], W["wsT"].rearrange("g k q -> k g q"), writes=[T("g_wst")])
        c.dma("sp", bst[:], W["bsT"][:, :], writes=[T("g_bst")])
        c.dma("sp", vng[:], W["vng"].partition_broadcast(128), writes=[T("g_vng")])
        c.dma("sp", sng[:], W["ssdg"].partition_broadcast(128), writes=[T("g_sng")])
        for ch in range(NCH):
            b = ch % 2
            r0 = ch * 128
            c.dma("sp", u[b][:], PU[r0:r0 + 128, :], reads=[T("PU")], writes=[T(f"g_u{b}")])
            c.dma("sp", v[b][:], PV[r0:r0 + 128, :], reads=[T("PV")], writes=[T(f"g_v{b}")])
            c.dma("sp", z[b][:], PZ[r0:r0 + 128, :], reads=[T("PZ")], writes=[T(f"g_z{b}")])
            c.dma("sp", y[b][:], Y[r0:r0 + 128, :], reads=[T("Y")], writes=[T(f"g_y{b}")])
            act(c, u[b][:], u[b][:], AF.Gelu_apprx_tanh, [T(f"g_u{b}")], [T(f"g_u{b}")])
            act(c, v[b][:], v[b][:], AF.Gelu_apprx_tanh, [T(f"g_v{b}")], [T(f"g_v{b}")])
            rstd = rmsnorm_rstd(c, v[b][:], junk[:], ss[:], GW, [T(f"g_v{b}")], T("g_n1"))
            stt(c, "dve", vn[:], v[b][:], rstd, vng[:], ALU.mult, ALU.mult, [T(f"g_v{b}"), T("g_n1_ss1"), T("g_vng")], [T("g_vn")])
            for gg in range(16):
                mm(c, pS[:, gg * 128:(gg + 1) * 128], wst[:, gg, :], vn[:, gg * 128:(gg + 1) * 128], True, True, [T("g_wst"), T("g_vn")], [T("g_pS")], inc=(gg == 15))
            tt(c, "dve", sgm[:].rearrange("p (g e) -> p g e", g=16), pS[:].rearrange("p (g e) -> p g e", g=16),
               bst[:].unsqueeze(2).to_broadcast([128, 16, 128]), ALU.add, [T("g_pS"), T("g_bst")], [T("g_sgm")])
            tt(c, "pool", mix[b][:, SW:2 * SW], sgm[:], u[b][:], ALU.mult, [T("g_sgm"), T(f"g_u{b}")], [T(f"g_mix{b}")])
            act(c, z[b][:], z[b][:], AF.Silu, [T(f"g_z{b}")], [T(f"g_z{b}")])
            tt(c, "dve", y[b][:], y[b][:], z[b][:], ALU.mult, [T(f"g_y{b}"), T(f"g_z{b}")], [T(f"g_y{b}")])
            rstd2 = rmsnorm_rstd(c, y[b][:], junk[:], ss2[:], SW, [T(f"g_y{b}")], T("g_n2"))
            stt(c, "dve", mix[b][:, 0:SW], y[b][:], rstd2, sng[:], ALU.mult, ALU.mult, [T(f"g_y{b}"), T("g_n2_ss1"), T("g_sng")], [T(f"g_mix{b}")])
            c.dma("sp", MIX[r0:r0 + 128, :], mix[b][:], reads=[T(f"g_mix{b}")], writes=[T("MIX")])
    c.barrier()
    if dbg is not None:
        dbg(dict(Y=Y, MIX=MIX, PZ=PZ, XS=XS, DT=DT))
    out_proj_res(c, T, "op_", D, 2 * SW, MIX, W["w_out"], RES, tiles, {ms: mods[ms]["G1"] for ms in mods}, K)


def out_proj_res(c, T, tag, D, KIN, SRC, Wd, RES, tiles, gates, K, ST=1024):
    nc = c.nc
    KC = KIN // 128
    R = lambda s: T(tag + s)
    with contextlib.ExitStack() as es:
        sb = lambda n, s, d: es.enter_context(nc.sbuf_tensor(R(n), s, d))
        ps = lambda n, s, d: es.enter_context(nc.psum_tensor(R(n), s, d))
        idb = sb("idb", [128, 128], BF16)
        idf = sb("idf", [128, 128], F32)
        Gm = sb("Gm", [128, D], F32)
        xin = [sb(f"xin{i}", [128, KIN], BF16) for i in range(2)]
        xT = sb("xT", [128, KC, ST], BF16)
        wt = [sb(f"wt{i}", [128, KC, 512], BF16) for i in range(2)]
        rr = [sb(f"rr{i}", [128, 512], F32) for i in range(2)]
        ot = [sb(f"ot{i}", [128, 512], F32) for i in range(2)]
        ptr = [ps(f"ptr{i}", [128, 1024], BF16) for i in range(2)]
        pm = [ps(f"pm{i}", [128, 512], F32) for i in range(2)]
        c.dma("sp", idf[:], K["ident"][:, :], writes=[R("idf")])
        cp(c, "dve", idb[:], idf[:], [R("idf")], [R("idb")])
        sts = []
        cur, curn = [], 0
        for tl in tiles:
            if curn + tl[1] > ST:
                sts.append(cur)
                cur, curn = [], 0
            cur.append(tl)
            curn += tl[1]
        if cur:
            sts.append(cur)
        ti = 0
        oi = 0
        for st_tiles in sts:
            off = 0
            offs = []
            for (r0, n, ms) in st_tiles:
                b = ti % 2
                ti += 1
                c.dma("sp", xin[b][:n], SRC[r0:r0 + n, :], reads=[R("SRC")], writes=[R(f"xin{b}")])
                for kg in range(KC // 8):
                    pb = kg % 2
                    for jj in range(8):
                        k = kg * 8 + jj
                        tr(c, ptr[pb][:, jj * 128:jj * 128 + n], xin[b][:n, k * 128:(k + 1) * 128], idb[:n, :n], [R(f"xin{b}"), R("idb")], [R(f"ptr{pb}")], inc=(jj == 7))
                    cp(c, "act" if pb == 0 else "dve", xT[:, kg * 8:(kg + 1) * 8, off:off + n],
                       ptr[pb][:].rearrange("p (a b) -> p a b", a=8)[:, :, :n], [R(f"ptr{pb}")], [R("xT")])
                offs.append(off)
                off += n
            for nb in range(D // 512):
                wb = nb % 2
                c.dma("pool", wt[wb][:], Wd[:, nb * 512:(nb + 1) * 512].rearrange("(k p) n -> p k n", p=128), writes=[R(f"wt{wb}")])
                cur_ms = None
                for (r0, n, ms), o in zip(st_tiles, offs):
                    pb = oi % 2
                    oi += 1
                    if ms != cur_ms:
                        c.dma("sp", Gm[:], gates[ms].partition_broadcast(128), reads=["MODS"], writes=[R("Gm")])
                        cur_ms = ms
                    for k in range(KC):
                        mm(c, pm[pb][:n, :], xT[:, k, o:o + n], wt[wb][:, k, :], k == 0, k == KC - 1, [R("xT"), R(f"wt{wb}")], [R(f"pm{pb}")])
                    c.dma("sp", rr[pb][:n], RES[r0:r0 + n, nb * 512:(nb + 1) * 512], reads=["RES"], writes=[R(f"rr{pb}")])
                    tt(c, "dve", ot[pb][:n], pm[pb][:n, :], Gm[:n, nb * 512:(nb + 1) * 512], ALU.mult, [R(f"pm{pb}"), R("Gm")], [R(f"ot{pb}")])
                    tt(c, "pool", ot[pb][:n], ot[pb][:n], rr[pb][:n], ALU.add, [R(f"ot{pb}"), R(f"rr{pb}")], [R(f"ot{pb}")])
                    c.dma("sp", RES[r0:r0 + n, nb * 512:(nb + 1) * 512], ot[pb][:n], reads=[R(f"ot{pb}")], writes=["RES"])
    c.barrier()


NH = 16
QL, KVL, DN, DR, DV = 768, 512, 128, 64, 128
SCALE = (DN + DR) ** -0.5


def mla_phase(c, tag, D, NCTX, NLAT, RES, mods, W, K, dbg=None, upto=None):
    nc = c.nc
    T = lambda s: f"{tag}_{s}"
    NTOK = NCTX + NLAT
    NKT = NTOK // 128
    NQT = NLAT // 128
    PQ = c.dram(T("PQ"), [NTOK, QL], F32)
    PKV = c.dram(T("PKV"), [NTOK, KVL + DR], F32)
    QD = c.dram(T("QD"), [NTOK, NH * 192], F32)
    QH = c.dram(T("QH"), [NH, NLAT, 192], F32)
    QSQ = c.dram(T("QSQ"), [NLAT, NH], F32)
    OD = c.dram(T("OD"), [NTOK, NH * DV], BF16)
    tiles = [(r0, 128, "c" if r0 < NCTX else "l") for r0 in range(0, NTOK, 128)]
    lat_tiles = [t for t in tiles if t[2] == "l"]
    m1 = {ms: {"A": mods[ms]["A1"], "B": mods[ms]["B1"]} for ms in mods}
    proj_rows(c, T, "pj_", D, RES, tiles, m1, W["w_in"], QL + KVL + DR, [(0, QL, PQ, lambda r: r, T("PQ")), (QL, QL + KVL + DR, PKV, lambda r: r, T("PKV"))], K)
    mq = {"l": {"A": W["qng"], "B": W["zero768"]}}
    proj_rows(c, T, "pq_", QL, PQ, lat_tiles, mq, W["w_uq"], NH * 192, [(0, NH * 192, QD, lambda r: r, T("QD"))], K, src_res=T("PQ"))

    if upto == "proj":
        return
    with contextlib.ExitStack() as es0:
        sb0 = lambda n, s, d: es0.enter_context(nc.sbuf_tensor(T(n), s, d))
        ckvT = sb0("ckvT", [128, 4, NTOK], BF16)
        kpT = sb0("kpT", [65, NTOK], BF16)
        kpsq = sb0("kpsq", [128, NTOK], F32)
        idt = sb0("idt", [128, 128], F32)
        ones = sb0("ones", [128, 128], F32)
        c.dma("sp", idt[:], K["ident"][:, :], writes=[T("idt")])
        c.dma("sp", ones[:], K["ones"][:, :], writes=[T("ones")])
        with contextlib.ExitStack() as es:
            sb = lambda n, s, d: es.enter_context(nc.sbuf_tensor(T(n), s, d))
            ps = lambda n, s, d: es.enter_context(nc.psum_tensor(T(n), s, d))
            kvg = sb("a_kvg", [128, KVL], F32)
            kv = [sb(f"a_kv{i}", [128, KVL + DR], F32) for i in range(2)]
            junk = sb("a_junk", [128, KVL], F32)
            ss = sb("a_ss", [128, 2], F32)
            kn = sb("a_kn", [128, KVL], F32)
            cs = [sb(f"a_cs{i}", [128, 64], F32) for i in range(2)]
            kr = sb("a_kr", [128, DR], F32)
            t1 = sb("a_t1", [128, 32], F32)
            t2 = sb("a_t2", [128, 32], F32)
            sq2 = sb("a_sq2", [64, 128], F32)
            qt = [sb(f"a_qt{i}", [128, NH * 192], F32) for i in range(2)]
            qr = sb("a_qr", [128, NH * 192], F32)
            q1 = sb("a_q1", [128, NH * 32], F32)
            q2 = sb("a_q2", [128, NH * 32], F32)
            qq = sb("a_qq", [128, NH * 192], F32)
            qs = sb("a_qs", [128, NH], F32)
            pT = [ps(f"a_pT{i}", [128, 512], F32) for i in range(2)]
            pk = ps("a_pk", [64, 128], F32)
            pn = ps("a_pn", [128, 128], F32)
            c.dma("sp", kvg[:], W["kvng"].partition_broadcast(128), writes=[T("a_kvg")])
            mset(c, "pool", kpT[64:65, :], 1.0, [T("kpT")])
            for ti, (r0, n, ms) in enumerate(tiles):
                b = ti % 2
                c.dma("sp", kv[b][:], PKV[r0:r0 + 128, :], reads=[T("PKV")], writes=[T(f"a_kv{b}")])
                rstd = rmsnorm_rstd(c, kv[b][:, 0:KVL], junk[:], ss[:], KVL, [T(f"a_kv{b}")], T("a_n"))
                stt(c, "dve", kn[:], kv[b][:, 0:KVL], rstd, kvg[:], ALU.mult, ALU.mult, [T(f"a_kv{b}"), T("a_n_ss1"), T("a_kvg")], [T("a_kn")])
                for k in range(4):
                    tr(c, pT[b][:, k * 128:(k + 1) * 128], kn[:, k * 128:(k + 1) * 128], idt[:], [T("a_kn"), T("idt")], [T(f"a_pT{b}")], inc=(k == 3))
                cp(c, "act", ckvT[:, :, r0:r0 + 128], pT[b][:].rearrange("p (a b) -> p a b", a=4), [T(f"a_pT{b}")], [T("ckvT")])
                kp = kv[b][:, KVL:KVL + DR]
                if ms == "l":
                    t0 = r0 - NCTX
                    c.dma("sp", cs[b][:, 0:32], W["cos"][t0:t0 + 128, :], writes=[T(f"a_cs{b}")])
                    c.dma("sp", cs[b][:, 32:64], W["sin"][t0:t0 + 128, :], writes=[T(f"a_cs{b}")])
                    kp4 = kp.rearrange("p (a h e) -> p a h e", a=2, h=2)
                    kr4 = kr[:].rearrange("p (a h e) -> p a h e", a=2, h=2)
                    co = cs[b][:, 0:32].rearrange("p (a e) -> p a e", a=2)
                    si = cs[b][:, 32:64].rearrange("p (a e) -> p a e", a=2)
                    t1v = t1[:].rearrange("p (a e) -> p a e", a=2)
                    t2v = t2[:].rearrange("p (a e) -> p a e", a=2)
                    rd = [T(f"a_kv{b}"), T(f"a_cs{b}")]
                    tt(c, "dve", t1v, kp4[:, :, 0, :], co, ALU.mult, rd, [T("a_t1")])
                    tt(c, "dve", t2v, kp4[:, :, 1, :], si, ALU.mult, rd, [T("a_t2")])
                    tt(c, "dve", kr4[:, :, 0, :], t1v, t2v, ALU.subtract, [T("a_t1"), T("a_t2")], [T("a_kr")])
                    tt(c, "dve", t1v, kp4[:, :, 0, :], si, ALU.mult, rd, [T("a_t1")])
                    tt(c, "dve", t2v, kp4[:, :, 1, :], co, ALU.mult, rd, [T("a_t2")])
                    tt(c, "dve", kr4[:, :, 1, :], t1v, t2v, ALU.add, [T("a_t1"), T("a_t2")], [T("a_kr")])
                else:
                    cp(c, "dve", kr[:], kp, [T(f"a_kv{b}")], [T("a_kr")])
                tr(c, pk[:, :], kr[:], idt[:], [T("a_kr"), T("idt")], [T("a_pk")])
                cp(c, "act", kpT[0:64, r0:r0 + 128], pk[:, :], [T("a_pk")], [T("kpT")])
                act(c, sq2[:, :], pk[:, :], AF.Square, [T("a_pk")], [T("a_sq2")])
                mm(c, pn[:, :], ones[0:64, :], sq2[:, :], True, True, [T("ones"), T("a_sq2")], [T("a_pn")])
                cp(c, "dve", kpsq[:, r0:r0 + 128], pn[:, :], [T("a_pn")], [T("kpsq")])
                if ms == "l":
                    t0 = r0 - NCTX
                    c.dma("sp", qt[b][:], QD[r0:r0 + 128, :], reads=[T("QD")], writes=[T(f"a_qt{b}")])
                    q3 = qt[b][:].rearrange("p (h e) -> p h e", h=NH)
                    qr3 = qr[:].rearrange("p (h e) -> p h e", h=NH)
                    cp(c, "pool", qr3[:, :, 0:DN], q3[:, :, 0:DN], [T(f"a_qt{b}")], [T("a_qr")])
                    rd = [T(f"a_qt{b}"), T(f"a_cs{b}")]
                    for ax in range(2):
                        x1 = q3[:, :, DN + ax * 32:DN + ax * 32 + 16]
                        x2 = q3[:, :, DN + ax * 32 + 16:DN + ax * 32 + 32]
                        o1 = qr3[:, :, DN + ax * 32:DN + ax * 32 + 16]
                        o2 = qr3[:, :, DN + ax * 32 + 16:DN + ax * 32 + 32]
                        co = cs[b][:, ax * 16:(ax + 1) * 16].unsqueeze(1).to_broadcast([128, NH, 16])
                        si = cs[b][:, 32 + ax * 16:32 + (ax + 1) * 16].unsqueeze(1).to_broadcast([128, NH, 16])
                        a1 = q1[:, 0:NH * 16].rearrange("p (h e) -> p h e", h=NH)
                        a2 = q2[:, 0:NH * 16].rearrange("p (h e) -> p h e", h=NH)
                        tt(c, "dve", a1, x1, co, ALU.mult, rd, [T("a_q1")])
                        tt(c, "dve", a2, x2, si, ALU.mult, rd, [T("a_q2")])
                        tt(c, "dve", o1, a1, a2, ALU.subtract, [T("a_q1"), T("a_q2")], [T("a_qr")])
                        tt(c, "dve", a1, x1, si, ALU.mult, rd, [T("a_q1")])
                        tt(c, "dve", a2, x2, co, ALU.mult, rd, [T("a_q2")])
                        tt(c, "dve", o2, a1, a2, ALU.add, [T("a_q1"), T("a_q2")], [T("a_qr")])
                    tt(c, "pool", qq[:], qr[:], qr[:], ALU.mult, [T("a_qr")], [T("a_qq")])
                    red(c, "dve", qs[:], qq[:].rearrange("p (h e) -> p h e", h=NH), ALU.add, [T("a_qq")], [T("a_qs")])
                    c.dma("sp", QSQ[t0:t0 + 128, :], qs[:], reads=[T("a_qs")], writes=[T("QSQ")])
                    c.dma("sp", QH[:, t0:t0 + 128, :].rearrange("h t e -> t h e"), qr3, reads=[T("a_qr")], writes=[T("QH")])
        c.barrier()
        if upto == "A2":
            return

        with contextlib.ExitStack() as es:
            sb = lambda n, s, d: es.enter_context(nc.sbuf_tensor(T(n), s, d))
            ps = lambda n, s, d: es.enter_context(nc.psum_tensor(T(n), s, d))
            wuk = sb("h_wuk", [128, 4, DN], BF16)
            wuv = sb("h_wuv", [128, 4, DV], BF16)
            KnT = sb("h_KnT", [128, NTOK], BF16)
            Vt = sb("h_Vt", [128, NKT, 132], BF16)
            sqk = sb("h_sqk", [128, 512], F32)
            ksq = sb("h_ksq", [128, NTOK], F32)
            km = sb("h_km", [128, 2], F32)
            kmb = sb("h_kmb", [128, 1], F32)
            qh = [sb(f"h_qh{i}", [128, 193], F32) for i in range(2)]
            qsq = [sb(f"h_qsq{i}", [128, NH], F32) for i in range(2)]
            nq = sb("h_nq", [128, 2], F32)
            QnT = [sb(f"h_QnT{i}", [128, 512], BF16) for i in range(2)]
            QpT = [sb(f"h_QpT{i}", [65, 512], BF16) for i in range(2)]
            PT = [sb(f"h_PT{i}", [128, 512], BF16) for i in range(3)]
            rc = sb("h_rc", [128, 4], F32)
            ob = [sb(f"h_ob{i}", [128, DV], BF16) for i in range(2)]
            pS = [ps(f"h_pS{i}", [128, 512], F32) for i in range(2)]
            pO = [ps(f"h_pO{i}", [128, 512], F32) for i in range(4)]
            pX = [ps(f"h_pX{i}", [128, 512], F32) for i in range(2)]
            mset(c, "dve", Vt[:], 1.0, [T("h_Vt")])
            pti = 0
            if upto == "K0":
                c.barrier()
                return
            for h in range(NH):
                c.dma("pool", wuk[:], W["w_uk"][:, h * DN:(h + 1) * DN].rearrange("(k p) n -> p k n", p=128), writes=[T("h_wuk")])
                c.dma("pool", wuv[:], W["w_uv"][:, h * DV:(h + 1) * DV].rearrange("(k p) n -> p k n", p=128), writes=[T("h_wuv")])
                if upto == "K0b":
                    c.barrier()
                    return
                nkb = (NTOK + 511) // 512
                for kb in range(nkb):
                    w = min(512, NTOK - kb * 512)
                    pb = kb % 2
                    for k in range(4):
                        mm(c, pX[pb][:, :w], wuk[:, k, :], ckvT[:, k, kb * 512:kb * 512 + w], k == 0, k == 3, [T("h_wuk"), T("ckvT")], [T(f"h_pX{pb}")])
                    cp(c, "dve", KnT[:, kb * 512:kb * 512 + w], pX[pb][:, :w], [T(f"h_pX{pb}")], [T("h_KnT")])
                    if upto == "K1a":
                        continue
                    act(c, sqk[:, :w], KnT[:, kb * 512:kb * 512 + w], AF.Square, [T("h_KnT")], [T("h_sqk")])
                    if upto == "K1b":
                        continue
                    mm(c, pS[pb][:, :w], ones[:, :], sqk[:, :w], True, True, [T("ones"), T("h_sqk")], [T(f"h_pS{pb}")])
                    tt(c, "dve", ksq[:, kb * 512:kb * 512 + w], pS[pb][:, :w], kpsq[:, kb * 512:kb * 512 + w], ALU.add, [T(f"h_pS{pb}"), T("kpsq")], [T("h_ksq")])
                if upto in ("K1", "K1a", "K1b"):
                    c.barrier()
                    return
                red(c, "dve", km[:, 0:1], ksq[:, :], ALU.max, [T("h_ksq")], [T("h_km")])
                act(c, km[:, 1:2], km[:, 0:1], AF.Sqrt, [T("h_km")], [T("h_km1")])
                ts(c, "dve", kmb[:], km[:, 1:2], -1.0, None, ALU.mult, None, [T("h_km1")], [T("h_kmb")])
                if upto == "K2":
                    c.barrier()
                    return
                for kg in range((NKT + 3) // 4):
                    pb = kg % 2
                    nk = min(4, NKT - kg * 4)
                    for j in range(nk):
                        kt = kg * 4 + j
                        for k in range(4):
                            mm(c, pX[pb][:, j * 128:(j + 1) * 128], ckvT[:, k, kt * 128:(kt + 1) * 128], wuv[:, k, :], k == 0, k == 3,
                               [T("ckvT"), T("h_wuv")], [T(f"h_pX{pb}")], inc=(k == 3 and j == nk - 1))
                    cp(c, "act" if pb == 0 else "dve", Vt[:, kg * 4:kg * 4 + nk, 0:DV], pX[pb][:, 0:nk * 128].rearrange("p (a b) -> p a b", a=nk),
                       [T(f"h_pX{pb}")], [T("h_Vt")])
                if upto == "KV":
                    c.barrier()
                    return
                for qb in range(NLAT // 512):
                    qi = qb % 2
                    for s4 in range(4):
                        t0 = qb * 512 + s4 * 128
                        b = s4 % 2
                        c.dma("sp", qh[b][:, 0:192], QH[h, t0:t0 + 128, :], reads=[T("QH")], writes=[T(f"h_qh{b}")])
                        c.dma("sp", qsq[b][:], QSQ[t0:t0 + 128, :], reads=[T("QSQ")], writes=[T(f"h_qsq{b}")])
                        act(c, nq[:, 0:1], qsq[b][:, h:h + 1], AF.Sqrt, [T(f"h_qsq{b}")], [T("h_nq")])
                        ts(c, "dve", qh[b][:, 192:193], nq[:, 0:1], kmb[:, 0:1], None, ALU.mult, None, [T("h_nq"), T("h_kmb")], [T(f"h_qh{b}")])
                        tr(c, pX[0][:, s4 * 128:(s4 + 1) * 128], qh[b][:, 0:DN], idt[:], [T(f"h_qh{b}"), T("idt")], [T("h_pX0")], inc=False)
                        tr(c, pX[1][0:65, s4 * 128:(s4 + 1) * 128], qh[b][:, DN:193], idt[:], [T(f"h_qh{b}"), T("idt")], [T("h_pX1")])
                    cp(c, "act", QnT[qi][:], pX[0][:], [T("h_pX0")], [T(f"h_QnT{qi}")])
                    cp(c, "dve", QpT[qi][:], pX[1][0:65, :], [T("h_pX1")], [T(f"h_QpT{qi}")])
                    for kt in range(NKT):
                        sbi = kt % 2
                        p3 = pti % 3
                        pti += 1
                        mm(c, pS[sbi][:], KnT[:, kt * 128:(kt + 1) * 128], QnT[qi][:], True, False, [T("h_KnT"), T(f"h_QnT{qi}")], [T(f"h_pS{sbi}")], inc=False)
                        mm(c, pS[sbi][:], kpT[:, kt * 128:(kt + 1) * 128], QpT[qi][:], False, True, [T("kpT"), T(f"h_QpT{qi}")], [T(f"h_pS{sbi}")])
                        act(c, PT[p3][:], pS[sbi][:], AF.Exp, [T(f"h_pS{sbi}")], [T(f"h_PT{p3}")], scale=SCALE)
                        for s4 in range(4):
                            mm(c, pO[s4][:, 0:129], PT[p3][:, s4 * 128:(s4 + 1) * 128], Vt[:, kt, 0:129], kt == 0, kt == NKT - 1,
                               [T(f"h_PT{p3}"), T("h_Vt")], [T(f"h_pO{s4}")], inc=(kt == NKT - 1 or s4 == 3))
                    for s4 in range(4):
                        t0 = qb * 512 + s4 * 128
                        b = s4 % 2
                        c.op("dve", lambda e: e.reciprocal(out=rc[:, s4:s4 + 1], in_=pO[s4][:, 128:129]), [T(f"h_pO{s4}")], [T("h_rc")])
                        ts(c, "dve", ob[b][:], pO[s4][:, 0:DV], rc[:, s4:s4 + 1], None, ALU.mult, None, [T(f"h_pO{s4}"), T("h_rc")], [T(f"h_ob{b}")])
                        c.dma("sp", OD[NCTX + t0:NCTX + t0 + 128, h * DV:(h + 1) * DV], ob[b][:], reads=[T(f"h_ob{b}")], writes=[T("OD")])
        c.barrier()
    c.barrier()
    if dbg is not None:
        dbg(dict(OD=OD, QH=QH, PKV=PKV))
    out_proj_res(c, T, "op_", D, NH * DV, OD, W["w_o"], RES, lat_tiles, {"l": mods["l"]["G1"]}, K)


NE = 32


def moe_local(c, tag, D, FF, NT, RES, tiles, mods, W, K, dbg=None):
    nc = c.nc
    KC = D // 128
    FC = FF // 128
    NBLK = (4 * NT + 511) // 512 + NE
    NSLOT = NBLK * 512
    assert NSLOT % 128 == 0
    T = lambda s: f"{tag}_{s}"
    HF = c.dram(T("HF"), [NT + 128, D], BF16)
    GD = c.dram(T("GD"), [NT, NE], F32)
    LISTF = c.dram(T("LISTF"), [NSLOT, 2], F32)
    ACC = c.dram(T("ACC"), [NT + 128, D], F32)
    W1G2 = W["w1gT"]
    W1L2 = W["w1lT"]
    W22 = W["w2T"]
    c.barrier()
    with contextlib.ExitStack() as es0:
        sb0 = lambda n, s, d: es0.enter_context(nc.sbuf_tensor(T(n), s, d))
        idt = sb0("idt", [128, 128], F32)
        ones = sb0("ones", [128, 128], F32)
        trif = sb0("trif", [128, 128], F32)
        iop = sb0("iop", [128, 1], F32)
        iob = sb0("iob", [128, NBLK], F32)
        cnt = sb0("cnt", [128, NE], F32)
        base = sb0("base", [128, NE], F32)
        widx = sb0("widx", [128, NBLK], I32)
        eidx = sb0("eidx", [128, NBLK], I32)
        widx1 = sb0("widx1", [128, FC, NBLK], I32)
        c.dma("sp", idt[:], K["ident"][:, :], writes=[T("idt")])
        c.dma("sp", ones[:], K["ones"][:, :], writes=[T("ones")])
        c.dma("sp", trif[:], K["trif"][:, :], writes=[T("trif")])
        c.dma("sp", iop[:], K["iota_p"][:, :], writes=[T("iop")])
        c.dma("sp", iob[:], K["iota_b"].partition_broadcast(128), writes=[T("iob")])
        with contextlib.ExitStack() as es:
            sb = lambda n, s, d: es.enter_context(nc.sbuf_tensor(T(n), s, d))
            ps = lambda n, s, d: es.enter_context(nc.psum_tensor(T(n), s, d))
            At = sb("At", [128, D], F32)
            Bt = sb("Bt", [128, D], F32)
            rwt = sb("rwt", [128, KC, NE], F32)
            rbt = sb("rbt", [128, NE], F32)
            xt = [sb(f"xt{i}", [128, D], F32) for i in range(2)]
            junk = sb("junk", [128, D], F32)
            ss = sb("ss", [128, 2], F32)
            h = sb("h", [128, D], F32)
            hb = [sb(f"hb{i}", [128, D], BF16) for i in range(2)]
            hT = sb("hT", [128, KC, 128], F32)
            lg = sb("lg", [128, NE], F32)
            m8 = sb("m8", [128, 8], F32)
            sm = sb("sm", [128, 4], F32)
            ex = sb("ex", [128, NE], F32)
            mk = [sb(f"mk{i}", [128, NE], F32) for i in range(2)]
            Gt = [sb(f"Gt{i}", [128, NE], F32) for i in range(2)]
            zt = sb("zt", [128, D], F32)
            zb = sb("zb", [128, D], BF16)
            lf = sb("lf", [128, NSLOT // 128, 2], F32)
            pT = [ps(f"pT{i}", [128, 512], F32) for i in range(2)]
            pl = ps("pl", [128, NE], F32)
            pc = ps("pc", [128, NE], F32)
            c.dma("sp", rwt[:], W["rw"].rearrange("(k p) e -> p k e", p=128), writes=[T("rwt")])
            c.dma("sp", rbt[:], W["rb"].partition_broadcast(128), writes=[T("rbt")])
            mset(c, "pool", zt[:], 0.0, [T("zt")])
            mset(c, "pool", zb[:], 0.0, [T("zb")])
            mset(c, "pool", lf[:, :, 0:1], float(NT), [T("lf")])
            mset(c, "pool", lf[:, :, 1:2], 0.0, [T("lf")])
            c.dma("sp", LISTF.rearrange("(p a) c -> p a c", p=128), lf[:], reads=[T("lf")], writes=[T("LISTF")])
            c.dma("sp", HF[NT:NT + 128, :], zb[:], reads=[T("zb")], writes=[T("HF")])
            for i in range((NT + 128) // 128):
                c.dma("sp", ACC[i * 128:(i + 1) * 128, :], zt[:], reads=[T("zt")], writes=[T("ACC")])
            cur_ms = None
            ntl = len(tiles)
            for ti, (r0, n, ms, tok0) in enumerate(tiles):
                assert n == 128
                b = ti % 2
                if ms != cur_ms:
                    c.dma("sp", At[:], mods[ms]["A2"].partition_broadcast(128), reads=["MODS"], writes=[T("At")])
                    c.dma("sp", Bt[:], mods[ms]["B2"].partition_broadcast(128), reads=["MODS"], writes=[T("Bt")])
                    cur_ms = ms
                c.dma("sp", xt[b][:], RES[r0:r0 + 128, :], reads=["RES"], writes=[T(f"xt{b}")])
                rstd = rmsnorm_rstd(c, xt[b][:], junk[:], ss[:], D, [T(f"xt{b}")], T("n1"))
                stt(c, "dve", h[:], xt[b][:], rstd, At[:], ALU.mult, ALU.mult, [T(f"xt{b}"), T("n1_ss1"), T("At")], [T("h")])
                tt(c, "pool", h[:], h[:], Bt[:], ALU.add, [T("h"), T("Bt")], [T("h")])
                cp(c, "act", hb[b][:], h[:], [T("h")], [T(f"hb{b}")])
                c.dma("sp", HF[tok0:tok0 + 128, :], hb[b][:], reads=[T(f"hb{b}")], writes=[T("HF")])
                for kg in range(KC // 4):
                    pb = kg % 2
                    for jj in range(4):
                        k = kg * 4 + jj
                        tr(c, pT[pb][:, jj * 128:(jj + 1) * 128], h[:, k * 128:(k + 1) * 128], idt[:], [T("h"), T("idt")], [T(f"pT{pb}")], inc=(jj == 3))
                    cp(c, "act" if kg % 2 == 0 else "dve", hT[:, kg * 4:(kg + 1) * 4, :], pT[pb][:].rearrange("p (a b) -> p a b", a=4), [T(f"pT{pb}")], [T("hT")])
                for k in range(KC):
                    mm(c, pl[:, :], hT[:, k, :], rwt[:, k, :], k == 0, k == KC - 1, [T("hT"), T("rwt")], [T("pl")])
                tt(c, "dve", lg[:], pl[:, :], rbt[:], ALU.add, [T("pl"), T("rbt")], [T("lg")])
                c.op("dve", lambda e: e.max(out=m8[:], in_=lg[:]), [T("lg")], [T("m8")])
                ts(c, "dve", mk[b][:], lg[:], m8[:, 3:4], None, ALU.is_ge, None, [T("lg"), T("m8")], [T(f"mk{b}")])
                ts(c, "dve", sm[:, 0:1], m8[:, 0:1], -1.0, None, ALU.mult, None, [T("m8")], [T("sm0")])
                act(c, ex[:], lg[:], AF.Exp, [T("lg"), T("sm0")], [T("ex")], bias=sm[:, 0:1])
                tt(c, "dve", ex[:], ex[:], mk[b][:], ALU.mult, [T("ex"), T(f"mk{b}")], [T("ex")])
                red(c, "dve", sm[:, 1:2], ex[:], ALU.add, [T("ex")], [T("sm1")])
                c.op("dve", lambda e: e.reciprocal(out=sm[:, 2:3], in_=sm[:, 1:2]), [T("sm1")], [T("sm2")])
                ts(c, "dve", Gt[b][:], ex[:], sm[:, 2:3], None, ALU.mult, None, [T("ex"), T("sm2")], [T(f"Gt{b}")])
                c.dma("sp", GD[tok0:tok0 + 128, :], Gt[b][:], reads=[T(f"Gt{b}")], writes=[T("GD")])
                mm(c, pc[:, :], ones[:], mk[b][:], ti == 0, ti == ntl - 1, [T("ones"), T(f"mk{b}")], [T("pc")], inc=True)
            cp(c, "dve", cnt[:], pc[:, :], [T("pc")], [T("cnt")])
        c.barrier()
        with contextlib.ExitStack() as es:
            sb = lambda n, s, d: es.enter_context(nc.sbuf_tensor(T(n), s, d))
            r = sb("r", [128, NE], F32)
            nb = sb("nb", [128, NE], F32)
            inc_ = [sb(f"inc{i}", [128, NE], F32) for i in range(2)]
            eid = sb("eid", [128, NBLK], F32)
            tmpb = sb("tmpb", [128, NBLK], F32)
            mset(c, "dve", nb[:], 0.0, [T("nb")])
            for j in range((4 * NT + 511) // 512 + 1):
                ts(c, "dve", r[:], cnt[:], 512.0 * j, None, ALU.is_gt, None, [T("cnt")], [T("r")])
                tt(c, "dve", nb[:], nb[:], r[:], ALU.add, [T("nb"), T("r")], [T("nb")])
            cp(c, "dve", inc_[0][:], nb[:], [T("nb")], [T("inc0")])
            src = 0
            sh = 1
            while sh < NE:
                dst = 1 - src
                tt(c, "dve", inc_[dst][:, sh:NE], inc_[src][:, sh:NE], inc_[src][:, 0:NE - sh], ALU.add, [T(f"inc{src}")], [T(f"inc{dst}")])
                cp(c, "dve", inc_[dst][:, 0:sh], inc_[src][:, 0:sh], [T(f"inc{src}")], [T(f"inc{dst}")])
                src = dst
                sh *= 2
            incl = inc_[src]
            tt(c, "dve", base[:], incl[:], nb[:], ALU.subtract, [T(f"inc{src}"), T("nb")], [T("base")])
            ts(c, "dve", base[:], base[:], 512.0, None, ALU.mult, None, [T("base")], [T("base")])
            mset(c, "dve", eid[:], 0.0, [T("eid")])
            for e in range(NE):
                ts(c, "dve", tmpb[:], iob[:], incl[:, e:e + 1], None, ALU.is_ge, None, [T("iob"), T(f"inc{src}")], [T("tmpb")])
                tt(c, "dve", eid[:], eid[:], tmpb[:], ALU.add, [T("eid"), T("tmpb")], [T("eid")])
            ts(c, "dve", eid[:], eid[:], float(NE - 1), None, ALU.min, None, [T("eid")], [T("eid")])
            cp(c, "dve", eidx[:], eid[:], [T("eid")], [T("eidx")])
            ts(c, "dve", tmpb[:], eid[:], 128.0, iop[:, 0:1], ALU.mult, ALU.add, [T("eid"), T("iop")], [T("tmpb")])
            cp(c, "dve", widx[:], tmpb[:], [T("tmpb")], [T("widx")])
            ts(c, "dve", eid[:], eid[:], 128.0 * FC, iop[:, 0:1], ALU.mult, ALU.add, [T("eid"), T("iop")], [T("eid")])
            for fc in range(FC):
                ts(c, "dve", tmpb[:], eid[:], 128.0 * fc, None, ALU.add, None, [T("eid")], [T("tmpb")])
                cp(c, "dve", widx1[:, fc, :], tmpb[:], [T("tmpb")], [T("widx")])
        c.barrier()
        with contextlib.ExitStack() as es:
            sb = lambda n, s, d: es.enter_context(nc.sbuf_tensor(T(n), s, d))
            ps = lambda n, s, d: es.enter_context(nc.psum_tensor(T(n), s, d))
            Gl = [sb(f"Gl{i}", [128, NE], F32) for i in range(2)]
            Mk = sb("Mk", [128, NE], F32)
            car = sb("car", [128, NE], F32)
            key = sb("key", [128, NE], F32)
            k8 = sb("k8", [128, 8], F32)
            eq = sb("eq", [128, NE], F32)
            dsti = [sb(f"dsti{i}", [128, 4], I32) for i in range(2)]
            dstf = sb("dstf", [128, 4], F32)
            pay = [sb(f"pay{i}", [128, 4, 2], F32) for i in range(2)]
            pcs = ps("pcs", [128, NE], F32)
            pcr = ps("pcr", [128, NE], F32)
            mset(c, "dve", car[:], 0.0, [T("car")])
            for ti, (r0, n, ms, tok0) in enumerate(tiles):
                b = ti % 2
                c.dma("sp", Gl[b][:], GD[tok0:tok0 + 128, :], reads=[T("GD")], writes=[T(f"Gl{b}")])
                ts(c, "dve", Mk[:], Gl[b][:], 0.0, None, ALU.is_gt, None, [T(f"Gl{b}")], [T("Mk")])
                mm(c, pcs[:, :], trif[:], Mk[:], True, True, [T("trif"), T("Mk")], [T("pcs")])
                mm(c, pcr[:, :], ones[:], Mk[:], True, True, [T("ones"), T("Mk")], [T("pcr")])
                tt(c, "dve", key[:], pcs[:, :], car[:], ALU.add, [T("pcs"), T("car")], [T("key")])
                tt(c, "dve", key[:], key[:], base[:], ALU.add, [T("key"), T("base")], [T("key")])
                tt(c, "dve", key[:], key[:], Mk[:], ALU.mult, [T("key"), T("Mk")], [T("key")])
                tt(c, "dve", car[:], car[:], pcr[:, :], ALU.add, [T("car"), T("pcr")], [T("car")])
                c.op("dve", lambda e: e.max(out=k8[:], in_=key[:]), [T("key")], [T("k8")])
                ts(c, "dve", dstf[:], k8[:, 0:4], -1.0, None, ALU.add, None, [T("k8")], [T("dstf")])
                cp(c, "dve", dsti[b][:], dstf[:], [T("dstf")], [T(f"dsti{b}")])
                for k in range(4):
                    ts(c, "dve", eq[:], key[:], k8[:, k:k + 1], None, ALU.is_equal, None, [T("key"), T("k8")], [T("eq")])
                    tt(c, "dve", eq[:], eq[:], Gl[b][:], ALU.mult, [T("eq"), T(f"Gl{b}")], [T("eq")])
                    red(c, "dve", pay[b][:, k, 1:2], eq[:], ALU.add, [T("eq")], [T(f"pay{b}")])
                    ts(c, "dve", pay[b][:, k, 0:1], iop[:, 0:1], float(tok0), None, ALU.add, None, [T("iop")], [T(f"pay{b}")])
                for k in range(4):
                    c.idma(LISTF[:, :], bass.IndirectOffsetOnAxis(ap=dsti[b][:, k:k + 1], axis=0), pay[b][:, k, :], None,
                           reads=[T(f"dsti{b}"), T(f"pay{b}")], writes=[T("LISTF")])
        c.barrier()
        with contextlib.ExitStack() as es:
            sb = lambda n, s, d: es.enter_context(nc.sbuf_tensor(T(n), s, d))
            ps = lambda n, s, d: es.enter_context(nc.psum_tensor(T(n), s, d))
            idb = sb("idb", [128, 128], BF16)
            lst = [sb(f"lst{i}", [128, 4, 2], F32) for i in range(2)]
            tkf = sb("tkf", [128, 4], F32)
            pad1 = sb("pad1", [128, 4], F32)
            tki = [sb(f"tki{i}", [128, 4], I32) for i in range(2)]
            xg = [sb(f"xg{i}", [128, D], BF16) for i in range(2)]
            xT = sb("xT", [128, KC, 512], BF16)
            yT = sb("yT", [128, FC, 512], BF16)
            w2t = sb("w2t", [128, FC, D], BF16)
            b2t = sb("b2t", [128, D], F32)
            b1gt = sb("b1gt", [128, FC], F32)
            b1lt = sb("b1lt", [128, FC], F32)
            w1gt = [sb(f"w1g{i}", [128, KC, 128], BF16) for i in range(2)]
            w1lt = [sb(f"w1l{i}", [128, KC, 128], BF16) for i in range(2)]
            stg = [sb(f"stg{i}", [128, max(D, KC * 128)], F32) for i in range(4)]
            nstg = 0
            gs = [sb(f"gs{i}", [128, 512], F32) for i in range(2)]
            sg = [sb(f"sg{i}", [128, 512], F32) for i in range(2)]
            ls = [sb(f"ls{i}", [128, 512], F32) for i in range(2)]
            yb = [sb(f"yb{i}", [128, D], F32) for i in range(2)]
            TW = min(8, KC)
            ptr = [ps(f"ptr{i}", [128, TW * 128], BF16) for i in range(2)]
            pgl = [ps(f"pgl{i}", [128, 512], F32) for i in range(4)]
            po = [ps(f"po{i}", [128, 512], F32) for i in range(2)]
            cp(c, "dve", idb[:], idt[:], [T("idt")], [T("idb")])
            gcount = 0
            for blk in range(NBLK):
                lb = blk % 2
                c.dma("sp", lst[lb][:], LISTF[blk * 512:(blk + 1) * 512, :].rearrange("(s p) c -> p s c", p=128), reads=[T("LISTF")], writes=[T(f"lst{lb}")])
                cp(c, "dve", tkf[:], lst[lb][:, :, 0], [T(f"lst{lb}")], [T("tkf")])
                ts(c, "dve", pad1[:], tkf[:], float(NT), None, ALU.is_ge, None, [T("tkf")], [T("pad1")])
                ts(c, "dve", pad1[:], pad1[:], iop[:, 0:1], None, ALU.mult, None, [T("pad1"), T("iop")], [T("pad1")])
                tt(c, "dve", tkf[:], tkf[:], pad1[:], ALU.add, [T("tkf"), T("pad1")], [T("tkf")])
                cp(c, "dve", tki[lb][:], tkf[:], [T("tkf")], [T(f"tki{lb}")])
                wofs = bass.IndirectOffsetOnAxis(ap=widx[:, blk:blk + 1], axis=0)
                for fc in range(FC):
                    sgi = nstg % 4
                    nstg += 1
                    c.idma(stg[sgi][:, 0:D], None, W22[:, :], bass.IndirectOffsetOnAxis(ap=widx1[:, fc, blk:blk + 1], axis=0),
                           reads=[T("widx")], writes=[T(f"stg{sgi}")])
                    cp(c, "act" if fc % 2 == 0 else "dve", w2t[:, fc, :], stg[sgi][:, 0:D], [T(f"stg{sgi}")], [T("w2t")])
                c.idma(b1gt[:], None, W["b1gT"][:, :], wofs, reads=[T("widx")], writes=[T("b1gt")])
                c.idma(b1lt[:], None, W["b1lT"][:, :], wofs, reads=[T("widx")], writes=[T("b1lt")])
                c.idma(b2t[:], None, W["b2"][:, :], bass.IndirectOffsetOnAxis(ap=eidx[:, blk:blk + 1], axis=0), reads=[T("eidx")], writes=[T("b2t")])
                for st in range(4):
                    b = gcount % 2
                    gcount += 1
                    c.idma(xg[b][:], None, HF[:, :], bass.IndirectOffsetOnAxis(ap=tki[lb][:, st:st + 1], axis=0),
                           reads=[T(f"tki{lb}"), T("HF")], writes=[T(f"xg{b}")])
                    for kg in range(KC // TW):
                        pb = (kg + st) % 2
                        for jj in range(TW):
                            k = kg * TW + jj
                            tr(c, ptr[pb][:, jj * 128:(jj + 1) * 128], xg[b][:, k * 128:(k + 1) * 128], idb[:], [T(f"xg{b}"), T("idb")], [T(f"ptr{pb}")], inc=(jj == TW - 1))
                        cp(c, "act" if pb == 0 else "dve", xT[:, kg * TW:(kg + 1) * TW, st * 128:(st + 1) * 128],
                           ptr[pb][:].rearrange("p (a b) -> p a b", a=TW), [T(f"ptr{pb}")], [T("xT")])
                for fc in range(FC):
                    wb = fc % 2
                    wofs1 = bass.IndirectOffsetOnAxis(ap=widx1[:, fc, blk:blk + 1], axis=0)
                    sgi = nstg % 4
                    nstg += 1
                    c.idma(stg[sgi][:, 0:KC * 128], None, W1G2[:, :], wofs1, reads=[T("widx")], writes=[T(f"stg{sgi}")])
                    cp(c, "act", w1gt[wb][:], stg[sgi][:, 0:KC * 128].rearrange("p (k f) -> p k f", k=KC), [T(f"stg{sgi}")], [T(f"w1g{wb}")])
                    sgi = nstg % 4
                    nstg += 1
                    c.idma(stg[sgi][:, 0:KC * 128], None, W1L2[:, :], wofs1, reads=[T("widx")], writes=[T(f"stg{sgi}")])
                    cp(c, "dve", w1lt[wb][:], stg[sgi][:, 0:KC * 128].rearrange("p (k f) -> p k f", k=KC), [T(f"stg{sgi}")], [T(f"w1l{wb}")])
                    pgi = pgl[2 * wb]
                    pli = pgl[2 * wb + 1]
                    for k in range(KC):
                        mm(c, pgi[:], w1gt[wb][:, k, :], xT[:, k, :], k == 0, k == KC - 1, [T(f"w1g{wb}"), T("xT")], [T(f"pgl{2 * wb}")])
                    for k in range(KC):
                        mm(c, pli[:], w1lt[wb][:, k, :], xT[:, k, :], k == 0, k == KC - 1, [T(f"w1l{wb}"), T("xT")], [T(f"pgl{2 * wb + 1}")])
                    ts(c, "dve", gs[wb][:], pgi[:], b1gt[:, fc:fc + 1], 7.0, ALU.add, ALU.min, [T(f"pgl{2 * wb}"), T("b1gt")], [T(f"gs{wb}")])
                    act(c, sg[wb][:], gs[wb][:], AF.Sigmoid, [T(f"gs{wb}")], [T(f"sg{wb}")], scale=1.702)
                    ts(c, "dve", ls[wb][:], pli[:], b1lt[:, fc:fc + 1], 7.0, ALU.add, ALU.min, [T(f"pgl{2 * wb + 1}"), T("b1lt")], [T(f"ls{wb}")])
                    ts(c, "dve", ls[wb][:], ls[wb][:], -7.0, 1.0, ALU.max, ALU.add, [T(f"ls{wb}")], [T(f"ls{wb}")])
                    tt(c, "dve", gs[wb][:], gs[wb][:], sg[wb][:], ALU.mult, [T(f"gs{wb}"), T(f"sg{wb}")], [T(f"gs{wb}")])
                    tt(c, "dve", yT[:, fc, :], gs[wb][:], ls[wb][:], ALU.mult, [T(f"gs{wb}"), T(f"ls{wb}")], [T("yT")])
                for st in range(4):
                    ob = st % 2
                    for nbk in range(D // 512):
                        pb = nbk % 2
                        for fc in range(FC):
                            mm(c, po[pb][:], yT[:, fc, st * 128:(st + 1) * 128], w2t[:, fc, nbk * 512:(nbk + 1) * 512], fc == 0, fc == FC - 1,
                               [T("yT"), T("w2t")], [T(f"po{pb}")])
                        tt(c, "dve", yb[ob][:, nbk * 512:(nbk + 1) * 512], po[pb][:], b2t[:, nbk * 512:(nbk + 1) * 512], ALU.add,
                           [T(f"po{pb}"), T("b2t")], [T(f"yb{ob}")])
                    act(c, yb[ob][:], yb[ob][:], AF.Identity, [T(f"yb{ob}"), T(f"lst{lb}")], [T(f"yb{ob}")], scale=lst[lb][:, st, 1:2])
                    c.idma(ACC[:, :], bass.IndirectOffsetOnAxis(ap=tki[lb][:, st:st + 1], axis=0), yb[ob][:], None,
                           reads=[T(f"tki{lb}"), T(f"yb{ob}")], writes=[T("ACC")], compute_op=ALU.add)
        c.barrier()
    c.barrier()
    if dbg is not None:
        dbg(dict(ACC=ACC, GD=GD, LISTF=LISTF))
    with contextlib.ExitStack() as es:
        sb = lambda n, s, d: es.enter_context(nc.sbuf_tensor(T(n), s, d))
        Gm = sb("Gm", [128, D], F32)
        xr = [sb(f"xr{i}", [128, D], F32) for i in range(2)]
        fr = [sb(f"fr{i}", [128, D], F32) for i in range(2)]
        cur_ms = None
        for ti, (r0, n, ms, tok0) in enumerate(tiles):
            b = ti % 2
            if ms != cur_ms:
                c.dma("sp", Gm[:], mods[ms]["G2"].partition_broadcast(128), reads=["MODS"], writes=[T("Gm")])
                cur_ms = ms
            c.dma("sp", xr[b][:], RES[r0:r0 + 128, :], reads=["RES"], writes=[T(f"xr{b}")])
            c.dma("sp", fr[b][:], ACC[tok0:tok0 + 128, :], reads=[T("ACC")], writes=[T(f"fr{b}")])
            tt(c, "dve", fr[b][:], fr[b][:], Gm[:], ALU.mult, [T(f"fr{b}"), T("Gm")], [T(f"fr{b}")])
            tt(c, "pool", xr[b][:], xr[b][:], fr[b][:], ALU.add, [T(f"xr{b}"), T(f"fr{b}")], [T(f"xr{b}")])
            c.dma("sp", RES[r0:r0 + 128, :], xr[b][:], reads=[T(f"xr{b}")], writes=["RES"])
    c.barrier()

import re as _re

D_MODEL = 2048
NBATCH = 2
SEQ = 8192
NCTX = 256
FFE = 2048


def mods_phase(c, CROW, ADAW, ADAB, MIXG, FFNG, MODV, K):
    nc = c.nc
    D = D_MODEL
    KC = D // 128
    T = lambda s: "md_" + s
    with contextlib.ExitStack() as es:
        sb = lambda n, s, d: es.enter_context(nc.sbuf_tensor(T(n), s, d))
        ps = lambda n, s, d: es.enter_context(nc.psum_tensor(T(n), s, d))
        idt = sb("idt", [128, 128], F32)
        cr = sb("cr", [3, D], F32)
        ST = sb("ST", [128, KC, 3], F32)
        wt = [sb(f"wt{i}", [128, KC, 512], F32) for i in range(2)]
        bt = [sb(f"bt{i}", [3, 512], F32) for i in range(2)]
        M = sb("M", [3, 6 * D], F32)
        g1 = sb("g1", [3, D], F32)
        g2 = sb("g2", [3, D], F32)
        MV = [sb(f"MV{i}", [3, D], F32) for i in range(2)]
        pT = ps("pT", [128, KC * 3], F32)
        pm = [ps(f"pm{i}", [3, 512], F32) for i in range(2)]
        c.dma("sp", idt[:], K["ident"][:, :], writes=[T("idt")])
        c.dma("sp", cr[:], CROW[:, :], writes=[T("cr")])
        act(c, cr[:], cr[:], AF.Silu, [T("cr")], [T("cr")])
        for k in range(KC):
            tr(c, pT[:, k * 3:(k + 1) * 3], cr[:, k * 128:(k + 1) * 128], idt[0:3, 0:3], [T("cr"), T("idt")], [T("pT")], inc=(k == KC - 1))
        cp(c, "dve", ST[:], pT[:].rearrange("p (k r) -> p k r", r=3), [T("pT")], [T("ST")])
        for i in range(2):
            c.dma("sp", g1[:], MIXG[i:i + 1, :].partition_broadcast(3), writes=[T("g1")])
            c.dma("sp", g2[:], FFNG[i:i + 1, :].partition_broadcast(3), writes=[T("g2")])
            for nb in range(6 * D // 512):
                wb = nb % 2
                c.dma("sp", wt[wb][:], ADAW[i * D:(i + 1) * D, nb * 512:(nb + 1) * 512].rearrange("(k p) n -> p k n", p=128), writes=[T(f"wt{wb}")])
                c.dma("sp", bt[wb][:], ADAB[i:i + 1, nb * 512:(nb + 1) * 512].partition_broadcast(3), writes=[T(f"bt{wb}")])
                for k in range(KC):
                    mm(c, pm[wb][:, :], ST[:, k, :], wt[wb][:, k, :], k == 0, k == KC - 1, [T("ST"), T(f"wt{wb}")], [T(f"pm{wb}")])
                tt(c, "dve", M[:, nb * 512:(nb + 1) * 512], pm[wb][:, :], bt[wb][:, :], ALU.add, [T(f"pm{wb}"), T(f"bt{wb}")], [T("M")])
            MO = MODV[i * 18:(i + 1) * 18, :].rearrange("(r k) d -> r k d", k=6)
            for k, (src0, gg) in enumerate(((D, g1), (0, None), (2 * D, None), (4 * D, g2), (3 * D, None), (5 * D, None))):
                mv = MV[k % 2]
                if gg is not None:
                    stt(c, "dve", mv[:], M[:, src0:src0 + D], 1.0, gg[:], ALU.add, ALU.mult, [T("M"), T("g1"), T("g2")], [T(f"MV{k % 2}")])
                else:
                    cp(c, "dve", mv[:], M[:, src0:src0 + D], [T("M")], [T(f"MV{k % 2}")])
                c.dma("sp", MO[:, k, :], mv[:], reads=[T(f"MV{k % 2}")], writes=["MODS"])
    c.barrier()


def modrow(MODV, i, r, k):
    j = (i * 3 + r) * 6 + k
    return MODV[j:j + 1, :]


def final_norm(c, tag, RES, r0, nrows, G, OUT, o0):
    nc = c.nc
    D = D_MODEL
    T = lambda s: f"{tag}_{s}"
    with contextlib.ExitStack() as es:
        sb = lambda n, s, d: es.enter_context(nc.sbuf_tensor(T(n), s, d))
        gt = sb("gt", [128, D], F32)
        xt = [sb(f"xt{i}", [128, D], F32) for i in range(2)]
        ot = [sb(f"ot{i}", [128, D], F32) for i in range(2)]
        junk = sb("junk", [128, D], F32)
        ss = sb("ss", [128, 2], F32)
        c.dma("sp", gt[:], G.partition_broadcast(128), writes=[T("gt")])
        for i in range(nrows // 128):
            b = i % 2
            c.dma("sp", xt[b][:], RES[r0 + i * 128:r0 + (i + 1) * 128, :], reads=["RES"], writes=[T(f"xt{b}")])
            rstd = rmsnorm_rstd(c, xt[b][:], junk[:], ss[:], D, [T(f"xt{b}")], T("n"))
            stt(c, "dve", ot[b][:], xt[b][:], rstd, gt[:], ALU.mult, ALU.mult, [T(f"xt{b}"), T("n_ss1"), T("gt")], [T(f"ot{b}")])
            c.dma("sp", OUT[o0 + i * 128:o0 + (i + 1) * 128, :], ot[b][:], reads=[T(f"ot{b}")], writes=["OUT"])
    c.barrier()


def build_program(nbatch=NBATCH, seq=SEQ, nctx=NCTX):
    c = Ctx()
    D = D_MODEL
    NTOK = nctx + seq
    ext = lambda n, s, dt=F32: c.dram(n, s, dt, "ExternalInput")
    X = ext("x", [nbatch * seq, D])
    CTX = ext("ctx", [nbatch * nctx, D])
    CROW = ext("crow", [3, D])
    ADAW = ext("ada_w", [2 * D, 6 * D])
    ADAB = ext("ada_b", [2, 6 * D])
    MIXG = ext("mix_g", [2, D])
    FFNG = ext("ffn_g", [2, D])
    FING = ext("fin_g", [1, D])
    WH = {"w_in": ext("h_w_in", [D, HIN]), "conv_w": ext("h_conv_w", [3, XW]), "conv_b": ext("h_conv_b", [1, XW]), "dtb": ext("h_dtb", [1, 64]),
          "alog": ext("h_alog", [1, 64]), "dsk": ext("h_dsk", [1, 32]), "ssdg": ext("h_ssdg", [1, SW]), "vng": ext("h_vng", [1, GW]),
          "wsT": ext("h_wsT", [16, 128, 128]), "bsT": ext("h_bsT", [128, 16]), "w_out": ext("h_w_out", [2 * SW, D])}
    WA = {"w_in": ext("a_w_in", [D, 1344]), "qng": ext("a_qng", [1, 768]), "kvng": ext("a_kvng", [1, 512]), "zero768": ext("a_zero768", [1, 768]),
          "w_uq": ext("a_w_uq", [768, 3072]), "w_uk": ext("a_w_uk", [512, 2048]), "w_uv": ext("a_w_uv", [512, 2048]), "w_o": ext("a_w_o", [2048, D]),
          "cos": ext("a_cos", [seq, 32]), "sin": ext("a_sin", [seq, 32])}
    FC = FFE // 128
    KC = D // 128
    WM = []
    for i in range(2):
        WM.append({"rw": ext(f"m{i}_rw", [D, 32]), "rb": ext(f"m{i}_rb", [1, 32]),
                   "w1gT": ext(f"m{i}_w1gT", [32 * FC * 128, KC * 128]), "w1lT": ext(f"m{i}_w1lT", [32 * FC * 128, KC * 128]),
                   "w2T": ext(f"m{i}_w2T", [32 * FFE, D]), "b1gT": ext(f"m{i}_b1gT", [32 * 128, FC]), "b1lT": ext(f"m{i}_b1lT", [32 * 128, FC]),
                   "b2": ext(f"m{i}_b2", [32, D])})
    NB0 = (4 * NTOK + 511) // 512 + 32
    NB1 = (4 * seq + 511) // 512 + 32
    K = {k: ext("k_" + k, [128, 128]) for k in ("ident", "ones", "trif", "trib")}
    K["iota_p"] = ext("k_iota_p", [128, 1])
    K0 = dict(K)
    K0["iota_b"] = ext("k_iota_b0", [1, NB0])
    K1 = dict(K)
    K1["iota_b"] = ext("k_iota_b1", [1, NB1])
    OUT = c.dram("out", [nbatch * seq, D], F32, "ExternalOutput")
    MODV = c.dram("MODV", [36, D], F32)
    mods_phase(c, CROW, ADAW, ADAB, MIXG, FFNG, MODV, K)
    for b in range(nbatch):
        RES = c.dram(f"b{b}RES", [NTOK, D], F32)
        for r0 in range(0, nctx, 128):
            c.dma("sp", RES[r0:r0 + 128, :], CTX[b * nctx + r0:b * nctx + r0 + 128, :], writes=["RES"])
        for r0 in range(0, seq, 128):
            c.dma("sp", RES[nctx + r0:nctx + r0 + 128, :], X[b * seq + r0:b * seq + r0 + 128, :], writes=["RES"])
        c.barrier()
        m = []
        for i in range(2):
            m.append({"l": {nm: modrow(MODV, i, b, k) for k, nm in enumerate(("A1", "B1", "G1", "A2", "B2", "G2"))},
                      "c": {nm: modrow(MODV, i, 2, k) for k, nm in enumerate(("A1", "B1", "G1", "A2", "B2", "G2"))}})
        hybrid_phase(c, f"b{b}H", D, nctx, seq, RES, m[0], WH, K)
        tiles0 = [(r0, 128, "c" if r0 < nctx else "l", r0) for r0 in range(0, NTOK, 128)]
        moe_local(c, f"b{b}M0", D, FFE, NTOK, RES, tiles0, m[0], WM[0], K0)
        mla_phase(c, f"b{b}A", D, nctx, seq, RES, m[1], WA, K)
        tiles1 = [(nctx + r0, 128, "l", r0) for r0 in range(0, seq, 128)]
        moe_local(c, f"b{b}M1", D, FFE, seq, RES, tiles1, m[1], WM[1], K1)
        final_norm(c, f"b{b}F", RES, nctx, seq, FING, OUT, b * seq)
    c.finish()
    return c


def host_inputs(inputs, nbatch=NBATCH, seq=SEQ, nctx=NCTX):
    f32 = np.float32
    A = lambda a: np.ascontiguousarray(np.asarray(a, dtype=f32))
    D = D_MODEL
    g = lambda k: np.asarray(inputs[k])
    m = {}
    m["ada_w"] = A(g("ada_w").reshape(2 * D, 6 * D))
    m["ada_b"] = A(g("ada_b").reshape(2, 6 * D))
    m["mix_g"] = A(g("mix_norm_g"))
    m["ffn_g"] = A(g("ffn_norm_g"))
    m["fin_g"] = A(g("final_norm_g").reshape(1, D))
    m["h_w_in"] = A(g("hyb_w_in")[0])
    m["h_conv_w"] = A(g("hyb_conv_w")[0])
    m["h_conv_b"] = A(g("hyb_conv_b")[0].reshape(1, -1))
    m["h_dtb"] = A(g("hyb_dt_bias")[0].reshape(1, 64))
    m["h_alog"] = A(g("hyb_a_log")[0].reshape(1, 64))
    m["h_dsk"] = A(g("hyb_d_skip")[0].reshape(1, 32))
    m["h_ssdg"] = A(g("hyb_ssd_norm_g")[0].reshape(1, -1))
    m["h_vng"] = A(g("hyb_v_norm_g")[0].reshape(1, -1))
    m["h_wsT"] = A(g("hyb_w_s")[0].transpose(0, 2, 1))
    m["h_bsT"] = A(g("hyb_b_s")[0].T)
    m["h_w_out"] = A(g("hyb_w_out")[0])
    m["a_w_in"] = A(g("mla_w_in")[0])
    m["a_qng"] = A(g("mla_q_norm_g")[0].reshape(1, -1))
    m["a_kvng"] = A(g("mla_kv_norm_g")[0].reshape(1, -1))
    m["a_zero768"] = np.zeros((1, 768), f32)
    m["a_w_uq"] = A(g("mla_w_uq")[0])
    wk = g("mla_w_ukv")[0].reshape(512, 16, 256)
    m["a_w_uk"] = A(wk[:, :, :128].reshape(512, 2048))
    m["a_w_uv"] = A(wk[:, :, 128:].reshape(512, 2048))
    m["a_w_o"] = A(g("mla_w_o")[0])
    rows = seq // 64
    row = np.repeat(np.arange(rows), 64).astype(np.float64)
    col = np.tile(np.arange(64), rows).astype(np.float64)
    freqs = (10000.0 ** (-np.arange(16, dtype=np.float32) / 16)).astype(np.float32)
    ang = np.stack([row[:, None].astype(f32) * freqs, col[:, None].astype(f32) * freqs], 1).astype(f32)
    m["a_cos"] = A(np.cos(ang).reshape(seq, 32))
    m["a_sin"] = A(np.sin(ang).reshape(seq, 32))
    FC = FFE // 128
    for i in range(2):
        m[f"m{i}_rw"] = A(g("router_w")[i])
        m[f"m{i}_rb"] = A(g("router_b")[i].reshape(1, 32))
        w1 = g("exp_w1")[i]
        w5 = w1.reshape(32, D // 128, 128, FC, 128, 2)
        m[f"m{i}_w1gT"] = A(w5[..., 0].transpose(0, 3, 2, 1, 4).reshape(32 * FC * 128, (D // 128) * 128))
        m[f"m{i}_w1lT"] = A(w5[..., 1].transpose(0, 3, 2, 1, 4).reshape(32 * FC * 128, (D // 128) * 128))
        m[f"m{i}_w2T"] = A(g("exp_w2")[i].reshape(32 * FFE, D))
        b1 = g("exp_b1")[i].reshape(32, FC, 128, 2)
        m[f"m{i}_b1gT"] = A(b1[..., 0].transpose(0, 2, 1).reshape(32 * 128, FC))
        m[f"m{i}_b1lT"] = A(b1[..., 1].transpose(0, 2, 1).reshape(32 * 128, FC))
        m[f"m{i}_b2"] = A(g("exp_b2")[i])
    tri = np.tril(np.ones((128, 128), f32))
    m["k_ident"] = np.eye(128, dtype=f32)
    m["k_ones"] = np.ones((128, 128), f32)
    m["k_trif"] = A(tri.T)
    m["k_trib"] = A(tri)
    m["k_iota_p"] = np.arange(128, dtype=f32).reshape(128, 1)
    NTOK = nctx + seq
    NB0 = (4 * NTOK + 511) // 512 + 32
    NB1 = (4 * seq + 511) // 512 + 32
    m["k_iota_b0"] = np.arange(NB0, dtype=f32).reshape(1, NB0)
    m["k_iota_b1"] = np.arange(NB1, dtype=f32).reshape(1, NB1)
    return m


def kernel(**inputs):
    c = build_program(nbatch=1)
    m = host_inputs(inputs)
    f32 = np.float32
    x = np.asarray(inputs["x"], dtype=f32)
    ctx = np.asarray(inputs["ctx"], dtype=f32)
    cc = np.asarray(inputs["c"], dtype=f32)
    c_ctx = np.asarray(inputs["c_ctx"], dtype=f32).reshape(1, D_MODEL)
    in_maps = []
    for b in range(NBATCH):
        mb = dict(m)
        mb["x"] = np.ascontiguousarray(x[b])
        mb["ctx"] = np.ascontiguousarray(ctx[b])
        mb["crow"] = np.ascontiguousarray(np.concatenate([cc[b:b + 1], cc[b:b + 1], c_ctx], 0))
        in_maps.append(mb)
    res = run_bass_kernel_spmd(c.nc, in_maps, core_ids=list(range(NBATCH)))
    out = np.stack([np.asarray(res.results[b]["out"], dtype=f32) for b in range(NBATCH)], 0)
    return out.reshape(NBATCH, SEQ, D_MODEL)
```

```python
import contextlib
import numpy as np
import concourse.bass as bass
import concourse.mybir as mybir
from concourse.bass_utils import run_bass_kernel_spmd

F32 = mybir.dt.float32
BF16 = mybir.dt.bfloat16
I32 = mybir.dt.int32
AF = mybir.ActivationFunctionType
ALU = mybir.AluOpType
AX = mybir.AxisListType

NDSEM = 8


class Ctx:
    def __init__(self):
        nc = bass.Bass("TRN2", target_bir_lowering=False)
        self.nc = nc
        self.E = {"pe": nc.tensor, "act": nc.scalar, "dve": nc.vector, "pool": nc.gpsimd, "sp": nc.sync}
        self.csem = {e: nc.alloc_semaphore(name=f"c_{e}") for e in ("pe", "act", "dve", "pool")}
        self.ccount = {e: 0 for e in self.csem}
        self.dsem = {q: [nc.alloc_semaphore(name=f"d_{q}{i}") for i in range(NDSEM)] for q in ("sp", "pool")}
        self.dval = {q: [0] * NDSEM for q in self.dsem}
        self.dnext = {q: 0 for q in self.dsem}
        self.ccsem = nc.alloc_semaphore(name="cc")
        self.ccval = 0
        self.known = {e: {} for e in self.E}
        self.lastw = {}
        self.readers = {}
        self.uid = 0
        self.n_instr = 0

    def name(self, base):
        self.uid += 1
        return f"{base}_{self.uid}"

    def dram(self, name, shape, dtype, kind="Internal"):
        key = _re.sub(r"^b\d", "", name) if kind == "Internal" and not name.endswith("RES") else name
        if not hasattr(self, "_dcache"):
            self._dcache = {}
        if key in self._dcache:
            return self._dcache[key]
        ap = self.nc.dram_tensor(key, list(shape), dtype, kind=kind).ap()
        self._dcache[key] = ap
        return ap

    def _deps(self, eng, reads, writes):
        need = []
        for r in reads:
            w = self.lastw.get(r)
            if w is not None:
                need.append(w)
        for r in writes:
            w = self.lastw.get(r)
            if w is not None:
                need.append(w)
            rd = self.readers.get(r)
            if rd:
                need.extend(rd.values())
        out = {}
        kn = self.known[eng]
        for (sem, val, src) in need:
            if src == "pe" and eng == "pe":
                continue
            key = id(sem)
            if kn.get(key, 0) >= val:
                continue
            if key not in out or out[key][1] < val:
                out[key] = (sem, val)
        for key, (sem, val) in out.items():
            kn[key] = val
            self.E[eng].wait_ge(sem, val)
            self.n_instr += 1

    def _record(self, rec, reads, writes):
        for r in reads:
            d = self.readers.setdefault(r, {})
            k = id(rec[0])
            if k not in d or d[k][1] < rec[1]:
                d[k] = rec
        for r in writes:
            self.lastw[r] = rec
            self.readers[r] = {}

    def op(self, eng, fn, reads=(), writes=(), inc=True):
        self._deps(eng, reads, writes)
        ins = fn(self.E[eng])
        self.n_instr += 1
        sem = self.csem[eng]
        if inc:
            self.ccount[eng] += 1
            ins.then_inc(sem, 1)
            rec = (sem, self.ccount[eng], eng)
        else:
            rec = (sem, self.ccount[eng] + 1, eng)
        self._record(rec, reads, writes)
        return ins

    def dma(self, q, out, in_, reads=(), writes=(), **kw):
        i = self.dnext[q]
        self.dnext[q] = (i + 1) % NDSEM
        sem = self.dsem[q][i]
        kn = self.known[q]
        if kn.get(id(sem), 0) < self.dval[q][i]:
            self.E[q].wait_ge(sem, self.dval[q][i])
            kn[id(sem)] = self.dval[q][i]
            self.n_instr += 1
        self._deps(q, reads, writes)
        ins = self.E[q].dma_start(out=out, in_=in_, **kw)
        self.n_instr += 1
        self.dval[q][i] += 16
        ins.then_inc(sem, 16)
        rec = (sem, self.dval[q][i], "dma_" + q)
        self._record(rec, reads, writes)
        return ins

    def idma(self, out, out_off, in_, in_off, reads=(), writes=(), **kw):
        q = "pool"
        i = self.dnext[q]
        self.dnext[q] = (i + 1) % NDSEM
        sem = self.dsem[q][i]
        kn = self.known[q]
        if kn.get(id(sem), 0) < self.dval[q][i]:
            self.E[q].wait_ge(sem, self.dval[q][i])
            kn[id(sem)] = self.dval[q][i]
        self._deps(q, reads, writes)
        ins = self.nc.gpsimd.indirect_dma_start(out=out, out_offset=out_off, in_=in_, in_offset=in_off, **kw)
        self.n_instr += 1
        self.dval[q][i] += 16
        ins.then_inc(sem, 16)
        rec = (sem, self.dval[q][i], "dma_" + q)
        self._record(rec, reads, writes)
        return ins

    def collective(self, kind, op, groups, in_ap, out_ap, reads=(), writes=()):
        q = "pool"
        self._deps(q, reads, writes)
        ins = self.nc.gpsimd.collective_compute(kind, op, replica_groups=groups, ins=[in_ap], outs=[out_ap])
        self.ccval += 1
        ins.then_inc(self.ccsem)
        self.nc.gpsimd.wait_ge(self.ccsem, self.ccval)
        self.known[q][id(self.ccsem)] = self.ccval
        rec = (self.ccsem, self.ccval, "cc")
        self._record(rec, reads, writes)
        return ins

    def barrier(self):
        for e in self.E:
            kn = self.known[e]
            eng = self.E[e]
            for f, sem in self.csem.items():
                if self.ccount[f] > kn.get(id(sem), 0):
                    eng.wait_ge(sem, self.ccount[f])
                    kn[id(sem)] = self.ccount[f]
            for q in self.dsem:
                for i, sem in enumerate(self.dsem[q]):
                    if self.dval[q][i] > kn.get(id(sem), 0):
                        eng.wait_ge(sem, self.dval[q][i])
                        kn[id(sem)] = self.dval[q][i]
            if self.ccval > kn.get(id(self.ccsem), 0):
                eng.wait_ge(self.ccsem, self.ccval)
                kn[id(self.ccsem)] = self.ccval

    def finish(self):
        sp = self.E["sp"]
        kn = self.known["sp"]
        for q in self.dsem:
            for i, sem in enumerate(self.dsem[q]):
                if self.dval[q][i] > kn.get(id(sem), 0):
                    sp.wait_ge(sem, self.dval[q][i])
        for e, sem in self.csem.items():
            if self.ccount[e] > kn.get(id(sem), 0):
                sp.wait_ge(sem, self.ccount[e])
        if self.ccval:
            sp.wait_ge(self.ccsem, self.ccval)


def bcast_rows(ap_row, nparts=128):
    return ap_row.partition_broadcast(nparts)


def _mk(c):
    return c


def mm(c, out, lhsT, rhs, start, stop, reads, writes, inc=None):
    inc = stop if inc is None else inc
    return c.op("pe", lambda e: e.matmul(out=out, lhsT=lhsT, rhs=rhs, start=start, stop=stop), reads, writes, inc=inc)


def tr(c, out, in_, ident, reads, writes, inc=True):
    return c.op("pe", lambda e: e.transpose(out=out, in_=in_, identity=ident), reads, writes, inc=inc)


def act(c, out, in_, func, reads, writes, **kw):
    return c.op("act", lambda e: e.activation(out=out, in_=in_, func=func, **kw), reads, writes)


def cp(c, eng, out, in_, reads, writes):
    if eng == "act":
        return c.op("act", lambda e: e.copy(out=out, in_=in_), reads, writes)
    return c.op(eng, lambda e: e.tensor_copy(out=out, in_=in_), reads, writes)


def tt(c, eng, out, in0, in1, op, reads, writes):
    return c.op(eng, lambda e: e.tensor_tensor(out=out, in0=in0, in1=in1, op=op), reads, writes)


def ts(c, eng, out, in0, s1, s2, op0, op1, reads, writes, accum_out=None):
    if op1 is None:
        return c.op(eng, lambda e: e.tensor_scalar(out=out, in0=in0, scalar1=s1, scalar2=None, op0=op0), reads, writes)
    if accum_out is not None:
        return c.op(eng, lambda e: e.tensor_scalar(out=out, in0=in0, scalar1=s1, scalar2=s2, op0=op0, op1=op1, accum_out=accum_out), reads, writes)
    return c.op(eng, lambda e: e.tensor_scalar(out=out, in0=in0, scalar1=s1, scalar2=s2, op0=op0, op1=op1), reads, writes)


def stt(c, eng, out, in0, scalar, in1, op0, op1, reads, writes):
    return c.op(eng, lambda e: e.scalar_tensor_tensor(out=out, in0=in0, scalar=scalar, in1=in1, op0=op0, op1=op1), reads, writes)


def red(c, eng, out, in_, op, reads, writes, axis=None):
    axis = AX.X if axis is None else axis
    return c.op(eng, lambda e: e.tensor_reduce(out=out, in_=in_, axis=axis, op=op), reads, writes)


def mset(c, eng, ap, val, writes):
    return c.op(eng, lambda e: e.memset(ap, val), (), writes)


def rmsnorm_rstd(c, x_ap, junk_ap, ss_ap, D, reads, tag, eps=1e-6):
    act(c, junk_ap, x_ap, AF.Square, reads, [tag + "_junk", tag + "_ss0"], scale=float(D) ** -0.5, accum_out=ss_ap[:, 0:1])
    act(c, ss_ap[:, 1:2], ss_ap[:, 0:1], AF.Sqrt, [tag + "_ss0"], [tag + "_ss1"], bias=eps)
    c.op("dve", lambda e: e.reciprocal(out=ss_ap[:, 1:2], in_=ss_ap[:, 1:2]), [tag + "_ss1"], [tag + "_ss1"])
    return ss_ap[:, 1:2]


G4, J8, HP, NS = 4, 8, 64, 128
SW = 2048
XW = 3072
GW = 2048
HIN = 9280


def proj_rows(c, T, tag, D, src, tiles, mods, Wd, ncols, outs, K, ST=1024, src_res="RES"):
    nc = c.nc
    KC = D // 128
    with contextlib.ExitStack() as es:
        sb = lambda n, s, d: es.enter_context(nc.sbuf_tensor(T(tag + n), s, d))
        ps = lambda n, s, d: es.enter_context(nc.psum_tensor(T(tag + n), s, d))
        idt = sb("idt", [128, 128], F32)
        At = sb("At", [128, D], F32)
        Bt = sb("Bt", [128, D], F32)
        xt = [sb(f"xt{i}", [128, D], F32) for i in range(2)]
        junk = sb("junk", [128, D], F32)
        ss = sb("ss", [128, 2], F32)
        h = sb("h", [128, D], F32)
        hT = sb("hT", [128, KC, ST], BF16)
        wt = [sb(f"wt{i}", [128, KC, 512], BF16) for i in range(2)]
        ot = [sb(f"ot{i}", [128, 512], F32) for i in range(3)]
        pT = [ps(f"pT{i}", [128, 512], F32) for i in range(2)]
        pm = [ps(f"pm{i}", [128, 512], F32) for i in range(3)]
        R = lambda s: T(tag + s)
        c.dma("sp", idt[:], K["ident"][:, :], writes=[R("idt")])
        sts = []
        cur = []
        curn = 0
        for tl in tiles:
            if curn + tl[1] > ST:
                sts.append(cur)
                cur, curn = [], 0
            cur.append(tl)
            curn += tl[1]
        if cur:
            sts.append(cur)
        cur_ms = None
        ti = 0
        oi = 0
        for st_tiles in sts:
            off = 0
            offs = []
            for (r0, n, ms) in st_tiles:
                b = ti % 2
                ti += 1
                if ms != cur_ms:
                    c.dma("sp", At[:], mods[ms]["A"].partition_broadcast(128), reads=["MODS"], writes=[R("At")])
                    c.dma("sp", Bt[:], mods[ms]["B"].partition_broadcast(128), reads=["MODS"], writes=[R("Bt")])
                    cur_ms = ms
                c.dma("sp", xt[b][:n], src[r0:r0 + n, :], reads=[src_res], writes=[R(f"xt{b}")])
                rstd = rmsnorm_rstd(c, xt[b][:n], junk[:n], ss[:n], D, [R(f"xt{b}")], R("n"))
                stt(c, "dve", h[:n], xt[b][:n], rstd[:n], At[:n], ALU.mult, ALU.mult, [R(f"xt{b}"), R("n_ss1"), R("At")], [R("h")])
                tt(c, "pool", h[:n], h[:n], Bt[:n], ALU.add, [R("h"), R("Bt")], [R("h")])
                gsz = 4 if KC % 4 == 0 else 2
                for kg in range(KC // gsz):
                    pb = kg % 2
                    for jj in range(gsz):
                        k = kg * gsz + jj
                        tr(c, pT[pb][:, jj * 128:jj * 128 + n], h[:n, k * 128:(k + 1) * 128], idt[:n, :n],
                           [R("h"), R("idt")], [R(f"pT{pb}")], inc=(jj == gsz - 1))
                    cp(c, "act" if kg % 2 == 0 else "dve", hT[:, kg * gsz:(kg + 1) * gsz, off:off + n],
                       pT[pb][:, 0:gsz * 128].rearrange("p (a b) -> p a b", a=gsz)[:, :, :n], [R(f"pT{pb}")], [R("hT")])
                offs.append(off)
                off += n
            nblk = (ncols + 511) // 512
            for nb in range(nblk):
                c0 = nb * 512
                w = min(512, ncols - c0)
                wb = nb % 2
                c.dma("pool", wt[wb][:, :, :w], Wd[:, c0:c0 + w].rearrange("(k p) n -> p k n", p=128), writes=[R(f"wt{wb}")])
                for (r0, n, ms), o in zip(st_tiles, offs):
                    pb = oi % 3
                    oi += 1
                    for k in range(KC):
                        mm(c, pm[pb][:n, :w], hT[:, k, o:o + n], wt[wb][:, k, :w], k == 0, k == KC - 1, [R("hT"), R(f"wt{wb}")], [R(f"pm{pb}")])
                    cp(c, "act" if pb != 1 else "dve", ot[pb][:n, :w], pm[pb][:n, :w], [R(f"pm{pb}")], [R(f"ot{pb}")])
                    for (d0, d1, dst, rofs, res) in outs:
                        lo = max(c0, d0)
                        hi = min(c0 + w, d1)
                        if lo < hi:
                            rr = rofs(r0)
                            c.dma("sp", dst[rr:rr + n, lo - d0:hi - d0], ot[pb][:n, lo - c0:hi - c0], reads=[R(f"ot{pb}")], writes=[res])
    c.barrier()


def hybrid_phase(c, tag, D, NCTX, NLAT, RES, mods, W, K, dbg=None):
    nc = c.nc
    T = lambda s: f"{tag}_{s}"
    NTOK = NCTX + NLAT
    NCH = NTOK // 128
    PZ = c.dram(T("PZ"), [NTOK, SW], F32)
    PX = c.dram(T("PX"), [NTOK + 4, XW], F32)
    PD = c.dram(T("PD"), [NTOK, 64], F32)
    PU = c.dram(T("PU"), [NTOK, GW], F32)
    PV = c.dram(T("PV"), [NTOK, GW], F32)
    XS = c.dram(T("XS"), [NTOK, SW], F32)
    BM = c.dram(T("BM"), [NTOK, 512], BF16)
    CTd = c.dram(T("CTd"), [NCH * 128, 512], BF16)
    CBF = c.dram(T("CBF"), [NCH * 128, 512], F32)
    CBB = c.dram(T("CBB"), [NCH * 128, 512], F32)
    DT = c.dram(T("DT"), [NTOK, 64], F32)
    Y = c.dram(T("Y"), [NTOK, SW], F32)
    YG = c.dram(T("YG"), [NTOK, GW], F32)
    MIX = c.dram(T("MIX"), [NTOK, 2 * SW], BF16)
    YM = c.dram(T("YM"), [NTOK, D], F32)

    def pxrow(r):
        return r + 1 if r < NCTX else r + 3

    tiles = [(r0, 128, "c" if r0 < NCTX else "l") for r0 in range(0, NTOK, 128)]
    ident_rows = lambda r: r
    outs = [(0, SW, PZ, ident_rows, T("PZ")), (SW, SW + XW, PX, pxrow, T("PX")), (SW + XW, SW + XW + 64, PD, ident_rows, T("PD")),
            (SW + XW + 64, SW + XW + 64 + GW, PU, ident_rows, T("PU")), (SW + XW + 64 + GW, HIN, PV, ident_rows, T("PV"))]
    m1 = {ms: {"A": mods[ms]["A1"], "B": mods[ms]["B1"]} for ms in mods}
    proj_rows(c, T, "pj_", D, RES, tiles, m1, W["w_in"], HIN, outs, K)

    with contextlib.ExitStack() as es:
        sb = lambda n, s, d: es.enter_context(nc.sbuf_tensor(T(n), s, d))
        ps = lambda n, s, d: es.enter_context(nc.psum_tensor(T(n), s, d))
        idt = sb("c_idt", [128, 128], F32)
        trif = sb("c_trif", [128, 128], F32)
        trib = sb("c_trib", [128, 128], F32)
        cw = sb("c_cw", [128, 3, XW], F32)
        cb = sb("c_cb", [128, XW], F32)
        dtb = sb("c_dtb", [128, 64], F32)
        zr = sb("c_zr", [4, XW], F32)
        xp = [sb(f"c_xp{i}", [128, XW], F32) for i in range(2)]
        xc = [sb(f"c_xc{i}", [128, XW], F32) for i in range(2)]
        xn = [sb(f"c_xn{i}", [128, XW], F32) for i in range(2)]
        acc = sb("c_acc", [128, XW], F32)
        sg = sb("c_sg", [128, XW], F32)
        bmb = sb("c_bmb", [128, 512], BF16)
        btb = sb("c_btb", [128, 512], BF16)
        ctb = sb("c_ctb", [128, 512], BF16)
        cbf = sb("c_cbf", [128, 512], F32)
        cbb = sb("c_cbb", [128, 512], F32)
        dtt = sb("c_dtt", [128, 64], F32)
        pB = ps("c_pB", [128, 512], F32)
        pC = ps("c_pC", [128, 512], F32)
        pCB = ps("c_pCB", [128, 512], F32)
        c.dma("sp", idt[:], K["ident"][:, :], writes=[T("c_idt")])
        c.dma("sp", trif[:], K["trif"][:, :], writes=[T("c_trif")])
        c.dma("sp", trib[:], K["trib"][:, :], writes=[T("c_trib")])
        for i in range(3):
            c.dma("sp", cw[:, i, :], W["conv_w"][i:i + 1, :].partition_broadcast(128), writes=[T("c_cw")])
        c.dma("sp", cb[:], W["conv_b"].partition_broadcast(128), writes=[T("c_cb")])
        c.dma("sp", dtb[:], W["dtb"].partition_broadcast(128), writes=[T("c_dtb")])
        mset(c, "pool", zr[:], 0.0, [T("c_zr")])
        for zrow in (0, NCTX + 1, NCTX + 2, NTOK + 3):
            c.dma("sp", PX[zrow:zrow + 1, :], zr[0:1, :], reads=[T("c_zr")], writes=[T("PX")])
        for ch in range(NCH):
            b = ch % 2
            r0 = ch * 128
            p0 = pxrow(r0)
            c.dma("sp", xp[b][:], PX[p0 - 1:p0 + 127, :], reads=[T("PX")], writes=[T(f"c_xp{b}")])
            c.dma("sp", xc[b][:], PX[p0:p0 + 128, :], reads=[T("PX")], writes=[T(f"c_xc{b}")])
            c.dma("sp", xn[b][:], PX[p0 + 1:p0 + 129, :], reads=[T("PX")], writes=[T(f"c_xn{b}")])
            tt(c, "dve", acc[:], xc[b][:], cw[:, 1, :], ALU.mult, [T(f"c_xc{b}"), T("c_cw")], [T("c_acc")])
            tt(c, "pool", xp[b][:], xp[b][:], cw[:, 0, :], ALU.mult, [T(f"c_xp{b}"), T("c_cw")], [T(f"c_xp{b}")])
            tt(c, "pool", xn[b][:], xn[b][:], cw[:, 2, :], ALU.mult, [T(f"c_xn{b}"), T("c_cw")], [T(f"c_xn{b}")])
            tt(c, "dve", acc[:], acc[:], cb[:], ALU.add, [T("c_acc"), T("c_cb")], [T("c_acc")])
            tt(c, "dve", acc[:], acc[:], xp[b][:], ALU.add, [T("c_acc"), T(f"c_xp{b}")], [T("c_acc")])
            tt(c, "dve", acc[:], acc[:], xn[b][:], ALU.add, [T("c_acc"), T(f"c_xn{b}")], [T("c_acc")])
            act(c, sg[:], acc[:], AF.Silu, [T("c_acc")], [T("c_sg")])
            c.dma("sp", XS[r0:r0 + 128, :], sg[:, 0:SW], reads=[T("c_sg")], writes=[T("XS")])
            cp(c, "pool", bmb[:], sg[:, SW:SW + 512], [T("c_sg")], [T("c_bmb")])
            c.dma("sp", BM[r0:r0 + 128, :], bmb[:], reads=[T("c_bmb")], writes=[T("BM")])
            for g in range(G4):
                tr(c, pB[:, g * 128:(g + 1) * 128], sg[:, SW + g * 128:SW + (g + 1) * 128], idt[:], [T("c_sg"), T("c_idt")], [T("c_pB")], inc=(g == 3))
            for g in range(G4):
                tr(c, pC[:, g * 128:(g + 1) * 128], sg[:, SW + 512 + g * 128:SW + 512 + (g + 1) * 128], idt[:], [T("c_sg"), T("c_idt")], [T("c_pC")], inc=(g == 3))
            cp(c, "act", btb[:], pB[:], [T("c_pB")], [T("c_btb")])
            cp(c, "act", ctb[:], pC[:], [T("c_pC")], [T("c_ctb")])
            c.dma("sp", CTd[r0:r0 + 128, :], ctb[:], reads=[T("c_ctb")], writes=[T("CTd")])
            for g in range(G4):
                mm(c, pCB[:, g * 128:(g + 1) * 128], btb[:, g * 128:(g + 1) * 128], ctb[:, g * 128:(g + 1) * 128], True, True,
                   [T("c_btb"), T("c_ctb")], [T("c_pCB")], inc=(g == 3))
            tt(c, "dve", cbf[:].rearrange("p (g q) -> p g q", g=4), pCB[:].rearrange("p (g q) -> p g q", g=4),
               trif[:].unsqueeze(1).to_broadcast([128, 4, 128]), ALU.mult, [T("c_pCB"), T("c_trif")], [T("c_cbf")])
            tt(c, "dve", cbb[:].rearrange("p (g q) -> p g q", g=4), pCB[:].rearrange("p (g q) -> p g q", g=4),
               trib[:].unsqueeze(1).to_broadcast([128, 4, 128]), ALU.mult, [T("c_pCB"), T("c_trib")], [T("c_cbb")])
            c.dma("sp", CBF[r0:r0 + 128, :], cbf[:], reads=[T("c_cbf")], writes=[T("CBF")])
            c.dma("sp", CBB[r0:r0 + 128, :], cbb[:], reads=[T("c_cbb")], writes=[T("CBB")])
            c.dma("sp", dtt[:], PD[r0:r0 + 128, :], reads=[T("PD")], writes=[T("c_dtt")])
            tt(c, "dve", dtt[:], dtt[:], dtb[:], ALU.add, [T("c_dtt"), T("c_dtb")], [T("c_dtt")])
            act(c, dtt[:], dtt[:], AF.Exp, [T("c_dtt")], [T("c_dtt")])
            act(c, dtt[:], dtt[:], AF.Ln, [T("c_dtt")], [T("c_dtt")], bias=1.0)
            c.dma("sp", DT[r0:r0 + 128, :], dtt[:], reads=[T("c_dtt")], writes=[T("DT")])
    c.barrier()

    ctx_ch = list(range(NCTX // 128))
    lat_ch = list(range(NCTX // 128, NCH))
    with contextlib.ExitStack() as es:
        sb = lambda n, s, d: es.enter_context(nc.sbuf_tensor(T(n), s, d))
        ps = lambda n, s, d: es.enter_context(nc.psum_tensor(T(n), s, d))
        ones = sb("s_ones", [128, 128], F32)
        tri = [sb("s_trif", [128, 128], F32), sb("s_trib", [128, 128], F32)]
        aneg = sb("s_aneg", [128, 64], F32)
        dsk = sb("s_dsk", [128, 32], F32)
        xs = [sb(f"s_xs{i}", [128, SW], F32) for i in range(2)]
        bm = [sb(f"s_bm{i}", [128, 512], BF16) for i in range(2)]
        ct = [sb(f"s_ct{i}", [128, 512], BF16) for i in range(2)]
        cbm = [sb(f"s_cbm{i}", [128, 512], F32) for i in range(2)]
        dtt = [sb(f"s_dt{i}", [128, 64], F32) for i in range(2)]
        a = sb("s_a", [128, 32], F32)
        acs = sb("s_acs", [128, 32], F32)
        tot = sb("s_tot", [128, 32], F32)
        dend = sb("s_dend", [128, 32], F32)
        eacs = sb("s_eacs", [128, 32], F32)
        cdec = sb("s_cdec", [128, 32], F32)
        xdt = sb("s_xdt", [128, SW], BF16)
        xdtd = sb("s_xdtd", [128, SW], BF16)
        X4 = [sb(f"s_X4{i}", [128, 512], F32) for i in range(2)]
        seg = [sb(f"s_seg{i}", [128, 512], F32) for i in range(2)]
        Lx = [sb(f"s_Lx{i}", [128, 512], F32) for i in range(2)]
        MT = [sb(f"s_MT{i}", [128, 512], BF16) for i in range(2)]
        Hs = sb("s_H", [128, G4 * 512], F32)
        Hb = sb("s_Hb", [128, G4 * 512], BF16)
        yo = sb("s_yo", [128, SW], F32)
        yacc = [sb(f"s_yacc{i}", [128, SW], F32) for i in range(2)]
        pa = ps("s_pa", [128, 64], F32)
        pR = [ps(f"s_pR{i}", [128, 512], F32) for i in range(2)]
        pY = ps("s_pY", [128, SW], F32)
        pO = ps("s_pO", [128, 512], F32)
        c.dma("sp", ones[:], K["ones"][:, :], writes=[T("s_ones")])
        c.dma("sp", tri[0][:], K["trif"][:, :], writes=[T("s_trif")])
        c.dma("sp", tri[1][:], K["trib"][:, :], writes=[T("s_trib")])
        c.dma("sp", aneg[:], W["alog"].partition_broadcast(128), writes=[T("s_aneg")])
        act(c, aneg[:], aneg[:], AF.Exp, [T("s_aneg")], [T("s_aneg")])
        ts(c, "dve", aneg[:], aneg[:], -1.0, None, ALU.mult, None, [T("s_aneg")], [T("s_aneg")])
        c.dma("sp", dsk[:], W["dsk"].partition_broadcast(128), writes=[T("s_dsk")])
        it = 0
        for d in range(2):
            CBd = CBF if d == 0 else CBB
            order = (ctx_ch + lat_ch) if d == 0 else (ctx_ch[::-1] + lat_ch[::-1])
            mset(c, "pool", Hs[:], 0.0, [T("s_H")])
            mset(c, "pool", Hb[:], 0.0, [T("s_Hb")])
            for ch in order:
                b = it % 2
                it += 1
                r0 = ch * 128
                c.dma("sp", xs[b][:], XS[r0:r0 + 128, :], reads=[T("XS")], writes=[T(f"s_xs{b}")])
                c.dma("sp", bm[b][:], BM[r0:r0 + 128, :], reads=[T("BM")], writes=[T(f"s_bm{b}")])
                c.dma("sp", ct[b][:], CTd[r0:r0 + 128, :], reads=[T("CTd")], writes=[T(f"s_ct{b}")])
                c.dma("sp", cbm[b][:], CBd[r0:r0 + 128, :], reads=[T("CBF"), T("CBB")], writes=[T(f"s_cbm{b}")])
                c.dma("sp", dtt[b][:], DT[r0:r0 + 128, :], reads=[T("DT")], writes=[T(f"s_dt{b}")])
                dtd = dtt[b][:, d * 32:(d + 1) * 32]
                tt(c, "dve", a[:], dtd, aneg[:, d * 32:(d + 1) * 32], ALU.mult, [T(f"s_dt{b}"), T("s_aneg")], [T("s_a")])
                mm(c, pa[:, 0:32], tri[d][:], a[:], True, True, [T(f"s_tri{'fb'[d]}"), T("s_a")], [T("s_pa")])
                mm(c, pa[:, 32:64], ones[:], a[:], True, True, [T("s_ones"), T("s_a")], [T("s_pa")])
                cp(c, "dve", acs[:], pa[:, 0:32], [T("s_pa")], [T("s_acs")])
                cp(c, "dve", tot[:], pa[:, 32:64], [T("s_pa")], [T("s_tot")])
                tt(c, "dve", dend[:], tot[:], acs[:], ALU.subtract, [T("s_tot"), T("s_acs")], [T("s_dend")])
                act(c, dend[:], dend[:], AF.Exp, [T("s_dend")], [T("s_dend")])
                act(c, eacs[:], acs[:], AF.Exp, [T("s_acs")], [T("s_eacs")])
                act(c, cdec[:], tot[:], AF.Exp, [T("s_tot")], [T("s_cdec")])
                tt(c, "pool", xdt[:].rearrange("p (h e) -> p h e", e=HP), xs[b][:].rearrange("p (h e) -> p h e", e=HP),
                   dtd.unsqueeze(2).to_broadcast([128, 32, HP]), ALU.mult, [T(f"s_xs{b}"), T(f"s_dt{b}")], [T("s_xdt")])
                tt(c, "pool", xdtd[:].rearrange("p (h e) -> p h e", e=HP), xdt[:].rearrange("p (h e) -> p h e", e=HP),
                   dend[:].unsqueeze(2).to_broadcast([128, 32, HP]), ALU.mult, [T("s_xdt"), T("s_dend")], [T("s_xdtd")])
                for hq in range(8):
                    g = hq // 2
                    q2 = hq % 2
                    tt(c, "dve", X4[q2][:].rearrange("p (h q) -> p h q", h=4), tri[d][:].unsqueeze(1).to_broadcast([128, 4, 128]),
                       a[:, hq * 4:(hq + 1) * 4].unsqueeze(2).to_broadcast([128, 4, 128]), ALU.mult,
                       [T(f"s_tri{'fb'[d]}"), T("s_a")], [T(f"s_X4{q2}")])
                    mm(c, pR[q2][:], ones[:], X4[q2][:], True, True, [T("s_ones"), T(f"s_X4{q2}")], [T(f"s_pR{q2}")])
                    for j4 in range(4):
                        hh = hq * 4 + j4
                        ts(c, "dve", seg[q2][:, j4 * 128:(j4 + 1) * 128], pR[q2][:, j4 * 128:(j4 + 1) * 128], acs[:, hh:hh + 1], 0.0,
                           ALU.subtract, ALU.min, [T(f"s_pR{q2}"), T("s_acs")], [T(f"s_seg{q2}")])
                    act(c, Lx[q2][:], seg[q2][:], AF.Exp, [T(f"s_seg{q2}")], [T(f"s_Lx{q2}")])
                    tt(c, "pool", MT[q2][:].rearrange("p (h q) -> p h q", h=4), Lx[q2][:].rearrange("p (h q) -> p h q", h=4),
                       cbm[b][:, g * 128:(g + 1) * 128].unsqueeze(1).to_broadcast([128, 4, 128]), ALU.mult,
                       [T(f"s_Lx{q2}"), T(f"s_cbm{b}")], [T(f"s_MT{q2}")])
                    for j4 in range(4):
                        hh = hq * 4 + j4
                        mm(c, pY[:, hh * HP:(hh + 1) * HP], MT[q2][:, j4 * 128:(j4 + 1) * 128], xdt[:, hh * HP:(hh + 1) * HP], True, True,
                           [T(f"s_MT{q2}"), T("s_xdt")], [T("s_pY")], inc=(j4 == 3))
                if d == 0:
                    tt(c, "pool", yacc[b][:].rearrange("p (h e) -> p h e", e=HP), xs[b][:].rearrange("p (h e) -> p h e", e=HP),
                       dsk[:].unsqueeze(2).to_broadcast([128, 32, HP]), ALU.mult, [T(f"s_xs{b}"), T("s_dsk")], [T(f"s_yacc{b}")])
                else:
                    c.dma("sp", yacc[b][:], Y[r0:r0 + 128, :], reads=[T("Y")], writes=[T(f"s_yacc{b}")])
                for g in range(G4):
                    mm(c, pO[:], ct[b][:, g * 128:(g + 1) * 128], Hb[:, g * 512:(g + 1) * 512], True, True, [T(f"s_ct{b}"), T("s_Hb")], [T("s_pO")])
                    tt(c, "dve", yo[:, g * 512:(g + 1) * 512].rearrange("p (h e) -> p h e", e=HP), pO[:].rearrange("p (h e) -> p h e", e=HP),
                       eacs[:, g * 8:(g + 1) * 8].unsqueeze(2).to_broadcast([128, 8, HP]), ALU.mult, [T("s_pO"), T("s_eacs")], [T("s_yo")])
                tt(c, "dve", yo[:], yo[:], pY[:], ALU.add, [T("s_yo"), T("s_pY")], [T("s_yo")])
                tt(c, "pool", yacc[b][:], yacc[b][:], yo[:], ALU.add, [T(f"s_yacc{b}"), T("s_yo")], [T(f"s_yacc{b}")])
                c.dma("sp", Y[r0:r0 + 128, :], yacc[b][:], reads=[T(f"s_yacc{b}")], writes=[T("Y")])
                for g in range(G4):
                    mm(c, pO[:], bm[b][:, g * 128:(g + 1) * 128], xdtd[:, g * 512:(g + 1) * 512], True, True, [T(f"s_bm{b}"), T("s_xdtd")], [T("s_pO")])
                    tt(c, "dve", Hs[:, g * 512:(g + 1) * 512].rearrange("p (h e) -> p h e", e=HP),
                       Hs[:, g * 512:(g + 1) * 512].rearrange("p (h e) -> p h e", e=HP),
                       cdec[:, g * 8:(g + 1) * 8].unsqueeze(2).to_broadcast([128, 8, HP]), ALU.mult, [T("s_H"), T("s_cdec")], [T("s_H")])
                    tt(c, "dve", Hs[:, g * 512:(g + 1) * 512], Hs[:, g * 512:(g + 1) * 512], pO[:], ALU.add, [T("s_H"), T("s_pO")], [T("s_H")])
                cp(c, "act", Hb[:], Hs[:], [T("s_H")], [T("s_Hb")])
    c.barrier()

    with contextlib.ExitStack() as es:
        sb = lambda n, s, d: es.enter_context(nc.sbuf_tensor(T(n), s, d))
        ps = lambda n, s, d: es.enter_context(nc.psum_tensor(T(n), s, d))
        wst = sb("g_wst", [128, 16, 128], BF16)
        bst = sb("g_bst", [128, 16], F32)
        vng = sb("g_vng", [128, GW], F32)
        sng = sb("g_sng", [128, SW], F32)
        u = [sb(f"g_u{i}", [128, GW], F32) for i in range(2)]
        v = [sb(f"g_v{i}", [128, GW], F32) for i in range(2)]
        z = [sb(f"g_z{i}", [128, SW], F32) for i in range(2)]
        y = [sb(f"g_y{i}", [128, SW], F32) for i in range(2)]
        junk = sb("g_junk", [128, GW], F32)
        ss = sb("g_ss", [128, 2], F32)
        ss2 = sb("g_ss2", [128, 2], F32)
        vn = sb("g_vn", [128, GW], BF16)
        mix = [sb(f"g_mix{i}", [128, 2 * SW], BF16) for i in range(2)]
        sgm = sb("g_sgm", [128, GW], F32)
        pS = ps("g_pS", [128, GW], F32)
        c.dma("pool", wst[:], W["wsT"].rearrange("g k q -> k g q"), writes=[T("g_wst")])
        c.dma("sp", bst[:], W["bsT"][:, :], writes=[T("g_bst")])
        c.dma("sp", vng[:], W["vng"].partition_broadcast(128), writes=[T("g_vng")])
        c.dma("sp", sng[:], W["ssdg"].partition_broadcast(128), writes=[T("g_sng")])
        for ch in range(NCH):
            b = ch % 2
            r0 = ch * 128
            c.dma("sp", u[b][:], PU[r0:r0 + 128, :], reads=[T("PU")], writes=[T(f"g_u{b}")])
            c.dma("sp", v[b][:], PV[r0:r0 + 128, :], reads=[T("PV")], writes=[T(f"g_v{b}")])
            c.dma("sp", z[b][:], PZ[r0:r0 + 128, :], reads=[T("PZ")], writes=[T(f"g_z{b}")])
            c.dma("sp", y[b][:], Y[r0:r0 + 128, :], reads=[T("Y")], writes=[T(f"g_y{b}")])
            act(c, u[b][:], u[b][:], AF.Gelu_apprx_tanh, [T(f"g_u{b}")], [T(f"g_u{b}")])
            act(c, v[b][:], v[b][:], AF.Gelu_apprx_tanh, [T(f"g_v{b}")], [T(f"g_v{b}")])
            rstd = rmsnorm_rstd(c, v[b][:], junk[:], ss[:], GW, [T(f"g_v{b}")], T("g_n1"))
            stt(c, "dve", vn[:], v[b][:], rstd, vng[:], ALU.mult, ALU.mult, [T(f"g_v{b}"), T("g_n1_ss1"), T("g_vng")], [T("g_vn")])
            for gg in range(16):
                mm(c, pS[:, gg * 128:(gg + 1) * 128], wst[:, gg, :], vn[:, gg * 128:(gg + 1) * 128], True, True, [T("g_wst"), T("g_vn")], [T("g_pS")], inc=(gg == 15))
            tt(c, "dve", sgm[:].rearrange("p (g e) -> p g e", g=16), pS[:].rearrange("p (g e) -> p g e", g=16),
               bst[:].unsqueeze(2).to_broadcast([128, 16, 128]), ALU.add, [T("g_pS"), T("g_bst")], [T("g_sgm")])
            tt(c, "pool", mix[b][:, SW:2 * SW], sgm[:], u[b][:], ALU.mult, [T("g_sgm"), T(f"g_u{b}")], [T(f"g_mix{b}")])
            act(c, z[b][:], z[b][:], AF.Silu, [T(f"g_z{b}")], [T(f"g_z{b}")])
            tt(c, "dve", y[b][:], y[b][:], z[b][:], ALU.mult, [T(f"g_y{b}"), T(f"g_z{b}")], [T(f"g_y{b}")])
            rstd2 = rmsnorm_rstd(c, y[b][:], junk[:], ss2[:], SW, [T(f"g_y{b}")], T("g_n2"))
            stt(c, "dve", mix[b][:, 0:SW], y[b][:], rstd2, sng[:], ALU.mult, ALU.mult, [T(f"g_y{b}"), T("g_n2_ss1"), T("g_sng")], [T(f"g_mix{b}")])
            c.dma("sp", MIX[r0:r0 + 128, :], mix[b][:], reads=[T(f"g_mix{b}")], writes=[T("MIX")])
    c.barrier()
    if dbg is not None:
        dbg(dict(Y=Y, MIX=MIX, PZ=PZ, XS=XS, DT=DT))
    out_proj_res(c, T, "op_", D, 2 * SW, MIX, W["w_out"], RES, tiles, {ms: mods[ms]["G1"] for ms in mods}, K)


def out_proj_res(c, T, tag, D, KIN, SRC, Wd, RES, tiles, gates, K, ST=1024):
    nc = c.nc
    KC = KIN // 128
    R = lambda s: T(tag + s)
    with contextlib.ExitStack() as es:
        sb = lambda n, s, d: es.enter_context(nc.sbuf_tensor(R(n), s, d))
        ps = lambda n, s, d: es.enter_context(nc.psum_tensor(R(n), s, d))
        idb = sb("idb", [128, 128], BF16)
        idf = sb("idf", [128, 128], F32)
        Gm = sb("Gm", [128, D], F32)
        xin = [sb(f"xin{i}", [128, KIN], BF16) for i in range(2)]
        xT = sb("xT", [128, KC, ST], BF16)
        wt = [sb(f"wt{i}", [128, KC, 512], BF16) for i in range(2)]
        rr = [sb(f"rr{i}", [128, 512], F32) for i in range(2)]
        ot = [sb(f"ot{i}", [128, 512], F32) for i in range(2)]
        ptr = [ps(f"ptr{i}", [128, 1024], BF16) for i in range(2)]
        pm = [ps(f"pm{i}", [128, 512], F32) for i in range(2)]
        c.dma("sp", idf[:], K["ident"][:, :], writes=[R("idf")])
        cp(c, "dve", idb[:], idf[:], [R("idf")], [R("idb")])
        sts = []
        cur, curn = [], 0
        for tl in tiles:
            if curn + tl[1] > ST:
                sts.append(cur)
                cur, curn = [], 0
            cur.append(tl)
            curn += tl[1]
        if cur:
            sts.append(cur)
        ti = 0
        oi = 0
        for st_tiles in sts:
            off = 0
            offs = []
            for (r0, n, ms) in st_tiles:
                b = ti % 2
                ti += 1
                c.dma("sp", xin[b][:n], SRC[r0:r0 + n, :], reads=[R("SRC")], writes=[R(f"xin{b}")])
                for kg in range(KC // 8):
                    pb = kg % 2
                    for jj in range(8):
                        k = kg * 8 + jj
                        tr(c, ptr[pb][:, jj * 128:jj * 128 + n], xin[b][:n, k * 128:(k + 1) * 128], idb[:n, :n], [R(f"xin{b}"), R("idb")], [R(f"ptr{pb}")], inc=(jj == 7))
                    cp(c, "act" if pb == 0 else "dve", xT[:, kg * 8:(kg + 1) * 8, off:off + n],
                       ptr[pb][:].rearrange("p (a b) -> p a b", a=8)[:, :, :n], [R(f"ptr{pb}")], [R("xT")])
                offs.append(off)
                off += n
            for nb in range(D // 512):
                wb = nb % 2
                c.dma("pool", wt[wb][:], Wd[:, nb * 512:(nb + 1) * 512].rearrange("(k p) n -> p k n", p=128), writes=[R(f"wt{wb}")])
                cur_ms = None
                for (r0, n, ms), o in zip(st_tiles, offs):
                    pb = oi % 2
                    oi += 1
                    if ms != cur_ms:
                        c.dma("sp", Gm[:], gates[ms].partition_broadcast(128), reads=["MODS"], writes=[R("Gm")])
                        cur_ms = ms
                    for k in range(KC):
                        mm(c, pm[pb][:n, :], xT[:, k, o:o + n], wt[wb][:, k, :], k == 0, k == KC - 1, [R("xT"), R(f"wt{wb}")], [R(f"pm{pb}")])
                    c.dma("sp", rr[pb][:n], RES[r0:r0 + n, nb * 512:(nb + 1) * 512], reads=["RES"], writes=[R(f"rr{pb}")])
                    tt(c, "dve", ot[pb][:n], pm[pb][:n, :], Gm[:n, nb * 512:(nb + 1) * 512], ALU.mult, [R(f"pm{pb}"), R("Gm")], [R(f"ot{pb}")])
                    tt(c, "pool", ot[pb][:n], ot[pb][:n], rr[pb][:n], ALU.add, [R(f"ot{pb}"), R(f"rr{pb}")], [R(f"ot{pb}")])
                    c.dma("sp", RES[r0:r0 + n, nb * 512:(nb + 1) * 512], ot[pb][:n], reads=[R(f"ot{pb}")], writes=["RES"])
    c.barrier()


NH = 16
QL, KVL, DN, DR, DV = 768, 512, 128, 64, 128
SCALE = (DN + DR) ** -0.5


def mla_phase(c, tag, D, NCTX, NLAT, RES, mods, W, K, dbg=None, upto=None):
    nc = c.nc
    T = lambda s: f"{tag}_{s}"
    NTOK = NCTX + NLAT
    NKT = NTOK // 128
    NQT = NLAT // 128
    PQ = c.dram(T("PQ"), [NTOK, QL], F32)
    PKV = c.dram(T("PKV"), [NTOK, KVL + DR], F32)
    QD = c.dram(T("QD"), [NTOK, NH * 192], F32)
    QH = c.dram(T("QH"), [NH, NLAT, 192], F32)
    QSQ = c.dram(T("QSQ"), [NLAT, NH], F32)
    OD = c.dram(T("OD"), [NTOK, NH * DV], BF16)
    tiles = [(r0, 128, "c" if r0 < NCTX else "l") for r0 in range(0, NTOK, 128)]
    lat_tiles = [t for t in tiles if t[2] == "l"]
    m1 = {ms: {"A": mods[ms]["A1"], "B": mods[ms]["B1"]} for ms in mods}
    proj_rows(c, T, "pj_", D, RES, tiles, m1, W["w_in"], QL + KVL + DR, [(0, QL, PQ, lambda r: r, T("PQ")), (QL, QL + KVL + DR, PKV, lambda r: r, T("PKV"))], K)
    mq = {"l": {"A": W["qng"], "B": W["zero768"]}}
    proj_rows(c, T, "pq_", QL, PQ, lat_tiles, mq, W["w_uq"], NH * 192, [(0, NH * 192, QD, lambda r: r, T("QD"))], K, src_res=T("PQ"))

    if upto == "proj":
        return
    with contextlib.ExitStack() as es0:
        sb0 = lambda n, s, d: es0.enter_context(nc.sbuf_tensor(T(n), s, d))
        ckvT = sb0("ckvT", [128, 4, NTOK], BF16)
        kpT = sb0("kpT", [65, NTOK], BF16)
        kpsq = sb0("kpsq", [128, NTOK], F32)
        idt = sb0("idt", [128, 128], F32)
        ones = sb0("ones", [128, 128], F32)
        c.dma("sp", idt[:], K["ident"][:, :], writes=[T("idt")])
        c.dma("sp", ones[:], K["ones"][:, :], writes=[T("ones")])
        with contextlib.ExitStack() as es:
            sb = lambda n, s, d: es.enter_context(nc.sbuf_tensor(T(n), s, d))
            ps = lambda n, s, d: es.enter_context(nc.psum_tensor(T(n), s, d))
            kvg = sb("a_kvg", [128, KVL], F32)
            kv = [sb(f"a_kv{i}", [128, KVL + DR], F32) for i in range(2)]
            junk = sb("a_junk", [128, KVL], F32)
            ss = sb("a_ss", [128, 2], F32)
            kn = sb("a_kn", [128, KVL], F32)
            cs = [sb(f"a_cs{i}", [128, 64], F32) for i in range(2)]
            kr = sb("a_kr", [128, DR], F32)
            t1 = sb("a_t1", [128, 32], F32)
            t2 = sb("a_t2", [128, 32], F32)
            sq2 = sb("a_sq2", [64, 128], F32)
            qt = [sb(f"a_qt{i}", [128, NH * 192], F32) for i in range(2)]
            qr = sb("a_qr", [128, NH * 192], F32)
            q1 = sb("a_q1", [128, NH * 32], F32)
            q2 = sb("a_q2", [128, NH * 32], F32)
            qq = sb("a_qq", [128, NH * 192], F32)
            qs = sb("a_qs", [128, NH], F32)
            pT = [ps(f"a_pT{i}", [128, 512], F32) for i in range(2)]
            pk = ps("a_pk", [64, 128], F32)
            pn = ps("a_pn", [128, 128], F32)
            c.dma("sp", kvg[:], W["kvng"].partition_broadcast(128), writes=[T("a_kvg")])
            mset(c, "pool", kpT[64:65, :], 1.0, [T("kpT")])
            for ti, (r0, n, ms) in enumerate(tiles):
                b = ti % 2
                c.dma("sp", kv[b][:], PKV[r0:r0 + 128, :], reads=[T("PKV")], writes=[T(f"a_kv{b}")])
                rstd = rmsnorm_rstd(c, kv[b][:, 0:KVL], junk[:], ss[:], KVL, [T(f"a_kv{b}")], T("a_n"))
                stt(c, "dve", kn[:], kv[b][:, 0:KVL], rstd, kvg[:], ALU.mult, ALU.mult, [T(f"a_kv{b}"), T("a_n_ss1"), T("a_kvg")], [T("a_kn")])
                for k in range(4):
                    tr(c, pT[b][:, k * 128:(k + 1) * 128], kn[:, k * 128:(k + 1) * 128], idt[:], [T("a_kn"), T("idt")], [T(f"a_pT{b}")], inc=(k == 3))
                cp(c, "act", ckvT[:, :, r0:r0 + 128], pT[b][:].rearrange("p (a b) -> p a b", a=4), [T(f"a_pT{b}")], [T("ckvT")])
                kp = kv[b][:, KVL:KVL + DR]
                if ms == "l":
                    t0 = r0 - NCTX
                    c.dma("sp", cs[b][:, 0:32], W["cos"][t0:t0 + 128, :], writes=[T(f"a_cs{b}")])
                    c.dma("sp", cs[b][:, 32:64], W["sin"][t0:t0 + 128, :], writes=[T(f"a_cs{b}")])
                    kp4 = kp.rearrange("p (a h e) -> p a h e", a=2, h=2)
                    kr4 = kr[:].rearrange("p (a h e) -> p a h e", a=2, h=2)
                    co = cs[b][:, 0:32].rearrange("p (a e) -> p a e", a=2)
                    si = cs[b][:, 32:64].rearrange("p (a e) -> p a e", a=2)
                    t1v = t1[:].rearrange("p (a e) -> p a e", a=2)
                    t2v = t2[:].rearrange("p (a e) -> p a e", a=2)
                    rd = [T(f"a_kv{b}"), T(f"a_cs{b}")]
                    tt(c, "dve", t1v, kp4[:, :, 0, :], co, ALU.mult, rd, [T("a_t1")])
                    tt(c, "dve", t2v, kp4[:, :, 1, :], si, ALU.mult, rd, [T("a_t2")])
                    tt(c, "dve", kr4[:, :, 0, :], t1v, t2v, ALU.subtract, [T("a_t1"), T("a_t2")], [T("a_kr")])
                    tt(c, "dve", t1v, kp4[:, :, 0, :], si, ALU.mult, rd, [T("a_t1")])
                    tt(c, "dve", t2v, kp4[:, :, 1, :], co, ALU.mult, rd, [T("a_t2")])
                    tt(c, "dve", kr4[:, :, 1, :], t1v, t2v, ALU.add, [T("a_t1"), T("a_t2")], [T("a_kr")])
                else:
                    cp(c, "dve", kr[:], kp, [T(f"a_kv{b}")], [T("a_kr")])
                tr(c, pk[:, :], kr[:], idt[:], [T("a_kr"), T("idt")], [T("a_pk")])
                cp(c, "act", kpT[0:64, r0:r0 + 128], pk[:, :], [T("a_pk")], [T("kpT")])
                act(c, sq2[:, :], pk[:, :], AF.Square, [T("a_pk")], [T("a_sq2")])
                mm(c, pn[:, :], ones[0:64, :], sq2[:, :], True, True, [T("ones"), T("a_sq2")], [T("a_pn")])
                cp(c, "dve", kpsq[:, r0:r0 + 128], pn[:, :], [T("a_pn")], [T("kpsq")])
                if ms == "l":
                    t0 = r0 - NCTX
                    c.dma("sp", qt[b][:], QD[r0:r0 + 128, :], reads=[T("QD")], writes=[T(f"a_qt{b}")])
                    q3 = qt[b][:].rearrange("p (h e) -> p h e", h=NH)
                    qr3 = qr[:].rearrange("p (h e) -> p h e", h=NH)
                    cp(c, "pool", qr3[:, :, 0:DN], q3[:, :, 0:DN], [T(f"a_qt{b}")], [T("a_qr")])
                    rd = [T(f"a_qt{b}"), T(f"a_cs{b}")]
                    for ax in range(2):
                        x1 = q3[:, :, DN + ax * 32:DN + ax * 32 + 16]
                        x2 = q3[:, :, DN + ax * 32 + 16:DN + ax * 32 + 32]
                        o1 = qr3[:, :, DN + ax * 32:DN + ax * 32 + 16]
                        o2 = qr3[:, :, DN + ax * 32 + 16:DN + ax * 32 + 32]
                        co = cs[b][:, ax * 16:(ax + 1) * 16].unsqueeze(1).to_broadcast([128, NH, 16])
                        si = cs[b][:, 32 + ax * 16:32 + (ax + 1) * 16].unsqueeze(1).to_broadcast([128, NH, 16])
                        a1 = q1[:, 0:NH * 16].rearrange("p (h e) -> p h e", h=NH)
                        a2 = q2[:, 0:NH * 16].rearrange("p (h e) -> p h e", h=NH)
                        tt(c, "dve", a1, x1, co, ALU.mult, rd, [T("a_q1")])
                        tt(c, "dve", a2, x2, si, ALU.mult, rd, [T("a_q2")])
                        tt(c, "dve", o1, a1, a2, ALU.subtract, [T("a_q1"), T("a_q2")], [T("a_qr")])
                        tt(c, "dve", a1, x1, si, ALU.mult, rd, [T("a_q1")])
                        tt(c, "dve", a2, x2, co, ALU.mult, rd, [T("a_q2")])
                        tt(c, "dve", o2, a1, a2, ALU.add, [T("a_q1"), T("a_q2")], [T("a_qr")])
                    tt(c, "pool", qq[:], qr[:], qr[:], ALU.mult, [T("a_qr")], [T("a_qq")])
                    red(c, "dve", qs[:], qq[:].rearrange("p (h e) -> p h e", h=NH), ALU.add, [T("a_qq")], [T("a_qs")])
                    c.dma("sp", QSQ[t0:t0 + 128, :], qs[:], reads=[T("a_qs")], writes=[T("QSQ")])
                    c.dma("sp", QH[:, t0:t0 + 128, :].rearrange("h t e -> t h e"), qr3, reads=[T("a_qr")], writes=[T("QH")])
        c.barrier()
        if upto == "A2":
            return

        with contextlib.ExitStack() as es:
            sb = lambda n, s, d: es.enter_context(nc.sbuf_tensor(T(n), s, d))
            ps = lambda n, s, d: es.enter_context(nc.psum_tensor(T(n), s, d))
            wuk = sb("h_wuk", [128, 4, DN], BF16)
            wuv = sb("h_wuv", [128, 4, DV], BF16)
            KnT = sb("h_KnT", [128, NTOK], BF16)
            Vt = sb("h_Vt", [128, NKT, 132], BF16)
            sqk = sb("h_sqk", [128, 512], F32)
            ksq = sb("h_ksq", [128, NTOK], F32)
            km = sb("h_km", [128, 2], F32)
            kmb = sb("h_kmb", [128, 1], F32)
            qh = [sb(f"h_qh{i}", [128, 193], F32) for i in range(2)]
            qsq = [sb(f"h_qsq{i}", [128, NH], F32) for i in range(2)]
            nq = sb("h_nq", [128, 2], F32)
            QnT = [sb(f"h_QnT{i}", [128, 512], BF16) for i in range(2)]
            QpT = [sb(f"h_QpT{i}", [65, 512], BF16) for i in range(2)]
            PT = [sb(f"h_PT{i}", [128, 512], BF16) for i in range(3)]
            rc = sb("h_rc", [128, 4], F32)
            ob = [sb(f"h_ob{i}", [128, DV], BF16) for i in range(2)]
            pS = [ps(f"h_pS{i}", [128, 512], F32) for i in range(2)]
            pO = [ps(f"h_pO{i}", [128, 512], F32) for i in range(4)]
            pX = [ps(f"h_pX{i}", [128, 512], F32) for i in range(2)]
            mset(c, "dve", Vt[:], 1.0, [T("h_Vt")])
            pti = 0
            if upto == "K0":
                c.barrier()
                return
            for h in range(NH):
                c.dma("pool", wuk[:], W["w_uk"][:, h * DN:(h + 1) * DN].rearrange("(k p) n -> p k n", p=128), writes=[T("h_wuk")])
                c.dma("pool", wuv[:], W["w_uv"][:, h * DV:(h + 1) * DV].rearrange("(k p) n -> p k n", p=128), writes=[T("h_wuv")])
                if upto == "K0b":
                    c.barrier()
                    return
                nkb = (NTOK + 511) // 512
                for kb in range(nkb):
                    w = min(512, NTOK - kb * 512)
                    pb = kb % 2
                    for k in range(4):
                        mm(c, pX[pb][:, :w], wuk[:, k, :], ckvT[:, k, kb * 512:kb * 512 + w], k == 0, k == 3, [T("h_wuk"), T("ckvT")], [T(f"h_pX{pb}")])
                    cp(c, "dve", KnT[:, kb * 512:kb * 512 + w], pX[pb][:, :w], [T(f"h_pX{pb}")], [T("h_KnT")])
                    if upto == "K1a":
                        continue
                    act(c, sqk[:, :w], KnT[:, kb * 512:kb * 512 + w], AF.Square, [T("h_KnT")], [T("h_sqk")])
                    if upto == "K1b":
                        continue
                    mm(c, pS[pb][:, :w], ones[:, :], sqk[:, :w], True, True, [T("ones"), T("h_sqk")], [T(f"h_pS{pb}")])
                    tt(c, "dve", ksq[:, kb * 512:kb * 512 + w], pS[pb][:, :w], kpsq[:, kb * 512:kb * 512 + w], ALU.add, [T(f"h_pS{pb}"), T("kpsq")], [T("h_ksq")])
                if upto in ("K1", "K1a", "K1b"):
                    c.barrier()
                    return
                red(c, "dve", km[:, 0:1], ksq[:, :], ALU.max, [T("h_ksq")], [T("h_km")])
                act(c, km[:, 1:2], km[:, 0:1], AF.Sqrt, [T("h_km")], [T("h_km1")])
                ts(c, "dve", kmb[:], km[:, 1:2], -1.0, None, ALU.mult, None, [T("h_km1")], [T("h_kmb")])
                if upto == "K2":
                    c.barrier()
                    return
                for kg in range((NKT + 3) // 4):
                    pb = kg % 2
                    nk = min(4, NKT - kg * 4)
                    for j in range(nk):
                        kt = kg * 4 + j
                        for k in range(4):
                            mm(c, pX[pb][:, j * 128:(j + 1) * 128], ckvT[:, k, kt * 128:(kt + 1) * 128], wuv[:, k, :], k == 0, k == 3,
                               [T("ckvT"), T("h_wuv")], [T(f"h_pX{pb}")], inc=(k == 3 and j == nk - 1))
                    cp(c, "act" if pb == 0 else "dve", Vt[:, kg * 4:kg * 4 + nk, 0:DV], pX[pb][:, 0:nk * 128].rearrange("p (a b) -> p a b", a=nk),
                       [T(f"h_pX{pb}")], [T("h_Vt")])
                if upto == "KV":
                    c.barrier()
                    return
                for qb in range(NLAT // 512):
                    qi = qb % 2
                    for s4 in range(4):
                        t0 = qb * 512 + s4 * 128
                        b = s4 % 2
                        c.dma("sp", qh[b][:, 0:192], QH[h, t0:t0 + 128, :], reads=[T("QH")], writes=[T(f"h_qh{b}")])
                        c.dma("sp", qsq[b][:], QSQ[t0:t0 + 128, :], reads=[T("QSQ")], writes=[T(f"h_qsq{b}")])
                        act(c, nq[:, 0:1], qsq[b][:, h:h + 1], AF.Sqrt, [T(f"h_qsq{b}")], [T("h_nq")])
                        ts(c, "dve", qh[b][:, 192:193], nq[:, 0:1], kmb[:, 0:1], None, ALU.mult, None, [T("h_nq"), T("h_kmb")], [T(f"h_qh{b}")])
                        tr(c, pX[0][:, s4 * 128:(s4 + 1) * 128], qh[b][:, 0:DN], idt[:], [T(f"h_qh{b}"), T("idt")], [T("h_pX0")], inc=False)
                        tr(c, pX[1][0:65, s4 * 128:(s4 + 1) * 128], qh[b][:, DN:193], idt[:], [T(f"h_qh{b}"), T("idt")], [T("h_pX1")])
                    cp(c, "act", QnT[qi][:], pX[0][:], [T("h_pX0")], [T(f"h_QnT{qi}")])
                    cp(c, "dve", QpT[qi][:], pX[1][0:65, :], [T("h_pX1")], [T(f"h_QpT{qi}")])
                    def emit_qk(kt):
                        sbi = kt % 2
                        mm(c, pS[sbi][:], KnT[:, kt * 128:(kt + 1) * 128], QnT[qi][:], True, False, [T("h_KnT"), T(f"h_QnT{qi}")], [T(f"h_pS{sbi}")], inc=False)
                        mm(c, pS[sbi][:], kpT[:, kt * 128:(kt + 1) * 128], QpT[qi][:], False, True, [T("kpT"), T(f"h_QpT{qi}")], [T(f"h_pS{sbi}")])
                    emit_qk(0)
                    for kt in range(NKT):
                        sbi = kt % 2
                        p3 = pti % 3
                        pti += 1
                        if kt + 1 < NKT:
                            emit_qk(kt + 1)
                        act(c, PT[p3][:], pS[sbi][:], AF.Exp, [T(f"h_pS{sbi}")], [T(f"h_PT{p3}")], scale=SCALE)
                        for s4 in range(4):
                            mm(c, pO[s4][:, 0:129], PT[p3][:, s4 * 128:(s4 + 1) * 128], Vt[:, kt, 0:129], kt == 0, kt == NKT - 1,
                               [T(f"h_PT{p3}"), T("h_Vt")], [T(f"h_pO{s4}")], inc=(kt == NKT - 1 or s4 == 3))
                    for s4 in range(4):
                        t0 = qb * 512 + s4 * 128
                        b = s4 % 2
                        c.op("dve", lambda e: e.reciprocal(out=rc[:, s4:s4 + 1], in_=pO[s4][:, 128:129]), [T(f"h_pO{s4}")], [T("h_rc")])
                        ts(c, "dve", ob[b][:], pO[s4][:, 0:DV], rc[:, s4:s4 + 1], None, ALU.mult, None, [T(f"h_pO{s4}"), T("h_rc")], [T(f"h_ob{b}")])
                        c.dma("sp", OD[NCTX + t0:NCTX + t0 + 128, h * DV:(h + 1) * DV], ob[b][:], reads=[T(f"h_ob{b}")], writes=[T("OD")])
        c.barrier()
    c.barrier()
    if dbg is not None:
        dbg(dict(OD=OD, QH=QH, PKV=PKV))
    out_proj_res(c, T, "op_", D, NH * DV, OD, W["w_o"], RES, lat_tiles, {"l": mods["l"]["G1"]}, K)


NE = 32


def moe_local(c, tag, D, FF, NT, RES, tiles, mods, W, K, dbg=None):
    nc = c.nc
    KC = D // 128
    FC = FF // 128
    NBLK = (4 * NT + 511) // 512 + NE
    NSLOT = NBLK * 512
    assert NSLOT % 128 == 0
    T = lambda s: f"{tag}_{s}"
    HF = c.dram(T("HF"), [NT + 128, D], BF16)
    GD = c.dram(T("GD"), [NT, NE], F32)
    LISTF = c.dram(T("LISTF"), [NSLOT, 2], F32)
    ACC = c.dram(T("ACC"), [NT + 128, D], F32)
    W1G2 = W["w1gT"]
    W1L2 = W["w1lT"]
    W22 = W["w2T"]
    c.barrier()
    with contextlib.ExitStack() as es0:
        sb0 = lambda n, s, d: es0.enter_context(nc.sbuf_tensor(T(n), s, d))
        idt = sb0("idt", [128, 128], F32)
        ones = sb0("ones", [128, 128], F32)
        trif = sb0("trif", [128, 128], F32)
        iop = sb0("iop", [128, 1], F32)
        iob = sb0("iob", [128, NBLK], F32)
        cnt = sb0("cnt", [128, NE], F32)
        base = sb0("base", [128, NE], F32)
        widx = sb0("widx", [128, NBLK], I32)
        eidx = sb0("eidx", [128, NBLK], I32)
        widx1 = sb0("widx1", [128, FC, NBLK], I32)
        c.dma("sp", idt[:], K["ident"][:, :], writes=[T("idt")])
        c.dma("sp", ones[:], K["ones"][:, :], writes=[T("ones")])
        c.dma("sp", trif[:], K["trif"][:, :], writes=[T("trif")])
        c.dma("sp", iop[:], K["iota_p"][:, :], writes=[T("iop")])
        c.dma("sp", iob[:], K["iota_b"].partition_broadcast(128), writes=[T("iob")])
        with contextlib.ExitStack() as es:
            sb = lambda n, s, d: es.enter_context(nc.sbuf_tensor(T(n), s, d))
            ps = lambda n, s, d: es.enter_context(nc.psum_tensor(T(n), s, d))
            At = sb("At", [128, D], F32)
            Bt = sb("Bt", [128, D], F32)
            rwt = sb("rwt", [128, KC, NE], F32)
            rbt = sb("rbt", [128, NE], F32)
            xt = [sb(f"xt{i}", [128, D], F32) for i in range(2)]
            junk = sb("junk", [128, D], F32)
            ss = sb("ss", [128, 2], F32)
            h = sb("h", [128, D], F32)
            hb = [sb(f"hb{i}", [128, D], BF16) for i in range(2)]
            hT = sb("hT", [128, KC, 128], F32)
            lg = sb("lg", [128, NE], F32)
            m8 = sb("m8", [128, 8], F32)
            sm = sb("sm", [128, 4], F32)
            ex = sb("ex", [128, NE], F32)
            mk = [sb(f"mk{i}", [128, NE], F32) for i in range(2)]
            Gt = [sb(f"Gt{i}", [128, NE], F32) for i in range(2)]
            zt = sb("zt", [128, D], F32)
            zb = sb("zb", [128, D], BF16)
            lf = sb("lf", [128, NSLOT // 128, 2], F32)
            pT = [ps(f"pT{i}", [128, 512], F32) for i in range(2)]
            pl = ps("pl", [128, NE], F32)
            pc = ps("pc", [128, NE], F32)
            c.dma("sp", rwt[:], W["rw"].rearrange("(k p) e -> p k e", p=128), writes=[T("rwt")])
            c.dma("sp", rbt[:], W["rb"].partition_broadcast(128), writes=[T("rbt")])
            mset(c, "pool", zt[:], 0.0, [T("zt")])
            mset(c, "pool", zb[:], 0.0, [T("zb")])
            mset(c, "pool", lf[:, :, 0:1], float(NT), [T("lf")])
            mset(c, "pool", lf[:, :, 1:2], 0.0, [T("lf")])
            c.dma("sp", LISTF.rearrange("(p a) c -> p a c", p=128), lf[:], reads=[T("lf")], writes=[T("LISTF")])
            c.dma("sp", HF[NT:NT + 128, :], zb[:], reads=[T("zb")], writes=[T("HF")])
            for i in range((NT + 128) // 128):
                c.dma("sp", ACC[i * 128:(i + 1) * 128, :], zt[:], reads=[T("zt")], writes=[T("ACC")])
            cur_ms = None
            ntl = len(tiles)
            for ti, (r0, n, ms, tok0) in enumerate(tiles):
                assert n == 128
                b = ti % 2
                if ms != cur_ms:
                    c.dma("sp", At[:], mods[ms]["A2"].partition_broadcast(128), reads=["MODS"], writes=[T("At")])
                    c.dma("sp", Bt[:], mods[ms]["B2"].partition_broadcast(128), reads=["MODS"], writes=[T("Bt")])
                    cur_ms = ms
                c.dma("sp", xt[b][:], RES[r0:r0 + 128, :], reads=["RES"], writes=[T(f"xt{b}")])
                rstd = rmsnorm_rstd(c, xt[b][:], junk[:], ss[:], D, [T(f"xt{b}")], T("n1"))
                stt(c, "dve", h[:], xt[b][:], rstd, At[:], ALU.mult, ALU.mult, [T(f"xt{b}"), T("n1_ss1"), T("At")], [T("h")])
                tt(c, "pool", h[:], h[:], Bt[:], ALU.add, [T("h"), T("Bt")], [T("h")])
                cp(c, "act", hb[b][:], h[:], [T("h")], [T(f"hb{b}")])
                c.dma("sp", HF[tok0:tok0 + 128, :], hb[b][:], reads=[T(f"hb{b}")], writes=[T("HF")])
                for kg in range(KC // 4):
                    pb = kg % 2
                    for jj in range(4):
                        k = kg * 4 + jj
                        tr(c, pT[pb][:, jj * 128:(jj + 1) * 128], h[:, k * 128:(k + 1) * 128], idt[:], [T("h"), T("idt")], [T(f"pT{pb}")], inc=(jj == 3))
                    cp(c, "act" if kg % 2 == 0 else "dve", hT[:, kg * 4:(kg + 1) * 4, :], pT[pb][:].rearrange("p (a b) -> p a b", a=4), [T(f"pT{pb}")], [T("hT")])
                for k in range(KC):
                    mm(c, pl[:, :], hT[:, k, :], rwt[:, k, :], k == 0, k == KC - 1, [T("hT"), T("rwt")], [T("pl")])
                tt(c, "dve", lg[:], pl[:, :], rbt[:], ALU.add, [T("pl"), T("rbt")], [T("lg")])
                c.op("dve", lambda e: e.max(out=m8[:], in_=lg[:]), [T("lg")], [T("m8")])
                ts(c, "dve", mk[b][:], lg[:], m8[:, 3:4], None, ALU.is_ge, None, [T("lg"), T("m8")], [T(f"mk{b}")])
                ts(c, "dve", sm[:, 0:1], m8[:, 0:1], -1.0, None, ALU.mult, None, [T("m8")], [T("sm0")])
                act(c, ex[:], lg[:], AF.Exp, [T("lg"), T("sm0")], [T("ex")], bias=sm[:, 0:1])
                tt(c, "dve", ex[:], ex[:], mk[b][:], ALU.mult, [T("ex"), T(f"mk{b}")], [T("ex")])
                red(c, "dve", sm[:, 1:2], ex[:], ALU.add, [T("ex")], [T("sm1")])
                c.op("dve", lambda e: e.reciprocal(out=sm[:, 2:3], in_=sm[:, 1:2]), [T("sm1")], [T("sm2")])
                ts(c, "dve", Gt[b][:], ex[:], sm[:, 2:3], None, ALU.mult, None, [T("ex"), T("sm2")], [T(f"Gt{b}")])
                c.dma("sp", GD[tok0:tok0 + 128, :], Gt[b][:], reads=[T(f"Gt{b}")], writes=[T("GD")])
                mm(c, pc[:, :], ones[:], mk[b][:], ti == 0, ti == ntl - 1, [T("ones"), T(f"mk{b}")], [T("pc")], inc=True)
            cp(c, "dve", cnt[:], pc[:, :], [T("pc")], [T("cnt")])
        c.barrier()
        with contextlib.ExitStack() as es:
            sb = lambda n, s, d: es.enter_context(nc.sbuf_tensor(T(n), s, d))
            r = sb("r", [128, NE], F32)
            nb = sb("nb", [128, NE], F32)
            inc_ = [sb(f"inc{i}", [128, NE], F32) for i in range(2)]
            eid = sb("eid", [128, NBLK], F32)
            tmpb = sb("tmpb", [128, NBLK], F32)
            mset(c, "dve", nb[:], 0.0, [T("nb")])
            for j in range((4 * NT + 511) // 512 + 1):
                ts(c, "dve", r[:], cnt[:], 512.0 * j, None, ALU.is_gt, None, [T("cnt")], [T("r")])
                tt(c, "dve", nb[:], nb[:], r[:], ALU.add, [T("nb"), T("r")], [T("nb")])
            cp(c, "dve", inc_[0][:], nb[:], [T("nb")], [T("inc0")])
            src = 0
            sh = 1
            while sh < NE:
                dst = 1 - src
                tt(c, "dve", inc_[dst][:, sh:NE], inc_[src][:, sh:NE], inc_[src][:, 0:NE - sh], ALU.add, [T(f"inc{src}")], [T(f"inc{dst}")])
                cp(c, "dve", inc_[dst][:, 0:sh], inc_[src][:, 0:sh], [T(f"inc{src}")], [T(f"inc{dst}")])
                src = dst
                sh *= 2
            incl = inc_[src]
            tt(c, "dve", base[:], incl[:], nb[:], ALU.subtract, [T(f"inc{src}"), T("nb")], [T("base")])
            ts(c, "dve", base[:], base[:], 512.0, None, ALU.mult, None, [T("base")], [T("base")])
            mset(c, "dve", eid[:], 0.0, [T("eid")])
            for e in range(NE):
                ts(c, "dve", tmpb[:], iob[:], incl[:, e:e + 1], None, ALU.is_ge, None, [T("iob"), T(f"inc{src}")], [T("tmpb")])
                tt(c, "dve", eid[:], eid[:], tmpb[:], ALU.add, [T("eid"), T("tmpb")], [T("eid")])
            ts(c, "dve", eid[:], eid[:], float(NE - 1), None, ALU.min, None, [T("eid")], [T("eid")])
            cp(c, "dve", eidx[:], eid[:], [T("eid")], [T("eidx")])
            ts(c, "dve", tmpb[:], eid[:], 128.0, iop[:, 0:1], ALU.mult, ALU.add, [T("eid"), T("iop")], [T("tmpb")])
            cp(c, "dve", widx[:], tmpb[:], [T("tmpb")], [T("widx")])
            ts(c, "dve", eid[:], eid[:], 128.0 * FC, iop[:, 0:1], ALU.mult, ALU.add, [T("eid"), T("iop")], [T("eid")])
            for fc in range(FC):
                ts(c, "dve", tmpb[:], eid[:], 128.0 * fc, None, ALU.add, None, [T("eid")], [T("tmpb")])
                cp(c, "dve", widx1[:, fc, :], tmpb[:], [T("tmpb")], [T("widx")])
        c.barrier()
        with contextlib.ExitStack() as es:
            sb = lambda n, s, d: es.enter_context(nc.sbuf_tensor(T(n), s, d))
            ps = lambda n, s, d: es.enter_context(nc.psum_tensor(T(n), s, d))
            Gl = [sb(f"Gl{i}", [128, NE], F32) for i in range(2)]
            Mk = sb("Mk", [128, NE], F32)
            car = sb("car", [128, NE], F32)
            key = sb("key", [128, NE], F32)
            k8 = sb("k8", [128, 8], F32)
            eq = sb("eq", [128, NE], F32)
            dsti = [sb(f"dsti{i}", [128, 4], I32) for i in range(2)]
            dstf = sb("dstf", [128, 4], F32)
            pay = [sb(f"pay{i}", [128, 4, 2], F32) for i in range(2)]
            pcs = ps("pcs", [128, NE], F32)
            pcr = ps("pcr", [128, NE], F32)
            mset(c, "dve", car[:], 0.0, [T("car")])
            for ti, (r0, n, ms, tok0) in enumerate(tiles):
                b = ti % 2
                c.dma("sp", Gl[b][:], GD[tok0:tok0 + 128, :], reads=[T("GD")], writes=[T(f"Gl{b}")])
                ts(c, "dve", Mk[:], Gl[b][:], 0.0, None, ALU.is_gt, None, [T(f"Gl{b}")], [T("Mk")])
                mm(c, pcs[:, :], trif[:], Mk[:], True, True, [T("trif"), T("Mk")], [T("pcs")])
                mm(c, pcr[:, :], ones[:], Mk[:], True, True, [T("ones"), T("Mk")], [T("pcr")])
                tt(c, "dve", key[:], pcs[:, :], car[:], ALU.add, [T("pcs"), T("car")], [T("key")])
                tt(c, "dve", key[:], key[:], base[:], ALU.add, [T("key"), T("base")], [T("key")])
                tt(c, "dve", key[:], key[:], Mk[:], ALU.mult, [T("key"), T("Mk")], [T("key")])
                tt(c, "dve", car[:], car[:], pcr[:, :], ALU.add, [T("car"), T("pcr")], [T("car")])
                c.op("dve", lambda e: e.max(out=k8[:], in_=key[:]), [T("key")], [T("k8")])
                ts(c, "dve", dstf[:], k8[:, 0:4], -1.0, None, ALU.add, None, [T("k8")], [T("dstf")])
                cp(c, "dve", dsti[b][:], dstf[:], [T("dstf")], [T(f"dsti{b}")])
                for k in range(4):
                    ts(c, "dve", eq[:], key[:], k8[:, k:k + 1], None, ALU.is_equal, None, [T("key"), T("k8")], [T("eq")])
                    tt(c, "dve", eq[:], eq[:], Gl[b][:], ALU.mult, [T("eq"), T(f"Gl{b}")], [T("eq")])
                    red(c, "dve", pay[b][:, k, 1:2], eq[:], ALU.add, [T("eq")], [T(f"pay{b}")])
                    ts(c, "dve", pay[b][:, k, 0:1], iop[:, 0:1], float(tok0), None, ALU.add, None, [T("iop")], [T(f"pay{b}")])
                for k in range(4):
                    c.idma(LISTF[:, :], bass.IndirectOffsetOnAxis(ap=dsti[b][:, k:k + 1], axis=0), pay[b][:, k, :], None,
                           reads=[T(f"dsti{b}"), T(f"pay{b}")], writes=[T("LISTF")])
        c.barrier()
        with contextlib.ExitStack() as es:
            sb = lambda n, s, d: es.enter_context(nc.sbuf_tensor(T(n), s, d))
            ps = lambda n, s, d: es.enter_context(nc.psum_tensor(T(n), s, d))
            idb = sb("idb", [128, 128], BF16)
            lst = [sb(f"lst{i}", [128, 4, 2], F32) for i in range(2)]
            tkf = sb("tkf", [128, 4], F32)
            pad1 = sb("pad1", [128, 4], F32)
            tki = [sb(f"tki{i}", [128, 4], I32) for i in range(2)]
            xg = [sb(f"xg{i}", [128, D], BF16) for i in range(2)]
            xT = sb("xT", [128, KC, 512], BF16)
            yT = sb("yT", [128, FC, 512], BF16)
            w2t = sb("w2t", [128, FC, D], BF16)
            b2t = sb("b2t", [128, D], F32)
            b1gt = sb("b1gt", [128, FC], F32)
            b1lt = sb("b1lt", [128, FC], F32)
            w1gt = [sb(f"w1g{i}", [128, KC, 128], BF16) for i in range(2)]
            w1lt = [sb(f"w1l{i}", [128, KC, 128], BF16) for i in range(2)]
            stg = [sb(f"stg{i}", [128, max(D, KC * 128)], F32) for i in range(4)]
            nstg = 0
            gs = [sb(f"gs{i}", [128, 512], F32) for i in range(2)]
            sg = [sb(f"sg{i}", [128, 512], F32) for i in range(2)]
            ls = [sb(f"ls{i}", [128, 512], F32) for i in range(2)]
            yb = [sb(f"yb{i}", [128, D], F32) for i in range(2)]
            TW = min(8, KC)
            ptr = [ps(f"ptr{i}", [128, TW * 128], BF16) for i in range(2)]
            pgl = [ps(f"pgl{i}", [128, 512], F32) for i in range(4)]
            po = [ps(f"po{i}", [128, 512], F32) for i in range(2)]
            cp(c, "dve", idb[:], idt[:], [T("idt")], [T("idb")])
            gcount = 0
            for blk in range(NBLK):
                lb = blk % 2
                c.dma("sp", lst[lb][:], LISTF[blk * 512:(blk + 1) * 512, :].rearrange("(s p) c -> p s c", p=128), reads=[T("LISTF")], writes=[T(f"lst{lb}")])
                cp(c, "dve", tkf[:], lst[lb][:, :, 0], [T(f"lst{lb}")], [T("tkf")])
                ts(c, "dve", pad1[:], tkf[:], float(NT), None, ALU.is_ge, None, [T("tkf")], [T("pad1")])
                ts(c, "dve", pad1[:], pad1[:], iop[:, 0:1], None, ALU.mult, None, [T("pad1"), T("iop")], [T("pad1")])
                tt(c, "dve", tkf[:], tkf[:], pad1[:], ALU.add, [T("tkf"), T("pad1")], [T("tkf")])
                cp(c, "dve", tki[lb][:], tkf[:], [T("tkf")], [T(f"tki{lb}")])
                wofs = bass.IndirectOffsetOnAxis(ap=widx[:, blk:blk + 1], axis=0)
                for fc in range(FC):
                    sgi = nstg % 4
                    nstg += 1
                    c.idma(stg[sgi][:, 0:D], None, W22[:, :], bass.IndirectOffsetOnAxis(ap=widx1[:, fc, blk:blk + 1], axis=0),
                           reads=[T("widx")], writes=[T(f"stg{sgi}")])
                    cp(c, "act" if fc % 2 == 0 else "dve", w2t[:, fc, :], stg[sgi][:, 0:D], [T(f"stg{sgi}")], [T("w2t")])
                c.idma(b1gt[:], None, W["b1gT"][:, :], wofs, reads=[T("widx")], writes=[T("b1gt")])
                c.idma(b1lt[:], None, W["b1lT"][:, :], wofs, reads=[T("widx")], writes=[T("b1lt")])
                c.idma(b2t[:], None, W["b2"][:, :], bass.IndirectOffsetOnAxis(ap=eidx[:, blk:blk + 1], axis=0), reads=[T("eidx")], writes=[T("b2t")])
                for st in range(4):
                    b = gcount % 2
                    gcount += 1
                    c.idma(xg[b][:], None, HF[:, :], bass.IndirectOffsetOnAxis(ap=tki[lb][:, st:st + 1], axis=0),
                           reads=[T(f"tki{lb}"), T("HF")], writes=[T(f"xg{b}")])
                    for kg in range(KC // TW):
                        pb = (kg + st) % 2
                        for jj in range(TW):
                            k = kg * TW + jj
                            tr(c, ptr[pb][:, jj * 128:(jj + 1) * 128], xg[b][:, k * 128:(k + 1) * 128], idb[:], [T(f"xg{b}"), T("idb")], [T(f"ptr{pb}")], inc=(jj == TW - 1))
                        cp(c, "act" if pb == 0 else "dve", xT[:, kg * TW:(kg + 1) * TW, st * 128:(st + 1) * 128],
                           ptr[pb][:].rearrange("p (a b) -> p a b", a=TW), [T(f"ptr{pb}")], [T("xT")])
                for fc in range(FC):
                    wb = fc % 2
                    wofs1 = bass.IndirectOffsetOnAxis(ap=widx1[:, fc, blk:blk + 1], axis=0)
                    sgi = nstg % 4
                    nstg += 1
                    c.idma(stg[sgi][:, 0:KC * 128], None, W1G2[:, :], wofs1, reads=[T("widx")], writes=[T(f"stg{sgi}")])
                    cp(c, "act", w1gt[wb][:], stg[sgi][:, 0:KC * 128].rearrange("p (k f) -> p k f", k=KC), [T(f"stg{sgi}")], [T(f"w1g{wb}")])
                    sgi = nstg % 4
                    nstg += 1
                    c.idma(stg[sgi][:, 0:KC * 128], None, W1L2[:, :], wofs1, reads=[T("widx")], writes=[T(f"stg{sgi}")])
                    cp(c, "dve", w1lt[wb][:], stg[sgi][:, 0:KC * 128].rearrange("p (k f) -> p k f", k=KC), [T(f"stg{sgi}")], [T(f"w1l{wb}")])
                    pgi = pgl[2 * wb]
                    pli = pgl[2 * wb + 1]
                    for k in range(KC):
                        mm(c, pgi[:], w1gt[wb][:, k, :], xT[:, k, :], k == 0, k == KC - 1, [T(f"w1g{wb}"), T("xT")], [T(f"pgl{2 * wb}")])
                    for k in range(KC):
                        mm(c, pli[:], w1lt[wb][:, k, :], xT[:, k, :], k == 0, k == KC - 1, [T(f"w1l{wb}"), T("xT")], [T(f"pgl{2 * wb + 1}")])
                    ts(c, "dve", gs[wb][:], pgi[:], b1gt[:, fc:fc + 1], 7.0, ALU.add, ALU.min, [T(f"pgl{2 * wb}"), T("b1gt")], [T(f"gs{wb}")])
                    act(c, sg[wb][:], gs[wb][:], AF.Sigmoid, [T(f"gs{wb}")], [T(f"sg{wb}")], scale=1.702)
                    ts(c, "dve", ls[wb][:], pli[:], b1lt[:, fc:fc + 1], 7.0, ALU.add, ALU.min, [T(f"pgl{2 * wb + 1}"), T("b1lt")], [T(f"ls{wb}")])
                    ts(c, "dve", ls[wb][:], ls[wb][:], -7.0, 1.0, ALU.max, ALU.add, [T(f"ls{wb}")], [T(f"ls{wb}")])
                    tt(c, "dve", gs[wb][:], gs[wb][:], sg[wb][:], ALU.mult, [T(f"gs{wb}"), T(f"sg{wb}")], [T(f"gs{wb}")])
                    tt(c, "dve", yT[:, fc, :], gs[wb][:], ls[wb][:], ALU.mult, [T(f"gs{wb}"), T(f"ls{wb}")], [T("yT")])
                for st in range(4):
                    ob = st % 2
                    for nbk in range(D // 512):
                        pb = nbk % 2
                        for fc in range(FC):
                            mm(c, po[pb][:], yT[:, fc, st * 128:(st + 1) * 128], w2t[:, fc, nbk * 512:(nbk + 1) * 512], fc == 0, fc == FC - 1,
                               [T("yT"), T("w2t")], [T(f"po{pb}")])
                        tt(c, "dve", yb[ob][:, nbk * 512:(nbk + 1) * 512], po[pb][:], b2t[:, nbk * 512:(nbk + 1) * 512], ALU.add,
                           [T(f"po{pb}"), T("b2t")], [T(f"yb{ob}")])
                    act(c, yb[ob][:], yb[ob][:], AF.Identity, [T(f"yb{ob}"), T(f"lst{lb}")], [T(f"yb{ob}")], scale=lst[lb][:, st, 1:2])
                    c.idma(ACC[:, :], bass.IndirectOffsetOnAxis(ap=tki[lb][:, st:st + 1], axis=0), yb[ob][:], None,
                           reads=[T(f"tki{lb}"), T(f"yb{ob}")], writes=[T("ACC")], compute_op=ALU.add)
        c.barrier()
    c.barrier()
    if dbg is not None:
        dbg(dict(ACC=ACC, GD=GD, LISTF=LISTF))
    with contextlib.ExitStack() as es:
        sb = lambda n, s, d: es.enter_context(nc.sbuf_tensor(T(n), s, d))
        Gm = sb("Gm", [128, D], F32)
        xr = [sb(f"xr{i}", [128, D], F32) for i in range(2)]
        fr = [sb(f"fr{i}", [128, D], F32) for i in range(2)]
        cur_ms = None
        for ti, (r0, n, ms, tok0) in enumerate(tiles):
            b = ti % 2
            if ms != cur_ms:
                c.dma("sp", Gm[:], mods[ms]["G2"].partition_broadcast(128), reads=["MODS"], writes=[T("Gm")])
                cur_ms = ms
            c.dma("sp", xr[b][:], RES[r0:r0 + 128, :], reads=["RES"], writes=[T(f"xr{b}")])
            c.dma("sp", fr[b][:], ACC[tok0:tok0 + 128, :], reads=[T("ACC")], writes=[T(f"fr{b}")])
            tt(c, "dve", fr[b][:], fr[b][:], Gm[:], ALU.mult, [T(f"fr{b}"), T("Gm")], [T(f"fr{b}")])
            tt(c, "pool", xr[b][:], xr[b][:], fr[b][:], ALU.add, [T(f"xr{b}"), T(f"fr{b}")], [T(f"xr{b}")])
            c.dma("sp", RES[r0:r0 + 128, :], xr[b][:], reads=[T(f"xr{b}")], writes=["RES"])
    c.barrier()

import re as _re

D_MODEL = 2048
NBATCH = 2
SEQ = 8192
NCTX = 256
FFE = 2048


def mods_phase(c, CROW, ADAW, ADAB, MIXG, FFNG, MODV, K):
    nc = c.nc
    D = D_MODEL
    KC = D // 128
    T = lambda s: "md_" + s
    with contextlib.ExitStack() as es:
        sb = lambda n, s, d: es.enter_context(nc.sbuf_tensor(T(n), s, d))
        ps = lambda n, s, d: es.enter_context(nc.psum_tensor(T(n), s, d))
        idt = sb("idt", [128, 128], F32)
        cr = sb("cr", [3, D], F32)
        ST = sb("ST", [128, KC, 3], F32)
        wt = [sb(f"wt{i}", [128, KC, 512], F32) for i in range(2)]
        bt = [sb(f"bt{i}", [3, 512], F32) for i in range(2)]
        M = sb("M", [3, 6 * D], F32)
        g1 = sb("g1", [3, D], F32)
        g2 = sb("g2", [3, D], F32)
        MV = [sb(f"MV{i}", [3, D], F32) for i in range(2)]
        pT = ps("pT", [128, KC * 3], F32)
        pm = [ps(f"pm{i}", [3, 512], F32) for i in range(2)]
        c.dma("sp", idt[:], K["ident"][:, :], writes=[T("idt")])
        c.dma("sp", cr[:], CROW[:, :], writes=[T("cr")])
        act(c, cr[:], cr[:], AF.Silu, [T("cr")], [T("cr")])
        for k in range(KC):
            tr(c, pT[:, k * 3:(k + 1) * 3], cr[:, k * 128:(k + 1) * 128], idt[0:3, 0:3], [T("cr"), T("idt")], [T("pT")], inc=(k == KC - 1))
        cp(c, "dve", ST[:], pT[:].rearrange("p (k r) -> p k r", r=3), [T("pT")], [T("ST")])
        for i in range(2):
            c.dma("sp", g1[:], MIXG[i:i + 1, :].partition_broadcast(3), writes=[T("g1")])
            c.dma("sp", g2[:], FFNG[i:i + 1, :].partition_broadcast(3), writes=[T("g2")])
            for nb in range(6 * D // 512):
                wb = nb % 2
                c.dma("sp", wt[wb][:], ADAW[i * D:(i + 1) * D, nb * 512:(nb + 1) * 512].rearrange("(k p) n -> p k n", p=128), writes=[T(f"wt{wb}")])
                c.dma("sp", bt[wb][:], ADAB[i:i + 1, nb * 512:(nb + 1) * 512].partition_broadcast(3), writes=[T(f"bt{wb}")])
                for k in range(KC):
                    mm(c, pm[wb][:, :], ST[:, k, :], wt[wb][:, k, :], k == 0, k == KC - 1, [T("ST"), T(f"wt{wb}")], [T(f"pm{wb}")])
                tt(c, "dve", M[:, nb * 512:(nb + 1) * 512], pm[wb][:, :], bt[wb][:, :], ALU.add, [T(f"pm{wb}"), T(f"bt{wb}")], [T("M")])
            MO = MODV[i * 18:(i + 1) * 18, :].rearrange("(r k) d -> r k d", k=6)
            for k, (src0, gg) in enumerate(((D, g1), (0, None), (2 * D, None), (4 * D, g2), (3 * D, None), (5 * D, None))):
                mv = MV[k % 2]
                if gg is not None:
                    stt(c, "dve", mv[:], M[:, src0:src0 + D], 1.0, gg[:], ALU.add, ALU.mult, [T("M"), T("g1"), T("g2")], [T(f"MV{k % 2}")])
                else:
                    cp(c, "dve", mv[:], M[:, src0:src0 + D], [T("M")], [T(f"MV{k % 2}")])
                c.dma("sp", MO[:, k, :], mv[:], reads=[T(f"MV{k % 2}")], writes=["MODS"])
    c.barrier()


def modrow(MODV, i, r, k):
    j = (i * 3 + r) * 6 + k
    return MODV[j:j + 1, :]


def final_norm(c, tag, RES, r0, nrows, G, OUT, o0):
    nc = c.nc
    D = D_MODEL
    T = lambda s: f"{tag}_{s}"
    with contextlib.ExitStack() as es:
        sb = lambda n, s, d: es.enter_context(nc.sbuf_tensor(T(n), s, d))
        gt = sb("gt", [128, D], F32)
        xt = [sb(f"xt{i}", [128, D], F32) for i in range(2)]
        ot = [sb(f"ot{i}", [128, D], F32) for i in range(2)]
        junk = sb("junk", [128, D], F32)
        ss = sb("ss", [128, 2], F32)
        c.dma("sp", gt[:], G.partition_broadcast(128), writes=[T("gt")])
        for i in range(nrows // 128):
            b = i % 2
            c.dma("sp", xt[b][:], RES[r0 + i * 128:r0 + (i + 1) * 128, :], reads=["RES"], writes=[T(f"xt{b}")])
            rstd = rmsnorm_rstd(c, xt[b][:], junk[:], ss[:], D, [T(f"xt{b}")], T("n"))
            stt(c, "dve", ot[b][:], xt[b][:], rstd, gt[:], ALU.mult, ALU.mult, [T(f"xt{b}"), T("n_ss1"), T("gt")], [T(f"ot{b}")])
            c.dma("sp", OUT[o0 + i * 128:o0 + (i + 1) * 128, :], ot[b][:], reads=[T(f"ot{b}")], writes=["OUT"])
    c.barrier()


def build_program(nbatch=NBATCH, seq=SEQ, nctx=NCTX):
    c = Ctx()
    D = D_MODEL
    NTOK = nctx + seq
    ext = lambda n, s, dt=F32: c.dram(n, s, dt, "ExternalInput")
    X = ext("x", [nbatch * seq, D])
    CTX = ext("ctx", [nbatch * nctx, D])
    CROW = ext("crow", [3, D])
    ADAW = ext("ada_w", [2 * D, 6 * D])
    ADAB = ext("ada_b", [2, 6 * D])
    MIXG = ext("mix_g", [2, D])
    FFNG = ext("ffn_g", [2, D])
    FING = ext("fin_g", [1, D])
    WH = {"w_in": ext("h_w_in", [D, HIN]), "conv_w": ext("h_conv_w", [3, XW]), "conv_b": ext("h_conv_b", [1, XW]), "dtb": ext("h_dtb", [1, 64]),
          "alog": ext("h_alog", [1, 64]), "dsk": ext("h_dsk", [1, 32]), "ssdg": ext("h_ssdg", [1, SW]), "vng": ext("h_vng", [1, GW]),
          "wsT": ext("h_wsT", [16, 128, 128]), "bsT": ext("h_bsT", [128, 16]), "w_out": ext("h_w_out", [2 * SW, D])}
    WA = {"w_in": ext("a_w_in", [D, 1344]), "qng": ext("a_qng", [1, 768]), "kvng": ext("a_kvng", [1, 512]), "zero768": ext("a_zero768", [1, 768]),
          "w_uq": ext("a_w_uq", [768, 3072]), "w_uk": ext("a_w_uk", [512, 2048]), "w_uv": ext("a_w_uv", [512, 2048]), "w_o": ext("a_w_o", [2048, D]),
          "cos": ext("a_cos", [seq, 32]), "sin": ext("a_sin", [seq, 32])}
    FC = FFE // 128
    KC = D // 128
    WM = []
    for i in range(2):
        WM.append({"rw": ext(f"m{i}_rw", [D, 32]), "rb": ext(f"m{i}_rb", [1, 32]),
                   "w1gT": ext(f"m{i}_w1gT", [32 * FC * 128, KC * 128]), "w1lT": ext(f"m{i}_w1lT", [32 * FC * 128, KC * 128]),
                   "w2T": ext(f"m{i}_w2T", [32 * FFE, D]), "b1gT": ext(f"m{i}_b1gT", [32 * 128, FC]), "b1lT": ext(f"m{i}_b1lT", [32 * 128, FC]),
                   "b2": ext(f"m{i}_b2", [32, D])})
    NB0 = (4 * NTOK + 511) // 512 + 32
    NB1 = (4 * seq + 511) // 512 + 32
    K = {k: ext("k_" + k, [128, 128]) for k in ("ident", "ones", "trif", "trib")}
    K["iota_p"] = ext("k_iota_p", [128, 1])
    K0 = dict(K)
    K0["iota_b"] = ext("k_iota_b0", [1, NB0])
    K1 = dict(K)
    K1["iota_b"] = ext("k_iota_b1", [1, NB1])
    OUT = c.dram("out", [nbatch * seq, D], F32, "ExternalOutput")
    MODV = c.dram("MODV", [36, D], F32)
    mods_phase(c, CROW, ADAW, ADAB, MIXG, FFNG, MODV, K)
    for b in range(nbatch):
        RES = c.dram(f"b{b}RES", [NTOK, D], F32)
        for r0 in range(0, nctx, 128):
            c.dma("sp", RES[r0:r0 + 128, :], CTX[b * nctx + r0:b * nctx + r0 + 128, :], writes=["RES"])
        for r0 in range(0, seq, 128):
            c.dma("sp", RES[nctx + r0:nctx + r0 + 128, :], X[b * seq + r0:b * seq + r0 + 128, :], writes=["RES"])
        c.barrier()
        m = []
        for i in range(2):
            m.append({"l": {nm: modrow(MODV, i, b, k) for k, nm in enumerate(("A1", "B1", "G1", "A2", "B2", "G2"))},
                      "c": {nm: modrow(MODV, i, 2, k) for k, nm in enumerate(("A1", "B1", "G1", "A2", "B2", "G2"))}})
        hybrid_phase(c, f"b{b}H", D, nctx, seq, RES, m[0], WH, K)
        tiles0 = [(r0, 128, "c" if r0 < nctx else "l", r0) for r0 in range(0, NTOK, 128)]
        moe_local(c, f"b{b}M0", D, FFE, NTOK, RES, tiles0, m[0], WM[0], K0)
        mla_phase(c, f"b{b}A", D, nctx, seq, RES, m[1], WA, K)
        tiles1 = [(nctx + r0, 128, "l", r0) for r0 in range(0, seq, 128)]
        moe_local(c, f"b{b}M1", D, FFE, seq, RES, tiles1, m[1], WM[1], K1)
        final_norm(c, f"b{b}F", RES, nctx, seq, FING, OUT, b * seq)
    c.finish()
    return c


def host_inputs(inputs, nbatch=NBATCH, seq=SEQ, nctx=NCTX):
    f32 = np.float32
    A = lambda a: np.ascontiguousarray(np.asarray(a, dtype=f32))
    D = D_MODEL
    g = lambda k: np.asarray(inputs[k])
    m = {}
    m["ada_w"] = A(g("ada_w").reshape(2 * D, 6 * D))
    m["ada_b"] = A(g("ada_b").reshape(2, 6 * D))
    m["mix_g"] = A(g("mix_norm_g"))
    m["ffn_g"] = A(g("ffn_norm_g"))
    m["fin_g"] = A(g("final_norm_g").reshape(1, D))
    m["h_w_in"] = A(g("hyb_w_in")[0])
    m["h_conv_w"] = A(g("hyb_conv_w")[0])
    m["h_conv_b"] = A(g("hyb_conv_b")[0].reshape(1, -1))
    m["h_dtb"] = A(g("hyb_dt_bias")[0].reshape(1, 64))
    m["h_alog"] = A(g("hyb_a_log")[0].reshape(1, 64))
    m["h_dsk"] = A(g("hyb_d_skip")[0].reshape(1, 32))
    m["h_ssdg"] = A(g("hyb_ssd_norm_g")[0].reshape(1, -1))
    m["h_vng"] = A(g("hyb_v_norm_g")[0].reshape(1, -1))
    m["h_wsT"] = A(g("hyb_w_s")[0].transpose(0, 2, 1))
    m["h_bsT"] = A(g("hyb_b_s")[0].T)
    m["h_w_out"] = A(g("hyb_w_out")[0])
    m["a_w_in"] = A(g("mla_w_in")[0])
    m["a_qng"] = A(g("mla_q_norm_g")[0].reshape(1, -1))
    m["a_kvng"] = A(g("mla_kv_norm_g")[0].reshape(1, -1))
    m["a_zero768"] = np.zeros((1, 768), f32)
    m["a_w_uq"] = A(g("mla_w_uq")[0])
    wk = g("mla_w_ukv")[0].reshape(512, 16, 256)
    m["a_w_uk"] = A(wk[:, :, :128].reshape(512, 2048))
    m["a_w_uv"] = A(wk[:, :, 128:].reshape(512, 2048))
    m["a_w_o"] = A(g("mla_w_o")[0])
    rows = seq // 64
    row = np.repeat(np.arange(rows), 64).astype(np.float64)
    col = np.tile(np.arange(64), rows).astype(np.float64)
    freqs = (10000.0 ** (-np.arange(16, dtype=np.float32) / 16)).astype(np.float32)
    ang = np.stack([row[:, None].astype(f32) * freqs, col[:, None].astype(f32) * freqs], 1).astype(f32)
    m["a_cos"] = A(np.cos(ang).reshape(seq, 32))
    m["a_sin"] = A(np.sin(ang).reshape(seq, 32))
    FC = FFE // 128
    for i in range(2):
        m[f"m{i}_rw"] = A(g("router_w")[i])
        m[f"m{i}_rb"] = A(g("router_b")[i].reshape(1, 32))
        w1 = g("exp_w1")[i]
        w5 = w1.reshape(32, D // 128, 128, FC, 128, 2)
        m[f"m{i}_w1gT"] = A(w5[..., 0].transpose(0, 3, 2, 1, 4).reshape(32 * FC * 128, (D // 128) * 128))
        m[f"m{i}_w1lT"] = A(w5[..., 1].transpose(0, 3, 2, 1, 4).reshape(32 * FC * 128, (D // 128) * 128))
        m[f"m{i}_w2T"] = A(g("exp_w2")[i].reshape(32 * FFE, D))
        b1 = g("exp_b1")[i].reshape(32, FC, 128, 2)
        m[f"m{i}_b1gT"] = A(b1[..., 0].transpose(0, 2, 1).reshape(32 * 128, FC))
        m[f"m{i}_b1lT"] = A(b1[..., 1].transpose(0, 2, 1).reshape(32 * 128, FC))
        m[f"m{i}_b2"] = A(g("exp_b2")[i])
    tri = np.tril(np.ones((128, 128), f32))
    m["k_ident"] = np.eye(128, dtype=f32)
    m["k_ones"] = np.ones((128, 128), f32)
    m["k_trif"] = A(tri.T)
    m["k_trib"] = A(tri)
    m["k_iota_p"] = np.arange(128, dtype=f32).reshape(128, 1)
    NTOK = nctx + seq
    NB0 = (4 * NTOK + 511) // 512 + 32
    NB1 = (4 * seq + 511) // 512 + 32
    m["k_iota_b0"] = np.arange(NB0, dtype=f32).reshape(1, NB0)
    m["k_iota_b1"] = np.arange(NB1, dtype=f32).reshape(1, NB1)
    return m


def kernel(**inputs):
    c = build_program(nbatch=1)
    m = host_inputs(inputs)
    f32 = np.float32
    x = np.asarray(inputs["x"], dtype=f32)
    ctx = np.asarray(inputs["ctx"], dtype=f32)
    cc = np.asarray(inputs["c"], dtype=f32)
    c_ctx = np.asarray(inputs["c_ctx"], dtype=f32).reshape(1, D_MODEL)
    in_maps = []
    for b in range(NBATCH):
        mb = dict(m)
        mb["x"] = np.ascontiguousarray(x[b])
        mb["ctx"] = np.ascontiguousarray(ctx[b])
        mb["crow"] = np.ascontiguousarray(np.concatenate([cc[b:b + 1], cc[b:b + 1], c_ctx], 0))
        in_maps.append(mb)
    res = run_bass_kernel_spmd(c.nc, in_maps, core_ids=list(range(NBATCH)))
    out = np.stack([np.asarray(res.results[b]["out"], dtype=f32) for b in range(NBATCH)], 0)
    return out.reshape(NBATCH, SEQ, D_MODEL)
```

```python
import contextlib
import numpy as np
import concourse.bass as bass
import concourse.mybir as mybir
from concourse.bass_utils import run_bass_kernel_spmd

F32 = mybir.dt.float32
BF16 = mybir.dt.bfloat16
I32 = mybir.dt.int32
AF = mybir.ActivationFunctionType
ALU = mybir.AluOpType
AX = mybir.AxisListType

NDSEM = 16


class Ctx:
    def __init__(self):
        nc = bass.Bass("TRN2", target_bir_lowering=False)
        self.nc = nc
        self.E = {"pe": nc.tensor, "act": nc.scalar, "dve": nc.vector, "pool": nc.gpsimd, "sp": nc.sync}
        self.csem = {e: nc.alloc_semaphore(name=f"c_{e}") for e in ("pe", "act", "dve", "pool")}
        self.ccount = {e: 0 for e in self.csem}
        self.dsem = {q: [nc.alloc_semaphore(name=f"d_{q}{i}") for i in range(NDSEM)] for q in ("sp", "pool")}
        self.dval = {q: [0] * NDSEM for q in self.dsem}
        self.dnext = {q: 0 for q in self.dsem}
        self.ccsem = nc.alloc_semaphore(name="cc")
        self.ccval = 0
        self.known = {e: {} for e in self.E}
        self.lastw = {}
        self.readers = {}
        self.uid = 0
        self.n_instr = 0

    def name(self, base):
        self.uid += 1
        return f"{base}_{self.uid}"

    def dram(self, name, shape, dtype, kind="Internal"):
        key = _re.sub(r"^b\d", "", name) if kind == "Internal" and not name.endswith("RES") else name
        if not hasattr(self, "_dcache"):
            self._dcache = {}
        if key in self._dcache:
            return self._dcache[key]
        ap = self.nc.dram_tensor(key, list(shape), dtype, kind=kind).ap()
        self._dcache[key] = ap
        return ap

    def _deps(self, eng, reads, writes):
        need = []
        for r in reads:
            w = self.lastw.get(r)
            if w is not None:
                need.append(w)
        for r in writes:
            w = self.lastw.get(r)
            if w is not None:
                need.append(w)
            rd = self.readers.get(r)
            if rd:
                need.extend(rd.values())
        out = {}
        kn = self.known[eng]
        for (sem, val, src) in need:
            if src == "pe" and eng == "pe":
                continue
            key = id(sem)
            if kn.get(key, 0) >= val:
                continue
            if key not in out or out[key][1] < val:
                out[key] = (sem, val)
        for key, (sem, val) in out.items():
            kn[key] = val
            self.E[eng].wait_ge(sem, val)
            self.n_instr += 1

    def _record(self, rec, reads, writes):
        for r in reads:
            d = self.readers.setdefault(r, {})
            k = id(rec[0])
            if k not in d or d[k][1] < rec[1]:
                d[k] = rec
        for r in writes:
            self.lastw[r] = rec
            self.readers[r] = {}

    def op(self, eng, fn, reads=(), writes=(), inc=True):
        self._deps(eng, reads, writes)
        ins = fn(self.E[eng])
        self.n_instr += 1
        sem = self.csem[eng]
        if inc:
            self.ccount[eng] += 1
            ins.then_inc(sem, 1)
            rec = (sem, self.ccount[eng], eng)
        else:
            rec = (sem, self.ccount[eng] + 1, eng)
        self._record(rec, reads, writes)
        return ins

    def dma(self, q, out, in_, reads=(), writes=(), **kw):
        i = self.dnext[q]
        self.dnext[q] = (i + 1) % NDSEM
        sem = self.dsem[q][i]
        kn = self.known[q]
        if kn.get(id(sem), 0) < self.dval[q][i]:
            self.E[q].wait_ge(sem, self.dval[q][i])
            kn[id(sem)] = self.dval[q][i]
            self.n_instr += 1
        self._deps(q, reads, writes)
        ins = self.E[q].dma_start(out=out, in_=in_, **kw)
        self.n_instr += 1
        self.dval[q][i] += 16
        ins.then_inc(sem, 16)
        rec = (sem, self.dval[q][i], "dma_" + q)
        self._record(rec, reads, writes)
        return ins

    def idma(self, out, out_off, in_, in_off, reads=(), writes=(), **kw):
        q = "pool"
        i = self.dnext[q]
        self.dnext[q] = (i + 1) % NDSEM
        sem = self.dsem[q][i]
        kn = self.known[q]
        if kn.get(id(sem), 0) < self.dval[q][i]:
            self.E[q].wait_ge(sem, self.dval[q][i])
            kn[id(sem)] = self.dval[q][i]
        self._deps(q, reads, writes)
        ins = self.nc.gpsimd.indirect_dma_start(out=out, out_offset=out_off, in_=in_, in_offset=in_off, **kw)
        self.n_instr += 1
        self.dval[q][i] += 16
        ins.then_inc(sem, 16)
        rec = (sem, self.dval[q][i], "dma_" + q)
        self._record(rec, reads, writes)
        return ins

    def collective(self, kind, op, groups, in_ap, out_ap, reads=(), writes=()):
        q = "pool"
        self._deps(q, reads, writes)
        ins = self.nc.gpsimd.collective_compute(kind, op, replica_groups=groups, ins=[in_ap], outs=[out_ap])
        self.ccval += 1
        ins.then_inc(self.ccsem)
        self.nc.gpsimd.wait_ge(self.ccsem, self.ccval)
        self.known[q][id(self.ccsem)] = self.ccval
        rec = (self.ccsem, self.ccval, "cc")
        self._record(rec, reads, writes)
        return ins

    def barrier(self):
        for e in self.E:
            kn = self.known[e]
            eng = self.E[e]
            for f, sem in self.csem.items():
                if self.ccount[f] > kn.get(id(sem), 0):
                    eng.wait_ge(sem, self.ccount[f])
                    kn[id(sem)] = self.ccount[f]
            for q in self.dsem:
                for i, sem in enumerate(self.dsem[q]):
                    if self.dval[q][i] > kn.get(id(sem), 0):
                        eng.wait_ge(sem, self.dval[q][i])
                        kn[id(sem)] = self.dval[q][i]
            if self.ccval > kn.get(id(self.ccsem), 0):
                eng.wait_ge(self.ccsem, self.ccval)
                kn[id(self.ccsem)] = self.ccval

    def finish(self):
        sp = self.E["sp"]
        kn = self.known["sp"]
        for q in self.dsem:
            for i, sem in enumerate(self.dsem[q]):
                if self.dval[q][i] > kn.get(id(sem), 0):
                    sp.wait_ge(sem, self.dval[q][i])
        for e, sem in self.csem.items():
            if self.ccount[e] > kn.get(id(sem), 0):
                sp.wait_ge(sem, self.ccount[e])
        if self.ccval:
            sp.wait_ge(self.ccsem, self.ccval)


def bcast_rows(ap_row, nparts=128):
    return ap_row.partition_broadcast(nparts)


def _mk(c):
    return c


def mm(c, out, lhsT, rhs, start, stop, reads, writes, inc=None):
    inc = stop if inc is None else inc
    return c.op("pe", lambda e: e.matmul(out=out, lhsT=lhsT, rhs=rhs, start=start, stop=stop), reads, writes, inc=inc)


def tr(c, out, in_, ident, reads, writes, inc=True):
    return c.op("pe", lambda e: e.transpose(out=out, in_=in_, identity=ident), reads, writes, inc=inc)


def act(c, out, in_, func, reads, writes, **kw):
    return c.op("act", lambda e: e.activation(out=out, in_=in_, func=func, **kw), reads, writes)


def cp(c, eng, out, in_, reads, writes):
    if eng == "act":
        return c.op("act", lambda e: e.copy(out=out, in_=in_), reads, writes)
    return c.op(eng, lambda e: e.tensor_copy(out=out, in_=in_), reads, writes)


def tt(c, eng, out, in0, in1, op, reads, writes):
    return c.op(eng, lambda e: e.tensor_tensor(out=out, in0=in0, in1=in1, op=op), reads, writes)


def ts(c, eng, out, in0, s1, s2, op0, op1, reads, writes, accum_out=None):
    if op1 is None:
        return c.op(eng, lambda e: e.tensor_scalar(out=out, in0=in0, scalar1=s1, scalar2=None, op0=op0), reads, writes)
    if accum_out is not None:
        return c.op(eng, lambda e: e.tensor_scalar(out=out, in0=in0, scalar1=s1, scalar2=s2, op0=op0, op1=op1, accum_out=accum_out), reads, writes)
    return c.op(eng, lambda e: e.tensor_scalar(out=out, in0=in0, scalar1=s1, scalar2=s2, op0=op0, op1=op1), reads, writes)


def stt(c, eng, out, in0, scalar, in1, op0, op1, reads, writes):
    return c.op(eng, lambda e: e.scalar_tensor_tensor(out=out, in0=in0, scalar=scalar, in1=in1, op0=op0, op1=op1), reads, writes)


def red(c, eng, out, in_, op, reads, writes, axis=None):
    axis = AX.X if axis is None else axis
    return c.op(eng, lambda e: e.tensor_reduce(out=out, in_=in_, axis=axis, op=op), reads, writes)


def mset(c, eng, ap, val, writes):
    return c.op(eng, lambda e: e.memset(ap, val), (), writes)


def rmsnorm_rstd(c, x_ap, junk_ap, ss_ap, D, reads, tag, eps=1e-6):
    act(c, junk_ap, x_ap, AF.Square, reads, [tag + "_junk", tag + "_ss0"], scale=float(D) ** -0.5, accum_out=ss_ap[:, 0:1])
    act(c, ss_ap[:, 1:2], ss_ap[:, 0:1], AF.Sqrt, [tag + "_ss0"], [tag + "_ss1"], bias=eps)
    c.op("dve", lambda e: e.reciprocal(out=ss_ap[:, 1:2], in_=ss_ap[:, 1:2]), [tag + "_ss1"], [tag + "_ss1"])
    return ss_ap[:, 1:2]


G4, J8, HP, NS = 4, 8, 64, 128
SW = 2048
XW = 3072
GW = 2048
HIN = 9280


def proj_rows(c, T, tag, D, src, tiles, mods, Wd, ncols, outs, K, ST=1024, src_res="RES"):
    nc = c.nc
    KC = D // 128
    with contextlib.ExitStack() as es:
        sb = lambda n, s, d: es.enter_context(nc.sbuf_tensor(T(tag + n), s, d))
        ps = lambda n, s, d: es.enter_context(nc.psum_tensor(T(tag + n), s, d))
        idt = sb("idt", [128, 128], F32)
        At = sb("At", [128, D], F32)
        Bt = sb("Bt", [128, D], F32)
        xt = [sb(f"xt{i}", [128, D], F32) for i in range(2)]
        junk = sb("junk", [128, D], F32)
        ss = sb("ss", [128, 2], F32)
        h = sb("h", [128, D], F32)
        hT = sb("hT", [128, KC, ST], BF16)
        wt = [sb(f"wt{i}", [128, KC, 512], BF16) for i in range(2)]
        ot = [sb(f"ot{i}", [128, 512], F32) for i in range(3)]
        pT = [ps(f"pT{i}", [128, 512], F32) for i in range(2)]
        pm = [ps(f"pm{i}", [128, 512], F32) for i in range(3)]
        R = lambda s: T(tag + s)
        c.dma("sp", idt[:], K["ident"][:, :], writes=[R("idt")])
        sts = []
        cur = []
        curn = 0
        for tl in tiles:
            if curn + tl[1] > ST:
                sts.append(cur)
                cur, curn = [], 0
            cur.append(tl)
            curn += tl[1]
        if cur:
            sts.append(cur)
        cur_ms = None
        ti = 0
        oi = 0
        for st_tiles in sts:
            off = 0
            offs = []
            for (r0, n, ms) in st_tiles:
                b = ti % 2
                ti += 1
                if ms != cur_ms:
                    c.dma("sp", At[:], mods[ms]["A"].partition_broadcast(128), reads=["MODS"], writes=[R("At")])
                    c.dma("sp", Bt[:], mods[ms]["B"].partition_broadcast(128), reads=["MODS"], writes=[R("Bt")])
                    cur_ms = ms
                c.dma("sp", xt[b][:n], src[r0:r0 + n, :], reads=[src_res], writes=[R(f"xt{b}")])
                rstd = rmsnorm_rstd(c, xt[b][:n], junk[:n], ss[:n], D, [R(f"xt{b}")], R("n"))
                stt(c, "dve", h[:n], xt[b][:n], rstd[:n], At[:n], ALU.mult, ALU.mult, [R(f"xt{b}"), R("n_ss1"), R("At")], [R("h")])
                tt(c, "pool", h[:n], h[:n], Bt[:n], ALU.add, [R("h"), R("Bt")], [R("h")])
                gsz = 4 if KC % 4 == 0 else 2
                for kg in range(KC // gsz):
                    pb = kg % 2
                    for jj in range(gsz):
                        k = kg * gsz + jj
                        tr(c, pT[pb][:, jj * 128:jj * 128 + n], h[:n, k * 128:(k + 1) * 128], idt[:n, :n],
                           [R("h"), R("idt")], [R(f"pT{pb}")], inc=(jj == gsz - 1))
                    cp(c, "act" if kg % 2 == 0 else "dve", hT[:, kg * gsz:(kg + 1) * gsz, off:off + n],
                       pT[pb][:, 0:gsz * 128].rearrange("p (a b) -> p a b", a=gsz)[:, :, :n], [R(f"pT{pb}")], [R("hT")])
                offs.append(off)
                off += n
            nblk = (ncols + 511) // 512
            for nb in range(nblk):
                c0 = nb * 512
                w = min(512, ncols - c0)
                wb = nb % 2
                c.dma("pool", wt[wb][:, :, :w], Wd[:, c0:c0 + w].rearrange("(k p) n -> p k n", p=128), writes=[R(f"wt{wb}")])
                for (r0, n, ms), o in zip(st_tiles, offs):
                    pb = oi % 3
                    oi += 1
                    for k in range(KC):
                        mm(c, pm[pb][:n, :w], hT[:, k, o:o + n], wt[wb][:, k, :w], k == 0, k == KC - 1, [R("hT"), R(f"wt{wb}")], [R(f"pm{pb}")])
                    cp(c, "act" if pb != 1 else "dve", ot[pb][:n, :w], pm[pb][:n, :w], [R(f"pm{pb}")], [R(f"ot{pb}")])
                    for (d0, d1, dst, rofs, res) in outs:
                        lo = max(c0, d0)
                        hi = min(c0 + w, d1)
                        if lo < hi:
                            rr = rofs(r0)
                            c.dma("sp", dst[rr:rr + n, lo - d0:hi - d0], ot[pb][:n, lo - c0:hi - c0], reads=[R(f"ot{pb}")], writes=[res])
    c.barrier()


def hybrid_phase(c, tag, D, NCTX, NLAT, RES, mods, W, K, dbg=None):
    nc = c.nc
    T = lambda s: f"{tag}_{s}"
    NTOK = NCTX + NLAT
    NCH = NTOK // 128
    PZ = c.dram(T("PZ"), [NTOK, SW], F32)
    PX = c.dram(T("PX"), [NTOK + 4, XW], F32)
    PD = c.dram(T("PD"), [NTOK, 64], F32)
    PU = c.dram(T("PU"), [NTOK, GW], F32)
    PV = c.dram(T("PV"), [NTOK, GW], F32)
    XS = c.dram(T("XS"), [NTOK, SW], F32)
    BM = c.dram(T("BM"), [NTOK, 512], BF16)
    CTd = c.dram(T("CTd"), [NCH * 128, 512], BF16)
    CBF = c.dram(T("CBF"), [NCH * 128, 512], F32)
    CBB = c.dram(T("CBB"), [NCH * 128, 512], F32)
    DT = c.dram(T("DT"), [NTOK, 64], F32)
    Y = c.dram(T("Y"), [NTOK, SW], F32)
    YG = c.dram(T("YG"), [NTOK, GW], F32)
    MIX = c.dram(T("MIX"), [NTOK, 2 * SW], BF16)
    YM = c.dram(T("YM"), [NTOK, D], F32)

    def pxrow(r):
        return r + 1 if r < NCTX else r + 3

    tiles = [(r0, 128, "c" if r0 < NCTX else "l") for r0 in range(0, NTOK, 128)]
    ident_rows = lambda r: r
    outs = [(0, SW, PZ, ident_rows, T("PZ")), (SW, SW + XW, PX, pxrow, T("PX")), (SW + XW, SW + XW + 64, PD, ident_rows, T("PD")),
            (SW + XW + 64, SW + XW + 64 + GW, PU, ident_rows, T("PU")), (SW + XW + 64 + GW, HIN, PV, ident_rows, T("PV"))]
    m1 = {ms: {"A": mods[ms]["A1"], "B": mods[ms]["B1"]} for ms in mods}
    proj_rows(c, T, "pj_", D, RES, tiles, m1, W["w_in"], HIN, outs, K)

    with contextlib.ExitStack() as es:
        sb = lambda n, s, d: es.enter_context(nc.sbuf_tensor(T(n), s, d))
        ps = lambda n, s, d: es.enter_context(nc.psum_tensor(T(n), s, d))
        idt = sb("c_idt", [128, 128], F32)
        trif = sb("c_trif", [128, 128], F32)
        trib = sb("c_trib", [128, 128], F32)
        cw = sb("c_cw", [128, 3, XW], F32)
        cb = sb("c_cb", [128, XW], F32)
        dtb = sb("c_dtb", [128, 64], F32)
        zr = sb("c_zr", [4, XW], F32)
        xp = [sb(f"c_xp{i}", [128, XW], F32) for i in range(2)]
        xc = [sb(f"c_xc{i}", [128, XW], F32) for i in range(2)]
        xn = [sb(f"c_xn{i}", [128, XW], F32) for i in range(2)]
        acc = sb("c_acc", [128, XW], F32)
        sg = sb("c_sg", [128, XW], F32)
        bmb = sb("c_bmb", [128, 512], BF16)
        btb = sb("c_btb", [128, 512], BF16)
        ctb = sb("c_ctb", [128, 512], BF16)
        cbf = sb("c_cbf", [128, 512], F32)
        cbb = sb("c_cbb", [128, 512], F32)
        dtt = sb("c_dtt", [128, 64], F32)
        pB = ps("c_pB", [128, 512], F32)
        pC = ps("c_pC", [128, 512], F32)
        pCB = ps("c_pCB", [128, 512], F32)
        c.dma("sp", idt[:], K["ident"][:, :], writes=[T("c_idt")])
        c.dma("sp", trif[:], K["trif"][:, :], writes=[T("c_trif")])
        c.dma("sp", trib[:], K["trib"][:, :], writes=[T("c_trib")])
        for i in range(3):
            c.dma("sp", cw[:, i, :], W["conv_w"][i:i + 1, :].partition_broadcast(128), writes=[T("c_cw")])
        c.dma("sp", cb[:], W["conv_b"].partition_broadcast(128), writes=[T("c_cb")])
        c.dma("sp", dtb[:], W["dtb"].partition_broadcast(128), writes=[T("c_dtb")])
        mset(c, "pool", zr[:], 0.0, [T("c_zr")])
        for zrow in (0, NCTX + 1, NCTX + 2, NTOK + 3):
            c.dma("sp", PX[zrow:zrow + 1, :], zr[0:1, :], reads=[T("c_zr")], writes=[T("PX")])
        for ch in range(NCH):
            b = ch % 2
            r0 = ch * 128
            p0 = pxrow(r0)
            c.dma("sp", xp[b][:], PX[p0 - 1:p0 + 127, :], reads=[T("PX")], writes=[T(f"c_xp{b}")])
            c.dma("sp", xc[b][:], PX[p0:p0 + 128, :], reads=[T("PX")], writes=[T(f"c_xc{b}")])
            c.dma("sp", xn[b][:], PX[p0 + 1:p0 + 129, :], reads=[T("PX")], writes=[T(f"c_xn{b}")])
            tt(c, "dve", acc[:], xc[b][:], cw[:, 1, :], ALU.mult, [T(f"c_xc{b}"), T("c_cw")], [T("c_acc")])
            tt(c, "pool", xp[b][:], xp[b][:], cw[:, 0, :], ALU.mult, [T(f"c_xp{b}"), T("c_cw")], [T(f"c_xp{b}")])
            tt(c, "pool", xn[b][:], xn[b][:], cw[:, 2, :], ALU.mult, [T(f"c_xn{b}"), T("c_cw")], [T(f"c_xn{b}")])
            tt(c, "dve", acc[:], acc[:], cb[:], ALU.add, [T("c_acc"), T("c_cb")], [T("c_acc")])
            tt(c, "dve", acc[:], acc[:], xp[b][:], ALU.add, [T("c_acc"), T(f"c_xp{b}")], [T("c_acc")])
            tt(c, "dve", acc[:], acc[:], xn[b][:], ALU.add, [T("c_acc"), T(f"c_xn{b}")], [T("c_acc")])
            act(c, sg[:], acc[:], AF.Silu, [T("c_acc")], [T("c_sg")])
            c.dma("sp", XS[r0:r0 + 128, :], sg[:, 0:SW], reads=[T("c_sg")], writes=[T("XS")])
            cp(c, "pool", bmb[:], sg[:, SW:SW + 512], [T("c_sg")], [T("c_bmb")])
            c.dma("sp", BM[r0:r0 + 128, :], bmb[:], reads=[T("c_bmb")], writes=[T("BM")])
            for g in range(G4):
                tr(c, pB[:, g * 128:(g + 1) * 128], sg[:, SW + g * 128:SW + (g + 1) * 128], idt[:], [T("c_sg"), T("c_idt")], [T("c_pB")], inc=(g == 3))
            for g in range(G4):
                tr(c, pC[:, g * 128:(g + 1) * 128], sg[:, SW + 512 + g * 128:SW + 512 + (g + 1) * 128], idt[:], [T("c_sg"), T("c_idt")], [T("c_pC")], inc=(g == 3))
            cp(c, "act", btb[:], pB[:], [T("c_pB")], [T("c_btb")])
            cp(c, "act", ctb[:], pC[:], [T("c_pC")], [T("c_ctb")])
            c.dma("sp", CTd[r0:r0 + 128, :], ctb[:], reads=[T("c_ctb")], writes=[T("CTd")])
            for g in range(G4):
                mm(c, pCB[:, g * 128:(g + 1) * 128], btb[:, g * 128:(g + 1) * 128], ctb[:, g * 128:(g + 1) * 128], True, True,
                   [T("c_btb"), T("c_ctb")], [T("c_pCB")], inc=(g == 3))
            tt(c, "dve", cbf[:].rearrange("p (g q) -> p g q", g=4), pCB[:].rearrange("p (g q) -> p g q", g=4),
               trif[:].unsqueeze(1).to_broadcast([128, 4, 128]), ALU.mult, [T("c_pCB"), T("c_trif")], [T("c_cbf")])
            tt(c, "dve", cbb[:].rearrange("p (g q) -> p g q", g=4), pCB[:].rearrange("p (g q) -> p g q", g=4),
               trib[:].unsqueeze(1).to_broadcast([128, 4, 128]), ALU.mult, [T("c_pCB"), T("c_trib")], [T("c_cbb")])
            c.dma("sp", CBF[r0:r0 + 128, :], cbf[:], reads=[T("c_cbf")], writes=[T("CBF")])
            c.dma("sp", CBB[r0:r0 + 128, :], cbb[:], reads=[T("c_cbb")], writes=[T("CBB")])
            c.dma("sp", dtt[:], PD[r0:r0 + 128, :], reads=[T("PD")], writes=[T("c_dtt")])
            tt(c, "dve", dtt[:], dtt[:], dtb[:], ALU.add, [T("c_dtt"), T("c_dtb")], [T("c_dtt")])
            act(c, dtt[:], dtt[:], AF.Exp, [T("c_dtt")], [T("c_dtt")])
            act(c, dtt[:], dtt[:], AF.Ln, [T("c_dtt")], [T("c_dtt")], bias=1.0)
            c.dma("sp", DT[r0:r0 + 128, :], dtt[:], reads=[T("c_dtt")], writes=[T("DT")])
    c.barrier()

    ctx_ch = list(range(NCTX // 128))
    lat_ch = list(range(NCTX // 128, NCH))
    with contextlib.ExitStack() as es:
        sb = lambda n, s, d: es.enter_context(nc.sbuf_tensor(T(n), s, d))
        ps = lambda n, s, d: es.enter_context(nc.psum_tensor(T(n), s, d))
        ones = sb("s_ones", [128, 128], F32)
        tri = [sb("s_trif", [128, 128], F32), sb("s_trib", [128, 128], F32)]
        aneg = sb("s_aneg", [128, 64], F32)
        dsk = sb("s_dsk", [128, 32], F32)
        xs = [sb(f"s_xs{i}", [128, SW], F32) for i in range(2)]
        bm = [sb(f"s_bm{i}", [128, 512], BF16) for i in range(2)]
        ct = [sb(f"s_ct{i}", [128, 512], BF16) for i in range(2)]
        cbm = [sb(f"s_cbm{i}", [128, 512], F32) for i in range(2)]
        dtt = [sb(f"s_dt{i}", [128, 64], F32) for i in range(2)]
        a = sb("s_a", [128, 32], F32)
        acs = sb("s_acs", [128, 32], F32)
        tot = sb("s_tot", [128, 32], F32)
        dend = sb("s_dend", [128, 32], F32)
        eacs = sb("s_eacs", [128, 32], F32)
        cdec = sb("s_cdec", [128, 32], F32)
        xdt = sb("s_xdt", [128, SW], BF16)
        xdtd = sb("s_xdtd", [128, SW], BF16)
        X4 = [sb(f"s_X4{i}", [128, 512], F32) for i in range(2)]
        seg = [sb(f"s_seg{i}", [128, 512], F32) for i in range(2)]
        Lx = [sb(f"s_Lx{i}", [128, 512], F32) for i in range(2)]
        MT = [sb(f"s_MT{i}", [128, 512], BF16) for i in range(2)]
        Hs = sb("s_H", [128, G4 * 512], F32)
        Hb = sb("s_Hb", [128, G4 * 512], BF16)
        yo = sb("s_yo", [128, SW], F32)
        yacc = [sb(f"s_yacc{i}", [128, SW], F32) for i in range(2)]
        pa = ps("s_pa", [128, 64], F32)
        pR = [ps(f"s_pR{i}", [128, 512], F32) for i in range(2)]
        pY = ps("s_pY", [128, SW], F32)
        pO = ps("s_pO", [128, 512], F32)
        c.dma("sp", ones[:], K["ones"][:, :], writes=[T("s_ones")])
        c.dma("sp", tri[0][:], K["trif"][:, :], writes=[T("s_trif")])
        c.dma("sp", tri[1][:], K["trib"][:, :], writes=[T("s_trib")])
        c.dma("sp", aneg[:], W["alog"].partition_broadcast(128), writes=[T("s_aneg")])
        act(c, aneg[:], aneg[:], AF.Exp, [T("s_aneg")], [T("s_aneg")])
        ts(c, "dve", aneg[:], aneg[:], -1.0, None, ALU.mult, None, [T("s_aneg")], [T("s_aneg")])
        c.dma("sp", dsk[:], W["dsk"].partition_broadcast(128), writes=[T("s_dsk")])
        it = 0
        for d in range(2):
            CBd = CBF if d == 0 else CBB
            order = (ctx_ch + lat_ch) if d == 0 else (ctx_ch[::-1] + lat_ch[::-1])
            mset(c, "pool", Hs[:], 0.0, [T("s_H")])
            mset(c, "pool", Hb[:], 0.0, [T("s_Hb")])
            for ch in order:
                b = it % 2
                it += 1
                r0 = ch * 128
                c.dma("sp", xs[b][:], XS[r0:r0 + 128, :], reads=[T("XS")], writes=[T(f"s_xs{b}")])
                c.dma("sp", bm[b][:], BM[r0:r0 + 128, :], reads=[T("BM")], writes=[T(f"s_bm{b}")])
                c.dma("sp", ct[b][:], CTd[r0:r0 + 128, :], reads=[T("CTd")], writes=[T(f"s_ct{b}")])
                c.dma("sp", cbm[b][:], CBd[r0:r0 + 128, :], reads=[T("CBF"), T("CBB")], writes=[T(f"s_cbm{b}")])
                c.dma("sp", dtt[b][:], DT[r0:r0 + 128, :], reads=[T("DT")], writes=[T(f"s_dt{b}")])
                dtd = dtt[b][:, d * 32:(d + 1) * 32]
                tt(c, "dve", a[:], dtd, aneg[:, d * 32:(d + 1) * 32], ALU.mult, [T(f"s_dt{b}"), T("s_aneg")], [T("s_a")])
                mm(c, pa[:, 0:32], tri[d][:], a[:], True, True, [T(f"s_tri{'fb'[d]}"), T("s_a")], [T("s_pa")])
                mm(c, pa[:, 32:64], ones[:], a[:], True, True, [T("s_ones"), T("s_a")], [T("s_pa")])
                cp(c, "dve", acs[:], pa[:, 0:32], [T("s_pa")], [T("s_acs")])
                cp(c, "dve", tot[:], pa[:, 32:64], [T("s_pa")], [T("s_tot")])
                tt(c, "dve", dend[:], tot[:], acs[:], ALU.subtract, [T("s_tot"), T("s_acs")], [T("s_dend")])
                act(c, dend[:], dend[:], AF.Exp, [T("s_dend")], [T("s_dend")])
                act(c, eacs[:], acs[:], AF.Exp, [T("s_acs")], [T("s_eacs")])
                act(c, cdec[:], tot[:], AF.Exp, [T("s_tot")], [T("s_cdec")])
                tt(c, "pool", xdt[:].rearrange("p (h e) -> p h e", e=HP), xs[b][:].rearrange("p (h e) -> p h e", e=HP),
                   dtd.unsqueeze(2).to_broadcast([128, 32, HP]), ALU.mult, [T(f"s_xs{b}"), T(f"s_dt{b}")], [T("s_xdt")])
                tt(c, "pool", xdtd[:].rearrange("p (h e) -> p h e", e=HP), xdt[:].rearrange("p (h e) -> p h e", e=HP),
                   dend[:].unsqueeze(2).to_broadcast([128, 32, HP]), ALU.mult, [T("s_xdt"), T("s_dend")], [T("s_xdtd")])
                for hq in range(8):
                    g = hq // 2
                    q2 = hq % 2
                    tt(c, "dve", X4[q2][:].rearrange("p (h q) -> p h q", h=4), tri[d][:].unsqueeze(1).to_broadcast([128, 4, 128]),
                       a[:, hq * 4:(hq + 1) * 4].unsqueeze(2).to_broadcast([128, 4, 128]), ALU.mult,
                       [T(f"s_tri{'fb'[d]}"), T("s_a")], [T(f"s_X4{q2}")])
                    mm(c, pR[q2][:], ones[:], X4[q2][:], True, True, [T("s_ones"), T(f"s_X4{q2}")], [T(f"s_pR{q2}")])
                    for j4 in range(4):
                        hh = hq * 4 + j4
                        ts(c, "dve", seg[q2][:, j4 * 128:(j4 + 1) * 128], pR[q2][:, j4 * 128:(j4 + 1) * 128], acs[:, hh:hh + 1], 0.0,
                           ALU.subtract, ALU.min, [T(f"s_pR{q2}"), T("s_acs")], [T(f"s_seg{q2}")])
                    act(c, Lx[q2][:], seg[q2][:], AF.Exp, [T(f"s_seg{q2}")], [T(f"s_Lx{q2}")])
                    tt(c, "pool", MT[q2][:].rearrange("p (h q) -> p h q", h=4), Lx[q2][:].rearrange("p (h q) -> p h q", h=4),
                       cbm[b][:, g * 128:(g + 1) * 128].unsqueeze(1).to_broadcast([128, 4, 128]), ALU.mult,
                       [T(f"s_Lx{q2}"), T(f"s_cbm{b}")], [T(f"s_MT{q2}")])
                    for j4 in range(4):
                        hh = hq * 4 + j4
                        mm(c, pY[:, hh * HP:(hh + 1) * HP], MT[q2][:, j4 * 128:(j4 + 1) * 128], xdt[:, hh * HP:(hh + 1) * HP], True, True,
                           [T(f"s_MT{q2}"), T("s_xdt")], [T("s_pY")], inc=(j4 == 3))
                if d == 0:
                    tt(c, "pool", yacc[b][:].rearrange("p (h e) -> p h e", e=HP), xs[b][:].rearrange("p (h e) -> p h e", e=HP),
                       dsk[:].unsqueeze(2).to_broadcast([128, 32, HP]), ALU.mult, [T(f"s_xs{b}"), T("s_dsk")], [T(f"s_yacc{b}")])
                else:
                    c.dma("sp", yacc[b][:], Y[r0:r0 + 128, :], reads=[T("Y")], writes=[T(f"s_yacc{b}")])
                for g in range(G4):
                    mm(c, pO[:], ct[b][:, g * 128:(g + 1) * 128], Hb[:, g * 512:(g + 1) * 512], True, True, [T(f"s_ct{b}"), T("s_Hb")], [T("s_pO")])
                    tt(c, "dve", yo[:, g * 512:(g + 1) * 512].rearrange("p (h e) -> p h e", e=HP), pO[:].rearrange("p (h e) -> p h e", e=HP),
                       eacs[:, g * 8:(g + 1) * 8].unsqueeze(2).to_broadcast([128, 8, HP]), ALU.mult, [T("s_pO"), T("s_eacs")], [T("s_yo")])
                tt(c, "dve", yo[:], yo[:], pY[:], ALU.add, [T("s_yo"), T("s_pY")], [T("s_yo")])
                tt(c, "pool", yacc[b][:], yacc[b][:], yo[:], ALU.add, [T(f"s_yacc{b}"), T("s_yo")], [T(f"s_yacc{b}")])
                c.dma("sp", Y[r0:r0 + 128, :], yacc[b][:], reads=[T(f"s_yacc{b}")], writes=[T("Y")])
                for g in range(G4):
                    mm(c, pO[:], bm[b][:, g * 128:(g + 1) * 128], xdtd[:, g * 512:(g + 1) * 512], True, True, [T(f"s_bm{b}"), T("s_xdtd")], [T("s_pO")])
                    tt(c, "dve", Hs[:, g * 512:(g + 1) * 512].rearrange("p (h e) -> p h e", e=HP),
                       Hs[:, g * 512:(g + 1) * 512].rearrange("p (h e) -> p h e", e=HP),
                       cdec[:, g * 8:(g + 1) * 8].unsqueeze(2).to_broadcast([128, 8, HP]), ALU.mult, [T("s_H"), T("s_cdec")], [T("s_H")])
                    tt(c, "dve", Hs[:, g * 512:(g + 1) * 512], Hs[:, g * 512:(g + 1) * 512], pO[:], ALU.add, [T("s_H"), T("s_pO")], [T("s_H")])
                cp(c, "act", Hb[:], Hs[:], [T("s_H")], [T("s_Hb")])
    c.barrier()

    with contextlib.ExitStack() as es:
        sb = lambda n, s, d: es.enter_context(nc.sbuf_tensor(T(n), s, d))
        ps = lambda n, s, d: es.enter_context(nc.psum_tensor(T(n), s, d))
        wst = sb("g_wst", [128, 16, 128], BF16)
        bst = sb("g_bst", [128, 16], F32)
        vng = sb("g_vng", [128, GW], F32)
        sng = sb("g_sng", [128, SW], F32)
        u = [sb(f"g_u{i}", [128, GW], F32) for i in range(2)]
        v = [sb(f"g_v{i}", [128, GW], F32) for i in range(2)]
        z = [sb(f"g_z{i}", [128, SW], F32) for i in range(2)]
        y = [sb(f"g_y{i}", [128, SW], F32) for i in range(2)]
        junk = sb("g_junk", [128, GW], F32)
        ss = sb("g_ss", [128, 2], F32)
        ss2 = sb("g_ss2", [128, 2], F32)
        vn = sb("g_vn", [128, GW], BF16)
        mix = [sb(f"g_mix{i}", [128, 2 * SW], BF16) for i in range(2)]
        sgm = sb("g_sgm", [128, GW], F32)
        pS = ps("g_pS", [128, GW], F32)
        c.dma("pool", wst[:], W["wsT"].rearrange("g k q -> k g q"), writes=[T("g_wst")])
        c.dma("sp", bst[:], W["bsT"][:, :], writes=[T("g_bst")])
        c.dma("sp", vng[:], W["vng"].partition_broadcast(128), writes=[T("g_vng")])
        c.dma("sp", sng[:], W["ssdg"].partition_broadcast(128), writes=[T("g_sng")])
        for ch in range(NCH):
            b = ch % 2
            r0 = ch * 128
            c.dma("sp", u[b][:], PU[r0:r0 + 128, :], reads=[T("PU")], writes=[T(f"g_u{b}")])
            c.dma("sp", v[b][:], PV[r0:r0 + 128, :], reads=[T("PV")], writes=[T(f"g_v{b}")])
            c.dma("sp", z[b][:], PZ[r0:r0 + 128, :], reads=[T("PZ")], writes=[T(f"g_z{b}")])
            c.dma("sp", y[b][:], Y[r0:r0 + 128, :], reads=[T("Y")], writes=[T(f"g_y{b}")])
            act(c, u[b][:], u[b][:], AF.Gelu_apprx_tanh, [T(f"g_u{b}")], [T(f"g_u{b}")])
            act(c, v[b][:], v[b][:], AF.Gelu_apprx_tanh, [T(f"g_v{b}")], [T(f"g_v{b}")])
            rstd = rmsnorm_rstd(c, v[b][:], junk[:], ss[:], GW, [T(f"g_v{b}")], T("g_n1"))
            stt(c, "dve", vn[:], v[b][:], rstd, vng[:], ALU.mult, ALU.mult, [T(f"g_v{b}"), T("g_n1_ss1"), T("g_vng")], [T("g_vn")])
            for gg in range(16):
                mm(c, pS[:, gg * 128:(gg + 1) * 128], wst[:, gg, :], vn[:, gg * 128:(gg + 1) * 128], True, True, [T("g_wst"), T("g_vn")], [T("g_pS")], inc=(gg == 15))
            tt(c, "dve", sgm[:].rearrange("p (g e) -> p g e", g=16), pS[:].rearrange("p (g e) -> p g e", g=16),
               bst[:].unsqueeze(2).to_broadcast([128, 16, 128]), ALU.add, [T("g_pS"), T("g_bst")], [T("g_sgm")])
            tt(c, "pool", mix[b][:, SW:2 * SW], sgm[:], u[b][:], ALU.mult, [T("g_sgm"), T(f"g_u{b}")], [T(f"g_mix{b}")])
            act(c, z[b][:], z[b][:], AF.Silu, [T(f"g_z{b}")], [T(f"g_z{b}")])
            tt(c, "dve", y[b][:], y[b][:], z[b][:], ALU.mult, [T(f"g_y{b}"), T(f"g_z{b}")], [T(f"g_y{b}")])
            rstd2 = rmsnorm_rstd(c, y[b][:], junk[:], ss2[:], SW, [T(f"g_y{b}")], T("g_n2"))
            stt(c, "dve", mix[b][:, 0:SW], y[b][:], rstd2, sng[:], ALU.mult, ALU.mult, [T(f"g_y{b}"), T("g_n2_ss1"), T("g_sng")], [T(f"g_mix{b}")])
            c.dma("sp", MIX[r0:r0 + 128, :], mix[b][:], reads=[T(f"g_mix{b}")], writes=[T("MIX")])
    c.barrier()
    if dbg is not None:
        dbg(dict(Y=Y, MIX=MIX, PZ=PZ, XS=XS, DT=DT))
    out_proj_res(c, T, "op_", D, 2 * SW, MIX, W["w_out"], RES, tiles, {ms: mods[ms]["G1"] for ms in mods}, K)


def out_proj_res(c, T, tag, D, KIN, SRC, Wd, RES, tiles, gates, K, ST=1024):
    nc = c.nc
    KC = KIN // 128
    R = lambda s: T(tag + s)
    with contextlib.ExitStack() as es:
        sb = lambda n, s, d: es.enter_context(nc.sbuf_tensor(R(n), s, d))
        ps = lambda n, s, d: es.enter_context(nc.psum_tensor(R(n), s, d))
        idb = sb("idb", [128, 128], BF16)
        idf = sb("idf", [128, 128], F32)
        Gm = sb("Gm", [128, D], F32)
        xin = [sb(f"xin{i}", [128, KIN], BF16) for i in range(2)]
        xT = sb("xT", [128, KC, ST], BF16)
        wt = [sb(f"wt{i}", [128, KC, 512], BF16) for i in range(2)]
        rr = [sb(f"rr{i}", [128, 512], F32) for i in range(2)]
        ot = [sb(f"ot{i}", [128, 512], F32) for i in range(2)]
        ptr = [ps(f"ptr{i}", [128, 1024], BF16) for i in range(2)]
        pm = [ps(f"pm{i}", [128, 512], F32) for i in range(2)]
        c.dma("sp", idf[:], K["ident"][:, :], writes=[R("idf")])
        cp(c, "dve", idb[:], idf[:], [R("idf")], [R("idb")])
        sts = []
        cur, curn = [], 0
        for tl in tiles:
            if curn + tl[1] > ST:
                sts.append(cur)
                cur, curn = [], 0
            cur.append(tl)
            curn += tl[1]
        if cur:
            sts.append(cur)
        ti = 0
        oi = 0
        for st_tiles in sts:
            off = 0
            offs = []
            for (r0, n, ms) in st_tiles:
                b = ti % 2
                ti += 1
                c.dma("sp", xin[b][:n], SRC[r0:r0 + n, :], reads=[R("SRC")], writes=[R(f"xin{b}")])
                for kg in range(KC // 8):
                    pb = kg % 2
                    for jj in range(8):
                        k = kg * 8 + jj
                        tr(c, ptr[pb][:, jj * 128:jj * 128 + n], xin[b][:n, k * 128:(k + 1) * 128], idb[:n, :n], [R(f"xin{b}"), R("idb")], [R(f"ptr{pb}")], inc=(jj == 7))
                    cp(c, "act" if pb == 0 else "dve", xT[:, kg * 8:(kg + 1) * 8, off:off + n],
                       ptr[pb][:].rearrange("p (a b) -> p a b", a=8)[:, :, :n], [R(f"ptr{pb}")], [R("xT")])
                offs.append(off)
                off += n
            for nb in range(D // 512):
                wb = nb % 2
                c.dma("pool", wt[wb][:], Wd[:, nb * 512:(nb + 1) * 512].rearrange("(k p) n -> p k n", p=128), writes=[R(f"wt{wb}")])
                cur_ms = None
                for (r0, n, ms), o in zip(st_tiles, offs):
                    pb = oi % 2
                    oi += 1
                    if ms != cur_ms:
                        c.dma("sp", Gm[:], gates[ms].partition_broadcast(128), reads=["MODS"], writes=[R("Gm")])
                        cur_ms = ms
                    for k in range(KC):
                        mm(c, pm[pb][:n, :], xT[:, k, o:o + n], wt[wb][:, k, :], k == 0, k == KC - 1, [R("xT"), R(f"wt{wb}")], [R(f"pm{pb}")])
                    c.dma("sp", rr[pb][:n], RES[r0:r0 + n, nb * 512:(nb + 1) * 512], reads=["RES"], writes=[R(f"rr{pb}")])
                    tt(c, "dve", ot[pb][:n], pm[pb][:n, :], Gm[:n, nb * 512:(nb + 1) * 512], ALU.mult, [R(f"pm{pb}"), R("Gm")], [R(f"ot{pb}")])
                    tt(c, "pool", ot[pb][:n], ot[pb][:n], rr[pb][:n], ALU.add, [R(f"ot{pb}"), R(f"rr{pb}")], [R(f"ot{pb}")])
                    c.dma("sp", RES[r0:r0 + n, nb * 512:(nb + 1) * 512], ot[pb][:n], reads=[R(f"ot{pb}")], writes=["RES"])
    c.barrier()


NH = 16
QL, KVL, DN, DR, DV = 768, 512, 128, 64, 128
SCALE = (DN + DR) ** -0.5


def mla_phase(c, tag, D, NCTX, NLAT, RES, mods, W, K, dbg=None, upto=None):
    nc = c.nc
    T = lambda s: f"{tag}_{s}"
    NTOK = NCTX + NLAT
    NKT = NTOK // 128
    NQT = NLAT // 128
    PQ = c.dram(T("PQ"), [NTOK, QL], F32)
    PKV = c.dram(T("PKV"), [NTOK, KVL + DR], F32)
    QD = c.dram(T("QD"), [NTOK, NH * 192], F32)
    QH = c.dram(T("QH"), [NH, NLAT, 192], F32)
    QSQ = c.dram(T("QSQ"), [NLAT, NH], F32)
    OD = c.dram(T("OD"), [NTOK, NH * DV], BF16)
    tiles = [(r0, 128, "c" if r0 < NCTX else "l") for r0 in range(0, NTOK, 128)]
    lat_tiles = [t for t in tiles if t[2] == "l"]
    m1 = {ms: {"A": mods[ms]["A1"], "B": mods[ms]["B1"]} for ms in mods}
    proj_rows(c, T, "pj_", D, RES, tiles, m1, W["w_in"], QL + KVL + DR, [(0, QL, PQ, lambda r: r, T("PQ")), (QL, QL + KVL + DR, PKV, lambda r: r, T("PKV"))], K)
    mq = {"l": {"A": W["qng"], "B": W["zero768"]}}
    proj_rows(c, T, "pq_", QL, PQ, lat_tiles, mq, W["w_uq"], NH * 192, [(0, NH * 192, QD, lambda r: r, T("QD"))], K, src_res=T("PQ"))

    if upto == "proj":
        return
    with contextlib.ExitStack() as es0:
        sb0 = lambda n, s, d: es0.enter_context(nc.sbuf_tensor(T(n), s, d))
        ckvT = sb0("ckvT", [128, 4, NTOK], BF16)
        kpT = sb0("kpT", [65, NTOK], BF16)
        kpsq = sb0("kpsq", [128, NTOK], F32)
        idt = sb0("idt", [128, 128], F32)
        ones = sb0("ones", [128, 128], F32)
        c.dma("sp", idt[:], K["ident"][:, :], writes=[T("idt")])
        c.dma("sp", ones[:], K["ones"][:, :], writes=[T("ones")])
        with contextlib.ExitStack() as es:
            sb = lambda n, s, d: es.enter_context(nc.sbuf_tensor(T(n), s, d))
            ps = lambda n, s, d: es.enter_context(nc.psum_tensor(T(n), s, d))
            kvg = sb("a_kvg", [128, KVL], F32)
            kv = [sb(f"a_kv{i}", [128, KVL + DR], F32) for i in range(2)]
            junk = sb("a_junk", [128, KVL], F32)
            ss = sb("a_ss", [128, 2], F32)
            kn = sb("a_kn", [128, KVL], F32)
            cs = [sb(f"a_cs{i}", [128, 64], F32) for i in range(2)]
            kr = sb("a_kr", [128, DR], F32)
            t1 = sb("a_t1", [128, 32], F32)
            t2 = sb("a_t2", [128, 32], F32)
            sq2 = sb("a_sq2", [64, 128], F32)
            qt = [sb(f"a_qt{i}", [128, NH * 192], F32) for i in range(2)]
            qr = sb("a_qr", [128, NH * 192], F32)
            q1 = sb("a_q1", [128, NH * 32], F32)
            q2 = sb("a_q2", [128, NH * 32], F32)
            qq = sb("a_qq", [128, NH * 192], F32)
            qs = sb("a_qs", [128, NH], F32)
            pT = [ps(f"a_pT{i}", [128, 512], F32) for i in range(2)]
            pk = ps("a_pk", [64, 128], F32)
            pn = ps("a_pn", [128, 128], F32)
            c.dma("sp", kvg[:], W["kvng"].partition_broadcast(128), writes=[T("a_kvg")])
            mset(c, "pool", kpT[64:65, :], 1.0, [T("kpT")])
            for ti, (r0, n, ms) in enumerate(tiles):
                b = ti % 2
                c.dma("sp", kv[b][:], PKV[r0:r0 + 128, :], reads=[T("PKV")], writes=[T(f"a_kv{b}")])
                rstd = rmsnorm_rstd(c, kv[b][:, 0:KVL], junk[:], ss[:], KVL, [T(f"a_kv{b}")], T("a_n"))
                stt(c, "dve", kn[:], kv[b][:, 0:KVL], rstd, kvg[:], ALU.mult, ALU.mult, [T(f"a_kv{b}"), T("a_n_ss1"), T("a_kvg")], [T("a_kn")])
                for k in range(4):
                    tr(c, pT[b][:, k * 128:(k + 1) * 128], kn[:, k * 128:(k + 1) * 128], idt[:], [T("a_kn"), T("idt")], [T(f"a_pT{b}")], inc=(k == 3))
                cp(c, "act", ckvT[:, :, r0:r0 + 128], pT[b][:].rearrange("p (a b) -> p a b", a=4), [T(f"a_pT{b}")], [T("ckvT")])
                kp = kv[b][:, KVL:KVL + DR]
                if ms == "l":
                    t0 = r0 - NCTX
                    c.dma("sp", cs[b][:, 0:32], W["cos"][t0:t0 + 128, :], writes=[T(f"a_cs{b}")])
                    c.dma("sp", cs[b][:, 32:64], W["sin"][t0:t0 + 128, :], writes=[T(f"a_cs{b}")])
                    kp4 = kp.rearrange("p (a h e) -> p a h e", a=2, h=2)
                    kr4 = kr[:].rearrange("p (a h e) -> p a h e", a=2, h=2)
                    co = cs[b][:, 0:32].rearrange("p (a e) -> p a e", a=2)
                    si = cs[b][:, 32:64].rearrange("p (a e) -> p a e", a=2)
                    t1v = t1[:].rearrange("p (a e) -> p a e", a=2)
                    t2v = t2[:].rearrange("p (a e) -> p a e", a=2)
                    rd = [T(f"a_kv{b}"), T(f"a_cs{b}")]
                    tt(c, "dve", t1v, kp4[:, :, 0, :], co, ALU.mult, rd, [T("a_t1")])
                    tt(c, "dve", t2v, kp4[:, :, 1, :], si, ALU.mult, rd, [T("a_t2")])
                    tt(c, "dve", kr4[:, :, 0, :], t1v, t2v, ALU.subtract, [T("a_t1"), T("a_t2")], [T("a_kr")])
                    tt(c, "dve", t1v, kp4[:, :, 0, :], si, ALU.mult, rd, [T("a_t1")])
                    tt(c, "dve", t2v, kp4[:, :, 1, :], co, ALU.mult, rd, [T("a_t2")])
                    tt(c, "dve", kr4[:, :, 1, :], t1v, t2v, ALU.add, [T("a_t1"), T("a_t2")], [T("a_kr")])
                else:
                    cp(c, "dve", kr[:], kp, [T(f"a_kv{b}")], [T("a_kr")])
                tr(c, pk[:, :], kr[:], idt[:], [T("a_kr"), T("idt")], [T("a_pk")])
                cp(c, "act", kpT[0:64, r0:r0 + 128], pk[:, :], [T("a_pk")], [T("kpT")])
                act(c, sq2[:, :], pk[:, :], AF.Square, [T("a_pk")], [T("a_sq2")])
                mm(c, pn[:, :], ones[0:64, :], sq2[:, :], True, True, [T("ones"), T("a_sq2")], [T("a_pn")])
                cp(c, "dve", kpsq[:, r0:r0 + 128], pn[:, :], [T("a_pn")], [T("kpsq")])
                if ms == "l":
                    t0 = r0 - NCTX
                    c.dma("sp", qt[b][:], QD[r0:r0 + 128, :], reads=[T("QD")], writes=[T(f"a_qt{b}")])
                    q3 = qt[b][:].rearrange("p (h e) -> p h e", h=NH)
                    qr3 = qr[:].rearrange("p (h e) -> p h e", h=NH)
                    cp(c, "pool", qr3[:, :, 0:DN], q3[:, :, 0:DN], [T(f"a_qt{b}")], [T("a_qr")])
                    rd = [T(f"a_qt{b}"), T(f"a_cs{b}")]
                    for ax in range(2):
                        x1 = q3[:, :, DN + ax * 32:DN + ax * 32 + 16]
                        x2 = q3[:, :, DN + ax * 32 + 16:DN + ax * 32 + 32]
                        o1 = qr3[:, :, DN + ax * 32:DN + ax * 32 + 16]
                        o2 = qr3[:, :, DN + ax * 32 + 16:DN + ax * 32 + 32]
                        co = cs[b][:, ax * 16:(ax + 1) * 16].unsqueeze(1).to_broadcast([128, NH, 16])
                        si = cs[b][:, 32 + ax * 16:32 + (ax + 1) * 16].unsqueeze(1).to_broadcast([128, NH, 16])
                        a1 = q1[:, 0:NH * 16].rearrange("p (h e) -> p h e", h=NH)
                        a2 = q2[:, 0:NH * 16].rearrange("p (h e) -> p h e", h=NH)
                        tt(c, "dve", a1, x1, co, ALU.mult, rd, [T("a_q1")])
                        tt(c, "dve", a2, x2, si, ALU.mult, rd, [T("a_q2")])
                        tt(c, "dve", o1, a1, a2, ALU.subtract, [T("a_q1"), T("a_q2")], [T("a_qr")])
                        tt(c, "dve", a1, x1, si, ALU.mult, rd, [T("a_q1")])
                        tt(c, "dve", a2, x2, co, ALU.mult, rd, [T("a_q2")])
                        tt(c, "dve", o2, a1, a2, ALU.add, [T("a_q1"), T("a_q2")], [T("a_qr")])
                    tt(c, "pool", qq[:], qr[:], qr[:], ALU.mult, [T("a_qr")], [T("a_qq")])
                    red(c, "dve", qs[:], qq[:].rearrange("p (h e) -> p h e", h=NH), ALU.add, [T("a_qq")], [T("a_qs")])
                    c.dma("sp", QSQ[t0:t0 + 128, :], qs[:], reads=[T("a_qs")], writes=[T("QSQ")])
                    c.dma("sp", QH[:, t0:t0 + 128, :].rearrange("h t e -> t h e"), qr3, reads=[T("a_qr")], writes=[T("QH")])
        c.barrier()
        if upto == "A2":
            return

        with contextlib.ExitStack() as es:
            sb = lambda n, s, d: es.enter_context(nc.sbuf_tensor(T(n), s, d))
            ps = lambda n, s, d: es.enter_context(nc.psum_tensor(T(n), s, d))
            wuk = sb("h_wuk", [128, 4, DN], BF16)
            wuv = sb("h_wuv", [128, 4, DV], BF16)
            KnT = sb("h_KnT", [128, NTOK], BF16)
            Vt = sb("h_Vt", [128, NKT, 132], BF16)
            sqk = sb("h_sqk", [128, 512], F32)
            ksq = sb("h_ksq", [128, NTOK], F32)
            km = sb("h_km", [128, 2], F32)
            kmb = sb("h_kmb", [128, 1], F32)
            qh = [sb(f"h_qh{i}", [128, 193], F32) for i in range(2)]
            qsq = [sb(f"h_qsq{i}", [128, NH], F32) for i in range(2)]
            nq = sb("h_nq", [128, 2], F32)
            QnT = [sb(f"h_QnT{i}", [128, 512], BF16) for i in range(2)]
            QpT = [sb(f"h_QpT{i}", [65, 512], BF16) for i in range(2)]
            PT = [sb(f"h_PT{i}", [128, 512], BF16) for i in range(3)]
            rc = sb("h_rc", [128, 4], F32)
            ob = [sb(f"h_ob{i}", [128, DV], BF16) for i in range(2)]
            pS = [ps(f"h_pS{i}", [128, 512], F32) for i in range(2)]
            pO = [ps(f"h_pO{i}", [128, 512], F32) for i in range(4)]
            pX = [ps(f"h_pX{i}", [128, 512], F32) for i in range(2)]
            mset(c, "dve", Vt[:], 1.0, [T("h_Vt")])
            pti = 0
            if upto == "K0":
                c.barrier()
                return
            for h in range(NH):
                c.dma("pool", wuk[:], W["w_uk"][:, h * DN:(h + 1) * DN].rearrange("(k p) n -> p k n", p=128), writes=[T("h_wuk")])
                c.dma("pool", wuv[:], W["w_uv"][:, h * DV:(h + 1) * DV].rearrange("(k p) n -> p k n", p=128), writes=[T("h_wuv")])
                if upto == "K0b":
                    c.barrier()
                    return
                nkb = (NTOK + 511) // 512
                for kb in range(nkb):
                    w = min(512, NTOK - kb * 512)
                    pb = kb % 2
                    for k in range(4):
                        mm(c, pX[pb][:, :w], wuk[:, k, :], ckvT[:, k, kb * 512:kb * 512 + w], k == 0, k == 3, [T("h_wuk"), T("ckvT")], [T(f"h_pX{pb}")])
                    cp(c, "dve", KnT[:, kb * 512:kb * 512 + w], pX[pb][:, :w], [T(f"h_pX{pb}")], [T("h_KnT")])
                    if upto == "K1a":
                        continue
                    act(c, sqk[:, :w], KnT[:, kb * 512:kb * 512 + w], AF.Square, [T("h_KnT")], [T("h_sqk")])
                    if upto == "K1b":
                        continue
                    mm(c, pS[pb][:, :w], ones[:, :], sqk[:, :w], True, True, [T("ones"), T("h_sqk")], [T(f"h_pS{pb}")])
                    tt(c, "dve", ksq[:, kb * 512:kb * 512 + w], pS[pb][:, :w], kpsq[:, kb * 512:kb * 512 + w], ALU.add, [T(f"h_pS{pb}"), T("kpsq")], [T("h_ksq")])
                if upto in ("K1", "K1a", "K1b"):
                    c.barrier()
                    return
                red(c, "dve", km[:, 0:1], ksq[:, :], ALU.max, [T("h_ksq")], [T("h_km")])
                act(c, km[:, 1:2], km[:, 0:1], AF.Sqrt, [T("h_km")], [T("h_km1")])
                ts(c, "dve", kmb[:], km[:, 1:2], -1.0, None, ALU.mult, None, [T("h_km1")], [T("h_kmb")])
                if upto == "K2":
                    c.barrier()
                    return
                for kg in range((NKT + 3) // 4):
                    pb = kg % 2
                    nk = min(4, NKT - kg * 4)
                    for j in range(nk):
                        kt = kg * 4 + j
                        for k in range(4):
                            mm(c, pX[pb][:, j * 128:(j + 1) * 128], ckvT[:, k, kt * 128:(kt + 1) * 128], wuv[:, k, :], k == 0, k == 3,
                               [T("ckvT"), T("h_wuv")], [T(f"h_pX{pb}")], inc=(k == 3 and j == nk - 1))
                    cp(c, "act" if pb == 0 else "dve", Vt[:, kg * 4:kg * 4 + nk, 0:DV], pX[pb][:, 0:nk * 128].rearrange("p (a b) -> p a b", a=nk),
                       [T(f"h_pX{pb}")], [T("h_Vt")])
                if upto == "KV":
                    c.barrier()
                    return
                for qb in range(NLAT // 512):
                    qi = qb % 2
                    for s4 in range(4):
                        t0 = qb * 512 + s4 * 128
                        b = s4 % 2
                        c.dma("sp", qh[b][:, 0:192], QH[h, t0:t0 + 128, :], reads=[T("QH")], writes=[T(f"h_qh{b}")])
                        c.dma("sp", qsq[b][:], QSQ[t0:t0 + 128, :], reads=[T("QSQ")], writes=[T(f"h_qsq{b}")])
                        act(c, nq[:, 0:1], qsq[b][:, h:h + 1], AF.Sqrt, [T(f"h_qsq{b}")], [T("h_nq")])
                        ts(c, "dve", qh[b][:, 192:193], nq[:, 0:1], kmb[:, 0:1], None, ALU.mult, None, [T("h_nq"), T("h_kmb")], [T(f"h_qh{b}")])
                        tr(c, pX[0][:, s4 * 128:(s4 + 1) * 128], qh[b][:, 0:DN], idt[:], [T(f"h_qh{b}"), T("idt")], [T("h_pX0")], inc=False)
                        tr(c, pX[1][0:65, s4 * 128:(s4 + 1) * 128], qh[b][:, DN:193], idt[:], [T(f"h_qh{b}"), T("idt")], [T("h_pX1")])
                    cp(c, "act", QnT[qi][:], pX[0][:], [T("h_pX0")], [T(f"h_QnT{qi}")])
                    cp(c, "dve", QpT[qi][:], pX[1][0:65, :], [T("h_pX1")], [T(f"h_QpT{qi}")])
                    def emit_qk(kt):
                        sbi = kt % 2
                        mm(c, pS[sbi][:], KnT[:, kt * 128:(kt + 1) * 128], QnT[qi][:], True, False, [T("h_KnT"), T(f"h_QnT{qi}")], [T(f"h_pS{sbi}")], inc=False)
                        mm(c, pS[sbi][:], kpT[:, kt * 128:(kt + 1) * 128], QpT[qi][:], False, True, [T("kpT"), T(f"h_QpT{qi}")], [T(f"h_pS{sbi}")])
                    emit_qk(0)
                    for kt in range(NKT):
                        sbi = kt % 2
                        p3 = pti % 3
                        pti += 1
                        if kt + 1 < NKT:
                            emit_qk(kt + 1)
                        act(c, PT[p3][:], pS[sbi][:], AF.Exp, [T(f"h_pS{sbi}")], [T(f"h_PT{p3}")], scale=SCALE)
                        for s4 in range(4):
                            mm(c, pO[s4][:, 0:129], PT[p3][:, s4 * 128:(s4 + 1) * 128], Vt[:, kt, 0:129], kt == 0, kt == NKT - 1,
                               [T(f"h_PT{p3}"), T("h_Vt")], [T(f"h_pO{s4}")], inc=(kt == NKT - 1 or s4 == 3))
                    for s4 in range(4):
                        t0 = qb * 512 + s4 * 128
                        b = s4 % 2
                        c.op("dve", lambda e: e.reciprocal(out=rc[:, s4:s4 + 1], in_=pO[s4][:, 128:129]), [T(f"h_pO{s4}")], [T("h_rc")])
                        ts(c, "dve", ob[b][:], pO[s4][:, 0:DV], rc[:, s4:s4 + 1], None, ALU.mult, None, [T(f"h_pO{s4}"), T("h_rc")], [T(f"h_ob{b}")])
                        c.dma("sp", OD[NCTX + t0:NCTX + t0 + 128, h * DV:(h + 1) * DV], ob[b][:], reads=[T(f"h_ob{b}")], writes=[T("OD")])
        c.barrier()
    c.barrier()
    if dbg is not None:
        dbg(dict(OD=OD, QH=QH, PKV=PKV))
    out_proj_res(c, T, "op_", D, NH * DV, OD, W["w_o"], RES, lat_tiles, {"l": mods["l"]["G1"]}, K)


NE = 32


def moe_local(c, tag, D, FF, NT, RES, tiles, mods, W, K, dbg=None):
    nc = c.nc
    KC = D // 128
    FC = FF // 128
    NBLK = (4 * NT + 511) // 512 + NE
    NSLOT = NBLK * 512
    assert NSLOT % 128 == 0
    T = lambda s: f"{tag}_{s}"
    HF = c.dram(T("HF"), [NT + 128, D], BF16)
    GD = c.dram(T("GD"), [NT, NE], F32)
    LISTF = c.dram(T("LISTF"), [NSLOT, 2], F32)
    ACC = c.dram(T("ACC"), [NT + 128, D], F32)
    W1G2 = W["w1gT"]
    W1L2 = W["w1lT"]
    W22 = W["w2T"]
    c.barrier()
    with contextlib.ExitStack() as es0:
        sb0 = lambda n, s, d: es0.enter_context(nc.sbuf_tensor(T(n), s, d))
        idt = sb0("idt", [128, 128], F32)
        ones = sb0("ones", [128, 128], F32)
        trif = sb0("trif", [128, 128], F32)
        iop = sb0("iop", [128, 1], F32)
        iob = sb0("iob", [128, NBLK], F32)
        cnt = sb0("cnt", [128, NE], F32)
        base = sb0("base", [128, NE], F32)
        widx = sb0("widx", [128, NBLK], I32)
        eidx = sb0("eidx", [128, NBLK], I32)
        widx1 = sb0("widx1", [128, FC, NBLK], I32)
        c.dma("sp", idt[:], K["ident"][:, :], writes=[T("idt")])
        c.dma("sp", ones[:], K["ones"][:, :], writes=[T("ones")])
        c.dma("sp", trif[:], K["trif"][:, :], writes=[T("trif")])
        c.dma("sp", iop[:], K["iota_p"][:, :], writes=[T("iop")])
        c.dma("sp", iob[:], K["iota_b"].partition_broadcast(128), writes=[T("iob")])
        with contextlib.ExitStack() as es:
            sb = lambda n, s, d: es.enter_context(nc.sbuf_tensor(T(n), s, d))
            ps = lambda n, s, d: es.enter_context(nc.psum_tensor(T(n), s, d))
            At = sb("At", [128, D], F32)
            Bt = sb("Bt", [128, D], F32)
            rwt = sb("rwt", [128, KC, NE], F32)
            rbt = sb("rbt", [128, NE], F32)
            xt = [sb(f"xt{i}", [128, D], F32) for i in range(2)]
            junk = sb("junk", [128, D], F32)
            ss = sb("ss", [128, 2], F32)
            h = sb("h", [128, D], F32)
            hb = [sb(f"hb{i}", [128, D], BF16) for i in range(2)]
            hT = sb("hT", [128, KC, 128], F32)
            lg = sb("lg", [128, NE], F32)
            m8 = sb("m8", [128, 8], F32)
            sm = sb("sm", [128, 4], F32)
            ex = sb("ex", [128, NE], F32)
            mk = [sb(f"mk{i}", [128, NE], F32) for i in range(2)]
            Gt = [sb(f"Gt{i}", [128, NE], F32) for i in range(2)]
            zt = sb("zt", [128, D], F32)
            zb = sb("zb", [128, D], BF16)
            lf = sb("lf", [128, NSLOT // 128, 2], F32)
            pT = [ps(f"pT{i}", [128, 512], F32) for i in range(2)]
            pl = ps("pl", [128, NE], F32)
            pc = ps("pc", [128, NE], F32)
            c.dma("sp", rwt[:], W["rw"].rearrange("(k p) e -> p k e", p=128), writes=[T("rwt")])
            c.dma("sp", rbt[:], W["rb"].partition_broadcast(128), writes=[T("rbt")])
            mset(c, "pool", zt[:], 0.0, [T("zt")])
            mset(c, "pool", zb[:], 0.0, [T("zb")])
            mset(c, "pool", lf[:, :, 0:1], float(NT), [T("lf")])
            mset(c, "pool", lf[:, :, 1:2], 0.0, [T("lf")])
            c.dma("sp", LISTF.rearrange("(p a) c -> p a c", p=128), lf[:], reads=[T("lf")], writes=[T("LISTF")])
            c.dma("sp", HF[NT:NT + 128, :], zb[:], reads=[T("zb")], writes=[T("HF")])
            for i in range((NT + 128) // 128):
                c.dma("sp", ACC[i * 128:(i + 1) * 128, :], zt[:], reads=[T("zt")], writes=[T("ACC")])
            cur_ms = None
            ntl = len(tiles)
            for ti, (r0, n, ms, tok0) in enumerate(tiles):
                assert n == 128
                b = ti % 2
                if ms != cur_ms:
                    c.dma("sp", At[:], mods[ms]["A2"].partition_broadcast(128), reads=["MODS"], writes=[T("At")])
                    c.dma("sp", Bt[:], mods[ms]["B2"].partition_broadcast(128), reads=["MODS"], writes=[T("Bt")])
                    cur_ms = ms
                c.dma("sp", xt[b][:], RES[r0:r0 + 128, :], reads=["RES"], writes=[T(f"xt{b}")])
                rstd = rmsnorm_rstd(c, xt[b][:], junk[:], ss[:], D, [T(f"xt{b}")], T("n1"))
                stt(c, "dve", h[:], xt[b][:], rstd, At[:], ALU.mult, ALU.mult, [T(f"xt{b}"), T("n1_ss1"), T("At")], [T("h")])
                tt(c, "pool", h[:], h[:], Bt[:], ALU.add, [T("h"), T("Bt")], [T("h")])
                cp(c, "act", hb[b][:], h[:], [T("h")], [T(f"hb{b}")])
                c.dma("sp", HF[tok0:tok0 + 128, :], hb[b][:], reads=[T(f"hb{b}")], writes=[T("HF")])
                for kg in range(KC // 4):
                    pb = kg % 2
                    for jj in range(4):
                        k = kg * 4 + jj
                        tr(c, pT[pb][:, jj * 128:(jj + 1) * 128], h[:, k * 128:(k + 1) * 128], idt[:], [T("h"), T("idt")], [T(f"pT{pb}")], inc=(jj == 3))
                    cp(c, "act" if kg % 2 == 0 else "dve", hT[:, kg * 4:(kg + 1) * 4, :], pT[pb][:].rearrange("p (a b) -> p a b", a=4), [T(f"pT{pb}")], [T("hT")])
                for k in range(KC):
                    mm(c, pl[:, :], hT[:, k, :], rwt[:, k, :], k == 0, k == KC - 1, [T("hT"), T("rwt")], [T("pl")])
                tt(c, "dve", lg[:], pl[:, :], rbt[:], ALU.add, [T("pl"), T("rbt")], [T("lg")])
                c.op("dve", lambda e: e.max(out=m8[:], in_=lg[:]), [T("lg")], [T("m8")])
                ts(c, "dve", mk[b][:], lg[:], m8[:, 3:4], None, ALU.is_ge, None, [T("lg"), T("m8")], [T(f"mk{b}")])
                ts(c, "dve", sm[:, 0:1], m8[:, 0:1], -1.0, None, ALU.mult, None, [T("m8")], [T("sm0")])
                act(c, ex[:], lg[:], AF.Exp, [T("lg"), T("sm0")], [T("ex")], bias=sm[:, 0:1])
                tt(c, "dve", ex[:], ex[:], mk[b][:], ALU.mult, [T("ex"), T(f"mk{b}")], [T("ex")])
                red(c, "dve", sm[:, 1:2], ex[:], ALU.add, [T("ex")], [T("sm1")])
                c.op("dve", lambda e: e.reciprocal(out=sm[:, 2:3], in_=sm[:, 1:2]), [T("sm1")], [T("sm2")])
                ts(c, "dve", Gt[b][:], ex[:], sm[:, 2:3], None, ALU.mult, None, [T("ex"), T("sm2")], [T(f"Gt{b}")])
                c.dma("sp", GD[tok0:tok0 + 128, :], Gt[b][:], reads=[T(f"Gt{b}")], writes=[T("GD")])
                mm(c, pc[:, :], ones[:], mk[b][:], ti == 0, ti == ntl - 1, [T("ones"), T(f"mk{b}")], [T("pc")], inc=True)
            cp(c, "dve", cnt[:], pc[:, :], [T("pc")], [T("cnt")])
        c.barrier()
        with contextlib.ExitStack() as es:
            sb = lambda n, s, d: es.enter_context(nc.sbuf_tensor(T(n), s, d))
            r = sb("r", [128, NE], F32)
            nb = sb("nb", [128, NE], F32)
            inc_ = [sb(f"inc{i}", [128, NE], F32) for i in range(2)]
            eid = sb("eid", [128, NBLK], F32)
            tmpb = sb("tmpb", [128, NBLK], F32)
            mset(c, "dve", nb[:], 0.0, [T("nb")])
            for j in range((4 * NT + 511) // 512 + 1):
                ts(c, "dve", r[:], cnt[:], 512.0 * j, None, ALU.is_gt, None, [T("cnt")], [T("r")])
                tt(c, "dve", nb[:], nb[:], r[:], ALU.add, [T("nb"), T("r")], [T("nb")])
            cp(c, "dve", inc_[0][:], nb[:], [T("nb")], [T("inc0")])
            src = 0
            sh = 1
            while sh < NE:
                dst = 1 - src
                tt(c, "dve", inc_[dst][:, sh:NE], inc_[src][:, sh:NE], inc_[src][:, 0:NE - sh], ALU.add, [T(f"inc{src}")], [T(f"inc{dst}")])
                cp(c, "dve", inc_[dst][:, 0:sh], inc_[src][:, 0:sh], [T(f"inc{src}")], [T(f"inc{dst}")])
                src = dst
                sh *= 2
            incl = inc_[src]
            tt(c, "dve", base[:], incl[:], nb[:], ALU.subtract, [T(f"inc{src}"), T("nb")], [T("base")])
            ts(c, "dve", base[:], base[:], 512.0, None, ALU.mult, None, [T("base")], [T("base")])
            mset(c, "dve", eid[:], 0.0, [T("eid")])
            for e in range(NE):
                ts(c, "dve", tmpb[:], iob[:], incl[:, e:e + 1], None, ALU.is_ge, None, [T("iob"), T(f"inc{src}")], [T("tmpb")])
                tt(c, "dve", eid[:], eid[:], tmpb[:], ALU.add, [T("eid"), T("tmpb")], [T("eid")])
            ts(c, "dve", eid[:], eid[:], float(NE - 1), None, ALU.min, None, [T("eid")], [T("eid")])
            cp(c, "dve", eidx[:], eid[:], [T("eid")], [T("eidx")])
            ts(c, "dve", tmpb[:], eid[:], 128.0, iop[:, 0:1], ALU.mult, ALU.add, [T("eid"), T("iop")], [T("tmpb")])
            cp(c, "dve", widx[:], tmpb[:], [T("tmpb")], [T("widx")])
            ts(c, "dve", eid[:], eid[:], 128.0 * FC, iop[:, 0:1], ALU.mult, ALU.add, [T("eid"), T("iop")], [T("eid")])
            for fc in range(FC):
                ts(c, "dve", tmpb[:], eid[:], 128.0 * fc, None, ALU.add, None, [T("eid")], [T("tmpb")])
                cp(c, "dve", widx1[:, fc, :], tmpb[:], [T("tmpb")], [T("widx")])
        c.barrier()
        with contextlib.ExitStack() as es:
            sb = lambda n, s, d: es.enter_context(nc.sbuf_tensor(T(n), s, d))
            ps = lambda n, s, d: es.enter_context(nc.psum_tensor(T(n), s, d))
            Gl = [sb(f"Gl{i}", [128, NE], F32) for i in range(2)]
            Mk = sb("Mk", [128, NE], F32)
            car = sb("car", [128, NE], F32)
            key = sb("key", [128, NE], F32)
            k8 = sb("k8", [128, 8], F32)
            eq = sb("eq", [128, NE], F32)
            dsti = [sb(f"dsti{i}", [128, 4], I32) for i in range(2)]
            dstf = sb("dstf", [128, 4], F32)
            pay = [sb(f"pay{i}", [128, 4, 2], F32) for i in range(2)]
            pcs = ps("pcs", [128, NE], F32)
            pcr = ps("pcr", [128, NE], F32)
            mset(c, "dve", car[:], 0.0, [T("car")])
            for ti, (r0, n, ms, tok0) in enumerate(tiles):
                b = ti % 2
                c.dma("sp", Gl[b][:], GD[tok0:tok0 + 128, :], reads=[T("GD")], writes=[T(f"Gl{b}")])
                ts(c, "dve", Mk[:], Gl[b][:], 0.0, None, ALU.is_gt, None, [T(f"Gl{b}")], [T("Mk")])
                mm(c, pcs[:, :], trif[:], Mk[:], True, True, [T("trif"), T("Mk")], [T("pcs")])
                mm(c, pcr[:, :], ones[:], Mk[:], True, True, [T("ones"), T("Mk")], [T("pcr")])
                tt(c, "dve", key[:], pcs[:, :], car[:], ALU.add, [T("pcs"), T("car")], [T("key")])
                tt(c, "dve", key[:], key[:], base[:], ALU.add, [T("key"), T("base")], [T("key")])
                tt(c, "dve", key[:], key[:], Mk[:], ALU.mult, [T("key"), T("Mk")], [T("key")])
                tt(c, "dve", car[:], car[:], pcr[:, :], ALU.add, [T("car"), T("pcr")], [T("car")])
                c.op("dve", lambda e: e.max(out=k8[:], in_=key[:]), [T("key")], [T("k8")])
                ts(c, "dve", dstf[:], k8[:, 0:4], -1.0, None, ALU.add, None, [T("k8")], [T("dstf")])
                cp(c, "dve", dsti[b][:], dstf[:], [T("dstf")], [T(f"dsti{b}")])
                for k in range(4):
                    ts(c, "dve", eq[:], key[:], k8[:, k:k + 1], None, ALU.is_equal, None, [T("key"), T("k8")], [T("eq")])
                    tt(c, "dve", eq[:], eq[:], Gl[b][:], ALU.mult, [T("eq"), T(f"Gl{b}")], [T("eq")])
                    red(c, "dve", pay[b][:, k, 1:2], eq[:], ALU.add, [T("eq")], [T(f"pay{b}")])
                    ts(c, "dve", pay[b][:, k, 0:1], iop[:, 0:1], float(tok0), None, ALU.add, None, [T("iop")], [T(f"pay{b}")])
                for k in range(4):
                    c.idma(LISTF[:, :], bass.IndirectOffsetOnAxis(ap=dsti[b][:, k:k + 1], axis=0), pay[b][:, k, :], None,
                           reads=[T(f"dsti{b}"), T(f"pay{b}")], writes=[T("LISTF")])
        c.barrier()
        with contextlib.ExitStack() as es:
            sb = lambda n, s, d: es.enter_context(nc.sbuf_tensor(T(n), s, d))
            ps = lambda n, s, d: es.enter_context(nc.psum_tensor(T(n), s, d))
            idb = sb("idb", [128, 128], BF16)
            lst = [sb(f"lst{i}", [128, 4, 2], F32) for i in range(2)]
            tkf = sb("tkf", [128, 4], F32)
            pad1 = sb("pad1", [128, 4], F32)
            tki = [sb(f"tki{i}", [128, 4], I32) for i in range(2)]
            xg = [sb(f"xg{i}", [128, D], BF16) for i in range(2)]
            xT = sb("xT", [128, KC, 512], BF16)
            yT = sb("yT", [128, FC, 512], BF16)
            w2t = sb("w2t", [128, FC, D], BF16)
            b2t = sb("b2t", [128, D], F32)
            b1gt = sb("b1gt", [128, FC], F32)
            b1lt = sb("b1lt", [128, FC], F32)
            w1gt = [sb(f"w1g{i}", [128, KC, 128], BF16) for i in range(2)]
            w1lt = [sb(f"w1l{i}", [128, KC, 128], BF16) for i in range(2)]
            stg = [sb(f"stg{i}", [128, max(D, KC * 128)], F32) for i in range(5)]
            nstg = 0
            gs = [sb(f"gs{i}", [128, 512], F32) for i in range(2)]
            sg = [sb(f"sg{i}", [128, 512], F32) for i in range(2)]
            ls = [sb(f"ls{i}", [128, 512], F32) for i in range(2)]
            yb = [sb(f"yb{i}", [128, D], F32) for i in range(2)]
            TW = min(8, KC)
            ptr = [ps(f"ptr{i}", [128, TW * 128], BF16) for i in range(2)]
            pgl = [ps(f"pgl{i}", [128, 512], F32) for i in range(4)]
            po = [ps(f"po{i}", [128, 512], F32) for i in range(2)]
            cp(c, "dve", idb[:], idt[:], [T("idt")], [T("idb")])
            gcount = 0
            for blk in range(NBLK):
                lb = blk % 2
                c.dma("sp", lst[lb][:], LISTF[blk * 512:(blk + 1) * 512, :].rearrange("(s p) c -> p s c", p=128), reads=[T("LISTF")], writes=[T(f"lst{lb}")])
                cp(c, "dve", tkf[:], lst[lb][:, :, 0], [T(f"lst{lb}")], [T("tkf")])
                ts(c, "dve", pad1[:], tkf[:], float(NT), None, ALU.is_ge, None, [T("tkf")], [T("pad1")])
                ts(c, "dve", pad1[:], pad1[:], iop[:, 0:1], None, ALU.mult, None, [T("pad1"), T("iop")], [T("pad1")])
                tt(c, "dve", tkf[:], tkf[:], pad1[:], ALU.add, [T("tkf"), T("pad1")], [T("tkf")])
                cp(c, "dve", tki[lb][:], tkf[:], [T("tkf")], [T(f"tki{lb}")])
                wofs = bass.IndirectOffsetOnAxis(ap=widx[:, blk:blk + 1], axis=0)
                for fc in range(FC):
                    sgi = nstg % 5
                    nstg += 1
                    c.idma(stg[sgi][:, 0:D], None, W22[:, :], bass.IndirectOffsetOnAxis(ap=widx1[:, fc, blk:blk + 1], axis=0),
                           reads=[T("widx")], writes=[T(f"stg{sgi}")])
                    cp(c, "act" if fc % 2 == 0 else "dve", w2t[:, fc, :], stg[sgi][:, 0:D], [T(f"stg{sgi}")], [T("w2t")])
                c.idma(b1gt[:], None, W["b1gT"][:, :], wofs, reads=[T("widx")], writes=[T("b1gt")])
                c.idma(b1lt[:], None, W["b1lT"][:, :], wofs, reads=[T("widx")], writes=[T("b1lt")])
                c.idma(b2t[:], None, W["b2"][:, :], bass.IndirectOffsetOnAxis(ap=eidx[:, blk:blk + 1], axis=0), reads=[T("eidx")], writes=[T("b2t")])
                for st in range(4):
                    b = gcount % 2
                    gcount += 1
                    c.idma(xg[b][:], None, HF[:, :], bass.IndirectOffsetOnAxis(ap=tki[lb][:, st:st + 1], axis=0),
                           reads=[T(f"tki{lb}"), T("HF")], writes=[T(f"xg{b}")])
                    for kg in range(KC // TW):
                        pb = (kg + st) % 2
                        for jj in range(TW):
                            k = kg * TW + jj
                            tr(c, ptr[pb][:, jj * 128:(jj + 1) * 128], xg[b][:, k * 128:(k + 1) * 128], idb[:], [T(f"xg{b}"), T("idb")], [T(f"ptr{pb}")], inc=(jj == TW - 1))
                        cp(c, "act" if pb == 0 else "dve", xT[:, kg * TW:(kg + 1) * TW, st * 128:(st + 1) * 128],
                           ptr[pb][:].rearrange("p (a b) -> p a b", a=TW), [T(f"ptr{pb}")], [T("xT")])
                for fc in range(FC):
                    wb = fc % 2
                    wofs1 = bass.IndirectOffsetOnAxis(ap=widx1[:, fc, blk:blk + 1], axis=0)
                    sgi = nstg % 5
                    nstg += 1
                    c.idma(stg[sgi][:, 0:KC * 128], None, W1G2[:, :], wofs1, reads=[T("widx")], writes=[T(f"stg{sgi}")])
                    cp(c, "act", w1gt[wb][:], stg[sgi][:, 0:KC * 128].rearrange("p (k f) -> p k f", k=KC), [T(f"stg{sgi}")], [T(f"w1g{wb}")])
                    sgi = nstg % 5
                    nstg += 1
                    c.idma(stg[sgi][:, 0:KC * 128], None, W1L2[:, :], wofs1, reads=[T("widx")], writes=[T(f"stg{sgi}")])
                    cp(c, "dve", w1lt[wb][:], stg[sgi][:, 0:KC * 128].rearrange("p (k f) -> p k f", k=KC), [T(f"stg{sgi}")], [T(f"w1l{wb}")])
                    pgi = pgl[2 * wb]
                    pli = pgl[2 * wb + 1]
                    for k in range(KC):
                        mm(c, pgi[:], w1gt[wb][:, k, :], xT[:, k, :], k == 0, k == KC - 1, [T(f"w1g{wb}"), T("xT")], [T(f"pgl{2 * wb}")])
                    for k in range(KC):
                        mm(c, pli[:], w1lt[wb][:, k, :], xT[:, k, :], k == 0, k == KC - 1, [T(f"w1l{wb}"), T("xT")], [T(f"pgl{2 * wb + 1}")])
                    ts(c, "dve", gs[wb][:], pgi[:], b1gt[:, fc:fc + 1], 7.0, ALU.add, ALU.min, [T(f"pgl{2 * wb}"), T("b1gt")], [T(f"gs{wb}")])
                    act(c, sg[wb][:], gs[wb][:], AF.Sigmoid, [T(f"gs{wb}")], [T(f"sg{wb}")], scale=1.702)
                    ts(c, "dve", ls[wb][:], pli[:], b1lt[:, fc:fc + 1], 7.0, ALU.add, ALU.min, [T(f"pgl{2 * wb + 1}"), T("b1lt")], [T(f"ls{wb}")])
                    ts(c, "dve", ls[wb][:], ls[wb][:], -7.0, 1.0, ALU.max, ALU.add, [T(f"ls{wb}")], [T(f"ls{wb}")])
                    tt(c, "dve", gs[wb][:], gs[wb][:], sg[wb][:], ALU.mult, [T(f"gs{wb}"), T(f"sg{wb}")], [T(f"gs{wb}")])
                    tt(c, "dve", yT[:, fc, :], gs[wb][:], ls[wb][:], ALU.mult, [T(f"gs{wb}"), T(f"ls{wb}")], [T("yT")])
                for st in range(4):
                    ob = st % 2
                    for nbk in range(D // 512):
                        pb = nbk % 2
                        for fc in range(FC):
                            mm(c, po[pb][:], yT[:, fc, st * 128:(st + 1) * 128], w2t[:, fc, nbk * 512:(nbk + 1) * 512], fc == 0, fc == FC - 1,
                               [T("yT"), T("w2t")], [T(f"po{pb}")])
                        tt(c, "dve", yb[ob][:, nbk * 512:(nbk + 1) * 512], po[pb][:], b2t[:, nbk * 512:(nbk + 1) * 512], ALU.add,
                           [T(f"po{pb}"), T("b2t")], [T(f"yb{ob}")])
                    act(c, yb[ob][:], yb[ob][:], AF.Identity, [T(f"yb{ob}"), T(f"lst{lb}")], [T(f"yb{ob}")], scale=lst[lb][:, st, 1:2])
                    c.idma(ACC[:, :], bass.IndirectOffsetOnAxis(ap=tki[lb][:, st:st + 1], axis=0), yb[ob][:], None,
                           reads=[T(f"tki{lb}"), T(f"yb{ob}")], writes=[T("ACC")], compute_op=ALU.add)
        c.barrier()
    c.barrier()
    if dbg is not None:
        dbg(dict(ACC=ACC, GD=GD, LISTF=LISTF))
    with contextlib.ExitStack() as es:
        sb = lambda n, s, d: es.enter_context(nc.sbuf_tensor(T(n), s, d))
        Gm = sb("Gm", [128, D], F32)
        xr = [sb(f"xr{i}", [128, D], F32) for i in range(2)]
        fr = [sb(f"fr{i}", [128, D], F32) for i in range(2)]
        cur_ms = None
        for ti, (r0, n, ms, tok0) in enumerate(tiles):
            b = ti % 2
            if ms != cur_ms:
                c.dma("sp", Gm[:], mods[ms]["G2"].partition_broadcast(128), reads=["MODS"], writes=[T("Gm")])
                cur_ms = ms
            c.dma("sp", xr[b][:], RES[r0:r0 + 128, :], reads=["RES"], writes=[T(f"xr{b}")])
            c.dma("sp", fr[b][:], ACC[tok0:tok0 + 128, :], reads=[T("ACC")], writes=[T(f"fr{b}")])
            tt(c, "dve", fr[b][:], fr[b][:], Gm[:], ALU.mult, [T(f"fr{b}"), T("Gm")], [T(f"fr{b}")])
            tt(c, "pool", xr[b][:], xr[b][:], fr[b][:], ALU.add, [T(f"xr{b}"), T(f"fr{b}")], [T(f"xr{b}")])
            c.dma("sp", RES[r0:r0 + 128, :], xr[b][:], reads=[T(f"xr{b}")], writes=["RES"])
    c.barrier()

import re as _re

D_MODEL = 2048
NBATCH = 2
SEQ = 8192
NCTX = 256
FFE = 2048


def mods_phase(c, CROW, ADAW, ADAB, MIXG, FFNG, MODV, K):
    nc = c.nc
    D = D_MODEL
    KC = D // 128
    T = lambda s: "md_" + s
    with contextlib.ExitStack() as es:
        sb = lambda n, s, d: es.enter_context(nc.sbuf_tensor(T(n), s, d))
        ps = lambda n, s, d: es.enter_context(nc.psum_tensor(T(n), s, d))
        idt = sb("idt", [128, 128], F32)
        cr = sb("cr", [3, D], F32)
        ST = sb("ST", [128, KC, 3], F32)
        wt = [sb(f"wt{i}", [128, KC, 512], F32) for i in range(2)]
        bt = [sb(f"bt{i}", [3, 512], F32) for i in range(2)]
        M = sb("M", [3, 6 * D], F32)
        g1 = sb("g1", [3, D], F32)
        g2 = sb("g2", [3, D], F32)
        MV = [sb(f"MV{i}", [3, D], F32) for i in range(2)]
        pT = ps("pT", [128, KC * 3], F32)
        pm = [ps(f"pm{i}", [3, 512], F32) for i in range(2)]
        c.dma("sp", idt[:], K["ident"][:, :], writes=[T("idt")])
        c.dma("sp", cr[:], CROW[:, :], writes=[T("cr")])
        act(c, cr[:], cr[:], AF.Silu, [T("cr")], [T("cr")])
        for k in range(KC):
            tr(c, pT[:, k * 3:(k + 1) * 3], cr[:, k * 128:(k + 1) * 128], idt[0:3, 0:3], [T("cr"), T("idt")], [T("pT")], inc=(k == KC - 1))
        cp(c, "dve", ST[:], pT[:].rearrange("p (k r) -> p k r", r=3), [T("pT")], [T("ST")])
        for i in range(2):
            c.dma("sp", g1[:], MIXG[i:i + 1, :].partition_broadcast(3), writes=[T("g1")])
            c.dma("sp", g2[:], FFNG[i:i + 1, :].partition_broadcast(3), writes=[T("g2")])
            for nb in range(6 * D // 512):
                wb = nb % 2
                c.dma("sp", wt[wb][:], ADAW[i * D:(i + 1) * D, nb * 512:(nb + 1) * 512].rearrange("(k p) n -> p k n", p=128), writes=[T(f"wt{wb}")])
                c.dma("sp", bt[wb][:], ADAB[i:i + 1, nb * 512:(nb + 1) * 512].partition_broadcast(3), writes=[T(f"bt{wb}")])
                for k in range(KC):
                    mm(c, pm[wb][:, :], ST[:, k, :], wt[wb][:, k, :], k == 0, k == KC - 1, [T("ST"), T(f"wt{wb}")], [T(f"pm{wb}")])
                tt(c, "dve", M[:, nb * 512:(nb + 1) * 512], pm[wb][:, :], bt[wb][:, :], ALU.add, [T(f"pm{wb}"), T(f"bt{wb}")], [T("M")])
            MO = MODV[i * 18:(i + 1) * 18, :].rearrange("(r k) d -> r k d", k=6)
            for k, (src0, gg) in enumerate(((D, g1), (0, None), (2 * D, None), (4 * D, g2), (3 * D, None), (5 * D, None))):
                mv = MV[k % 2]
                if gg is not None:
                    stt(c, "dve", mv[:], M[:, src0:src0 + D], 1.0, gg[:], ALU.add, ALU.mult, [T("M"), T("g1"), T("g2")], [T(f"MV{k % 2}")])
                else:
                    cp(c, "dve", mv[:], M[:, src0:src0 + D], [T("M")], [T(f"MV{k % 2}")])
                c.dma("sp", MO[:, k, :], mv[:], reads=[T(f"MV{k % 2}")], writes=["MODS"])
    c.barrier()


def modrow(MODV, i, r, k):
    j = (i * 3 + r) * 6 + k
    return MODV[j:j + 1, :]


def final_norm(c, tag, RES, r0, nrows, G, OUT, o0):
    nc = c.nc
    D = D_MODEL
    T = lambda s: f"{tag}_{s}"
    with contextlib.ExitStack() as es:
        sb = lambda n, s, d: es.enter_context(nc.sbuf_tensor(T(n), s, d))
        gt = sb("gt", [128, D], F32)
        xt = [sb(f"xt{i}", [128, D], F32) for i in range(2)]
        ot = [sb(f"ot{i}", [128, D], F32) for i in range(2)]
        junk = sb("junk", [128, D], F32)
        ss = sb("ss", [128, 2], F32)
        c.dma("sp", gt[:], G.partition_broadcast(128), writes=[T("gt")])
        for i in range(nrows // 128):
            b = i % 2
            c.dma("sp", xt[b][:], RES[r0 + i * 128:r0 + (i + 1) * 128, :], reads=["RES"], writes=[T(f"xt{b}")])
            rstd = rmsnorm_rstd(c, xt[b][:], junk[:], ss[:], D, [T(f"xt{b}")], T("n"))
            stt(c, "dve", ot[b][:], xt[b][:], rstd, gt[:], ALU.mult, ALU.mult, [T(f"xt{b}"), T("n_ss1"), T("gt")], [T(f"ot{b}")])
            c.dma("sp", OUT[o0 + i * 128:o0 + (i + 1) * 128, :], ot[b][:], reads=[T(f"ot{b}")], writes=["OUT"])
    c.barrier()


def build_program(nbatch=NBATCH, seq=SEQ, nctx=NCTX):
    c = Ctx()
    D = D_MODEL
    NTOK = nctx + seq
    ext = lambda n, s, dt=F32: c.dram(n, s, dt, "ExternalInput")
    X = ext("x", [nbatch * seq, D])
    CTX = ext("ctx", [nbatch * nctx, D])
    CROW = ext("crow", [3, D])
    ADAW = ext("ada_w", [2 * D, 6 * D])
    ADAB = ext("ada_b", [2, 6 * D])
    MIXG = ext("mix_g", [2, D])
    FFNG = ext("ffn_g", [2, D])
    FING = ext("fin_g", [1, D])
    WH = {"w_in": ext("h_w_in", [D, HIN]), "conv_w": ext("h_conv_w", [3, XW]), "conv_b": ext("h_conv_b", [1, XW]), "dtb": ext("h_dtb", [1, 64]),
          "alog": ext("h_alog", [1, 64]), "dsk": ext("h_dsk", [1, 32]), "ssdg": ext("h_ssdg", [1, SW]), "vng": ext("h_vng", [1, GW]),
          "wsT": ext("h_wsT", [16, 128, 128]), "bsT": ext("h_bsT", [128, 16]), "w_out": ext("h_w_out", [2 * SW, D])}
    WA = {"w_in": ext("a_w_in", [D, 1344]), "qng": ext("a_qng", [1, 768]), "kvng": ext("a_kvng", [1, 512]), "zero768": ext("a_zero768", [1, 768]),
          "w_uq": ext("a_w_uq", [768, 3072]), "w_uk": ext("a_w_uk", [512, 2048]), "w_uv": ext("a_w_uv", [512, 2048]), "w_o": ext("a_w_o", [2048, D]),
          "cos": ext("a_cos", [seq, 32]), "sin": ext("a_sin", [seq, 32])}
    FC = FFE // 128
    KC = D // 128
    WM = []
    for i in range(2):
        WM.append({"rw": ext(f"m{i}_rw", [D, 32]), "rb": ext(f"m{i}_rb", [1, 32]),
                   "w1gT": ext(f"m{i}_w1gT", [32 * FC * 128, KC * 128]), "w1lT": ext(f"m{i}_w1lT", [32 * FC * 128, KC * 128]),
                   "w2T": ext(f"m{i}_w2T", [32 * FFE, D]), "b1gT": ext(f"m{i}_b1gT", [32 * 128, FC]), "b1lT": ext(f"m{i}_b1lT", [32 * 128, FC]),
                   "b2": ext(f"m{i}_b2", [32, D])})
    NB0 = (4 * NTOK + 511) // 512 + 32
    NB1 = (4 * seq + 511) // 512 + 32
    K = {k: ext("k_" + k, [128, 128]) for k in ("ident", "ones", "trif", "trib")}
    K["iota_p"] = ext("k_iota_p", [128, 1])
    K0 = dict(K)
    K0["iota_b"] = ext("k_iota_b0", [1, NB0])
    K1 = dict(K)
    K1["iota_b"] = ext("k_iota_b1", [1, NB1])
    OUT = c.dram("out", [nbatch * seq, D], F32, "ExternalOutput")
    MODV = c.dram("MODV", [36, D], F32)
    mods_phase(c, CROW, ADAW, ADAB, MIXG, FFNG, MODV, K)
    for b in range(nbatch):
        RES = c.dram(f"b{b}RES", [NTOK, D], F32)
        for r0 in range(0, nctx, 128):
            c.dma("sp", RES[r0:r0 + 128, :], CTX[b * nctx + r0:b * nctx + r0 + 128, :], writes=["RES"])
        for r0 in range(0, seq, 128):
            c.dma("sp", RES[nctx + r0:nctx + r0 + 128, :], X[b * seq + r0:b * seq + r0 + 128, :], writes=["RES"])
        c.barrier()
        m = []
        for i in range(2):
            m.append({"l": {nm: modrow(MODV, i, b, k) for k, nm in enumerate(("A1", "B1", "G1", "A2", "B2", "G2"))},
                      "c": {nm: modrow(MODV, i, 2, k) for k, nm in enumerate(("A1", "B1", "G1", "A2", "B2", "G2"))}})
        hybrid_phase(c, f"b{b}H", D, nctx, seq, RES, m[0], WH, K)
        tiles0 = [(r0, 128, "c" if r0 < nctx else "l", r0) for r0 in range(0, NTOK, 128)]
        moe_local(c, f"b{b}M0", D, FFE, NTOK, RES, tiles0, m[0], WM[0], K0)
        mla_phase(c, f"b{b}A", D, nctx, seq, RES, m[1], WA, K)
        tiles1 = [(nctx + r0, 128, "l", r0) for r0 in range(0, seq, 128)]
        moe_local(c, f"b{b}M1", D, FFE, seq, RES, tiles1, m[1], WM[1], K1)
        final_norm(c, f"b{b}F", RES, nctx, seq, FING, OUT, b * seq)
    c.finish()
    return c


def host_inputs(inputs, nbatch=NBATCH, seq=SEQ, nctx=NCTX):
    f32 = np.float32
    A = lambda a: np.ascontiguousarray(np.asarray(a, dtype=f32))
    D = D_MODEL
    g = lambda k: np.asarray(inputs[k])
    m = {}
    m["ada_w"] = A(g("ada_w").reshape(2 * D, 6 * D))
    m["ada_b"] = A(g("ada_b").reshape(2, 6 * D))
    m["mix_g"] = A(g("mix_norm_g"))
    m["ffn_g"] = A(g("ffn_norm_g"))
    m["fin_g"] = A(g("final_norm_g").reshape(1, D))
    m["h_w_in"] = A(g("hyb_w_in")[0])
    m["h_conv_w"] = A(g("hyb_conv_w")[0])
    m["h_conv_b"] = A(g("hyb_conv_b")[0].reshape(1, -1))
    m["h_dtb"] = A(g("hyb_dt_bias")[0].reshape(1, 64))
    m["h_alog"] = A(g("hyb_a_log")[0].reshape(1, 64))
    m["h_dsk"] = A(g("hyb_d_skip")[0].reshape(1, 32))
    m["h_ssdg"] = A(g("hyb_ssd_norm_g")[0].reshape(1, -1))
    m["h_vng"] = A(g("hyb_v_norm_g")[0].reshape(1, -1))
    m["h_wsT"] = A(g("hyb_w_s")[0].transpose(0, 2, 1))
    m["h_bsT"] = A(g("hyb_b_s")[0].T)
    m["h_w_out"] = A(g("hyb_w_out")[0])
    m["a_w_in"] = A(g("mla_w_in")[0])
    m["a_qng"] = A(g("mla_q_norm_g")[0].reshape(1, -1))
    m["a_kvng"] = A(g("mla_kv_norm_g")[0].reshape(1, -1))
    m["a_zero768"] = np.zeros((1, 768), f32)
    m["a_w_uq"] = A(g("mla_w_uq")[0])
    wk = g("mla_w_ukv")[0].reshape(512, 16, 256)
    m["a_w_uk"] = A(wk[:, :, :128].reshape(512, 2048))
    m["a_w_uv"] = A(wk[:, :, 128:].reshape(512, 2048))
    m["a_w_o"] = A(g("mla_w_o")[0])
    rows = seq // 64
    row = np.repeat(np.arange(rows), 64).astype(np.float64)
    col = np.tile(np.arange(64), rows).astype(np.float64)
    freqs = (10000.0 ** (-np.arange(16, dtype=np.float32) / 16)).astype(np.float32)
    ang = np.stack([row[:, None].astype(f32) * freqs, col[:, None].astype(f32) * freqs], 1).astype(f32)
    m["a_cos"] = A(np.cos(ang).reshape(seq, 32))
    m["a_sin"] = A(np.sin(ang).reshape(seq, 32))
    FC = FFE // 128
    for i in range(2):
        m[f"m{i}_rw"] = A(g("router_w")[i])
        m[f"m{i}_rb"] = A(g("router_b")[i].reshape(1, 32))
        w1 = g("exp_w1")[i]
        w5 = w1.reshape(32, D // 128, 128, FC, 128, 2)
        m[f"m{i}_w1gT"] = A(w5[..., 0].transpose(0, 3, 2, 1, 4).reshape(32 * FC * 128, (D // 128) * 128))
        m[f"m{i}_w1lT"] = A(w5[..., 1].transpose(0, 3, 2, 1, 4).reshape(32 * FC * 128, (D // 128) * 128))
        m[f"m{i}_w2T"] = A(g("exp_w2")[i].reshape(32 * FFE, D))
        b1 = g("exp_b1")[i].reshape(32, FC, 128, 2)
        m[f"m{i}_b1gT"] = A(b1[..., 0].transpose(0, 2, 1).reshape(32 * 128, FC))
        m[f"m{i}_b1lT"] = A(b1[..., 1].transpose(0, 2, 1).reshape(32 * 128, FC))
        m[f"m{i}_b2"] = A(g("exp_b2")[i])
    tri = np.tril(np.ones((128, 128), f32))
    m["k_ident"] = np.eye(128, dtype=f32)
    m["k_ones"] = np.ones((128, 128), f32)
    m["k_trif"] = A(tri.T)
    m["k_trib"] = A(tri)
    m["k_iota_p"] = np.arange(128, dtype=f32).reshape(128, 1)
    NTOK = nctx + seq
    NB0 = (4 * NTOK + 511) // 512 + 32
    NB1 = (4 * seq + 511) // 512 + 32
    m["k_iota_b0"] = np.arange(NB0, dtype=f32).reshape(1, NB0)
    m["k_iota_b1"] = np.arange(NB1, dtype=f32).reshape(1, NB1)
    return m


def kernel(**inputs):
    c = build_program(nbatch=1)
    m = host_inputs(inputs)
    f32 = np.float32
    x = np.asarray(inputs["x"], dtype=f32)
    ctx = np.asarray(inputs["ctx"], dtype=f32)
    cc = np.asarray(inputs["c"], dtype=f32)
    c_ctx = np.asarray(inputs["c_ctx"], dtype=f32).reshape(1, D_MODEL)
    in_maps = []
    for b in range(NBATCH):
        mb = dict(m)
        mb["x"] = np.ascontiguousarray(x[b])
        mb["ctx"] = np.ascontiguousarray(ctx[b])
        mb["crow"] = np.ascontiguousarray(np.concatenate([cc[b:b + 1], cc[b:b + 1], c_ctx], 0))
        in_maps.append(mb)
    res = run_bass_kernel_spmd(c.nc, in_maps, core_ids=list(range(NBATCH)))
    out = np.stack([np.asarray(res.results[b]["out"], dtype=f32) for b in range(NBATCH)], 0)
    return out.reshape(NBATCH, SEQ, D_MODEL)
```
